# Optimizing a Trainium2 kernel written in Bass

```python
import math
import jax, jax.numpy as jnp
from jax import lax
import numpy as np

D_MODEL = 1024
BATCH = 8
SEQ = 4096
DEPTH = 2

D_MIX = D_MODEL
D_A = D_MIX // 2
D_B = D_MIX - D_A
HEAD_DIM = 64
H_A = D_A // HEAD_DIM
H_B = D_B // HEAD_DIM
N_KV = 2
GQA = H_B // N_KV
D_KV = N_KV * HEAD_DIM
CHUNK = 128
CMP_BLOCK = 32
CMP_STRIDE = 16
CMP_HIDDEN = 256
SEL_BLOCK = 64
N_SELECT = 16
WINDOW = 512
Q_BLOCK = 128
N_GATES = 3
D_IN = 2 * D_A + D_B + 6 * D_KV + N_GATES * H_B
D_FF = int(math.ceil(8 * D_MODEL / 3 / 256)) * 256
EPS = 1e-6
NEG = -1e30
FORCE = 1e4

kernel_name = "hybrid_gmlp_nsa_parallel_heads"


def rmsnorm(x, w):
    xf = x.astype(jnp.float32)
    y = xf * lax.rsqrt(jnp.mean(xf * xf, axis=-1, keepdims=True) + EPS)
    return (y * w.astype(jnp.float32)).astype(x.dtype)


def masked_softmax(s, mask):
    p = jax.nn.softmax(jnp.where(mask, s.astype(jnp.float32), NEG), axis=-1)
    return jnp.where(mask, p, 0.0)


def split_offsets():
    sizes = [D_A, D_A, D_B, D_KV, D_KV, D_KV, D_KV, D_KV, D_KV, N_GATES * H_B]
    return [int(o) for o in np.cumsum(sizes)[:-1]]


def gmlp_group(u, v, norm_w, ws, bs):
    B, S, _ = u.shape
    nc = S // CHUNK
    u = jax.nn.gelu(u).reshape(B, nc, CHUNK, H_A, HEAD_DIM)
    v = rmsnorm(jax.nn.gelu(v), norm_w).reshape(B, nc, CHUNK, H_A, HEAD_DIM)
    causal = jnp.tril(jnp.ones((CHUNK, CHUNK), dtype=bool))
    w = jnp.where(causal[None], ws, 0).astype(v.dtype)
    sv = jnp.einsum('hts,bcshd->bcthd', w, v) + bs.T.astype(v.dtype)[None, None, :, :, None]
    return (u * sv).reshape(B, S, D_A)


def compress(kv, pos, w1, w2):
    B, S = kv.shape[0], kv.shape[1]
    n_cmp = (S - CMP_BLOCK) // CMP_STRIDE + 1
    idx = jnp.arange(n_cmp)[:, None] * CMP_STRIDE + jnp.arange(CMP_BLOCK)[None, :]
    blk = kv[:, idx] + pos[None, None, :, None, :]
    blk = blk.transpose(0, 1, 3, 2, 4).reshape(B, n_cmp, N_KV, CMP_BLOCK * HEAD_DIM)
    return jax.nn.gelu(blk @ w1) @ w2


def nsa_group(q, k_c, v_c, k_s, v_s, k_w, v_w, g_raw,
              cmp_pos_k, cmp_pos_v, cmp_k_w1, cmp_k_w2, cmp_v_w1, cmp_v_w2, gate_b):
    B, S = q.shape[0], q.shape[1]
    q = (q * (HEAD_DIM ** -0.5)).reshape(B, S, N_KV, GQA, HEAD_DIM)
    kv_shape = (B, S, N_KV, HEAD_DIM)
    k_c, v_c, k_s, v_s, k_w, v_w = [a.reshape(kv_shape) for a in (k_c, v_c, k_s, v_s, k_w, v_w)]
    t = jnp.arange(S)
    n_qb = S // Q_BLOCK

    kc = compress(k_c, cmp_pos_k, cmp_k_w1, cmp_k_w2)
    vc = compress(v_c, cmp_pos_v, cmp_v_w1, cmp_v_w2)
    n_cmp = kc.shape[1]
    cmp_start = jnp.arange(n_cmp) * CMP_STRIDE
    cmp_mask = (cmp_start + CMP_BLOCK - 1)[None, :] <= t[:, None]
    s_cmp = jnp.einsum('bskgd,bnkd->bkgsn', q, kc)
    p_cmp = masked_softmax(s_cmp, cmp_mask)
    o_cmp = jnp.einsum('bkgsn,bnkd->bskgd', p_cmp.astype(vc.dtype), vc)

    n_blk = S // SEL_BLOCK
    n_sel = min(N_SELECT, n_blk)
    blk_start = jnp.arange(n_blk) * SEL_BLOCK
    overlap = ((cmp_start[:, None] < blk_start[None, :] + SEL_BLOCK)
               & (cmp_start[:, None] + CMP_BLOCK > blk_start[None, :])).astype(jnp.float32)
    imp = jnp.einsum('bkgsn,nj->bksj', p_cmp, overlap)
    cur = t // SEL_BLOCK
    j = jnp.arange(n_blk)
    valid = j[None, :] <= cur[:, None]
    forced = (j[None, :] == 0) | (j[None, :] == cur[:, None]) | (j[None, :] == cur[:, None] - 1)
    score = jnp.where(forced, FORCE, jnp.where(valid, imp, -FORCE))
    _, sel = lax.top_k(score, n_sel)
    sel = sel.transpose(0, 2, 1, 3)

    kb = k_s.reshape(B, n_blk, SEL_BLOCK, N_KV, HEAD_DIM).transpose(0, 3, 1, 2, 4)
    vb = v_s.reshape(B, n_blk, SEL_BLOCK, N_KV, HEAD_DIM).transpose(0, 3, 1, 2, 4)
    b_ix = jnp.arange(B)[:, None, None, None]
    h_ix = jnp.arange(N_KV)[None, None, :, None]

    def sel_block(args):
        qc, ic, tc = args
        kg = kb[b_ix, h_ix, ic]
        vg = vb[b_ix, h_ix, ic]
        pos = ic[..., None] * SEL_BLOCK + jnp.arange(SEL_BLOCK)
        mask = (pos <= tc[None, :, None, None, None]).reshape(B, Q_BLOCK, N_KV, 1, n_sel * SEL_BLOCK)
        s = jnp.einsum('bqkgd,bqknrd->bqkgnr', qc, kg).reshape(B, Q_BLOCK, N_KV, GQA, n_sel * SEL_BLOCK)
        p = masked_softmax(s, mask)
        vg = vg.reshape(B, Q_BLOCK, N_KV, n_sel * SEL_BLOCK, HEAD_DIM)
        return jnp.einsum('bqkgm,bqkmd->bqkgd', p.astype(vg.dtype), vg)

    qs = q.reshape(B, n_qb, Q_BLOCK, N_KV, GQA, HEAD_DIM).transpose(1, 0, 2, 3, 4, 5)
    sels = sel.reshape(B, n_qb, Q_BLOCK, N_KV, n_sel).transpose(1, 0, 2, 3, 4)
    ts = t.reshape(n_qb, Q_BLOCK)
    o_slc = lax.map(sel_block, (qs, sels, ts))
    o_slc = o_slc.transpose(1, 0, 2, 3, 4, 5).reshape(B, S, N_KV, GQA, HEAD_DIM)

    span = WINDOW + Q_BLOCK
    pad = ((0, 0), (WINDOW, 0), (0, 0), (0, 0))
    band = jnp.arange(n_qb)[:, None] * Q_BLOCK + jnp.arange(span)[None, :]
    kw = jnp.pad(k_w, pad)[:, band]
    vw = jnp.pad(v_w, pad)[:, band]
    kpos = band - WINDOW
    diff = ts[:, :, None] - kpos[:, None, :]
    win_mask = (diff >= 0) & (diff < WINDOW) & (kpos[:, None, :] >= 0)
    qw = q.reshape(B, n_qb, Q_BLOCK, N_KV, GQA, HEAD_DIM)
    s_win = jnp.einsum('bcqkgd,bcmkd->bckgqm', qw, kw)
    p_win = masked_softmax(s_win, win_mask[None, :, None, None])
    o_win = jnp.einsum('bckgqm,bcmkd->bcqkgd', p_win.astype(vw.dtype), vw).reshape(B, S, N_KV, GQA, HEAD_DIM)

    g = jax.nn.sigmoid(g_raw + gate_b).reshape(B, S, N_GATES, N_KV, GQA, 1)
    o = g[:, :, 0] * o_cmp + g[:, :, 1] * o_slc + g[:, :, 2] * o_win
    return o.reshape(B, S, D_B)


def setup_inputs(seed: int = 0) -> dict:
    key = jax.random.key(seed)
    ks = iter(jax.random.split(key, 32))

    def nrm(shape, scale):
        return jax.random.normal(next(ks), shape, jnp.float32) * scale

    def gain(shape):
        return 1.0 + nrm(shape, 0.01)

    L = DEPTH
    return {
        "x": nrm((BATCH, SEQ, D_MODEL), 1.0),
        "norm_mix_w": gain((L, D_MODEL)),
        "w_in": nrm((L, D_MODEL, D_IN), D_MODEL ** -0.5),
        "gmlp_norm_w": gain((L, D_A)),
        "gmlp_ws": nrm((L, H_A, CHUNK, CHUNK), 0.5 * CHUNK ** -0.5),
        "gmlp_bs": 1.0 + nrm((L, H_A, CHUNK), 0.01),
        "cmp_pos_k": nrm((L, CMP_BLOCK, HEAD_DIM), 0.1),
        "cmp_pos_v": nrm((L, CMP_BLOCK, HEAD_DIM), 0.1),
        "cmp_k_w1": nrm((L, CMP_BLOCK * HEAD_DIM, CMP_HIDDEN), (CMP_BLOCK * HEAD_DIM) ** -0.5),
        "cmp_k_w2": nrm((L, CMP_HIDDEN, HEAD_DIM), CMP_HIDDEN ** -0.5),
        "cmp_v_w1": nrm((L, CMP_BLOCK * HEAD_DIM, CMP_HIDDEN), (CMP_BLOCK * HEAD_DIM) ** -0.5),
        "cmp_v_w2": nrm((L, CMP_HIDDEN, HEAD_DIM), CMP_HIDDEN ** -0.5),
        "gate_b": nrm((L, N_GATES * H_B), 0.01),
        "out_norm_a_w": gain((L, D_A)),
        "out_norm_b_w": gain((L, D_B)),
        "w_o": nrm((L, D_MIX, D_MODEL), D_MIX ** -0.5),
        "norm_ffn_w": gain((L, D_MODEL)),
        "w_gate": nrm((L, D_MODEL, D_FF), D_MODEL ** -0.5),
        "w_up": nrm((L, D_MODEL, D_FF), D_MODEL ** -0.5),
        "w_down": nrm((L, D_FF, D_MODEL), D_FF ** -0.5),
        "final_norm_w": gain((D_MODEL,)),
    }


def reference(x, norm_mix_w, w_in, gmlp_norm_w, gmlp_ws, gmlp_bs,
              cmp_pos_k, cmp_pos_v, cmp_k_w1, cmp_k_w2, cmp_v_w1, cmp_v_w2,
              gate_b, out_norm_a_w, out_norm_b_w, w_o, norm_ffn_w,
              w_gate, w_up, w_down, final_norm_w):
    offsets = split_offsets()
    for l in range(DEPTH):
        h = rmsnorm(x, norm_mix_w[l])
        z = h @ w_in[l]
        u, v, q, k_c, v_c, k_s, v_s, k_w, v_w, g_raw = jnp.split(z, offsets, axis=-1)
        a = gmlp_group(u, v, gmlp_norm_w[l], gmlp_ws[l], gmlp_bs[l])
        b = nsa_group(q, k_c, v_c, k_s, v_s, k_w, v_w, g_raw,
                      cmp_pos_k[l], cmp_pos_v[l], cmp_k_w1[l], cmp_k_w2[l],
                      cmp_v_w1[l], cmp_v_w2[l], gate_b[l])
        mix = jnp.concatenate([rmsnorm(a, out_norm_a_w[l]), rmsnorm(b, out_norm_b_w[l])], axis=-1)
        x = x + mix @ w_o[l]
        h = rmsnorm(x, norm_ffn_w[l])
        x = x + (jax.nn.silu(h @ w_gate[l]) * (h @ w_up[l])) @ w_down[l]
    return rmsnorm(x, final_norm_w)
```

```python
import numpy as np
import concourse.bass as bass
import concourse.mybir as mybir
from concourse.bass_utils import run_bass_kernel_spmd

F32 = mybir.dt.float32
BF16 = mybir.dt.bfloat16
AF = mybir.ActivationFunctionType
ALU = mybir.AluOpType
AX = mybir.AxisListType


class Prog:
    ENGS = ("sync", "scalar", "vector", "gpsimd", "tensor")
    NDMA = 8
    R = 8

    def __init__(self, nc, stack):
        self.nc = nc
        self.q = {e: [] for e in self.ENGS}
        self.sem = {e: [stack.enter_context(nc.semaphore("s_%s%d" % (e, i))) for i in range(self.R)]
                    for e in self.ENGS}
        self.cnt = {e: 0 for e in self.ENGS}
        self.dsem = {e: [stack.enter_context(nc.semaphore("d_%s%d" % (e, i))) for i in range(self.NDMA)]
                     for e in ("sync", "scalar", "gpsimd")}
        self.dcnt = {e: 0 for e in self.dsem}
        self.semobj = {}
        for e in self.ENGS:
            for i in range(self.R):
                self.semobj[("c", e, i)] = self.sem[e][i]
        for e in self.dsem:
            for i in range(self.NDMA):
                self.semobj[("d", e, i)] = self.dsem[e][i]
        self.waited = {e: {} for e in self.ENGS}
        self.lastw = {}
        self.readers = {}
        self.n_inst = 0

    def _waits(self, eng, deps):
        need = {}
        for (sid, val) in deps:
            if sid[0] == "c" and sid[1] == eng and (val - 1) * self.R + sid[2] + 1 > self.cnt[eng]:
                continue
            if self.waited[eng].get(sid, 0) >= val:
                continue
            if need.get(sid, 0) < val:
                need[sid] = val
        out = []
        for sid, val in need.items():
            self.waited[eng][sid] = val
            out.append((self.semobj[sid], val))
        return out

    def _deps(self, reads, writes):
        deps = []
        for k in reads:
            if k in self.lastw:
                deps.append(self.lastw[k])
        for k in writes:
            if k in self.lastw:
                deps.append(self.lastw[k])
            deps.extend(self.readers.get(k, ()))
        return deps

    def _commit(self, tok, reads, writes):
        for k in reads:
            self.readers.setdefault(k, []).append(tok)
        for k in writes:
            self.lastw[k] = tok
            self.readers[k] = []

    def op(self, eng, fn, reads=(), writes=(), signal=True):
        waits = self._waits(eng, self._deps(reads, writes))
        n = self.cnt[eng]
        tok = (("c", eng, n % self.R), n // self.R + 1)
        if signal:
            self.cnt[eng] += 1
        sem = self.sem[eng][n % self.R]

        def run(e, fn=fn, waits=waits, signal=signal, sem=sem):
            for (s, v) in waits:
                e.wait_ge(s, v)
            ins = fn(e)
            if signal:
                ins.then_inc(sem, 1)

        self.q[eng].append(run)
        self._commit(tok, reads, writes)
        self.n_inst += 1

    def dma(self, eng, out, in_, reads=(), writes=(), **kw):
        n = self.dcnt[eng]
        self.dcnt[eng] += 1
        slot = n % self.NDMA
        val = 16 * (n // self.NDMA + 1)
        sid = ("d", eng, slot)
        deps = self._deps(reads, writes)
        if val > 16:
            deps.append((sid, val - 16))
        waits = self._waits(eng, deps)
        sem = self.dsem[eng][slot]

        def run(e, waits=waits, sem=sem, out=out, in_=in_, kw=kw):
            for (s, v) in waits:
                e.wait_ge(s, v)
            e.dma_start(out=out, in_=in_, **kw).then_inc(sem, 16)

        self.q[eng].append(run)
        self._commit((sid, val), reads, writes)
        self.n_inst += 1

    def barrier(self):
        deps = []
        for e in self.ENGS:
            n = self.cnt[e]
            for i in range(self.R):
                if n >= i + 1:
                    deps.append((("c", e, i), (n - 1 - i) // self.R + 1))
        for e in self.dsem:
            n = self.dcnt[e]
            for i in range(self.NDMA):
                if n >= i + 1:
                    deps.append((("d", e, i), 16 * ((n - 1 - i) // self.NDMA + 1)))
        for e in self.ENGS:
            waits = self._waits(e, deps)

            def run(en, waits=waits):
                for (s, v) in waits:
                    en.wait_ge(s, v)

            self.q[e].append(run)

    def finish(self, eng, keys):
        waits = self._waits(eng, self._deps(keys, ()))

        def run(e, waits=waits):
            for (s, v) in waits:
                e.wait_ge(s, v)

        self.q[eng].append(run)

    def emit(self, block):
        q = self.q

        @block.sync
        def _(e):
            for f in q["sync"]:
                f(e)

        @block.scalar
        def _(e):
            for f in q["scalar"]:
                f(e)

        @block.vector
        def _(e):
            for f in q["vector"]:
                f(e)

        @block.gpsimd
        def _(e):
            for f in q["gpsimd"]:
                f(e)

        @block.tensor
        def _(e):
            for f in q["tensor"]:
                f(e)


S = 4096
D = 1024
NT = 32
DFF = 2816
NFC = 22
NEG = -30000.0
L = 2


def build(debug=None, nlayers=L):
    from contextlib import ExitStack
    nc = bass.Bass("TRN2", target_bir_lowering=False)
    dt_in = lambda name, shape: nc.dram_tensor(name, shape, F32, kind="ExternalInput").ap()
    x_in = dt_in("x", [S, D])
    norm_mix_w = dt_in("norm_mix_w", [L, D])
    w_in = dt_in("w_in", [L, D, 2328])
    gmlp_norm_w = dt_in("gmlp_norm_w", [L, 512])
    gmlp_ws = dt_in("gmlp_ws", [L, 8, 128, 128])
    gmlp_bs = dt_in("gmlp_bs", [L, 8, 128])
    cmp_pos = {"k": dt_in("cmp_pos_k", [L, 32, 64]), "v": dt_in("cmp_pos_v", [L, 32, 64])}
    cmp_w1 = {"k": dt_in("cmp_k_w1", [L, 2048, 256]), "v": dt_in("cmp_v_w1", [L, 2048, 256])}
    cmp_w2 = {"k": dt_in("cmp_k_w2", [L, 256, 64]), "v": dt_in("cmp_v_w2", [L, 256, 64])}
    gate_b = dt_in("gate_b", [L, 24])
    out_norm_a_w = dt_in("out_norm_a_w", [L, 512])
    out_norm_b_w = dt_in("out_norm_b_w", [L, 512])
    w_o = dt_in("w_o", [L, D, D])
    norm_ffn_w = dt_in("norm_ffn_w", [L, D])
    w_gate = dt_in("w_gate", [L, D, DFF])
    w_up = dt_in("w_up", [L, D, DFF])
    w_down = dt_in("w_down", [L, DFF, D])
    final_norm_w = dt_in("final_norm_w", [D])
    out = nc.dram_tensor("out", [S, D], F32, kind="ExternalOutput").ap()
    dbg = debug is not None
    mix_d = nc.dram_tensor("mix_d", [S, D], BF16, kind="ExternalOutput" if dbg else "Internal").ap()
    xres_d = nc.dram_tensor("xres_d", [S, D], F32, kind="ExternalOutput" if dbg else "Internal").ap()

    with ExitStack() as st:
        P = Prog(nc, st)
        op = P.op

        sfx = [""]

        def T(name, shape, dt, stack=None):
            return (stack or st).enter_context(nc.sbuf_tensor(name + sfx[0], shape, dt))

        B = [st.enter_context(nc.psum_tensor("B%d" % i, [128, 512], F32)) for i in range(7)]
        BT = st.enter_context(nc.psum_tensor("BT", [128, 8, 128], BF16))

        ident_b = T("ident_b", [128, 128], BF16)
        ident_f = T("ident_f", [128, 128], F32)
        tri_f = T("tri_f", [128, 128], F32)
        tri_b = T("tri_b", [128, 128], BF16)
        ntri_b = T("ntri_b", [128, 128], BF16)
        ones_f = T("ones_f", [128, 128], F32)
        ind8 = T("ind8", [8, 512], F32)
        A_big = T("A_big", [16, 512], BF16)
        Bm4 = T("Bm4", [16, 4, 128], BF16)
        VM = T("VM", [128, 128], F32)
        AMk = T("AMk", [128, 128], F32)
        tmpc = T("tmpc", [128, 512], F32)
        tmpc2 = T("tmpc2", [128, 512], F32)

        def G(fn, reads=(), writes=()):
            op("gpsimd", fn, reads=reads, writes=writes)

        def asel(t, pattern, cmp_, fill, base, cm, key):
            G(lambda e: e.affine_select(out=t, in_=t, pattern=pattern, compare_op=cmp_, fill=fill,
                                        base=base, channel_multiplier=cm), reads=[key], writes=[key])

        G(lambda e: e.memset(ones_f[:], 1.0), writes=["ones_f"])
        G(lambda e: e.memset(ident_f[:], 1.0), writes=["ident_f"])
        asel(ident_f[:], [[-1, 128]], ALU.is_equal, 0.0, 0, 1, "ident_f")
        G(lambda e: e.tensor_copy(ident_b[:], ident_f[:]), reads=["ident_f"], writes=["ident_b"])
        G(lambda e: e.memset(tri_f[:], 1.0), writes=["tri_f"])
        asel(tri_f[:], [[1, 128]], ALU.is_ge, 0.0, 0, -1, "tri_f")
        G(lambda e: e.tensor_copy(tri_b[:], tri_f[:]), reads=["tri_f"], writes=["tri_b"])
        G(lambda e: e.memset(tmpc[:, 0:128], 1.0), writes=["tmpc"])
        asel(tmpc[:, 0:128], [[-1, 128]], ALU.is_gt, 0.0, 0, 1, "tmpc")
        G(lambda e: e.tensor_copy(ntri_b[:], tmpc[:, 0:128]), reads=["tmpc"], writes=["ntri_b"])
        G(lambda e: e.memset(ind8[:], 1.0), writes=["ind8"])
        asel(ind8[:].rearrange("p (h d) -> p h d", h=8), [[1, 8], [0, 64]], ALU.is_equal, 0.0, 0, -1, "ind8")
        G(lambda e: e.memset(tmpc[0:16, :], 1.0), writes=["tmpc"])
        asel(tmpc[0:16, :], [[1, 512]], ALU.is_equal, 0.0, -255, -1, "tmpc")
        G(lambda e: e.memset(tmpc2[0:16, :], 1.0), writes=["tmpc2"])
        asel(tmpc2[0:16, :], [[1, 512]], ALU.is_ge, 0.0, -263, 0, "tmpc2")
        asel(tmpc2[0:16, :], [[0, 512]], ALU.is_equal, 0.0, -8, 1, "tmpc2")
        asel(tmpc[0:16, :], [[0, 512]], ALU.is_ge, 0.0, 7, -1, "tmpc")
        G(lambda e: e.tensor_tensor(out=A_big[:], in0=tmpc[0:16, :], in1=tmpc2[0:16, :], op=ALU.add),
          reads=["tmpc", "tmpc2"], writes=["A_big"])
        G(lambda e: e.memset(tmpc[0:16, :], NEG), writes=["tmpc"])
        asel(tmpc[0:16, :].rearrange("p (g t) -> p g t", g=4), [[0, 4], [-1, 128]], ALU.is_gt, 0.0, 15, 16, "tmpc")
        asel(tmpc[0:16, :], [[0, 512]], ALU.is_ge, 0.0, 7, -1, "tmpc")
        G(lambda e: e.memset(tmpc2[0:16, :], NEG), writes=["tmpc2"])
        asel(tmpc2[0:16, :], [[0, 512]], ALU.is_equal, 0.0, -8, 1, "tmpc2")
        G(lambda e: e.tensor_tensor(out=Bm4[:].rearrange("p g t -> p (g t)"), in0=tmpc[0:16, :], in1=tmpc2[0:16, :], op=ALU.add),
          reads=["tmpc", "tmpc2"], writes=["Bm4"])
        G(lambda e: e.memset(VM[:], 1.0), writes=["VM"])
        asel(VM[:], [[-64, 128]], ALU.is_ge, 0.0, 64 * 64 - 128, 1, "VM")
        G(lambda e: e.memset(AMk[:], 10000.0), writes=["AMk"])
        asel(AMk[:], [[-64, 128]], ALU.is_ge, 0.0, 64 * 64, 1, "AMk")
        asel(AMk[:], [[64, 128]], ALU.is_ge, 0.0, -64 * 64 + 63, -1, "AMk")
        G(lambda e: e.memset(tmpc[:, 0:128], 10001.0), writes=["tmpc"])
        asel(tmpc[:, 0:128], [[-64, 128]], ALU.is_ge, 0.0, 64 * 64 - 64, 1, "tmpc")
        asel(tmpc[:, 0:128], [[64, 128]], ALU.is_ge, 0.0, -64 * 64 + 64 + 63, -1, "tmpc")
        G(lambda e: e.tensor_tensor(out=AMk[:], in0=AMk[:], in1=tmpc[:, 0:128], op=ALU.add), reads=["AMk", "tmpc"], writes=["AMk"])
        G(lambda e: e.memset(tmpc2[:, 0:128], -10000.0), writes=["tmpc2"])
        asel(tmpc2[:, 0:128], [[64, 128]], ALU.is_ge, 0.0, -64 * 64 - 1, -1, "tmpc2")
        G(lambda e: e.tensor_tensor(out=AMk[:], in0=AMk[:], in1=tmpc2[:, 0:128], op=ALU.add), reads=["AMk", "tmpc2"], writes=["AMk"])


        def rms_stats(src_ap, width, key_src, junk, ssum, rstd, tag):
            sc = float(width) ** -0.5
            op("scalar", lambda e: e.activation(out=junk, in_=src_ap, func=AF.Square, scale=sc, accum_out=ssum[:, 0:1]),
               reads=[key_src], writes=["junk" + tag, "ss" + tag])
            op("vector", lambda e: e.tensor_scalar(rstd[:, 0:1], ssum[:, 0:1], 1e-6, None, ALU.add), reads=["ss" + tag], writes=["rstd" + tag])
            op("scalar", lambda e: e.activation(out=rstd[:, 0:1], in_=rstd[:, 0:1], func=AF.Sqrt), reads=["rstd" + tag], writes=["rstd" + tag])
            op("vector", lambda e: e.reciprocal(rstd[:, 0:1], rstd[:, 0:1]), reads=["rstd" + tag], writes=["rstd" + tag])

        def layer(l):
            sfx[0] = "_L%d" % l
            xsrc = x_in if l == 0 else xres_d
            last = (l == nlayers - 1)
            P.barrier()
            with ExitStack() as s1:
                nmw = T("nmw", [128, D], F32, s1)
                gnw = T("gnw", [128, 512], F32, s1)
                onaw = T("onaw", [128, 512], F32, s1)
                onbw = T("onbw", [128, 512], F32, s1)
                gbt = T("gbt", [128, 24], F32, s1)
                bs8 = T("bs8", [8, 128], F32, s1)
                gates = T("gates", [128, NT, 24], F32, s1)
                P.dma("sync", nmw[:], norm_mix_w[l].partition_broadcast(128), writes=["nmw"])
                P.dma("sync", gnw[:], gmlp_norm_w[l].partition_broadcast(128), writes=["gnw"])
                P.dma("sync", onaw[:], out_norm_a_w[l].partition_broadcast(128), writes=["onaw"])
                P.dma("sync", onbw[:], out_norm_b_w[l].partition_broadcast(128), writes=["onbw"])
                P.dma("sync", gbt[:], gate_b[l].partition_broadcast(128), writes=["gbt"])
                P.dma("sync", bs8[:], gmlp_bs[l], writes=["bs8"])

                wtm = T("wtm", [128, 8, 1304], BF16, s1)
                wfm = T("wfm", [128, 8, 1024], BF16, s1)
                wsT = T("wsT", [128, 8, 128], BF16, s1)
                kTE = [T("kTE%d" % k, [128, S], BF16, s1) for k in range(2)]
                kTw = T("kTw", [128, S], BF16, s1)
                kcT = T("kcT", [128, S], BF16, s1)
                vcT = T("vcT", [128, S], BF16, s1)
                vs_aug = T("vs_aug", [128, NT, 2, 65], BF16, s1)
                vw_aug = T("vw_aug", [128, NT, 2, 65], BF16, s1)
                qT_all = T("qT_all", [128, 4, S], BF16, s1)
                w2k_pad = T("w2k_pad", [128, 2, 2, 128], BF16, s1)
                w2v = T("w2v", [128, 2, 64], BF16, s1)
                hidT = T("hidT", [128, 2, 256], BF16, s1)
                kcmp = T("kcmp", [128, 256], BF16, s1)
                R_cmp = T("R_cmp", [128, 2, 2, 129], BF16, s1)

                with ExitStack() as s0:
                    stage = [T("stage%d" % i, [128, 2328], F32, s0) for i in range(2)]
                    for c in range(8):
                        sg = stage[c % 2]
                        sk = "stage%d" % (c % 2)
                        P.dma("sync", sg[:], w_in[l, c * 128:(c + 1) * 128, :], writes=[sk])
                        cp = [
                            (wtm[:, c, 0:1024], sg[:, 0:1024]),
                            (wtm[:, c, 1024:1152], sg[:, 1920:2048]),
                            (wtm[:, c, 1152:1280], sg[:, 2176:2304]),
                            (wtm[:, c, 1280:1304], sg[:, 2304:2328]),
                            (wfm[:, c, 0:512].rearrange("p (g k d) -> p g k d", g=4, k=2),
                             sg[:, 1024:1536].rearrange("p (k g d) -> p g k d", k=2, g=4)),
                            (wfm[:, c, 512:640], sg[:, 1536:1664]),
                            (wfm[:, c, 640:768], sg[:, 1664:1792]),
                            (wfm[:, c, 768:896], sg[:, 1792:1920]),
                            (wfm[:, c, 896:1024], sg[:, 2048:2176]),
                        ]
                        for ci, (o_, i_) in enumerate(cp):
                            eng = "gpsimd" if ci % 2 == 0 else "vector"
                            op(eng, lambda e, o_=o_, i_=i_: e.tensor_copy(o_, i_), reads=[sk], writes=["wtm" if ci < 4 else "wfm"])
                    wsf = T("wsf", [128, 8, 128], F32, s0)
                    P.dma("sync", wsf[:], gmlp_ws[l].rearrange("h t s -> t h s"), writes=["wsf"])
                    for h in range(8):
                        bk = B[h % 2]
                        op("tensor", lambda e, h=h, bk=bk: e.transpose(bk[:, 0:128], wsf[:, h, :], ident_f[:]),
                           reads=["wsf", "ident_f"], writes=["B%d" % (h % 2)])
                        op("vector", lambda e, h=h, bk=bk: e.tensor_tensor(out=wsT[:, h, :], in0=bk[:, 0:128], in1=tri_f[:], op=ALU.mult),
                           reads=["B%d" % (h % 2), "tri_f"], writes=["wsT"])
                    for nt_ in range(2):
                        G(lambda e: e.memset(tmpc[:, 0:64], 1.0), writes=["tmpc"])
                        asel(tmpc[:, 0:64], [[-64, 64]], ALU.is_ge, 0.0, 2048 * nt_ + 31, 16, "tmpc")
                        asel(tmpc[:, 0:64], [[64, 64]], ALU.is_ge, 0.0, 63 - 2048 * nt_, -16, "tmpc")
                        for k in range(2):
                            G(lambda e, nt_=nt_, k=k: e.tensor_copy(R_cmp[:, nt_, k, 64:128], tmpc[:, 0:64]), reads=["tmpc"], writes=["R_cmp"])
                    G(lambda e: e.memset(R_cmp[:, :, :, 128:129], 1.0), writes=["R_cmp"])
                    G(lambda e: e.memset(vs_aug[:, :, :, 64:65], 1.0), writes=["vs_aug"])
                    G(lambda e: e.memset(vw_aug[:, :, :, 64:65], 1.0), writes=["vw_aug"])
                    G(lambda e: e.memset(kTE[0][:], 1.0), writes=["kTE0"])
                    asel(kTE[0][:].rearrange("p (j r) -> p j r", j=64), [[1, 64], [0, 64]], ALU.is_equal, 0.0, 64, -1, "kTE0")
                    G(lambda e: e.memset(kTE[1][:], 1.0), writes=["kTE1"])
                    asel(kTE[1][:].rearrange("p (j r) -> p j r", j=64), [[1, 64], [0, 64]], ALU.is_equal, 0.0, 0, -1, "kTE1")
                    P.barrier()

                with ExitStack() as s2:
                    xt = [T("xt%d" % i, [128, D], F32, s2) for i in range(2)]
                    junk = T("junk", [128, D], BF16, s2)
                    ssx = T("ssx", [128, 1], F32, s2)
                    rsx = T("rsx", [128, 1], F32, s2)
                    hb = T("hb", [128, D], BF16, s2)
                    hT = [T("hT%d" % i, [128, 8, 512], BF16, s2) for i in range(2)]
                    gu = T("gu", [128, 512], F32, s2)
                    gv = T("gv", [128, 512], F32, s2)
                    ssv = T("ssv", [128, 1], F32, s2)
                    rsv = T("rsv", [128, 1], F32, s2)
                    vn = T("vn", [128, 512], BF16, s2)
                    a_t = T("a_t", [128, 512], F32, s2)
                    ssa = T("ssa", [128, 1], F32, s2)
                    rsa = T("rsa", [128, 1], F32, s2)
                    mixA = [T("mixA%d" % i, [128, 512], BF16, s2) for i in range(2)]
                    gpre = T("gpre", [128, 24], F32, s2)
                    for TT in range(8):
                        hTt = hT[TT % 2]
                        hk = "hT%d" % (TT % 2)
                        for tt in range(4):
                            i = TT * 4 + tt
                            xti = xt[i % 2]
                            xk = "xt%d" % (i % 2)
                            P.dma("sync", xti[:], xsrc[i * 128:(i + 1) * 128, :], writes=[xk])
                            rms_stats(xti[:], D, xk, junk[:], ssx, rsx, "x")
                            op("vector", lambda e, xti=xti: e.scalar_tensor_tensor(out=hb[:], in0=xti[:], scalar=rsx[:, 0:1], in1=nmw[:], op0=ALU.mult, op1=ALU.mult),
                               reads=[xk, "rstdx", "nmw"], writes=["hb"])
                            for c in range(8):
                                op("tensor", lambda e, c=c: e.transpose(BT[:, c, :], hb[:, c * 128:(c + 1) * 128], ident_b[:]),
                                   reads=["hb", "ident_b"], writes=["BT"], signal=(c == 7))
                            op("vector", lambda e, hTt=hTt, tt=tt: e.tensor_copy(hTt[:, :, tt * 128:(tt + 1) * 128], BT[:]), reads=["BT"], writes=[hk])
                            for cb, (c0, c1) in enumerate(((0, 512), (512, 1024), (1024, 1304))):
                                for c in range(8):
                                    op("tensor", lambda e, cb=cb, c=c, c0=c0, c1=c1, hTt=hTt, tt=tt: e.matmul(B[cb][:, 0:c1 - c0], lhsT=hTt[:, c, tt * 128:(tt + 1) * 128], rhs=wtm[:, c, c0:c1],
                                                                                                      start=(c == 0), stop=(c == 7)),
                                       reads=[hk, "wtm"], writes=["B%d" % cb], signal=(c == 7))
                            op("scalar", lambda e: e.activation(out=gu[:], in_=B[0][:], func=AF.Gelu_apprx_tanh), reads=["B0"], writes=["gu"])
                            op("scalar", lambda e: e.activation(out=gv[:], in_=B[1][:], func=AF.Gelu_apprx_tanh), reads=["B1"], writes=["gv"])
                            rms_stats(gv[:], 512, "gv", junk[:, 0:512], ssv, rsv, "v")
                            op("vector", lambda e: e.scalar_tensor_tensor(out=vn[:], in0=gv[:], scalar=rsv[:, 0:1], in1=gnw[:], op0=ALU.mult, op1=ALU.mult),
                               reads=["gv", "rstdv", "gnw"], writes=["vn"])
                            op("vector", lambda e, i=i: e.tensor_copy(vs_aug[:, i, :, 0:64], B[2][:, 0:128].rearrange("p (k d) -> p k d", k=2)), reads=["B2"], writes=["vs_aug"])
                            op("vector", lambda e, i=i: e.tensor_copy(vw_aug[:, i, :, 0:64], B[2][:, 128:256].rearrange("p (k d) -> p k d", k=2)), reads=["B2"], writes=["vw_aug"])
                            op("vector", lambda e: e.tensor_tensor(out=gpre[:], in0=B[2][:, 256:280], in1=gbt[:], op=ALU.add), reads=["B2", "gbt"], writes=["gpre"])
                            op("scalar", lambda e, i=i: e.activation(out=gates[:, i, :], in_=gpre[:], func=AF.Sigmoid), reads=["gpre"], writes=["gates"])
                            op("tensor", lambda e: e.matmul(B[3][:], lhsT=bs8[:], rhs=ind8[:], start=True, stop=False), reads=["bs8", "ind8"], writes=["B3"], signal=False)
                            for h in range(8):
                                op("tensor", lambda e, h=h: e.matmul(B[3][:, h * 64:(h + 1) * 64], lhsT=wsT[:, h, :], rhs=vn[:, h * 64:(h + 1) * 64], start=False, stop=(h == 7)),
                                   reads=["wsT", "vn"], writes=["B3"], signal=(h == 7))
                            op("vector", lambda e: e.tensor_tensor(out=a_t[:], in0=B[3][:], in1=gu[:], op=ALU.mult), reads=["B3", "gu"], writes=["a_t"])
                            rms_stats(a_t[:], 512, "a_t", junk[:, 512:1024], ssa, rsa, "a")
                            mA = mixA[i % 2]
                            mk = "mixA%d" % (i % 2)
                            op("vector", lambda e, mA=mA: e.scalar_tensor_tensor(out=mA[:], in0=a_t[:], scalar=rsa[:, 0:1], in1=onaw[:], op0=ALU.mult, op1=ALU.mult),
                               reads=["a_t", "rstda", "onaw"], writes=[mk])
                            P.dma("gpsimd", mix_d[i * 128:(i + 1) * 128, 0:512], mA[:], reads=[mk], writes=["mix_d"])
                        tok = slice(TT * 512, (TT + 1) * 512)
                        for ch in range(8):
                            bk = B[4 + ch % 2]
                            bkk = "B%d" % (4 + ch % 2)
                            for c in range(8):
                                op("tensor", lambda e, bk=bk, ch=ch, c=c, hTt=hTt: e.matmul(bk[:], lhsT=wfm[:, c, ch * 128:(ch + 1) * 128], rhs=hTt[:, c, :], start=(c == 0), stop=(c == 7)),
                                   reads=[hk, "wfm"], writes=[bkk], signal=(c == 7))
                            if ch < 4:
                                op("scalar", lambda e, bk=bk, ch=ch, tok=tok: e.activation(out=qT_all[:, ch, tok], in_=bk[:], func=AF.Copy, scale=0.125), reads=[bkk], writes=["qT_all"])
                            elif ch == 4:
                                op("vector", lambda e, bk=bk, tok=tok: e.tensor_copy(kcT[:, tok], bk[:]), reads=[bkk], writes=["kcT"])
                            elif ch == 5:
                                op("vector", lambda e, bk=bk, tok=tok: e.tensor_copy(vcT[:, tok], bk[:]), reads=[bkk], writes=["vcT"])
                            elif ch == 6:
                                op("vector", lambda e, bk=bk, tok=tok: e.tensor_copy(kTE[0][0:64, tok], bk[0:64, :]), reads=[bkk], writes=["kTE0"])
                                op("vector", lambda e, bk=bk, tok=tok: e.tensor_copy(kTE[1][64:128, tok], bk[64:128, :]), reads=[bkk], writes=["kTE1"])
                            else:
                                op("vector", lambda e, bk=bk, tok=tok: e.tensor_copy(kTw[:, tok], bk[:]), reads=[bkk], writes=["kTw"])
                    P.barrier()

                with ExitStack() as s25:
                    w1A = {kv: T("w1A" + kv, [128, 32, 256], BF16, s25) for kv in "kv"}
                    posT = {kv: T("posT" + kv, [128, 32], BF16, s25) for kv in "kv"}
                    bcol = {kv: T("bcol" + kv, [128, 2], F32, s25) for kv in "kv"}
                    w1f = T("w1f", [128, 8, 256], F32, s25)
                    posf = T("posf", [32, 64], F32, s25)
                    w2f = T("w2f", [128, 2, 64], F32, s25)
                    G(lambda e: e.memset(w2k_pad[:], 0.0), writes=["w2k_pad"])
                    for kv in "kv":
                        src = cmp_w1[kv][l].rearrange("(l d) n -> d l n", d=64)
                        for q4 in range(4):
                            P.dma("sync", w1f[0:64], src[:, q4 * 8:(q4 + 1) * 8, :], writes=["w1f"])
                            P.dma("sync", w1f[64:128], src[:, q4 * 8:(q4 + 1) * 8, :], writes=["w1f"])
                            G(lambda e, kv=kv, q4=q4: e.tensor_copy(w1A[kv][:, q4 * 8:(q4 + 1) * 8, :], w1f[:]), reads=["w1f"], writes=["w1A" + kv])
                        P.dma("sync", posf[:], cmp_pos[kv][l], writes=["posf"])
                        op("tensor", lambda e: e.transpose(B[2][0:64, 0:32], posf[:, :], ident_f[0:32, 0:32]), reads=["posf", "ident_f"], writes=["B2"])
                        op("vector", lambda e, kv=kv: e.tensor_copy(posT[kv][0:64, :], B[2][0:64, 0:32]), reads=["B2"], writes=["posT" + kv])
                        for hc in range(2):
                            for ll in range(32):
                                op("tensor", lambda e, kv=kv, hc=hc, ll=ll: e.matmul(B[3][:, hc:hc + 1], lhsT=w1A[kv][0:64, ll, hc * 128:(hc + 1) * 128],
                                                                                 rhs=posT[kv][0:64, ll:ll + 1], start=(ll == 0), stop=(ll == 31)),
                                   reads=["w1A" + kv, "posT" + kv], writes=["B3"], signal=(ll == 31))
                        op("vector", lambda e, kv=kv: e.tensor_copy(bcol[kv][:], B[3][:, 0:2]), reads=["B3"], writes=["bcol" + kv])
                        P.dma("sync", w2f[:], cmp_w2[kv][l].rearrange("(c p) d -> p c d", p=128), writes=["w2f"])
                        if kv == "k":
                            for k in range(2):
                                G(lambda e, k=k: e.tensor_copy(w2k_pad[:, :, k, 64 * k:64 * k + 64], w2f[:]), reads=["w2f"], writes=["w2k_pad"])
                        else:
                            G(lambda e: e.tensor_copy(w2v[:], w2f[:]), reads=["w2f"], writes=["w2v"])
                    for kv, srcT in (("k", kcT), ("v", vcT)):
                        for k in range(2):
                            pr = slice(64 * k, 64 * k + 64)
                            for hc in range(2):
                                bk = B[hc]
                                for ll in range(32):
                                    op("tensor", lambda e, kv=kv, hc=hc, ll=ll, bk=bk, pr=pr, srcT=srcT: e.matmul(bk[:, 0:255], lhsT=w1A[kv][pr, ll, hc * 128:(hc + 1) * 128],
                                                                                                       rhs=srcT[pr, ll:ll + 16 * 254 + 1:16], start=(ll == 0), stop=(ll == 31)),
                                       reads=["w1A" + kv, "kcT", "vcT"], writes=["B%d" % hc], signal=(ll == 31))
                                op("scalar", lambda e, kv=kv, hc=hc, bk=bk: e.activation(out=hidT[:, hc, 0:255], in_=bk[:, 0:255], func=AF.Gelu_apprx_tanh, bias=bcol[kv][:, hc:hc + 1]),
                                   reads=["B%d" % hc, "bcol" + kv], writes=["hidT"])
                            if kv == "k":
                                for hc in range(2):
                                    op("tensor", lambda e, hc=hc, k=k: e.matmul(B[2][:, 0:255], lhsT=w2k_pad[:, hc, k, :], rhs=hidT[:, hc, 0:255], start=(hc == 0), stop=(hc == 1)),
                                       reads=["w2k_pad", "hidT"], writes=["B2"], signal=(hc == 1))
                                op("vector", lambda e, pr=pr: e.tensor_copy(kcmp[pr, 0:255], B[2][pr, 0:255]), reads=["B2"], writes=["kcmp"])
                            else:
                                for nt_, M in ((0, 128), (1, 127)):
                                    for hc in range(2):
                                        op("tensor", lambda e, hc=hc, nt_=nt_, M=M: e.matmul(B[3][0:M, 0:64], lhsT=hidT[:, hc, nt_ * 128:nt_ * 128 + M], rhs=w2v[:, hc, :], start=(hc == 0), stop=(hc == 1)),
                                           reads=["w2v", "hidT"], writes=["B3"], signal=(hc == 1))
                                    op("vector", lambda e, nt_=nt_, M=M, k=k: e.tensor_copy(R_cmp[0:M, nt_, k, 0:64], B[3][0:M, 0:64]), reads=["B3"], writes=["R_cmp"])
                    P.barrier()

                with ExitStack() as s3:
                    qB = [T("qB%d" % k, [128, 4, 128], BF16, s3) for k in range(2)]
                    PTc = T("PTc", [128, 2, 512], BF16, s3)
                    PT = [T("PT%d" % i, [128, 512], BF16, s3) for i in range(4)]
                    oc2 = [T("oc%d" % j, [128, 4, 129], F32, s3) for j in range(2)]
                    rc2 = [T("rc%d" % j, [128, 4], F32, s3) for j in range(2)]
                    imp = T("imp", [128, 64], F32, s3)
                    score = T("score", [128, 64], F32, s3)
                    sc2 = T("sc2", [128, 64], F32, s3)
                    m8 = T("m8", [128, 16], F32, s3)
                    biasP = [T("biasP%d" % k, [128, 128], F32, s3) for k in range(2)]
                    oT_sb = T("oT_sb", [128, 2, 512], F32, s3)
                    rr = T("rr", [128, 2, 4], F32, s3)
                    rg = T("rg", [128, 3, 4], F32, s3)
                    bfull2 = [T("bfull%d" % j, [128, 512], F32, s3) for j in range(2)]
                    btmp2 = [T("btmp%d" % j, [128, 4, 64], F32, s3) for j in range(2)]
                    ssb = T("ssb", [128, 1], F32, s3)
                    rsb = T("rsb", [128, 1], F32, s3)
                    junkb = T("junkb", [128, 512], F32, s3)
                    mixB = [T("mixB%d" % i, [128, 512], BF16, s3) for i in range(2)]
                    for k in range(2):
                        G(lambda e, k=k: e.memset(qB[k][:], 0.0), writes=["qB%d" % k])
                        G(lambda e, k=k: e.memset(biasP[k][:], 0.0), writes=["biasP%d" % k])
                    psC = [B[4][:, 0:129], B[4][:, 129:258], B[5][:, 0:129], B[5][:, 129:258]]
                    psCk = ["B4", "B4", "B5", "B5"]
                    psB = B[4][:, 384:512]
                    psT = B[6][:, 0:260].rearrange("p (g d) -> p g d", g=4)
                    sidx = [0]
                    pidx = [0]

                    def stageA(i, k, n):
                        pr = slice(64 * k, 64 * k + 64)
                        br_ = slice(64, 128) if k == 0 else slice(0, 64)
                        qk = "qB%d" % k
                        ocn, rcn = oc2[n % 2], rc2[n % 2]
                        ock, rck = "oc%d" % (n % 2), "rc%d" % (n % 2)
                        G(lambda e: e.tensor_copy(qB[k][pr, :, :], qT_all[pr, :, i * 128:(i + 1) * 128]), reads=["qT_all"], writes=[qk])
                        yield
                        n_tiles = 1 if i <= 15 else 2
                        for nt_ in range(n_tiles):
                            M = 128 if nt_ == 0 else 127
                            bs_ = B[sidx[0] % 2]
                            bsk = "B%d" % (sidx[0] % 2)
                            sidx[0] += 1
                            a0 = 256 - 8 * i + nt_ * 128
                            op("tensor", lambda e, bs_=bs_, M=M, nt_=nt_: e.matmul(bs_[0:M, :], lhsT=kcmp[pr, nt_ * 128:nt_ * 128 + M], rhs=qB[k][pr, :, :], start=True, stop=False),
                               reads=["kcmp", qk], writes=[bsk], signal=False)
                            op("tensor", lambda e, bs_=bs_, M=M, a0=a0: e.matmul(bs_[0:M, :], lhsT=A_big[0:9, a0:a0 + M], rhs=Bm4[0:9, :, :], start=False, stop=True),
                               reads=["A_big", "Bm4"], writes=[bsk])
                            yield
                            op("scalar", lambda e, bs_=bs_, M=M, nt_=nt_: e.activation(out=PTc[0:M, nt_, :], in_=bs_[0:M, :], func=AF.Exp), reads=[bsk], writes=["PTc"])
                            yield
                        for g in range(4):
                            for nt_ in range(n_tiles):
                                M = 128 if nt_ == 0 else 127
                                op("tensor", lambda e, g=g, nt_=nt_, M=M: e.matmul(psC[g], lhsT=PTc[0:M, nt_, g * 128:(g + 1) * 128], rhs=R_cmp[0:M, nt_, k, :],
                                                                                start=(nt_ == 0), stop=(nt_ == n_tiles - 1)),
                                   reads=["PTc", "R_cmp"], writes=[psCk[g]], signal=(nt_ == n_tiles - 1))
                            if g % 2 == 1:
                                yield
                        for g in range(4):
                            op("vector", lambda e, g=g: e.tensor_copy(ocn[:, g, :], psC[g]), reads=[psCk[g]], writes=[ock])
                            if g % 2 == 1:
                                yield
                        op("vector", lambda e: e.tensor_scalar(rcn[:], ocn[:, :, 128], 1e-30, None, ALU.max), reads=[ock], writes=[rck])
                        yield
                        op("vector", lambda e: e.reciprocal(rcn[:], rcn[:]), reads=[rck], writes=[rck])
                        yield
                        if i >= 8:
                            op("vector", lambda e: e.tensor_scalar(imp[:], ocn[:, 0, 64:128], rcn[:, 0:1], None, ALU.mult), reads=[ock, rck], writes=["imp"])
                            yield
                            for g in range(1, 4):
                                op("vector", lambda e, g=g: e.scalar_tensor_tensor(out=imp[:], in0=ocn[:, g, 64:128], scalar=rcn[:, g:g + 1], in1=imp[:], op0=ALU.mult, op1=ALU.add),
                                   reads=[ock, rck, "imp"], writes=["imp"])
                                yield
                            c0 = 64 - 2 * i
                            op("vector", lambda e: e.tensor_tensor(out=score[:], in0=imp[:], in1=VM[:, c0:c0 + 64], op=ALU.mult), reads=["imp", "VM"], writes=["score"])
                            yield
                            op("vector", lambda e: e.tensor_tensor(out=score[:], in0=score[:], in1=AMk[:, c0:c0 + 64], op=ALU.add), reads=["score", "AMk"], writes=["score"])
                            yield
                            op("vector", lambda e: e.memset(score[:, 0:1], 10002.0), reads=["score"], writes=["score"])
                            yield
                            op("vector", lambda e: e.max(out=m8[:, 0:8], in_=score[:]), reads=["score"], writes=["m8"])
                            yield
                            op("vector", lambda e: e.match_replace(out=sc2[:], in_to_replace=m8[:, 0:8], in_values=score[:], imm_value=NEG), reads=["score", "m8"], writes=["sc2"])
                            yield
                            op("vector", lambda e: e.max(out=m8[:, 8:16], in_=sc2[:]), reads=["sc2"], writes=["m8"])
                            yield
                            bo = 64 if k == 0 else 0
                            op("vector", lambda e: e.tensor_scalar(biasP[k][:, bo:bo + 64], score[:], m8[:, 15:16], NEG, ALU.is_lt, ALU.mult),
                               reads=["score", "m8"], writes=["biasP%d" % k])
                            yield
                            op("tensor", lambda e: e.transpose(psB, biasP[k][:], ident_f[:]), reads=["biasP%d" % k, "ident_f", ock], writes=["B4"])
                            yield
                            op("vector", lambda e: e.tensor_copy(qB[k][br_, :, :], B[4][br_, 384:512].unsqueeze(1).to_broadcast([64, 4, 128])),
                               reads=["B4"], writes=[qk])
                            yield

                    def adv(gen, cnt):
                        for _ in range(cnt):
                            try:
                                next(gen)
                            except StopIteration:
                                return

                    def stageB(i, k, n, gnext):
                        pr = slice(64 * k, 64 * k + 64)
                        qk = "qB%d" % k
                        ocn, rcn = oc2[n % 2], rc2[n % 2]
                        ock, rck = "oc%d" % (n % 2), "rc%d" % (n % 2)
                        bfl = bfull2[i % 2]
                        bfk = "bfull%d" % (i % 2)
                        for bi, (cache, vaug, jts) in enumerate(((kTE[k], vs_aug, list(range(0, i + 1))), (kTw, vw_aug, list(range(max(0, i - 4), i + 1))))):
                            pso = B[2 + bi]
                            psok = "B%d" % (2 + bi)
                            ck = ("kTE%d" % k) if bi == 0 else "kTw"
                            for ji, jt in enumerate(jts):
                                bs_ = B[sidx[0] % 2]
                                bsk = "B%d" % (sidx[0] % 2)
                                sidx[0] += 1
                                pt = PT[pidx[0] % 4]
                                ptk = "PT%d" % (pidx[0] % 4)
                                pidx[0] += 1
                                if bi == 0:
                                    op("tensor", lambda e, bs_=bs_, cache=cache, jt=jt: e.matmul(bs_[:], lhsT=cache[:, jt * 128:(jt + 1) * 128], rhs=qB[k][:, :, :], start=True, stop=True),
                                       reads=[ck, qk], writes=[bsk])
                                else:
                                    op("tensor", lambda e, bs_=bs_, cache=cache, jt=jt: e.matmul(bs_[:], lhsT=cache[pr, jt * 128:(jt + 1) * 128], rhs=qB[k][pr, :, :], start=True, stop=True),
                                       reads=[ck, qk], writes=[bsk])
                                op("scalar", lambda e, bs_=bs_, pt=pt: e.activation(out=pt[:], in_=bs_[:], func=AF.Exp), reads=[bsk], writes=[ptk])
                                if jt == i:
                                    G(lambda e, pt=pt: e.tensor_tensor(out=pt[:].rearrange("p (g t) -> p g t", g=4), in0=pt[:].rearrange("p (g t) -> p g t", g=4),
                                                                      in1=tri_b[:, :].unsqueeze(1).to_broadcast([128, 4, 128]), op=ALU.mult), reads=[ptk, "tri_b"], writes=[ptk])
                                if bi == 1 and jt == i - 4:
                                    G(lambda e, pt=pt: e.tensor_tensor(out=pt[:].rearrange("p (g t) -> p g t", g=4), in0=pt[:].rearrange("p (g t) -> p g t", g=4),
                                                                      in1=ntri_b[:, :].unsqueeze(1).to_broadcast([128, 4, 128]), op=ALU.mult), reads=[ptk, "ntri_b"], writes=[ptk])
                                op("tensor", lambda e, pso=pso, vaug=vaug, jt=jt, pt=pt, ji=ji, nj=len(jts): e.matmul(pso[0:65, :], lhsT=vaug[:, jt, k, :], rhs=pt[:], start=(ji == 0), stop=(ji == nj - 1)),
                                   reads=[ptk, "vs_aug", "vw_aug"], writes=[psok], signal=(ji == len(jts) - 1))
                                adv(gnext, 3)
                            op("vector", lambda e, pso=pso, bi=bi: e.tensor_copy(oT_sb[0:65, bi, :], pso[0:65, :]), reads=[psok], writes=["oT_sb"])
                        adv(gnext, 1000)
                        gsl = lambda br: gates[:, i, br * 8 + k * 4: br * 8 + k * 4 + 4]
                        op("vector", lambda e: e.tensor_tensor(out=rg[:, 0, :], in0=rcn[:], in1=gsl(0), op=ALU.mult), reads=[rck, "gates"], writes=["rg"])
                        op("vector", lambda e: e.tensor_tensor(out=bfl[:, k * 256:(k + 1) * 256].rearrange("p (g d) -> p g d", g=4), in0=ocn[:, :, 0:64],
                                                               in1=rg[:, 0, :].unsqueeze(2).to_broadcast([128, 4, 64]), op=ALU.mult), reads=[ock, "rg"], writes=[bfk])
                        for bi in range(2):
                            for g in range(4):
                                op("tensor", lambda e, bi=bi, g=g: e.transpose(psT[:, g, :], oT_sb[0:65, bi, g * 128:(g + 1) * 128], ident_f[0:65, 0:65]),
                                   reads=["oT_sb", "ident_f"], writes=["B6"], signal=(g == 3))
                            op("vector", lambda e, bi=bi: e.tensor_scalar(rr[:, bi, :], psT[:, :, 64], 1e-30, None, ALU.max), reads=["B6"], writes=["rr"])
                            op("vector", lambda e, bi=bi: e.reciprocal(rr[:, bi, :], rr[:, bi, :]), reads=["rr"], writes=["rr"])
                            op("vector", lambda e, bi=bi: e.tensor_tensor(out=rg[:, 1 + bi, :], in0=rr[:, bi, :], in1=gsl(1 + bi), op=ALU.mult), reads=["rr", "gates"], writes=["rg"])
                            op("vector", lambda e, bi=bi: e.tensor_tensor(out=btmp2[bi][:], in0=psT[:, :, 0:64], in1=rg[:, 1 + bi, :].unsqueeze(2).to_broadcast([128, 4, 64]), op=ALU.mult),
                               reads=["B6", "rg"], writes=["btmp%d" % bi])
                            G(lambda e, bi=bi: e.tensor_tensor(out=bfl[:, k * 256:(k + 1) * 256], in0=bfl[:, k * 256:(k + 1) * 256], in1=btmp2[bi][:].rearrange("p g d -> p (g d)"), op=ALU.add),
                              reads=[bfk, "btmp%d" % bi], writes=[bfk])

                    steps = [(i, k) for i in range(NT) for k in range(2)]
                    adv(stageA(steps[0][0], steps[0][1], 0), 1000)
                    for n, (i, k) in enumerate(steps):
                        gnext = stageA(steps[n + 1][0], steps[n + 1][1], n + 1) if n + 1 < len(steps) else iter(())
                        stageB(i, k, n, gnext)
                        if k == 1:
                            bfl = bfull2[i % 2]
                            bfk = "bfull%d" % (i % 2)
                            rms_stats(bfl[:], 512, bfk, junkb[:], ssb, rsb, "b")
                            mB = mixB[i % 2]
                            mk = "mixB%d" % (i % 2)
                            op("vector", lambda e, mB=mB, bfl=bfl: e.scalar_tensor_tensor(out=mB[:], in0=bfl[:], scalar=rsb[:, 0:1], in1=onbw[:], op0=ALU.mult, op1=ALU.mult),
                               reads=[bfk, "rstdb", "onbw"], writes=[mk])
                            P.dma("gpsimd", mix_d[i * 128:(i + 1) * 128, 512:1024], mB[:], reads=[mk], writes=["mix_d"])
                    P.barrier()
            if debug == "p3":
                return
            with ExitStack() as s4:
                wo_b = T("wo_b", [128, 8, D], BF16, s4)
                wg_b = T("wg_b", [128, 8, DFF], BF16, s4)
                wu_b = T("wu_b", [128, 8, DFF], BF16, s4)
                wd_b = T("wd_b", [128, NFC, D], BF16, s4)
                nfw = T("nfw", [128, D], F32, s4)
                fnw = T("fnw", [128, D], F32, s4)
                P.dma("sync", nfw[:], norm_ffn_w[l].partition_broadcast(128), writes=["nfw"])
                P.dma("sync", fnw[:], final_norm_w.partition_broadcast(128), writes=["fnw"])
                s4a = ExitStack()
                stg = [T("stg%d" % i, [128, DFF], F32, s4a) for i in range(2)]
                si = 0
                for (wsrc, wdst, nck, wid, key) in ((w_o, wo_b, 8, D, "wo_b"), (w_gate, wg_b, 8, DFF, "wg_b"), (w_up, wu_b, 8, DFF, "wu_b"), (w_down, wd_b, NFC, D, "wd_b")):
                    for c in range(nck):
                        sg = stg[si % 2]
                        sk = "stg%d" % (si % 2)
                        P.dma("sync" if si % 2 == 0 else "scalar", sg[:, 0:wid], wsrc[l, c * 128:(c + 1) * 128, :], writes=[sk])
                        eng = ("gpsimd", "vector")[si % 2]
                        op(eng, lambda e, wdst=wdst, c=c, sg=sg, wid=wid: e.tensor_copy(wdst[:, c, :], sg[:, 0:wid]), reads=[sk], writes=[key])
                        si += 1
                P.barrier()
                s4a.close()
                mixt = [T("mixt%d" % i, [128, D], BF16, s4) for i in range(2)]
                mixT = T("mixT", [128, 8, 128], BF16, s4)
                x1 = T("x1", [128, 2, D], F32, s4)
                junk4 = T("junk4", [128, D], BF16, s4)
                ss4 = T("ss4", [128, 1], F32, s4)
                rs4 = T("rs4", [128, 1], F32, s4)
                h2 = T("h2", [128, D], BF16, s4)
                h2T = T("h2T", [128, 8, 256], BF16, s4)
                sgt = [T("sgt%d" % i, [128, 256], F32, s4) for i in range(2)]
                actT = T("actT", [128, NFC, 256], BF16, s4)
                x2 = [T("x2_%d" % i, [128, D], F32, s4) for i in range(1)]
                ss5 = T("ss5", [128, 1], F32, s4)
                rs5 = T("rs5", [128, 1], F32, s4)
                for TT in range(16):
                    for tt in range(2):
                        i = TT * 2 + tt
                        mt = mixt[i % 2]
                        mtk = "mixt%d" % (i % 2)
                        P.dma("sync", mt[:], mix_d[i * 128:(i + 1) * 128, :], reads=["mix_d"], writes=[mtk])
                        P.dma("scalar", x1[:, tt, :], xsrc[i * 128:(i + 1) * 128, :], reads=["xres_d"], writes=["x1"])
                        for c in range(8):
                            op("tensor", lambda e, c=c, mt=mt: e.transpose(BT[:, c, :], mt[:, c * 128:(c + 1) * 128], ident_b[:]), reads=[mtk, "ident_b"], writes=["BT"], signal=(c == 7))
                        op("vector", lambda e: e.tensor_copy(mixT[:], BT[:]), reads=["BT"], writes=["mixT"])
                        for half in range(2):
                            for c in range(8):
                                op("tensor", lambda e, half=half, c=c: e.matmul(B[half][:], lhsT=mixT[:, c, :], rhs=wo_b[:, c, half * 512:(half + 1) * 512], start=(c == 0), stop=(c == 7)),
                                   reads=["mixT", "wo_b"], writes=["B%d" % half], signal=(c == 7))
                            op("vector", lambda e, half=half, tt=tt: e.tensor_tensor(out=x1[:, tt, half * 512:(half + 1) * 512], in0=B[half][:], in1=x1[:, tt, half * 512:(half + 1) * 512], op=ALU.add),
                               reads=["B%d" % half, "x1"], writes=["x1"])
                        rms_stats(x1[:, tt, :], D, "x1", junk4[:], ss4, rs4, "4")
                        op("vector", lambda e, tt=tt: e.scalar_tensor_tensor(out=h2[:], in0=x1[:, tt, :], scalar=rs4[:, 0:1], in1=nfw[:], op0=ALU.mult, op1=ALU.mult),
                           reads=["x1", "rstd4", "nfw"], writes=["h2"])
                        for c in range(8):
                            op("tensor", lambda e, c=c: e.transpose(BT[:, c, :], h2[:, c * 128:(c + 1) * 128], ident_b[:]), reads=["h2", "ident_b"], writes=["BT"], signal=(c == 7))
                        op("vector", lambda e, tt=tt: e.tensor_copy(h2T[:, :, tt * 128:(tt + 1) * 128], BT[:]), reads=["BT"], writes=["h2T"])
                    for fc in range(NFC):
                        pg = B[2 + 2 * (fc % 2)]
                        pu = B[3 + 2 * (fc % 2)]
                        pgk = "B%d" % (2 + 2 * (fc % 2))
                        puk = "B%d" % (3 + 2 * (fc % 2))
                        for c in range(8):
                            op("tensor", lambda e, pg=pg, c=c, fc=fc: e.matmul(pg[:, 0:256], lhsT=wg_b[:, c, fc * 128:(fc + 1) * 128], rhs=h2T[:, c, :], start=(c == 0), stop=(c == 7)),
                               reads=["wg_b", "h2T"], writes=[pgk], signal=(c == 7))
                        for c in range(8):
                            op("tensor", lambda e, pu=pu, c=c, fc=fc: e.matmul(pu[:, 0:256], lhsT=wu_b[:, c, fc * 128:(fc + 1) * 128], rhs=h2T[:, c, :], start=(c == 0), stop=(c == 7)),
                               reads=["wu_b", "h2T"], writes=[puk], signal=(c == 7))
                        sg_ = sgt[fc % 2]
                        sgk = "sgt%d" % (fc % 2)
                        op("scalar", lambda e, sg_=sg_, pg=pg: e.activation(out=sg_[:], in_=pg[:, 0:256], func=AF.Silu), reads=[pgk], writes=[sgk])
                        op("vector", lambda e, sg_=sg_, pu=pu, fc=fc: e.tensor_tensor(out=actT[:, fc, :], in0=pu[:, 0:256], in1=sg_[:], op=ALU.mult), reads=[puk, sgk], writes=["actT"])
                    for tt in range(2):
                        i = TT * 2 + tt
                        x2i = x2[0]
                        x2k = "x2_0"
                        for half in range(2):
                            pd = B[(0, 6)[half]]
                            pdk = "B%d" % ((0, 6)[half])
                            for fc in range(NFC):
                                op("tensor", lambda e, pd=pd, fc=fc, tt=tt, half=half: e.matmul(pd[:], lhsT=actT[:, fc, tt * 128:(tt + 1) * 128], rhs=wd_b[:, fc, half * 512:(half + 1) * 512],
                                                                                      start=(fc == 0), stop=(fc == NFC - 1)),
                                   reads=["actT", "wd_b"], writes=[pdk], signal=(fc == NFC - 1))
                            op("vector", lambda e, pd=pd, half=half, tt=tt, x2i=x2i: e.tensor_tensor(out=x2i[:, half * 512:(half + 1) * 512], in0=pd[:], in1=x1[:, tt, half * 512:(half + 1) * 512], op=ALU.add),
                               reads=[pdk, "x1"], writes=[x2k])
                        if not last:
                            P.dma("gpsimd", xres_d[i * 128:(i + 1) * 128, :], x2i[:], reads=[x2k], writes=["xres_d"])
                        else:
                            rms_stats(x2i[:], D, x2k, junk4[:], ss5, rs5, "5")
                            op("vector", lambda e, x2i=x2i: e.scalar_tensor_tensor(out=x2i[:], in0=x2i[:], scalar=rs5[:, 0:1], in1=fnw[:], op0=ALU.mult, op1=ALU.mult),
                               reads=[x2k, "rstd5", "fnw"], writes=[x2k])
                            P.dma("gpsimd", out[i * 128:(i + 1) * 128, :], x2i[:], reads=[x2k], writes=["out"])
                P.barrier()
        for l_ in range(nlayers):
            layer(l_)
        P.finish("gpsimd", ["out", "mix_d", "xres_d"])
        P.barrier()
        with nc.Block() as block:
            P.emit(block)
    print("instructions:", P.n_inst)
    return nc


_NAMES = ["norm_mix_w", "w_in", "gmlp_norm_w", "gmlp_ws", "gmlp_bs", "cmp_pos_k", "cmp_pos_v", "cmp_k_w1", "cmp_k_w2",
          "cmp_v_w1", "cmp_v_w2", "gate_b", "out_norm_a_w", "out_norm_b_w", "w_o", "norm_ffn_w", "w_gate", "w_up",
          "w_down", "final_norm_w"]


def kernel(**inputs):
    x = np.ascontiguousarray(np.asarray(inputs["x"], dtype=np.float32))
    shared = {n: np.ascontiguousarray(np.asarray(inputs[n], dtype=np.float32)) for n in _NAMES}
    nc = build()
    in_maps = [dict(shared, x=x[b]) for b in range(8)]
    res = run_bass_kernel_spmd(nc, in_maps, core_ids=list(range(8)))
    return np.stack([np.asarray(r["out"], dtype=np.float32) for r in res.results], axis=0)
```

```python
import numpy as np
import concourse.bass as bass
import concourse.mybir as mybir
from concourse.bass_utils import run_bass_kernel_spmd

F32 = mybir.dt.float32
BF16 = mybir.dt.bfloat16
AF = mybir.ActivationFunctionType
ALU = mybir.AluOpType
AX = mybir.AxisListType


class Prog:
    ENGS = ("sync", "scalar", "vector", "gpsimd", "tensor")
    NDMA = 8
    R = 8

    def __init__(self, nc, stack):
        self.nc = nc
        self.q = {e: [] for e in self.ENGS}
        self.sem = {e: [stack.enter_context(nc.semaphore("s_%s%d" % (e, i))) for i in range(self.R)]
                    for e in self.ENGS}
        self.cnt = {e: 0 for e in self.ENGS}
        self.dsem = {e: [stack.enter_context(nc.semaphore("d_%s%d" % (e, i))) for i in range(self.NDMA)]
                     for e in ("sync", "scalar", "gpsimd")}
        self.dcnt = {e: 0 for e in self.dsem}
        self.semobj = {}
        for e in self.ENGS:
            for i in range(self.R):
                self.semobj[("c", e, i)] = self.sem[e][i]
        for e in self.dsem:
            for i in range(self.NDMA):
                self.semobj[("d", e, i)] = self.dsem[e][i]
        self.waited = {e: {} for e in self.ENGS}
        self.lastw = {}
        self.readers = {}
        self.n_inst = 0

    def _waits(self, eng, deps):
        need = {}
        for (sid, val) in deps:
            if sid[0] == "c" and sid[1] == eng and (val - 1) * self.R + sid[2] + 1 > self.cnt[eng]:
                continue
            if self.waited[eng].get(sid, 0) >= val:
                continue
            if need.get(sid, 0) < val:
                need[sid] = val
        out = []
        for sid, val in need.items():
            self.waited[eng][sid] = val
            out.append((self.semobj[sid], val))
        return out

    def _deps(self, reads, writes):
        deps = []
        for k in reads:
            if k in self.lastw:
                deps.append(self.lastw[k])
        for k in writes:
            if k in self.lastw:
                deps.append(self.lastw[k])
            deps.extend(self.readers.get(k, ()))
        return deps

    def _commit(self, tok, reads, writes):
        for k in reads:
            self.readers.setdefault(k, []).append(tok)
        for k in writes:
            self.lastw[k] = tok
            self.readers[k] = []

    def op(self, eng, fn, reads=(), writes=(), signal=True):
        waits = self._waits(eng, self._deps(reads, writes))
        n = self.cnt[eng]
        tok = (("c", eng, n % self.R), n // self.R + 1)
        if signal:
            self.cnt[eng] += 1
        sem = self.sem[eng][n % self.R]

        def run(e, fn=fn, waits=waits, signal=signal, sem=sem):
            for (s, v) in waits:
                e.wait_ge(s, v)
            ins = fn(e)
            if signal:
                ins.then_inc(sem, 1)

        self.q[eng].append(run)
        self._commit(tok, reads, writes)
        self.n_inst += 1

    def dma(self, eng, out, in_, reads=(), writes=(), **kw):
        n = self.dcnt[eng]
        self.dcnt[eng] += 1
        slot = n % self.NDMA
        val = 16 * (n // self.NDMA + 1)
        sid = ("d", eng, slot)
        deps = self._deps(reads, writes)
        if val > 16:
            deps.append((sid, val - 16))
        waits = self._waits(eng, deps)
        sem = self.dsem[eng][slot]

        def run(e, waits=waits, sem=sem, out=out, in_=in_, kw=kw):
            for (s, v) in waits:
                e.wait_ge(s, v)
            e.dma_start(out=out, in_=in_, **kw).then_inc(sem, 16)

        self.q[eng].append(run)
        self._commit((sid, val), reads, writes)
        self.n_inst += 1

    def barrier(self):
        deps = []
        for e in self.ENGS:
            n = self.cnt[e]
            for i in range(self.R):
                if n >= i + 1:
                    deps.append((("c", e, i), (n - 1 - i) // self.R + 1))
        for e in self.dsem:
            n = self.dcnt[e]
            for i in range(self.NDMA):
                if n >= i + 1:
                    deps.append((("d", e, i), 16 * ((n - 1 - i) // self.NDMA + 1)))
        for e in self.ENGS:
            waits = self._waits(e, deps)

            def run(en, waits=waits):
                for (s, v) in waits:
                    en.wait_ge(s, v)

            self.q[e].append(run)

    def finish(self, eng, keys):
        waits = self._waits(eng, self._deps(keys, ()))

        def run(e, waits=waits):
            for (s, v) in waits:
                e.wait_ge(s, v)

        self.q[eng].append(run)

    def emit(self, block):
        q = self.q

        @block.sync
        def _(e):
            for f in q["sync"]:
                f(e)

        @block.scalar
        def _(e):
            for f in q["scalar"]:
                f(e)

        @block.vector
        def _(e):
            for f in q["vector"]:
                f(e)

        @block.gpsimd
        def _(e):
            for f in q["gpsimd"]:
                f(e)

        @block.tensor
        def _(e):
            for f in q["tensor"]:
                f(e)


S = 4096
D = 1024
NT = 32
DFF = 2816
NFC = 22
NEG = -30000.0
L = 2


def build(debug=None, nlayers=L):
    from contextlib import ExitStack
    nc = bass.Bass("TRN2", target_bir_lowering=False)
    dt_in = lambda name, shape: nc.dram_tensor(name, shape, F32, kind="ExternalInput").ap()
    x_in = dt_in("x", [S, D])
    norm_mix_w = dt_in("norm_mix_w", [L, D])
    w_in = dt_in("w_in", [L, D, 2328])
    gmlp_norm_w = dt_in("gmlp_norm_w", [L, 512])
    gmlp_ws = dt_in("gmlp_ws", [L, 8, 128, 128])
    gmlp_bs = dt_in("gmlp_bs", [L, 8, 128])
    cmp_pos = {"k": dt_in("cmp_pos_k", [L, 32, 64]), "v": dt_in("cmp_pos_v", [L, 32, 64])}
    cmp_w1 = {"k": dt_in("cmp_k_w1", [L, 2048, 256]), "v": dt_in("cmp_v_w1", [L, 2048, 256])}
    cmp_w2 = {"k": dt_in("cmp_k_w2", [L, 256, 64]), "v": dt_in("cmp_v_w2", [L, 256, 64])}
    gate_b = dt_in("gate_b", [L, 24])
    out_norm_a_w = dt_in("out_norm_a_w", [L, 512])
    out_norm_b_w = dt_in("out_norm_b_w", [L, 512])
    w_o = dt_in("w_o", [L, D, D])
    norm_ffn_w = dt_in("norm_ffn_w", [L, D])
    w_gate = dt_in("w_gate", [L, D, DFF])
    w_up = dt_in("w_up", [L, D, DFF])
    w_down = dt_in("w_down", [L, DFF, D])
    final_norm_w = dt_in("final_norm_w", [D])
    out = nc.dram_tensor("out", [S, D], F32, kind="ExternalOutput").ap()
    dbg = debug is not None
    mix_d = nc.dram_tensor("mix_d", [S, D], BF16, kind="ExternalOutput" if dbg else "Internal").ap()
    xres_d = nc.dram_tensor("xres_d", [S, D], F32, kind="ExternalOutput" if dbg else "Internal").ap()

    with ExitStack() as st:
        P = Prog(nc, st)
        op = P.op

        sfx = [""]

        def T(name, shape, dt, stack=None):
            return (stack or st).enter_context(nc.sbuf_tensor(name + sfx[0], shape, dt))

        B = [st.enter_context(nc.psum_tensor("B%d" % i, [128, 512], F32)) for i in range(7)]
        BT = st.enter_context(nc.psum_tensor("BT", [128, 8, 128], BF16))

        ident_b = T("ident_b", [128, 128], BF16)
        ident_f = T("ident_f", [128, 128], F32)
        tri_f = T("tri_f", [128, 128], F32)
        tri_b = T("tri_b", [128, 128], BF16)
        ntri_b = T("ntri_b", [128, 128], BF16)
        ones_f = T("ones_f", [128, 128], F32)
        ind8 = T("ind8", [8, 512], F32)
        A_big = T("A_big", [16, 512], BF16)
        Bm4 = T("Bm4", [16, 4, 128], BF16)
        VM = T("VM", [128, 128], F32)
        AMk = T("AMk", [128, 128], F32)
        tmpc = T("tmpc", [128, 512], F32)
        tmpc2 = T("tmpc2", [128, 512], F32)

        def G(fn, reads=(), writes=()):
            op("gpsimd", fn, reads=reads, writes=writes)

        def asel(t, pattern, cmp_, fill, base, cm, key):
            G(lambda e: e.affine_select(out=t, in_=t, pattern=pattern, compare_op=cmp_, fill=fill,
                                        base=base, channel_multiplier=cm), reads=[key], writes=[key])

        G(lambda e: e.memset(ones_f[:], 1.0), writes=["ones_f"])
        G(lambda e: e.memset(ident_f[:], 1.0), writes=["ident_f"])
        asel(ident_f[:], [[-1, 128]], ALU.is_equal, 0.0, 0, 1, "ident_f")
        G(lambda e: e.tensor_copy(ident_b[:], ident_f[:]), reads=["ident_f"], writes=["ident_b"])
        G(lambda e: e.memset(tri_f[:], 1.0), writes=["tri_f"])
        asel(tri_f[:], [[1, 128]], ALU.is_ge, 0.0, 0, -1, "tri_f")
        G(lambda e: e.tensor_copy(tri_b[:], tri_f[:]), reads=["tri_f"], writes=["tri_b"])
        G(lambda e: e.memset(tmpc[:, 0:128], 1.0), writes=["tmpc"])
        asel(tmpc[:, 0:128], [[-1, 128]], ALU.is_gt, 0.0, 0, 1, "tmpc")
        G(lambda e: e.tensor_copy(ntri_b[:], tmpc[:, 0:128]), reads=["tmpc"], writes=["ntri_b"])
        G(lambda e: e.memset(ind8[:], 1.0), writes=["ind8"])
        asel(ind8[:].rearrange("p (h d) -> p h d", h=8), [[1, 8], [0, 64]], ALU.is_equal, 0.0, 0, -1, "ind8")
        G(lambda e: e.memset(tmpc[0:16, :], 1.0), writes=["tmpc"])
        asel(tmpc[0:16, :], [[1, 512]], ALU.is_equal, 0.0, -255, -1, "tmpc")
        G(lambda e: e.memset(tmpc2[0:16, :], 1.0), writes=["tmpc2"])
        asel(tmpc2[0:16, :], [[1, 512]], ALU.is_ge, 0.0, -263, 0, "tmpc2")
        asel(tmpc2[0:16, :], [[0, 512]], ALU.is_equal, 0.0, -8, 1, "tmpc2")
        asel(tmpc[0:16, :], [[0, 512]], ALU.is_ge, 0.0, 7, -1, "tmpc")
        G(lambda e: e.tensor_tensor(out=A_big[:], in0=tmpc[0:16, :], in1=tmpc2[0:16, :], op=ALU.add),
          reads=["tmpc", "tmpc2"], writes=["A_big"])
        G(lambda e: e.memset(tmpc[0:16, :], NEG), writes=["tmpc"])
        asel(tmpc[0:16, :].rearrange("p (g t) -> p g t", g=4), [[0, 4], [-1, 128]], ALU.is_gt, 0.0, 15, 16, "tmpc")
        asel(tmpc[0:16, :], [[0, 512]], ALU.is_ge, 0.0, 7, -1, "tmpc")
        G(lambda e: e.memset(tmpc2[0:16, :], NEG), writes=["tmpc2"])
        asel(tmpc2[0:16, :], [[0, 512]], ALU.is_equal, 0.0, -8, 1, "tmpc2")
        G(lambda e: e.tensor_tensor(out=Bm4[:].rearrange("p g t -> p (g t)"), in0=tmpc[0:16, :], in1=tmpc2[0:16, :], op=ALU.add),
          reads=["tmpc", "tmpc2"], writes=["Bm4"])
        G(lambda e: e.memset(VM[:], 1.0), writes=["VM"])
        asel(VM[:], [[-64, 128]], ALU.is_ge, 0.0, 64 * 64 - 128, 1, "VM")
        G(lambda e: e.memset(AMk[:], 10000.0), writes=["AMk"])
        asel(AMk[:], [[-64, 128]], ALU.is_ge, 0.0, 64 * 64, 1, "AMk")
        asel(AMk[:], [[64, 128]], ALU.is_ge, 0.0, -64 * 64 + 63, -1, "AMk")
        G(lambda e: e.memset(tmpc[:, 0:128], 10001.0), writes=["tmpc"])
        asel(tmpc[:, 0:128], [[-64, 128]], ALU.is_ge, 0.0, 64 * 64 - 64, 1, "tmpc")
        asel(tmpc[:, 0:128], [[64, 128]], ALU.is_ge, 0.0, -64 * 64 + 64 + 63, -1, "tmpc")
        G(lambda e: e.tensor_tensor(out=AMk[:], in0=AMk[:], in1=tmpc[:, 0:128], op=ALU.add), reads=["AMk", "tmpc"], writes=["AMk"])
        G(lambda e: e.memset(tmpc2[:, 0:128], -10000.0), writes=["tmpc2"])
        asel(tmpc2[:, 0:128], [[64, 128]], ALU.is_ge, 0.0, -64 * 64 - 1, -1, "tmpc2")
        G(lambda e: e.tensor_tensor(out=AMk[:], in0=AMk[:], in1=tmpc2[:, 0:128], op=ALU.add), reads=["AMk", "tmpc2"], writes=["AMk"])


        def rms_stats(src_ap, width, key_src, junk, ssum, rstd, tag):
            sc = float(width) ** -0.5
            op("scalar", lambda e: e.activation(out=junk, in_=src_ap, func=AF.Square, scale=sc, accum_out=ssum[:, 0:1]),
               reads=[key_src], writes=["junk" + tag, "ss" + tag])
            op("vector", lambda e: e.tensor_scalar(rstd[:, 0:1], ssum[:, 0:1], 1e-6, None, ALU.add), reads=["ss" + tag], writes=["rstd" + tag])
            op("scalar", lambda e: e.activation(out=rstd[:, 0:1], in_=rstd[:, 0:1], func=AF.Sqrt), reads=["rstd" + tag], writes=["rstd" + tag])
            op("vector", lambda e: e.reciprocal(rstd[:, 0:1], rstd[:, 0:1]), reads=["rstd" + tag], writes=["rstd" + tag])

        def layer(l):
            sfx[0] = "_L%d" % l
            xsrc = x_in if l == 0 else xres_d
            last = (l == nlayers - 1)
            P.barrier()
            with ExitStack() as s1:
                nmw = T("nmw", [128, D], F32, s1)
                gnw = T("gnw", [128, 512], F32, s1)
                onaw = T("onaw", [128, 512], F32, s1)
                onbw = T("onbw", [128, 512], F32, s1)
                gbt = T("gbt", [128, 24], F32, s1)
                bs8 = T("bs8", [8, 128], F32, s1)
                gates = T("gates", [128, NT, 24], F32, s1)
                P.dma("sync", nmw[:], norm_mix_w[l].partition_broadcast(128), writes=["nmw"])
                P.dma("sync", gnw[:], gmlp_norm_w[l].partition_broadcast(128), writes=["gnw"])
                P.dma("sync", onaw[:], out_norm_a_w[l].partition_broadcast(128), writes=["onaw"])
                P.dma("sync", onbw[:], out_norm_b_w[l].partition_broadcast(128), writes=["onbw"])
                P.dma("sync", gbt[:], gate_b[l].partition_broadcast(128), writes=["gbt"])
                P.dma("sync", bs8[:], gmlp_bs[l], writes=["bs8"])

                wtm = T("wtm", [128, 8, 1304], BF16, s1)
                wfm = T("wfm", [128, 8, 1024], BF16, s1)
                wsT = T("wsT", [128, 8, 128], BF16, s1)
                kTE = [T("kTE%d" % k, [128, S], BF16, s1) for k in range(2)]
                kTw = T("kTw", [128, S], BF16, s1)
                kcT = T("kcT", [128, S], BF16, s1)
                vcT = T("vcT", [128, S], BF16, s1)
                vs_aug = T("vs_aug", [128, NT, 2, 65], BF16, s1)
                vw_aug = T("vw_aug", [128, NT, 2, 65], BF16, s1)
                qT_all = T("qT_all", [128, 4, S], BF16, s1)
                w2k_pad = T("w2k_pad", [128, 2, 2, 128], BF16, s1)
                w2v = T("w2v", [128, 2, 64], BF16, s1)
                hidT = T("hidT", [128, 2, 256], BF16, s1)
                kcmp = T("kcmp", [128, 256], BF16, s1)
                R_cmp = T("R_cmp", [128, 2, 2, 129], BF16, s1)

                with ExitStack() as s0:
                    stage = [T("stage%d" % i, [128, 2328], F32, s0) for i in range(2)]
                    for c in range(8):
                        sg = stage[c % 2]
                        sk = "stage%d" % (c % 2)
                        P.dma("sync", sg[:], w_in[l, c * 128:(c + 1) * 128, :], writes=[sk])
                        cp = [
                            (wtm[:, c, 0:1024], sg[:, 0:1024]),
                            (wtm[:, c, 1024:1152], sg[:, 1920:2048]),
                            (wtm[:, c, 1152:1280], sg[:, 2176:2304]),
                            (wtm[:, c, 1280:1304], sg[:, 2304:2328]),
                            (wfm[:, c, 0:512].rearrange("p (g k d) -> p g k d", g=4, k=2),
                             sg[:, 1024:1536].rearrange("p (k g d) -> p g k d", k=2, g=4)),
                            (wfm[:, c, 512:640], sg[:, 1536:1664]),
                            (wfm[:, c, 640:768], sg[:, 1664:1792]),
                            (wfm[:, c, 768:896], sg[:, 1792:1920]),
                            (wfm[:, c, 896:1024], sg[:, 2048:2176]),
                        ]
                        for ci, (o_, i_) in enumerate(cp):
                            eng = "gpsimd" if ci % 2 == 0 else "vector"
                            op(eng, lambda e, o_=o_, i_=i_: e.tensor_copy(o_, i_), reads=[sk], writes=["wtm" if ci < 4 else "wfm"])
                    wsf = T("wsf", [128, 8, 128], F32, s0)
                    P.dma("sync", wsf[:], gmlp_ws[l].rearrange("h t s -> t h s"), writes=["wsf"])
                    for h in range(8):
                        bk = B[h % 2]
                        op("tensor", lambda e, h=h, bk=bk: e.transpose(bk[:, 0:128], wsf[:, h, :], ident_f[:]),
                           reads=["wsf", "ident_f"], writes=["B%d" % (h % 2)])
                        op("vector", lambda e, h=h, bk=bk: e.tensor_tensor(out=wsT[:, h, :], in0=bk[:, 0:128], in1=tri_f[:], op=ALU.mult),
                           reads=["B%d" % (h % 2), "tri_f"], writes=["wsT"])
                    for nt_ in range(2):
                        G(lambda e: e.memset(tmpc[:, 0:64], 1.0), writes=["tmpc"])
                        asel(tmpc[:, 0:64], [[-64, 64]], ALU.is_ge, 0.0, 2048 * nt_ + 31, 16, "tmpc")
                        asel(tmpc[:, 0:64], [[64, 64]], ALU.is_ge, 0.0, 63 - 2048 * nt_, -16, "tmpc")
                        for k in range(2):
                            G(lambda e, nt_=nt_, k=k: e.tensor_copy(R_cmp[:, nt_, k, 64:128], tmpc[:, 0:64]), reads=["tmpc"], writes=["R_cmp"])
                    G(lambda e: e.memset(R_cmp[:, :, :, 128:129], 1.0), writes=["R_cmp"])
                    G(lambda e: e.memset(vs_aug[:, :, :, 64:65], 1.0), writes=["vs_aug"])
                    G(lambda e: e.memset(vw_aug[:, :, :, 64:65], 1.0), writes=["vw_aug"])
                    G(lambda e: e.memset(kTE[0][:], 1.0), writes=["kTE0"])
                    asel(kTE[0][:].rearrange("p (j r) -> p j r", j=64), [[1, 64], [0, 64]], ALU.is_equal, 0.0, 64, -1, "kTE0")
                    G(lambda e: e.memset(kTE[1][:], 1.0), writes=["kTE1"])
                    asel(kTE[1][:].rearrange("p (j r) -> p j r", j=64), [[1, 64], [0, 64]], ALU.is_equal, 0.0, 0, -1, "kTE1")
                    P.barrier()

                with ExitStack() as s2:
                    xt = [T("xt%d" % i, [128, D], F32, s2) for i in range(2)]
                    junk = T("junk", [128, D], BF16, s2)
                    ssx = T("ssx", [128, 1], F32, s2)
                    rsx = T("rsx", [128, 1], F32, s2)
                    hb = T("hb", [128, D], BF16, s2)
                    hT = [T("hT%d" % i, [128, 8, 512], BF16, s2) for i in range(2)]
                    gu = T("gu", [128, 512], F32, s2)
                    gv = T("gv", [128, 512], F32, s2)
                    ssv = T("ssv", [128, 1], F32, s2)
                    rsv = T("rsv", [128, 1], F32, s2)
                    vn = T("vn", [128, 512], BF16, s2)
                    a_t = T("a_t", [128, 512], F32, s2)
                    ssa = T("ssa", [128, 1], F32, s2)
                    rsa = T("rsa", [128, 1], F32, s2)
                    mixA = [T("mixA%d" % i, [128, 512], BF16, s2) for i in range(2)]
                    gpre = T("gpre", [128, 24], F32, s2)
                    for TT in range(8):
                        hTt = hT[TT % 2]
                        hk = "hT%d" % (TT % 2)
                        for tt in range(4):
                            i = TT * 4 + tt
                            xti = xt[i % 2]
                            xk = "xt%d" % (i % 2)
                            P.dma("sync", xti[:], xsrc[i * 128:(i + 1) * 128, :], writes=[xk])
                            rms_stats(xti[:], D, xk, junk[:], ssx, rsx, "x")
                            op("vector", lambda e, xti=xti: e.scalar_tensor_tensor(out=hb[:], in0=xti[:], scalar=rsx[:, 0:1], in1=nmw[:], op0=ALU.mult, op1=ALU.mult),
                               reads=[xk, "rstdx", "nmw"], writes=["hb"])
                            for c in range(8):
                                op("tensor", lambda e, c=c: e.transpose(BT[:, c, :], hb[:, c * 128:(c + 1) * 128], ident_b[:]),
                                   reads=["hb", "ident_b"], writes=["BT"], signal=(c == 7))
                            op("vector", lambda e, hTt=hTt, tt=tt: e.tensor_copy(hTt[:, :, tt * 128:(tt + 1) * 128], BT[:]), reads=["BT"], writes=[hk])
                            for cb, (c0, c1) in enumerate(((0, 512), (512, 1024), (1024, 1304))):
                                for c in range(8):
                                    op("tensor", lambda e, cb=cb, c=c, c0=c0, c1=c1, hTt=hTt, tt=tt: e.matmul(B[cb][:, 0:c1 - c0], lhsT=hTt[:, c, tt * 128:(tt + 1) * 128], rhs=wtm[:, c, c0:c1],
                                                                                                      start=(c == 0), stop=(c == 7)),
                                       reads=[hk, "wtm"], writes=["B%d" % cb], signal=(c == 7))
                            op("scalar", lambda e: e.activation(out=gu[:], in_=B[0][:], func=AF.Gelu_apprx_tanh), reads=["B0"], writes=["gu"])
                            op("scalar", lambda e: e.activation(out=gv[:], in_=B[1][:], func=AF.Gelu_apprx_tanh), reads=["B1"], writes=["gv"])
                            rms_stats(gv[:], 512, "gv", junk[:, 0:512], ssv, rsv, "v")
                            op("vector", lambda e: e.scalar_tensor_tensor(out=vn[:], in0=gv[:], scalar=rsv[:, 0:1], in1=gnw[:], op0=ALU.mult, op1=ALU.mult),
                               reads=["gv", "rstdv", "gnw"], writes=["vn"])
                            op("vector", lambda e, i=i: e.tensor_copy(vs_aug[:, i, :, 0:64], B[2][:, 0:128].rearrange("p (k d) -> p k d", k=2)), reads=["B2"], writes=["vs_aug"])
                            op("vector", lambda e, i=i: e.tensor_copy(vw_aug[:, i, :, 0:64], B[2][:, 128:256].rearrange("p (k d) -> p k d", k=2)), reads=["B2"], writes=["vw_aug"])
                            op("vector", lambda e: e.tensor_tensor(out=gpre[:], in0=B[2][:, 256:280], in1=gbt[:], op=ALU.add), reads=["B2", "gbt"], writes=["gpre"])
                            op("scalar", lambda e, i=i: e.activation(out=gates[:, i, :], in_=gpre[:], func=AF.Sigmoid), reads=["gpre"], writes=["gates"])
                            op("tensor", lambda e: e.matmul(B[3][:], lhsT=bs8[:], rhs=ind8[:], start=True, stop=False), reads=["bs8", "ind8"], writes=["B3"], signal=False)
                            for h in range(8):
                                op("tensor", lambda e, h=h: e.matmul(B[3][:, h * 64:(h + 1) * 64], lhsT=wsT[:, h, :], rhs=vn[:, h * 64:(h + 1) * 64], start=False, stop=(h == 7)),
                                   reads=["wsT", "vn"], writes=["B3"], signal=(h == 7))
                            op("vector", lambda e: e.tensor_tensor(out=a_t[:], in0=B[3][:], in1=gu[:], op=ALU.mult), reads=["B3", "gu"], writes=["a_t"])
                            rms_stats(a_t[:], 512, "a_t", junk[:, 512:1024], ssa, rsa, "a")
                            mA = mixA[i % 2]
                            mk = "mixA%d" % (i % 2)
                            op("vector", lambda e, mA=mA: e.scalar_tensor_tensor(out=mA[:], in0=a_t[:], scalar=rsa[:, 0:1], in1=onaw[:], op0=ALU.mult, op1=ALU.mult),
                               reads=["a_t", "rstda", "onaw"], writes=[mk])
                            P.dma("gpsimd", mix_d[i * 128:(i + 1) * 128, 0:512], mA[:], reads=[mk], writes=["mix_d"])
                        tok = slice(TT * 512, (TT + 1) * 512)
                        for ch in range(8):
                            bk = B[4 + ch % 2]
                            bkk = "B%d" % (4 + ch % 2)
                            for c in range(8):
                                op("tensor", lambda e, bk=bk, ch=ch, c=c, hTt=hTt: e.matmul(bk[:], lhsT=wfm[:, c, ch * 128:(ch + 1) * 128], rhs=hTt[:, c, :], start=(c == 0), stop=(c == 7)),
                                   reads=[hk, "wfm"], writes=[bkk], signal=(c == 7))
                            if ch < 4:
                                op("scalar", lambda e, bk=bk, ch=ch, tok=tok: e.activation(out=qT_all[:, ch, tok], in_=bk[:], func=AF.Copy, scale=0.125), reads=[bkk], writes=["qT_all"])
                            elif ch == 4:
                                op("vector", lambda e, bk=bk, tok=tok: e.tensor_copy(kcT[:, tok], bk[:]), reads=[bkk], writes=["kcT"])
                            elif ch == 5:
                                op("vector", lambda e, bk=bk, tok=tok: e.tensor_copy(vcT[:, tok], bk[:]), reads=[bkk], writes=["vcT"])
                            elif ch == 6:
                                op("vector", lambda e, bk=bk, tok=tok: e.tensor_copy(kTE[0][0:64, tok], bk[0:64, :]), reads=[bkk], writes=["kTE0"])
                                op("vector", lambda e, bk=bk, tok=tok: e.tensor_copy(kTE[1][64:128, tok], bk[64:128, :]), reads=[bkk], writes=["kTE1"])
                            else:
                                op("vector", lambda e, bk=bk, tok=tok: e.tensor_copy(kTw[:, tok], bk[:]), reads=[bkk], writes=["kTw"])
                    P.barrier()

                with ExitStack() as s25:
                    w1A = {kv: T("w1A" + kv, [128, 32, 256], BF16, s25) for kv in "kv"}
                    posT = {kv: T("posT" + kv, [128, 32], BF16, s25) for kv in "kv"}
                    bcol = {kv: T("bcol" + kv, [128, 2], F32, s25) for kv in "kv"}
                    w1f = T("w1f", [128, 8, 256], F32, s25)
                    posf = T("posf", [32, 64], F32, s25)
                    w2f = T("w2f", [128, 2, 64], F32, s25)
                    G(lambda e: e.memset(w2k_pad[:], 0.0), writes=["w2k_pad"])
                    for kv in "kv":
                        src = cmp_w1[kv][l].rearrange("(l d) n -> d l n", d=64)
                        for q4 in range(4):
                            P.dma("sync", w1f[0:64], src[:, q4 * 8:(q4 + 1) * 8, :], writes=["w1f"])
                            P.dma("sync", w1f[64:128], src[:, q4 * 8:(q4 + 1) * 8, :], writes=["w1f"])
                            G(lambda e, kv=kv, q4=q4: e.tensor_copy(w1A[kv][:, q4 * 8:(q4 + 1) * 8, :], w1f[:]), reads=["w1f"], writes=["w1A" + kv])
                        P.dma("sync", posf[:], cmp_pos[kv][l], writes=["posf"])
                        op("tensor", lambda e: e.transpose(B[2][0:64, 0:32], posf[:, :], ident_f[0:32, 0:32]), reads=["posf", "ident_f"], writes=["B2"])
                        op("vector", lambda e, kv=kv: e.tensor_copy(posT[kv][0:64, :], B[2][0:64, 0:32]), reads=["B2"], writes=["posT" + kv])
                        for hc in range(2):
                            for ll in range(32):
                                op("tensor", lambda e, kv=kv, hc=hc, ll=ll: e.matmul(B[3][:, hc:hc + 1], lhsT=w1A[kv][0:64, ll, hc * 128:(hc + 1) * 128],
                                                                                 rhs=posT[kv][0:64, ll:ll + 1], start=(ll == 0), stop=(ll == 31)),
                                   reads=["w1A" + kv, "posT" + kv], writes=["B3"], signal=(ll == 31))
                        op("vector", lambda e, kv=kv: e.tensor_copy(bcol[kv][:], B[3][:, 0:2]), reads=["B3"], writes=["bcol" + kv])
                        P.dma("sync", w2f[:], cmp_w2[kv][l].rearrange("(c p) d -> p c d", p=128), writes=["w2f"])
                        if kv == "k":
                            for k in range(2):
                                G(lambda e, k=k: e.tensor_copy(w2k_pad[:, :, k, 64 * k:64 * k + 64], w2f[:]), reads=["w2f"], writes=["w2k_pad"])
                        else:
                            G(lambda e: e.tensor_copy(w2v[:], w2f[:]), reads=["w2f"], writes=["w2v"])
                    for kv, srcT in (("k", kcT), ("v", vcT)):
                        for k in range(2):
                            pr = slice(64 * k, 64 * k + 64)
                            for hc in range(2):
                                bk = B[hc]
                                for ll in range(32):
                                    op("tensor", lambda e, kv=kv, hc=hc, ll=ll, bk=bk, pr=pr, srcT=srcT: e.matmul(bk[:, 0:255], lhsT=w1A[kv][pr, ll, hc * 128:(hc + 1) * 128],
                                                                                                       rhs=srcT[pr, ll:ll + 16 * 254 + 1:16], start=(ll == 0), stop=(ll == 31)),
                                       reads=["w1A" + kv, "kcT", "vcT"], writes=["B%d" % hc], signal=(ll == 31))
                                op("scalar", lambda e, kv=kv, hc=hc, bk=bk: e.activation(out=hidT[:, hc, 0:255], in_=bk[:, 0:255], func=AF.Gelu_apprx_tanh, bias=bcol[kv][:, hc:hc + 1]),
                                   reads=["B%d" % hc, "bcol" + kv], writes=["hidT"])
                            if kv == "k":
                                for hc in range(2):
                                    op("tensor", lambda e, hc=hc, k=k: e.matmul(B[2][:, 0:255], lhsT=w2k_pad[:, hc, k, :], rhs=hidT[:, hc, 0:255], start=(hc == 0), stop=(hc == 1)),
                                       reads=["w2k_pad", "hidT"], writes=["B2"], signal=(hc == 1))
                                op("vector", lambda e, pr=pr: e.tensor_copy(kcmp[pr, 0:255], B[2][pr, 0:255]), reads=["B2"], writes=["kcmp"])
                            else:
                                for nt_, M in ((0, 128), (1, 127)):
                                    for hc in range(2):
                                        op("tensor", lambda e, hc=hc, nt_=nt_, M=M: e.matmul(B[3][0:M, 0:64], lhsT=hidT[:, hc, nt_ * 128:nt_ * 128 + M], rhs=w2v[:, hc, :], start=(hc == 0), stop=(hc == 1)),
                                           reads=["w2v", "hidT"], writes=["B3"], signal=(hc == 1))
                                    op("vector", lambda e, nt_=nt_, M=M, k=k: e.tensor_copy(R_cmp[0:M, nt_, k, 0:64], B[3][0:M, 0:64]), reads=["B3"], writes=["R_cmp"])
                    P.barrier()

                with ExitStack() as s3:
                    qB = [T("qB%d" % k, [128, 4, 128], BF16, s3) for k in range(2)]
                    PTc = T("PTc", [128, 2, 512], BF16, s3)
                    PT = [T("PT%d" % i, [128, 512], BF16, s3) for i in range(4)]
                    oc2 = [T("oc%d" % j, [128, 4, 129], F32, s3) for j in range(2)]
                    rc2 = [T("rc%d" % j, [128, 4], F32, s3) for j in range(2)]
                    imp = T("imp", [128, 64], F32, s3)
                    score = T("score", [128, 64], F32, s3)
                    sc2 = T("sc2", [128, 64], F32, s3)
                    m8 = T("m8", [128, 16], F32, s3)
                    biasP = [T("biasP%d" % k, [128, 128], BF16, s3) for k in range(2)]
                    oT_sb = T("oT_sb", [128, 2, 512], F32, s3)
                    rr = T("rr", [128, 2, 4], F32, s3)
                    rg = T("rg", [128, 3, 4], F32, s3)
                    bfull2 = [T("bfull%d" % j, [128, 512], F32, s3) for j in range(2)]
                    btmp2 = [T("btmp%d" % j, [128, 4, 64], F32, s3) for j in range(2)]
                    ssb = T("ssb", [128, 1], F32, s3)
                    rsb = T("rsb", [128, 1], F32, s3)
                    junkb = T("junkb", [128, 512], F32, s3)
                    mixB = [T("mixB%d" % i, [128, 512], BF16, s3) for i in range(2)]
                    for k in range(2):
                        G(lambda e, k=k: e.memset(qB[k][:], 0.0), writes=["qB%d" % k])
                        G(lambda e, k=k: e.memset(biasP[k][:], 0.0), writes=["biasP%d" % k])
                    psC = [B[6][:, 260:389], B[5][:, 0:129], B[5][:, 129:258], B[5][:, 258:387]]
                    psCk = ["B6", "B5", "B5", "B5"]
                    psB = BT[:, 0, :]
                    psT = B[6][:, 0:260].rearrange("p (g d) -> p g d", g=4)
                    sidx = [0]
                    pidx = [0]

                    def stageA(i, k, n):
                        pr = slice(64 * k, 64 * k + 64)
                        br_ = slice(64, 128) if k == 0 else slice(0, 64)
                        qk = "qB%d" % k
                        ocn, rcn = oc2[n % 2], rc2[n % 2]
                        ock, rck = "oc%d" % (n % 2), "rc%d" % (n % 2)
                        G(lambda e: e.tensor_copy(qB[k][pr, :, :], qT_all[pr, :, i * 128:(i + 1) * 128]), reads=["qT_all"], writes=[qk])
                        yield
                        n_tiles = 1 if i <= 15 else 2
                        for nt_ in range(n_tiles):
                            M = 128 if nt_ == 0 else 127
                            bs_ = B[4]
                            bsk = "B4"
                            a0 = 256 - 8 * i + nt_ * 128
                            op("tensor", lambda e, bs_=bs_, M=M, nt_=nt_: e.matmul(bs_[0:M, :], lhsT=kcmp[pr, nt_ * 128:nt_ * 128 + M], rhs=qB[k][pr, :, :], start=True, stop=False),
                               reads=["kcmp", qk], writes=[bsk], signal=False)
                            op("tensor", lambda e, bs_=bs_, M=M, a0=a0: e.matmul(bs_[0:M, :], lhsT=A_big[0:9, a0:a0 + M], rhs=Bm4[0:9, :, :], start=False, stop=True),
                               reads=["A_big", "Bm4"], writes=[bsk])
                            yield
                            op("scalar", lambda e, bs_=bs_, M=M, nt_=nt_: e.activation(out=PTc[0:M, nt_, :], in_=bs_[0:M, :], func=AF.Exp), reads=[bsk], writes=["PTc"])
                            yield
                        for g in range(4):
                            for nt_ in range(n_tiles):
                                M = 128 if nt_ == 0 else 127
                                op("tensor", lambda e, g=g, nt_=nt_, M=M: e.matmul(psC[g], lhsT=PTc[0:M, nt_, g * 128:(g + 1) * 128], rhs=R_cmp[0:M, nt_, k, :],
                                                                                start=(nt_ == 0), stop=(nt_ == n_tiles - 1)),
                                   reads=["PTc", "R_cmp"], writes=[psCk[g]], signal=(nt_ == n_tiles - 1))
                            if g % 2 == 1:
                                yield
                        for g in range(4):
                            op("vector", lambda e, g=g: e.tensor_copy(ocn[:, g, :], psC[g]), reads=[psCk[g]], writes=[ock])
                            if g % 2 == 1:
                                yield
                        op("vector", lambda e: e.tensor_scalar(rcn[:], ocn[:, :, 128], 1e-30, None, ALU.max), reads=[ock], writes=[rck])
                        yield
                        op("vector", lambda e: e.reciprocal(rcn[:], rcn[:]), reads=[rck], writes=[rck])
                        yield
                        if i >= 8:
                            op("vector", lambda e: e.tensor_scalar(imp[:], ocn[:, 0, 64:128], rcn[:, 0:1], None, ALU.mult), reads=[ock, rck], writes=["imp"])
                            yield
                            for g in range(1, 4):
                                op("vector", lambda e, g=g: e.scalar_tensor_tensor(out=imp[:], in0=ocn[:, g, 64:128], scalar=rcn[:, g:g + 1], in1=imp[:], op0=ALU.mult, op1=ALU.add),
                                   reads=[ock, rck, "imp"], writes=["imp"])
                                yield
                            c0 = 64 - 2 * i
                            op("vector", lambda e: e.tensor_tensor(out=score[:], in0=imp[:], in1=VM[:, c0:c0 + 64], op=ALU.mult), reads=["imp", "VM"], writes=["score"])
                            yield
                            op("vector", lambda e: e.tensor_tensor(out=score[:], in0=score[:], in1=AMk[:, c0:c0 + 64], op=ALU.add), reads=["score", "AMk"], writes=["score"])
                            yield
                            op("vector", lambda e: e.memset(score[:, 0:1], 10002.0), reads=["score"], writes=["score"])
                            yield
                            op("vector", lambda e: e.max(out=m8[:, 0:8], in_=score[:]), reads=["score"], writes=["m8"])
                            yield
                            op("vector", lambda e: e.match_replace(out=sc2[:], in_to_replace=m8[:, 0:8], in_values=score[:], imm_value=NEG), reads=["score", "m8"], writes=["sc2"])
                            yield
                            op("vector", lambda e: e.max(out=m8[:, 8:16], in_=sc2[:]), reads=["sc2"], writes=["m8"])
                            yield
                            bo = 64 if k == 0 else 0
                            op("vector", lambda e: e.tensor_scalar(biasP[k][:, bo:bo + 64], score[:], m8[:, 15:16], NEG, ALU.is_lt, ALU.mult),
                               reads=["score", "m8"], writes=["biasP%d" % k])
                            yield
                            op("tensor", lambda e: e.transpose(psB, biasP[k][:], ident_b[:]), reads=["biasP%d" % k, "ident_b"], writes=["BT"])
                            yield
                            op("vector", lambda e: e.tensor_copy(qB[k][br_, :, :], BT[br_, 0, :].unsqueeze(1).to_broadcast([64, 4, 128])),
                               reads=["BT"], writes=[qk])
                            yield

                    def adv(gen, cnt):
                        for _ in range(cnt):
                            try:
                                next(gen)
                            except StopIteration:
                                return

                    def stageB(i, k, n, gnext):
                        pr = slice(64 * k, 64 * k + 64)
                        qk = "qB%d" % k
                        ocn, rcn = oc2[n % 2], rc2[n % 2]
                        ock, rck = "oc%d" % (n % 2), "rc%d" % (n % 2)
                        bfl = bfull2[i % 2]
                        bfk = "bfull%d" % (i % 2)
                        cfg = ((kTE[k], vs_aug, list(range(0, i + 1))), (kTw, vw_aug, list(range(max(0, i - 4), i + 1))))
                        stp = [(bi, ji, jt, len(cfg[bi][2])) for bi in range(2) for ji, jt in enumerate(cfg[bi][2])]
                        banks = {}

                        def emitS(idx):
                            bi, ji, jt, nj = stp[idx]
                            cache = cfg[bi][0]
                            ck = ("kTE%d" % k) if bi == 0 else "kTw"
                            bs_ = B[sidx[0] % 2]
                            bsk = "B%d" % (sidx[0] % 2)
                            sidx[0] += 1
                            banks[idx] = (bs_, bsk)
                            if bi == 0:
                                op("tensor", lambda e: e.matmul(bs_[:], lhsT=cache[:, jt * 128:(jt + 1) * 128], rhs=qB[k][:, :, :], start=True, stop=True),
                                   reads=[ck, qk], writes=[bsk])
                            else:
                                op("tensor", lambda e: e.matmul(bs_[:], lhsT=cache[pr, jt * 128:(jt + 1) * 128], rhs=qB[k][pr, :, :], start=True, stop=True),
                                   reads=[ck, qk], writes=[bsk])

                        def emitPV(idx):
                            bi, ji, jt, nj = stp[idx]
                            vaug = cfg[bi][1]
                            pso = B[2 + bi]
                            psok = "B%d" % (2 + bi)
                            bs_, bsk = banks[idx]
                            pt = PT[pidx[0] % 4]
                            ptk = "PT%d" % (pidx[0] % 4)
                            pidx[0] += 1
                            op("scalar", lambda e: e.activation(out=pt[:], in_=bs_[:], func=AF.Exp), reads=[bsk], writes=[ptk])
                            if jt == i:
                                G(lambda e: e.tensor_tensor(out=pt[:].rearrange("p (g t) -> p g t", g=4), in0=pt[:].rearrange("p (g t) -> p g t", g=4),
                                                            in1=tri_b[:, :].unsqueeze(1).to_broadcast([128, 4, 128]), op=ALU.mult), reads=[ptk, "tri_b"], writes=[ptk])
                            if bi == 1 and jt == i - 4:
                                G(lambda e: e.tensor_tensor(out=pt[:].rearrange("p (g t) -> p g t", g=4), in0=pt[:].rearrange("p (g t) -> p g t", g=4),
                                                            in1=ntri_b[:, :].unsqueeze(1).to_broadcast([128, 4, 128]), op=ALU.mult), reads=[ptk, "ntri_b"], writes=[ptk])
                            op("tensor", lambda e: e.matmul(pso[0:65, :], lhsT=vaug[:, jt, k, :], rhs=pt[:], start=(ji == 0), stop=(ji == nj - 1)),
                               reads=[ptk, "vs_aug", "vw_aug"], writes=[psok], signal=(ji == nj - 1))
                            if ji == nj - 1:
                                op("vector", lambda e: e.tensor_copy(oT_sb[0:65, bi, :], pso[0:65, :]), reads=[psok], writes=["oT_sb"])

                        emitS(0)
                        for idx in range(len(stp)):
                            if idx + 1 < len(stp):
                                emitS(idx + 1)
                            emitPV(idx)
                            adv(gnext, 3)
                        adv(gnext, 1000)
                        gsl = lambda br: gates[:, i, br * 8 + k * 4: br * 8 + k * 4 + 4]
                        op("vector", lambda e: e.tensor_tensor(out=rg[:, 0, :], in0=rcn[:], in1=gsl(0), op=ALU.mult), reads=[rck, "gates"], writes=["rg"])
                        op("vector", lambda e: e.tensor_tensor(out=bfl[:, k * 256:(k + 1) * 256].rearrange("p (g d) -> p g d", g=4), in0=ocn[:, :, 0:64],
                                                               in1=rg[:, 0, :].unsqueeze(2).to_broadcast([128, 4, 64]), op=ALU.mult), reads=[ock, "rg"], writes=[bfk])
                        for bi in range(2):
                            for g in range(4):
                                op("tensor", lambda e, bi=bi, g=g: e.transpose(psT[:, g, :], oT_sb[0:65, bi, g * 128:(g + 1) * 128], ident_f[0:65, 0:65]),
                                   reads=["oT_sb", "ident_f"], writes=["B6"], signal=(g == 3))
                            op("vector", lambda e, bi=bi: e.tensor_scalar(rr[:, bi, :], psT[:, :, 64], 1e-30, None, ALU.max), reads=["B6"], writes=["rr"])
                            op("vector", lambda e, bi=bi: e.reciprocal(rr[:, bi, :], rr[:, bi, :]), reads=["rr"], writes=["rr"])
                            op("vector", lambda e, bi=bi: e.tensor_tensor(out=rg[:, 1 + bi, :], in0=rr[:, bi, :], in1=gsl(1 + bi), op=ALU.mult), reads=["rr", "gates"], writes=["rg"])
                            op("vector", lambda e, bi=bi: e.tensor_tensor(out=btmp2[bi][:], in0=psT[:, :, 0:64], in1=rg[:, 1 + bi, :].unsqueeze(2).to_broadcast([128, 4, 64]), op=ALU.mult),
                               reads=["B6", "rg"], writes=["btmp%d" % bi])
                            G(lambda e, bi=bi: e.tensor_tensor(out=bfl[:, k * 256:(k + 1) * 256], in0=bfl[:, k * 256:(k + 1) * 256], in1=btmp2[bi][:].rearrange("p g d -> p (g d)"), op=ALU.add),
                              reads=[bfk, "btmp%d" % bi], writes=[bfk])

                    steps = [(i, k) for i in range(NT) for k in range(2)]
                    adv(stageA(steps[0][0], steps[0][1], 0), 1000)
                    for n, (i, k) in enumerate(steps):
                        gnext = stageA(steps[n + 1][0], steps[n + 1][1], n + 1) if n + 1 < len(steps) else iter(())
                        stageB(i, k, n, gnext)
                        if k == 1:
                            bfl = bfull2[i % 2]
                            bfk = "bfull%d" % (i % 2)
                            rms_stats(bfl[:], 512, bfk, junkb[:], ssb, rsb, "b")
                            mB = mixB[i % 2]
                            mk = "mixB%d" % (i % 2)
                            op("vector", lambda e, mB=mB, bfl=bfl: e.scalar_tensor_tensor(out=mB[:], in0=bfl[:], scalar=rsb[:, 0:1], in1=onbw[:], op0=ALU.mult, op1=ALU.mult),
                               reads=[bfk, "rstdb", "onbw"], writes=[mk])
                            P.dma("gpsimd", mix_d[i * 128:(i + 1) * 128, 512:1024], mB[:], reads=[mk], writes=["mix_d"])
                    P.barrier()
            if debug == "p3":
                return
            with ExitStack() as s4:
                wo_b = T("wo_b", [128, 8, D], BF16, s4)
                wg_b = T("wg_b", [128, 8, DFF], BF16, s4)
                wu_b = T("wu_b", [128, 8, DFF], BF16, s4)
                wd_b = T("wd_b", [128, NFC, D], BF16, s4)
                nfw = T("nfw", [128, D], F32, s4)
                fnw = T("fnw", [128, D], F32, s4)
                P.dma("sync", nfw[:], norm_ffn_w[l].partition_broadcast(128), writes=["nfw"])
                P.dma("sync", fnw[:], final_norm_w.partition_broadcast(128), writes=["fnw"])
                s4a = ExitStack()
                stg = [T("stg%d" % i, [128, DFF], F32, s4a) for i in range(2)]
                si = 0
                for (wsrc, wdst, nck, wid, key) in ((w_o, wo_b, 8, D, "wo_b"), (w_gate, wg_b, 8, DFF, "wg_b"), (w_up, wu_b, 8, DFF, "wu_b"), (w_down, wd_b, NFC, D, "wd_b")):
                    for c in range(nck):
                        sg = stg[si % 2]
                        sk = "stg%d" % (si % 2)
                        P.dma("sync" if si % 2 == 0 else "scalar", sg[:, 0:wid], wsrc[l, c * 128:(c + 1) * 128, :], writes=[sk])
                        eng = ("gpsimd", "vector")[si % 2]
                        op(eng, lambda e, wdst=wdst, c=c, sg=sg, wid=wid: e.tensor_copy(wdst[:, c, :], sg[:, 0:wid]), reads=[sk], writes=[key])
                        si += 1
                P.barrier()
                s4a.close()
                mixt = [T("mixt%d" % i, [128, D], BF16, s4) for i in range(2)]
                mixT = T("mixT", [128, 8, 128], BF16, s4)
                x1 = T("x1", [128, 2, D], F32, s4)
                junk4 = T("junk4", [128, D], BF16, s4)
                ss4 = T("ss4", [128, 1], F32, s4)
                rs4 = T("rs4", [128, 1], F32, s4)
                h2 = T("h2", [128, D], BF16, s4)
                h2T = T("h2T", [128, 8, 256], BF16, s4)
                sgt = [T("sgt%d" % i, [128, 256], F32, s4) for i in range(2)]
                actT = T("actT", [128, NFC, 256], BF16, s4)
                x2 = [T("x2_%d" % i, [128, D], F32, s4) for i in range(1)]
                ss5 = T("ss5", [128, 1], F32, s4)
                rs5 = T("rs5", [128, 1], F32, s4)
                for TT in range(16):
                    for tt in range(2):
                        i = TT * 2 + tt
                        mt = mixt[i % 2]
                        mtk = "mixt%d" % (i % 2)
                        P.dma("sync", mt[:], mix_d[i * 128:(i + 1) * 128, :], reads=["mix_d"], writes=[mtk])
                        P.dma("scalar", x1[:, tt, :], xsrc[i * 128:(i + 1) * 128, :], reads=["xres_d"], writes=["x1"])
                        for c in range(8):
                            op("tensor", lambda e, c=c, mt=mt: e.transpose(BT[:, c, :], mt[:, c * 128:(c + 1) * 128], ident_b[:]), reads=[mtk, "ident_b"], writes=["BT"], signal=(c == 7))
                        op("vector", lambda e: e.tensor_copy(mixT[:], BT[:]), reads=["BT"], writes=["mixT"])
                        for half in range(2):
                            for c in range(8):
                                op("tensor", lambda e, half=half, c=c: e.matmul(B[half][:], lhsT=mixT[:, c, :], rhs=wo_b[:, c, half * 512:(half + 1) * 512], start=(c == 0), stop=(c == 7)),
                                   reads=["mixT", "wo_b"], writes=["B%d" % half], signal=(c == 7))
                            op("vector", lambda e, half=half, tt=tt: e.tensor_tensor(out=x1[:, tt, half * 512:(half + 1) * 512], in0=B[half][:], in1=x1[:, tt, half * 512:(half + 1) * 512], op=ALU.add),
                               reads=["B%d" % half, "x1"], writes=["x1"])
                        rms_stats(x1[:, tt, :], D, "x1", junk4[:], ss4, rs4, "4")
                        op("vector", lambda e, tt=tt: e.scalar_tensor_tensor(out=h2[:], in0=x1[:, tt, :], scalar=rs4[:, 0:1], in1=nfw[:], op0=ALU.mult, op1=ALU.mult),
                           reads=["x1", "rstd4", "nfw"], writes=["h2"])
                        for c in range(8):
                            op("tensor", lambda e, c=c: e.transpose(BT[:, c, :], h2[:, c * 128:(c + 1) * 128], ident_b[:]), reads=["h2", "ident_b"], writes=["BT"], signal=(c == 7))
                        op("vector", lambda e, tt=tt: e.tensor_copy(h2T[:, :, tt * 128:(tt + 1) * 128], BT[:]), reads=["BT"], writes=["h2T"])
                    for fc in range(NFC):
                        pg = B[2 + 2 * (fc % 2)]
                        pu = B[3 + 2 * (fc % 2)]
                        pgk = "B%d" % (2 + 2 * (fc % 2))
                        puk = "B%d" % (3 + 2 * (fc % 2))
                        for c in range(8):
                            op("tensor", lambda e, pg=pg, c=c, fc=fc: e.matmul(pg[:, 0:256], lhsT=wg_b[:, c, fc * 128:(fc + 1) * 128], rhs=h2T[:, c, :], start=(c == 0), stop=(c == 7)),
                               reads=["wg_b", "h2T"], writes=[pgk], signal=(c == 7))
                        for c in range(8):
                            op("tensor", lambda e, pu=pu, c=c, fc=fc: e.matmul(pu[:, 0:256], lhsT=wu_b[:, c, fc * 128:(fc + 1) * 128], rhs=h2T[:, c, :], start=(c == 0), stop=(c == 7)),
                               reads=["wu_b", "h2T"], writes=[puk], signal=(c == 7))
                        sg_ = sgt[fc % 2]
                        sgk = "sgt%d" % (fc % 2)
                        op("scalar", lambda e, sg_=sg_, pg=pg: e.activation(out=sg_[:], in_=pg[:, 0:256], func=AF.Silu), reads=[pgk], writes=[sgk])
                        op("vector", lambda e, sg_=sg_, pu=pu, fc=fc: e.tensor_tensor(out=actT[:, fc, :], in0=pu[:, 0:256], in1=sg_[:], op=ALU.mult), reads=[puk, sgk], writes=["actT"])
                    for tt in range(2):
                        i = TT * 2 + tt
                        x2i = x2[0]
                        x2k = "x2_0"
                        for half in range(2):
                            pd = B[(0, 6)[half]]
                            pdk = "B%d" % ((0, 6)[half])
                            for fc in range(NFC):
                                op("tensor", lambda e, pd=pd, fc=fc, tt=tt, half=half: e.matmul(pd[:], lhsT=actT[:, fc, tt * 128:(tt + 1) * 128], rhs=wd_b[:, fc, half * 512:(half + 1) * 512],
                                                                                      start=(fc == 0), stop=(fc == NFC - 1)),
                                   reads=["actT", "wd_b"], writes=[pdk], signal=(fc == NFC - 1))
                            op("vector", lambda e, pd=pd, half=half, tt=tt, x2i=x2i: e.tensor_tensor(out=x2i[:, half * 512:(half + 1) * 512], in0=pd[:], in1=x1[:, tt, half * 512:(half + 1) * 512], op=ALU.add),
                               reads=[pdk, "x1"], writes=[x2k])
                        if not last:
                            P.dma("gpsimd", xres_d[i * 128:(i + 1) * 128, :], x2i[:], reads=[x2k], writes=["xres_d"])
                        else:
                            rms_stats(x2i[:], D, x2k, junk4[:], ss5, rs5, "5")
                            op("vector", lambda e, x2i=x2i: e.scalar_tensor_tensor(out=x2i[:], in0=x2i[:], scalar=rs5[:, 0:1], in1=fnw[:], op0=ALU.mult, op1=ALU.mult),
                               reads=[x2k, "rstd5", "fnw"], writes=[x2k])
                            P.dma("gpsimd", out[i * 128:(i + 1) * 128, :], x2i[:], reads=[x2k], writes=["out"])
                P.barrier()
        for l_ in range(nlayers):
            layer(l_)
        P.finish("gpsimd", ["out", "mix_d", "xres_d"])
        P.barrier()
        with nc.Block() as block:
            P.emit(block)
    print("instructions:", P.n_inst)
    return nc


_NAMES = ["norm_mix_w", "w_in", "gmlp_norm_w", "gmlp_ws", "gmlp_bs", "cmp_pos_k", "cmp_pos_v", "cmp_k_w1", "cmp_k_w2",
          "cmp_v_w1", "cmp_v_w2", "gate_b", "out_norm_a_w", "out_norm_b_w", "w_o", "norm_ffn_w", "w_gate", "w_up",
          "w_down", "final_norm_w"]


def kernel(**inputs):
    x = np.ascontiguousarray(np.asarray(inputs["x"], dtype=np.float32))
    shared = {n: np.ascontiguousarray(np.asarray(inputs[n], dtype=np.float32)) for n in _NAMES}
    nc = build()
    in_maps = [dict(shared, x=x[b]) for b in range(8)]
    res = run_bass_kernel_spmd(nc, in_maps, core_ids=list(range(8)))
    return np.stack([np.asarray(r["out"], dtype=np.float32) for r in res.results], axis=0)
```

```python
import numpy as np
import concourse.bass as bass
import concourse.mybir as mybir
from concourse.bass_utils import run_bass_kernel_spmd

F32 = mybir.dt.float32
BF16 = mybir.dt.bfloat16
AF = mybir.ActivationFunctionType
ALU = mybir.AluOpType
AX = mybir.AxisListType


class Prog:
    ENGS = ("sync", "scalar", "vector", "gpsimd", "tensor")
    NDMA = 8
    R = 8

    def __init__(self, nc, stack):
        self.nc = nc
        self.q = {e: [] for e in self.ENGS}
        self.sem = {e: [stack.enter_context(nc.semaphore("s_%s%d" % (e, i))) for i in range(self.R)]
                    for e in self.ENGS}
        self.cnt = {e: 0 for e in self.ENGS}
        self.dsem = {e: [stack.enter_context(nc.semaphore("d_%s%d" % (e, i))) for i in range(self.NDMA)]
                     for e in ("sync", "scalar", "gpsimd")}
        self.dcnt = {e: 0 for e in self.dsem}
        self.semobj = {}
        for e in self.ENGS:
            for i in range(self.R):
                self.semobj[("c", e, i)] = self.sem[e][i]
        for e in self.dsem:
            for i in range(self.NDMA):
                self.semobj[("d", e, i)] = self.dsem[e][i]
        self.waited = {e: {} for e in self.ENGS}
        self.lastw = {}
        self.readers = {}
        self.n_inst = 0

    def _waits(self, eng, deps):
        need = {}
        for (sid, val) in deps:
            if sid[0] == "c" and sid[1] == eng and (val - 1) * self.R + sid[2] + 1 > self.cnt[eng]:
                continue
            if self.waited[eng].get(sid, 0) >= val:
                continue
            if need.get(sid, 0) < val:
                need[sid] = val
        out = []
        for sid, val in need.items():
            self.waited[eng][sid] = val
            out.append((self.semobj[sid], val))
        return out

    def _deps(self, reads, writes):
        deps = []
        for k in reads:
            if k in self.lastw:
                deps.append(self.lastw[k])
        for k in writes:
            if k in self.lastw:
                deps.append(self.lastw[k])
            deps.extend(self.readers.get(k, ()))
        return deps

    def _commit(self, tok, reads, writes):
        for k in reads:
            self.readers.setdefault(k, []).append(tok)
        for k in writes:
            self.lastw[k] = tok
            self.readers[k] = []

    def op(self, eng, fn, reads=(), writes=(), signal=True):
        waits = self._waits(eng, self._deps(reads, writes))
        n = self.cnt[eng]
        tok = (("c", eng, n % self.R), n // self.R + 1)
        if signal:
            self.cnt[eng] += 1
        sem = self.sem[eng][n % self.R]

        def run(e, fn=fn, waits=waits, signal=signal, sem=sem):
            for (s, v) in waits:
                e.wait_ge(s, v)
            ins = fn(e)
            if signal:
                ins.then_inc(sem, 1)

        self.q[eng].append(run)
        self._commit(tok, reads, writes)
        self.n_inst += 1

    def dma(self, eng, out, in_, reads=(), writes=(), **kw):
        n = self.dcnt[eng]
        self.dcnt[eng] += 1
        slot = n % self.NDMA
        val = 16 * (n // self.NDMA + 1)
        sid = ("d", eng, slot)
        deps = self._deps(reads, writes)
        if val > 16:
            deps.append((sid, val - 16))
        waits = self._waits(eng, deps)
        sem = self.dsem[eng][slot]

        def run(e, waits=waits, sem=sem, out=out, in_=in_, kw=kw):
            for (s, v) in waits:
                e.wait_ge(s, v)
            e.dma_start(out=out, in_=in_, **kw).then_inc(sem, 16)

        self.q[eng].append(run)
        self._commit((sid, val), reads, writes)
        self.n_inst += 1

    def barrier(self):
        deps = []
        for e in self.ENGS:
            n = self.cnt[e]
            for i in range(self.R):
                if n >= i + 1:
                    deps.append((("c", e, i), (n - 1 - i) // self.R + 1))
        for e in self.dsem:
            n = self.dcnt[e]
            for i in range(self.NDMA):
                if n >= i + 1:
                    deps.append((("d", e, i), 16 * ((n - 1 - i) // self.NDMA + 1)))
        for e in self.ENGS:
            waits = self._waits(e, deps)

            def run(en, waits=waits):
                for (s, v) in waits:
                    en.wait_ge(s, v)

            self.q[e].append(run)

    def finish(self, eng, keys):
        waits = self._waits(eng, self._deps(keys, ()))

        def run(e, waits=waits):
            for (s, v) in waits:
                e.wait_ge(s, v)

        self.q[eng].append(run)

    def emit(self, block):
        q = self.q

        @block.sync
        def _(e):
            for f in q["sync"]:
                f(e)

        @block.scalar
        def _(e):
            for f in q["scalar"]:
                f(e)

        @block.vector
        def _(e):
            for f in q["vector"]:
                f(e)

        @block.gpsimd
        def _(e):
            for f in q["gpsimd"]:
                f(e)

        @block.tensor
        def _(e):
            for f in q["tensor"]:
                f(e)


S = 4096
D = 1024
NT = 32
DFF = 2816
NFC = 22
NEG = -30000.0
L = 2


def build(debug=None, nlayers=L):
    from contextlib import ExitStack
    nc = bass.Bass("TRN2", target_bir_lowering=False)
    dt_in = lambda name, shape: nc.dram_tensor(name, shape, F32, kind="ExternalInput").ap()
    x_in = dt_in("x", [S, D])
    norm_mix_w = dt_in("norm_mix_w", [L, D])
    w_in = dt_in("w_in", [L, D, 2328])
    gmlp_norm_w = dt_in("gmlp_norm_w", [L, 512])
    gmlp_ws = dt_in("gmlp_ws", [L, 8, 128, 128])
    gmlp_bs = dt_in("gmlp_bs", [L, 8, 128])
    cmp_pos = {"k": dt_in("cmp_pos_k", [L, 32, 64]), "v": dt_in("cmp_pos_v", [L, 32, 64])}
    cmp_w1 = {"k": dt_in("cmp_k_w1", [L, 2048, 256]), "v": dt_in("cmp_v_w1", [L, 2048, 256])}
    cmp_w2 = {"k": dt_in("cmp_k_w2", [L, 256, 64]), "v": dt_in("cmp_v_w2", [L, 256, 64])}
    gate_b = dt_in("gate_b", [L, 24])
    out_norm_a_w = dt_in("out_norm_a_w", [L, 512])
    out_norm_b_w = dt_in("out_norm_b_w", [L, 512])
    w_o = dt_in("w_o", [L, D, D])
    norm_ffn_w = dt_in("norm_ffn_w", [L, D])
    w_gate = dt_in("w_gate", [L, D, DFF])
    w_up = dt_in("w_up", [L, D, DFF])
    w_down = dt_in("w_down", [L, DFF, D])
    final_norm_w = dt_in("final_norm_w", [D])
    out = nc.dram_tensor("out", [S, D], F32, kind="ExternalOutput").ap()
    dbg = debug is not None
    mix_d = nc.dram_tensor("mix_d", [S, D], BF16, kind="ExternalOutput" if dbg else "Internal").ap()
    xres_d = nc.dram_tensor("xres_d", [S, D], F32, kind="ExternalOutput" if dbg else "Internal").ap()

    with ExitStack() as st:
        P = Prog(nc, st)
        op = P.op

        sfx = [""]

        def T(name, shape, dt, stack=None):
            return (stack or st).enter_context(nc.sbuf_tensor(name + sfx[0], shape, dt))

        B = [st.enter_context(nc.psum_tensor("B%d" % i, [128, 512], F32)) for i in range(7)]
        BT = st.enter_context(nc.psum_tensor("BT", [128, 8, 128], BF16))

        ident_b = T("ident_b", [128, 128], BF16)
        ident_f = T("ident_f", [128, 128], F32)
        tri_f = T("tri_f", [128, 128], F32)
        tri_b = T("tri_b", [128, 128], BF16)
        ntri_b = T("ntri_b", [128, 128], BF16)
        ones_f = T("ones_f", [128, 128], F32)
        ind8 = T("ind8", [8, 512], F32)
        A_big = T("A_big", [16, 512], BF16)
        Bm4 = T("Bm4", [16, 4, 128], BF16)
        VM = T("VM", [128, 128], F32)
        AMk = T("AMk", [128, 128], F32)
        tmpc = T("tmpc", [128, 512], F32)
        tmpc2 = T("tmpc2", [128, 512], F32)

        def G(fn, reads=(), writes=()):
            op("gpsimd", fn, reads=reads, writes=writes)

        def asel(t, pattern, cmp_, fill, base, cm, key):
            G(lambda e: e.affine_select(out=t, in_=t, pattern=pattern, compare_op=cmp_, fill=fill,
                                        base=base, channel_multiplier=cm), reads=[key], writes=[key])

        G(lambda e: e.memset(ones_f[:], 1.0), writes=["ones_f"])
        G(lambda e: e.memset(ident_f[:], 1.0), writes=["ident_f"])
        asel(ident_f[:], [[-1, 128]], ALU.is_equal, 0.0, 0, 1, "ident_f")
        G(lambda e: e.tensor_copy(ident_b[:], ident_f[:]), reads=["ident_f"], writes=["ident_b"])
        G(lambda e: e.memset(tri_f[:], 1.0), writes=["tri_f"])
        asel(tri_f[:], [[1, 128]], ALU.is_ge, 0.0, 0, -1, "tri_f")
        G(lambda e: e.tensor_copy(tri_b[:], tri_f[:]), reads=["tri_f"], writes=["tri_b"])
        G(lambda e: e.memset(tmpc[:, 0:128], 1.0), writes=["tmpc"])
        asel(tmpc[:, 0:128], [[-1, 128]], ALU.is_gt, 0.0, 0, 1, "tmpc")
        G(lambda e: e.tensor_copy(ntri_b[:], tmpc[:, 0:128]), reads=["tmpc"], writes=["ntri_b"])
        G(lambda e: e.memset(ind8[:], 1.0), writes=["ind8"])
        asel(ind8[:].rearrange("p (h d) -> p h d", h=8), [[1, 8], [0, 64]], ALU.is_equal, 0.0, 0, -1, "ind8")
        G(lambda e: e.memset(tmpc[0:16, :], 1.0), writes=["tmpc"])
        asel(tmpc[0:16, :], [[1, 512]], ALU.is_equal, 0.0, -255, -1, "tmpc")
        G(lambda e: e.memset(tmpc2[0:16, :], 1.0), writes=["tmpc2"])
        asel(tmpc2[0:16, :], [[1, 512]], ALU.is_ge, 0.0, -263, 0, "tmpc2")
        asel(tmpc2[0:16, :], [[0, 512]], ALU.is_equal, 0.0, -8, 1, "tmpc2")
        asel(tmpc[0:16, :], [[0, 512]], ALU.is_ge, 0.0, 7, -1, "tmpc")
        G(lambda e: e.tensor_tensor(out=A_big[:], in0=tmpc[0:16, :], in1=tmpc2[0:16, :], op=ALU.add),
          reads=["tmpc", "tmpc2"], writes=["A_big"])
        G(lambda e: e.memset(tmpc[0:16, :], NEG), writes=["tmpc"])
        asel(tmpc[0:16, :].rearrange("p (g t) -> p g t", g=4), [[0, 4], [-1, 128]], ALU.is_gt, 0.0, 15, 16, "tmpc")
        asel(tmpc[0:16, :], [[0, 512]], ALU.is_ge, 0.0, 7, -1, "tmpc")
        G(lambda e: e.memset(tmpc2[0:16, :], NEG), writes=["tmpc2"])
        asel(tmpc2[0:16, :], [[0, 512]], ALU.is_equal, 0.0, -8, 1, "tmpc2")
        G(lambda e: e.tensor_tensor(out=Bm4[:].rearrange("p g t -> p (g t)"), in0=tmpc[0:16, :], in1=tmpc2[0:16, :], op=ALU.add),
          reads=["tmpc", "tmpc2"], writes=["Bm4"])
        G(lambda e: e.memset(VM[:], 1.0), writes=["VM"])
        asel(VM[:], [[-64, 128]], ALU.is_ge, 0.0, 64 * 64 - 128, 1, "VM")
        G(lambda e: e.memset(AMk[:], 10000.0), writes=["AMk"])
        asel(AMk[:], [[-64, 128]], ALU.is_ge, 0.0, 64 * 64, 1, "AMk")
        asel(AMk[:], [[64, 128]], ALU.is_ge, 0.0, -64 * 64 + 63, -1, "AMk")
        G(lambda e: e.memset(tmpc[:, 0:128], 10001.0), writes=["tmpc"])
        asel(tmpc[:, 0:128], [[-64, 128]], ALU.is_ge, 0.0, 64 * 64 - 64, 1, "tmpc")
        asel(tmpc[:, 0:128], [[64, 128]], ALU.is_ge, 0.0, -64 * 64 + 64 + 63, -1, "tmpc")
        G(lambda e: e.tensor_tensor(out=AMk[:], in0=AMk[:], in1=tmpc[:, 0:128], op=ALU.add), reads=["AMk", "tmpc"], writes=["AMk"])
        G(lambda e: e.memset(tmpc2[:, 0:128], -10000.0), writes=["tmpc2"])
        asel(tmpc2[:, 0:128], [[64, 128]], ALU.is_ge, 0.0, -64 * 64 - 1, -1, "tmpc2")
        G(lambda e: e.tensor_tensor(out=AMk[:], in0=AMk[:], in1=tmpc2[:, 0:128], op=ALU.add), reads=["AMk", "tmpc2"], writes=["AMk"])


        def rms_stats(src_ap, width, key_src, junk, ssum, rstd, tag):
            sc = float(width) ** -0.5
            op("scalar", lambda e: e.activation(out=junk, in_=src_ap, func=AF.Square, scale=sc, accum_out=ssum[:, 0:1]),
               reads=[key_src], writes=["junk" + tag, "ss" + tag])
            op("vector", lambda e: e.tensor_scalar(rstd[:, 0:1], ssum[:, 0:1], 1e-6, None, ALU.add), reads=["ss" + tag], writes=["rstd" + tag])
            op("scalar", lambda e: e.activation(out=rstd[:, 0:1], in_=rstd[:, 0:1], func=AF.Sqrt), reads=["rstd" + tag], writes=["rstd" + tag])
            op("vector", lambda e: e.reciprocal(rstd[:, 0:1], rstd[:, 0:1]), reads=["rstd" + tag], writes=["rstd" + tag])

        def layer(l):
            sfx[0] = "_L%d" % l
            xsrc = x_in if l == 0 else xres_d
            last = (l == nlayers - 1)
            P.barrier()
            with ExitStack() as s1:
                nmw = T("nmw", [128, D], F32, s1)
                gnw = T("gnw", [128, 512], F32, s1)
                onaw = T("onaw", [128, 512], F32, s1)
                onbw = T("onbw", [128, 512], F32, s1)
                gbt = T("gbt", [128, 24], F32, s1)
                bs8 = T("bs8", [8, 128], F32, s1)
                gates = T("gates", [128, NT, 24], F32, s1)
                P.dma("sync", nmw[:], norm_mix_w[l].partition_broadcast(128), writes=["nmw"])
                P.dma("sync", gnw[:], gmlp_norm_w[l].partition_broadcast(128), writes=["gnw"])
                P.dma("sync", onaw[:], out_norm_a_w[l].partition_broadcast(128), writes=["onaw"])
                P.dma("sync", onbw[:], out_norm_b_w[l].partition_broadcast(128), writes=["onbw"])
                P.dma("sync", gbt[:], gate_b[l].partition_broadcast(128), writes=["gbt"])
                P.dma("sync", bs8[:], gmlp_bs[l], writes=["bs8"])

                wtm = T("wtm", [128, 8, 1304], BF16, s1)
                wfm = T("wfm", [128, 8, 1024], BF16, s1)
                wsT = T("wsT", [128, 8, 128], BF16, s1)
                kTE = [T("kTE%d" % k, [128, S], BF16, s1) for k in range(2)]
                kTw = T("kTw", [128, S], BF16, s1)
                kcT = T("kcT", [128, S], BF16, s1)
                vcT = T("vcT", [128, S], BF16, s1)
                vs_aug = T("vs_aug", [128, NT, 2, 65], BF16, s1)
                vw_aug = T("vw_aug", [128, NT, 2, 65], BF16, s1)
                qT_all = T("qT_all", [128, 4, S], BF16, s1)
                w2k_pad = T("w2k_pad", [128, 2, 2, 128], BF16, s1)
                w2v = T("w2v", [128, 2, 64], BF16, s1)
                hidT = T("hidT", [128, 2, 256], BF16, s1)
                kcmp = T("kcmp", [128, 256], BF16, s1)
                R_cmp = T("R_cmp", [128, 2, 2, 129], BF16, s1)

                with ExitStack() as s0:
                    stage = [T("stage%d" % i, [128, 2328], F32, s0) for i in range(2)]
                    for c in range(8):
                        sg = stage[c % 2]
                        sk = "stage%d" % (c % 2)
                        P.dma("sync", sg[:], w_in[l, c * 128:(c + 1) * 128, :], writes=[sk])
                        cp = [
                            (wtm[:, c, 0:1024], sg[:, 0:1024]),
                            (wtm[:, c, 1024:1152], sg[:, 1920:2048]),
                            (wtm[:, c, 1152:1280], sg[:, 2176:2304]),
                            (wtm[:, c, 1280:1304], sg[:, 2304:2328]),
                            (wfm[:, c, 0:512].rearrange("p (g k d) -> p g k d", g=4, k=2),
                             sg[:, 1024:1536].rearrange("p (k g d) -> p g k d", k=2, g=4)),
                            (wfm[:, c, 512:640], sg[:, 1536:1664]),
                            (wfm[:, c, 640:768], sg[:, 1664:1792]),
                            (wfm[:, c, 768:896], sg[:, 1792:1920]),
                            (wfm[:, c, 896:1024], sg[:, 2048:2176]),
                        ]
                        for ci, (o_, i_) in enumerate(cp):
                            eng = "gpsimd" if ci % 2 == 0 else "vector"
                            op(eng, lambda e, o_=o_, i_=i_: e.tensor_copy(o_, i_), reads=[sk], writes=["wtm" if ci < 4 else "wfm"])
                    wsf = T("wsf", [128, 8, 128], F32, s0)
                    P.dma("sync", wsf[:], gmlp_ws[l].rearrange("h t s -> t h s"), writes=["wsf"])
                    for h in range(8):
                        bk = B[h % 2]
                        op("tensor", lambda e, h=h, bk=bk: e.transpose(bk[:, 0:128], wsf[:, h, :], ident_f[:]),
                           reads=["wsf", "ident_f"], writes=["B%d" % (h % 2)])
                        op("vector", lambda e, h=h, bk=bk: e.tensor_tensor(out=wsT[:, h, :], in0=bk[:, 0:128], in1=tri_f[:], op=ALU.mult),
                           reads=["B%d" % (h % 2), "tri_f"], writes=["wsT"])
                    for nt_ in range(2):
                        G(lambda e: e.memset(tmpc[:, 0:64], 1.0), writes=["tmpc"])
                        asel(tmpc[:, 0:64], [[-64, 64]], ALU.is_ge, 0.0, 2048 * nt_ + 31, 16, "tmpc")
                        asel(tmpc[:, 0:64], [[64, 64]], ALU.is_ge, 0.0, 63 - 2048 * nt_, -16, "tmpc")
                        for k in range(2):
                            G(lambda e, nt_=nt_, k=k: e.tensor_copy(R_cmp[:, nt_, k, 64:128], tmpc[:, 0:64]), reads=["tmpc"], writes=["R_cmp"])
                    G(lambda e: e.memset(R_cmp[:, :, :, 128:129], 1.0), writes=["R_cmp"])
                    G(lambda e: e.memset(vs_aug[:, :, :, 64:65], 1.0), writes=["vs_aug"])
                    G(lambda e: e.memset(vw_aug[:, :, :, 64:65], 1.0), writes=["vw_aug"])
                    G(lambda e: e.memset(kTE[0][:], 1.0), writes=["kTE0"])
                    asel(kTE[0][:].rearrange("p (j r) -> p j r", j=64), [[1, 64], [0, 64]], ALU.is_equal, 0.0, 64, -1, "kTE0")
                    G(lambda e: e.memset(kTE[1][:], 1.0), writes=["kTE1"])
                    asel(kTE[1][:].rearrange("p (j r) -> p j r", j=64), [[1, 64], [0, 64]], ALU.is_equal, 0.0, 0, -1, "kTE1")
                    P.barrier()

                with ExitStack() as s2:
                    xt = [T("xt%d" % i, [128, D], F32, s2) for i in range(2)]
                    junk = T("junk", [128, D], BF16, s2)
                    ssx = T("ssx", [128, 1], F32, s2)
                    rsx = T("rsx", [128, 1], F32, s2)
                    hb = T("hb", [128, D], BF16, s2)
                    hT = [T("hT%d" % i, [128, 8, 512], BF16, s2) for i in range(2)]
                    gu = T("gu", [128, 512], F32, s2)
                    gv = T("gv", [128, 512], F32, s2)
                    ssv = T("ssv", [128, 1], F32, s2)
                    rsv = T("rsv", [128, 1], F32, s2)
                    vn = T("vn", [128, 512], BF16, s2)
                    a_t = T("a_t", [128, 512], F32, s2)
                    ssa = T("ssa", [128, 1], F32, s2)
                    rsa = T("rsa", [128, 1], F32, s2)
                    mixA = [T("mixA%d" % i, [128, 512], BF16, s2) for i in range(2)]
                    gpre = T("gpre", [128, 24], F32, s2)
                    for TT in range(8):
                        hTt = hT[TT % 2]
                        hk = "hT%d" % (TT % 2)
                        for tt in range(4):
                            i = TT * 4 + tt
                            xti = xt[i % 2]
                            xk = "xt%d" % (i % 2)
                            P.dma("sync", xti[:], xsrc[i * 128:(i + 1) * 128, :], writes=[xk])
                            rms_stats(xti[:], D, xk, junk[:], ssx, rsx, "x")
                            op("vector", lambda e, xti=xti: e.scalar_tensor_tensor(out=hb[:], in0=xti[:], scalar=rsx[:, 0:1], in1=nmw[:], op0=ALU.mult, op1=ALU.mult),
                               reads=[xk, "rstdx", "nmw"], writes=["hb"])
                            for c in range(8):
                                op("tensor", lambda e, c=c: e.transpose(BT[:, c, :], hb[:, c * 128:(c + 1) * 128], ident_b[:]),
                                   reads=["hb", "ident_b"], writes=["BT"], signal=(c == 7))
                            op("vector", lambda e, hTt=hTt, tt=tt: e.tensor_copy(hTt[:, :, tt * 128:(tt + 1) * 128], BT[:]), reads=["BT"], writes=[hk])
                            for cb, (c0, c1) in enumerate(((0, 512), (512, 1024), (1024, 1304))):
                                for c in range(8):
                                    op("tensor", lambda e, cb=cb, c=c, c0=c0, c1=c1, hTt=hTt, tt=tt: e.matmul(B[cb][:, 0:c1 - c0], lhsT=hTt[:, c, tt * 128:(tt + 1) * 128], rhs=wtm[:, c, c0:c1],
                                                                                                      start=(c == 0), stop=(c == 7)),
                                       reads=[hk, "wtm"], writes=["B%d" % cb], signal=(c == 7))
                            op("scalar", lambda e: e.activation(out=gu[:], in_=B[0][:], func=AF.Gelu_apprx_tanh), reads=["B0"], writes=["gu"])
                            op("scalar", lambda e: e.activation(out=gv[:], in_=B[1][:], func=AF.Gelu_apprx_tanh), reads=["B1"], writes=["gv"])
                            rms_stats(gv[:], 512, "gv", junk[:, 0:512], ssv, rsv, "v")
                            op("vector", lambda e: e.scalar_tensor_tensor(out=vn[:], in0=gv[:], scalar=rsv[:, 0:1], in1=gnw[:], op0=ALU.mult, op1=ALU.mult),
                               reads=["gv", "rstdv", "gnw"], writes=["vn"])
                            op("vector", lambda e, i=i: e.tensor_copy(vs_aug[:, i, :, 0:64], B[2][:, 0:128].rearrange("p (k d) -> p k d", k=2)), reads=["B2"], writes=["vs_aug"])
                            op("vector", lambda e, i=i: e.tensor_copy(vw_aug[:, i, :, 0:64], B[2][:, 128:256].rearrange("p (k d) -> p k d", k=2)), reads=["B2"], writes=["vw_aug"])
                            op("vector", lambda e: e.tensor_tensor(out=gpre[:], in0=B[2][:, 256:280], in1=gbt[:], op=ALU.add), reads=["B2", "gbt"], writes=["gpre"])
                            op("scalar", lambda e, i=i: e.activation(out=gates[:, i, :], in_=gpre[:], func=AF.Sigmoid), reads=["gpre"], writes=["gates"])
                            op("tensor", lambda e: e.matmul(B[3][:], lhsT=bs8[:], rhs=ind8[:], start=True, stop=False), reads=["bs8", "ind8"], writes=["B3"], signal=False)
                            for h in range(8):
                                op("tensor", lambda e, h=h: e.matmul(B[3][:, h * 64:(h + 1) * 64], lhsT=wsT[:, h, :], rhs=vn[:, h * 64:(h + 1) * 64], start=False, stop=(h == 7)),
                                   reads=["wsT", "vn"], writes=["B3"], signal=(h == 7))
                            op("vector", lambda e: e.tensor_tensor(out=a_t[:], in0=B[3][:], in1=gu[:], op=ALU.mult), reads=["B3", "gu"], writes=["a_t"])
                            rms_stats(a_t[:], 512, "a_t", junk[:, 512:1024], ssa, rsa, "a")
                            mA = mixA[i % 2]
                            mk = "mixA%d" % (i % 2)
                            op("vector", lambda e, mA=mA: e.scalar_tensor_tensor(out=mA[:], in0=a_t[:], scalar=rsa[:, 0:1], in1=onaw[:], op0=ALU.mult, op1=ALU.mult),
                               reads=["a_t", "rstda", "onaw"], writes=[mk])
                            P.dma("gpsimd", mix_d[i * 128:(i + 1) * 128, 0:512], mA[:], reads=[mk], writes=["mix_d"])
                        tok = slice(TT * 512, (TT + 1) * 512)
                        for ch in range(8):
                            bk = B[4 + ch % 2]
                            bkk = "B%d" % (4 + ch % 2)
                            for c in range(8):
                                op("tensor", lambda e, bk=bk, ch=ch, c=c, hTt=hTt: e.matmul(bk[:], lhsT=wfm[:, c, ch * 128:(ch + 1) * 128], rhs=hTt[:, c, :], start=(c == 0), stop=(c == 7)),
                                   reads=[hk, "wfm"], writes=[bkk], signal=(c == 7))
                            if ch < 4:
                                op("scalar", lambda e, bk=bk, ch=ch, tok=tok: e.activation(out=qT_all[:, ch, tok], in_=bk[:], func=AF.Copy, scale=0.125), reads=[bkk], writes=["qT_all"])
                            elif ch == 4:
                                op("vector", lambda e, bk=bk, tok=tok: e.tensor_copy(kcT[:, tok], bk[:]), reads=[bkk], writes=["kcT"])
                            elif ch == 5:
                                op("vector", lambda e, bk=bk, tok=tok: e.tensor_copy(vcT[:, tok], bk[:]), reads=[bkk], writes=["vcT"])
                            elif ch == 6:
                                op("vector", lambda e, bk=bk, tok=tok: e.tensor_copy(kTE[0][0:64, tok], bk[0:64, :]), reads=[bkk], writes=["kTE0"])
                                op("vector", lambda e, bk=bk, tok=tok: e.tensor_copy(kTE[1][64:128, tok], bk[64:128, :]), reads=[bkk], writes=["kTE1"])
                            else:
                                op("vector", lambda e, bk=bk, tok=tok: e.tensor_copy(kTw[:, tok], bk[:]), reads=[bkk], writes=["kTw"])
                    P.barrier()

                with ExitStack() as s25:
                    w1A = {kv: T("w1A" + kv, [128, 32, 256], BF16, s25) for kv in "kv"}
                    posT = {kv: T("posT" + kv, [128, 32], BF16, s25) for kv in "kv"}
                    bcol = {kv: T("bcol" + kv, [128, 2], F32, s25) for kv in "kv"}
                    w1f = T("w1f", [128, 8, 256], F32, s25)
                    posf = T("posf", [32, 64], F32, s25)
                    w2f = T("w2f", [128, 2, 64], F32, s25)
                    G(lambda e: e.memset(w2k_pad[:], 0.0), writes=["w2k_pad"])
                    for kv in "kv":
                        src = cmp_w1[kv][l].rearrange("(l d) n -> d l n", d=64)
                        for q4 in range(4):
                            P.dma("sync", w1f[0:64], src[:, q4 * 8:(q4 + 1) * 8, :], writes=["w1f"])
                            P.dma("sync", w1f[64:128], src[:, q4 * 8:(q4 + 1) * 8, :], writes=["w1f"])
                            G(lambda e, kv=kv, q4=q4: e.tensor_copy(w1A[kv][:, q4 * 8:(q4 + 1) * 8, :], w1f[:]), reads=["w1f"], writes=["w1A" + kv])
                        P.dma("sync", posf[:], cmp_pos[kv][l], writes=["posf"])
                        op("tensor", lambda e: e.transpose(B[2][0:64, 0:32], posf[:, :], ident_f[0:32, 0:32]), reads=["posf", "ident_f"], writes=["B2"])
                        op("vector", lambda e, kv=kv: e.tensor_copy(posT[kv][0:64, :], B[2][0:64, 0:32]), reads=["B2"], writes=["posT" + kv])
                        for hc in range(2):
                            for ll in range(32):
                                op("tensor", lambda e, kv=kv, hc=hc, ll=ll: e.matmul(B[3][:, hc:hc + 1], lhsT=w1A[kv][0:64, ll, hc * 128:(hc + 1) * 128],
                                                                                 rhs=posT[kv][0:64, ll:ll + 1], start=(ll == 0), stop=(ll == 31)),
                                   reads=["w1A" + kv, "posT" + kv], writes=["B3"], signal=(ll == 31))
                        op("vector", lambda e, kv=kv: e.tensor_copy(bcol[kv][:], B[3][:, 0:2]), reads=["B3"], writes=["bcol" + kv])
                        P.dma("sync", w2f[:], cmp_w2[kv][l].rearrange("(c p) d -> p c d", p=128), writes=["w2f"])
                        if kv == "k":
                            for k in range(2):
                                G(lambda e, k=k: e.tensor_copy(w2k_pad[:, :, k, 64 * k:64 * k + 64], w2f[:]), reads=["w2f"], writes=["w2k_pad"])
                        else:
                            G(lambda e: e.tensor_copy(w2v[:], w2f[:]), reads=["w2f"], writes=["w2v"])
                    for kv, srcT in (("k", kcT), ("v", vcT)):
                        for k in range(2):
                            pr = slice(64 * k, 64 * k + 64)
                            for hc in range(2):
                                bk = B[hc]
                                for ll in range(32):
                                    op("tensor", lambda e, kv=kv, hc=hc, ll=ll, bk=bk, pr=pr, srcT=srcT: e.matmul(bk[:, 0:255], lhsT=w1A[kv][pr, ll, hc * 128:(hc + 1) * 128],
                                                                                                       rhs=srcT[pr, ll:ll + 16 * 254 + 1:16], start=(ll == 0), stop=(ll == 31)),
                                       reads=["w1A" + kv, "kcT", "vcT"], writes=["B%d" % hc], signal=(ll == 31))
                                op("scalar", lambda e, kv=kv, hc=hc, bk=bk: e.activation(out=hidT[:, hc, 0:255], in_=bk[:, 0:255], func=AF.Gelu_apprx_tanh, bias=bcol[kv][:, hc:hc + 1]),
                                   reads=["B%d" % hc, "bcol" + kv], writes=["hidT"])
                            if kv == "k":
                                for hc in range(2):
                                    op("tensor", lambda e, hc=hc, k=k: e.matmul(B[2][:, 0:255], lhsT=w2k_pad[:, hc, k, :], rhs=hidT[:, hc, 0:255], start=(hc == 0), stop=(hc == 1)),
                                       reads=["w2k_pad", "hidT"], writes=["B2"], signal=(hc == 1))
                                op("vector", lambda e, pr=pr: e.tensor_copy(kcmp[pr, 0:255], B[2][pr, 0:255]), reads=["B2"], writes=["kcmp"])
                            else:
                                for nt_, M in ((0, 128), (1, 127)):
                                    for hc in range(2):
                                        op("tensor", lambda e, hc=hc, nt_=nt_, M=M: e.matmul(B[3][0:M, 0:64], lhsT=hidT[:, hc, nt_ * 128:nt_ * 128 + M], rhs=w2v[:, hc, :], start=(hc == 0), stop=(hc == 1)),
                                           reads=["w2v", "hidT"], writes=["B3"], signal=(hc == 1))
                                    op("vector", lambda e, nt_=nt_, M=M, k=k: e.tensor_copy(R_cmp[0:M, nt_, k, 0:64], B[3][0:M, 0:64]), reads=["B3"], writes=["R_cmp"])
                    P.barrier()

                with ExitStack() as s3:
                    qB = [T("qB%d" % k, [128, 4, 128], BF16, s3) for k in range(2)]
                    PTc = T("PTc", [128, 2, 512], BF16, s3)
                    PT = [T("PT%d" % i, [128, 512], BF16, s3) for i in range(4)]
                    oc2 = [T("oc%d" % j, [128, 4, 129], F32, s3) for j in range(2)]
                    rc2 = [T("rc%d" % j, [128, 4], F32, s3) for j in range(2)]
                    imp = T("imp", [128, 64], F32, s3)
                    score = T("score", [128, 64], F32, s3)
                    sc2 = T("sc2", [128, 64], F32, s3)
                    m8 = T("m8", [128, 16], F32, s3)
                    biasP = [T("biasP%d" % k, [128, 128], BF16, s3) for k in range(2)]
                    oT_sb = T("oT_sb", [128, 2, 512], F32, s3)
                    rr = T("rr", [128, 2, 4], F32, s3)
                    rg = T("rg", [128, 3, 4], F32, s3)
                    bfull2 = [T("bfull%d" % j, [128, 512], F32, s3) for j in range(2)]
                    btmp2 = [T("btmp%d" % j, [128, 4, 64], F32, s3) for j in range(2)]
                    ssb = T("ssb", [128, 1], F32, s3)
                    rsb = T("rsb", [128, 1], F32, s3)
                    junkb = T("junkb", [128, 512], F32, s3)
                    mixB = [T("mixB%d" % i, [128, 512], BF16, s3) for i in range(2)]
                    for k in range(2):
                        G(lambda e, k=k: e.memset(qB[k][:], 0.0), writes=["qB%d" % k])
                        G(lambda e, k=k: e.memset(biasP[k][:], 0.0), writes=["biasP%d" % k])
                    psC = [B[6][:, 260:389], B[5][:, 0:129], B[5][:, 129:258], B[5][:, 258:387]]
                    psCk = ["B6", "B5", "B5", "B5"]
                    psB = BT[:, 0, :]
                    psT = B[6][:, 0:260].rearrange("p (g d) -> p g d", g=4)
                    sidx = [0]
                    pidx = [0]

                    def stageA(i, k, n):
                        pr = slice(64 * k, 64 * k + 64)
                        br_ = slice(64, 128) if k == 0 else slice(0, 64)
                        qk = "qB%d" % k
                        ocn, rcn = oc2[n % 2], rc2[n % 2]
                        ock, rck = "oc%d" % (n % 2), "rc%d" % (n % 2)
                        G(lambda e: e.tensor_copy(qB[k][pr, :, :], qT_all[pr, :, i * 128:(i + 1) * 128]), reads=["qT_all"], writes=[qk])
                        yield
                        n_tiles = 1 if i <= 15 else 2
                        for nt_ in range(n_tiles):
                            M = 128 if nt_ == 0 else 127
                            bs_ = B[5]
                            bsk = "B5"
                            a0 = 256 - 8 * i + nt_ * 128
                            op("tensor", lambda e, bs_=bs_, M=M, nt_=nt_: e.matmul(bs_[0:M, :], lhsT=kcmp[pr, nt_ * 128:nt_ * 128 + M], rhs=qB[k][pr, :, :], start=True, stop=False),
                               reads=["kcmp", qk], writes=[bsk], signal=False)
                            op("tensor", lambda e, bs_=bs_, M=M, a0=a0: e.matmul(bs_[0:M, :], lhsT=A_big[0:9, a0:a0 + M], rhs=Bm4[0:9, :, :], start=False, stop=True),
                               reads=["A_big", "Bm4"], writes=[bsk])
                            yield
                            op("scalar", lambda e, bs_=bs_, M=M, nt_=nt_: e.activation(out=PTc[0:M, nt_, :], in_=bs_[0:M, :], func=AF.Exp), reads=[bsk], writes=["PTc"])
                            yield
                        for g in range(4):
                            for nt_ in range(n_tiles):
                                M = 128 if nt_ == 0 else 127
                                op("tensor", lambda e, g=g, nt_=nt_, M=M: e.matmul(psC[g], lhsT=PTc[0:M, nt_, g * 128:(g + 1) * 128], rhs=R_cmp[0:M, nt_, k, :],
                                                                                start=(nt_ == 0), stop=(nt_ == n_tiles - 1)),
                                   reads=["PTc", "R_cmp"], writes=[psCk[g]], signal=(nt_ == n_tiles - 1))
                            if g % 2 == 1:
                                yield
                        for g in range(4):
                            op("vector", lambda e, g=g: e.tensor_copy(ocn[:, g, :], psC[g]), reads=[psCk[g]], writes=[ock])
                            if g % 2 == 1:
                                yield
                        op("vector", lambda e: e.tensor_scalar(rcn[:], ocn[:, :, 128], 1e-30, None, ALU.max), reads=[ock], writes=[rck])
                        yield
                        op("vector", lambda e: e.reciprocal(rcn[:], rcn[:]), reads=[rck], writes=[rck])
                        yield
                        if i >= 8:
                            op("vector", lambda e: e.tensor_scalar(imp[:], ocn[:, 0, 64:128], rcn[:, 0:1], None, ALU.mult), reads=[ock, rck], writes=["imp"])
                            yield
                            for g in range(1, 4):
                                op("vector", lambda e, g=g: e.scalar_tensor_tensor(out=imp[:], in0=ocn[:, g, 64:128], scalar=rcn[:, g:g + 1], in1=imp[:], op0=ALU.mult, op1=ALU.add),
                                   reads=[ock, rck, "imp"], writes=["imp"])
                                yield
                            c0 = 64 - 2 * i
                            op("vector", lambda e: e.tensor_tensor(out=score[:], in0=imp[:], in1=VM[:, c0:c0 + 64], op=ALU.mult), reads=["imp", "VM"], writes=["score"])
                            yield
                            op("vector", lambda e: e.tensor_tensor(out=score[:], in0=score[:], in1=AMk[:, c0:c0 + 64], op=ALU.add), reads=["score", "AMk"], writes=["score"])
                            yield
                            op("vector", lambda e: e.memset(score[:, 0:1], 10002.0), reads=["score"], writes=["score"])
                            yield
                            op("vector", lambda e: e.max(out=m8[:, 0:8], in_=score[:]), reads=["score"], writes=["m8"])
                            yield
                            op("vector", lambda e: e.match_replace(out=sc2[:], in_to_replace=m8[:, 0:8], in_values=score[:], imm_value=NEG), reads=["score", "m8"], writes=["sc2"])
                            yield
                            op("vector", lambda e: e.max(out=m8[:, 8:16], in_=sc2[:]), reads=["sc2"], writes=["m8"])
                            yield
                            bo = 64 if k == 0 else 0
                            op("vector", lambda e: e.tensor_scalar(biasP[k][:, bo:bo + 64], score[:], m8[:, 15:16], NEG, ALU.is_lt, ALU.mult),
                               reads=["score", "m8"], writes=["biasP%d" % k])
                            yield
                            op("tensor", lambda e: e.transpose(psB, biasP[k][:], ident_b[:]), reads=["biasP%d" % k, "ident_b"], writes=["BT"])
                            yield
                            op("vector", lambda e: e.tensor_copy(qB[k][br_, :, :], BT[br_, 0, :].unsqueeze(1).to_broadcast([64, 4, 128])),
                               reads=["BT"], writes=[qk])
                            yield

                    def adv(gen, cnt):
                        for _ in range(cnt):
                            try:
                                next(gen)
                            except StopIteration:
                                return

                    def stageB(i, k, n, gnext):
                        pr = slice(64 * k, 64 * k + 64)
                        qk = "qB%d" % k
                        ocn, rcn = oc2[n % 2], rc2[n % 2]
                        ock, rck = "oc%d" % (n % 2), "rc%d" % (n % 2)
                        bfl = bfull2[i % 2]
                        bfk = "bfull%d" % (i % 2)
                        cfg = ((kTE[k], vs_aug, list(range(0, i + 1))), (kTw, vw_aug, list(range(max(0, i - 4), i + 1))))
                        stp = [(bi, ji, jt, len(cfg[bi][2])) for bi in range(2) for ji, jt in enumerate(cfg[bi][2])]
                        banks = {}

                        def emitS(idx):
                            bi, ji, jt, nj = stp[idx]
                            cache = cfg[bi][0]
                            ck = ("kTE%d" % k) if bi == 0 else "kTw"
                            bnum = (0, 1, 4)[sidx[0] % 3]
                            bs_ = B[bnum]
                            bsk = "B%d" % bnum
                            sidx[0] += 1
                            banks[idx] = (bs_, bsk)
                            if bi == 0:
                                op("tensor", lambda e: e.matmul(bs_[:], lhsT=cache[:, jt * 128:(jt + 1) * 128], rhs=qB[k][:, :, :], start=True, stop=True),
                                   reads=[ck, qk], writes=[bsk])
                            else:
                                op("tensor", lambda e: e.matmul(bs_[:], lhsT=cache[pr, jt * 128:(jt + 1) * 128], rhs=qB[k][pr, :, :], start=True, stop=True),
                                   reads=[ck, qk], writes=[bsk])

                        def emitPV(idx):
                            bi, ji, jt, nj = stp[idx]
                            vaug = cfg[bi][1]
                            pso = B[2 + bi]
                            psok = "B%d" % (2 + bi)
                            bs_, bsk = banks[idx]
                            pt = PT[pidx[0] % 4]
                            ptk = "PT%d" % (pidx[0] % 4)
                            pidx[0] += 1
                            op("scalar", lambda e: e.activation(out=pt[:], in_=bs_[:], func=AF.Exp), reads=[bsk], writes=[ptk])
                            if jt == i:
                                G(lambda e: e.tensor_tensor(out=pt[:].rearrange("p (g t) -> p g t", g=4), in0=pt[:].rearrange("p (g t) -> p g t", g=4),
                                                            in1=tri_b[:, :].unsqueeze(1).to_broadcast([128, 4, 128]), op=ALU.mult), reads=[ptk, "tri_b"], writes=[ptk])
                            if bi == 1 and jt == i - 4:
                                G(lambda e: e.tensor_tensor(out=pt[:].rearrange("p (g t) -> p g t", g=4), in0=pt[:].rearrange("p (g t) -> p g t", g=4),
                                                            in1=ntri_b[:, :].unsqueeze(1).to_broadcast([128, 4, 128]), op=ALU.mult), reads=[ptk, "ntri_b"], writes=[ptk])
                            op("tensor", lambda e: e.matmul(pso[0:65, :], lhsT=vaug[:, jt, k, :], rhs=pt[:], start=(ji == 0), stop=(ji == nj - 1)),
                               reads=[ptk, "vs_aug", "vw_aug"], writes=[psok], signal=(ji == nj - 1))
                            if ji == nj - 1:
                                op("vector", lambda e: e.tensor_copy(oT_sb[0:65, bi, :], pso[0:65, :]), reads=[psok], writes=["oT_sb"])

                        emitS(0)
                        if len(stp) > 1:
                            emitS(1)
                        for idx in range(len(stp)):
                            if idx + 2 < len(stp):
                                emitS(idx + 2)
                            emitPV(idx)
                            adv(gnext, 3)
                        adv(gnext, 1000)
                        gsl = lambda br: gates[:, i, br * 8 + k * 4: br * 8 + k * 4 + 4]
                        op("vector", lambda e: e.tensor_tensor(out=rg[:, 0, :], in0=rcn[:], in1=gsl(0), op=ALU.mult), reads=[rck, "gates"], writes=["rg"])
                        op("vector", lambda e: e.tensor_tensor(out=bfl[:, k * 256:(k + 1) * 256].rearrange("p (g d) -> p g d", g=4), in0=ocn[:, :, 0:64],
                                                               in1=rg[:, 0, :].unsqueeze(2).to_broadcast([128, 4, 64]), op=ALU.mult), reads=[ock, "rg"], writes=[bfk])
                        for bi in range(2):
                            for g in range(4):
                                op("tensor", lambda e, bi=bi, g=g: e.transpose(psT[:, g, :], oT_sb[0:65, bi, g * 128:(g + 1) * 128], ident_f[0:65, 0:65]),
                                   reads=["oT_sb", "ident_f"], writes=["B6"], signal=(g == 3))
                            op("vector", lambda e, bi=bi: e.tensor_scalar(rr[:, bi, :], psT[:, :, 64], 1e-30, None, ALU.max), reads=["B6"], writes=["rr"])
                            op("vector", lambda e, bi=bi: e.reciprocal(rr[:, bi, :], rr[:, bi, :]), reads=["rr"], writes=["rr"])
                            op("vector", lambda e, bi=bi: e.tensor_tensor(out=rg[:, 1 + bi, :], in0=rr[:, bi, :], in1=gsl(1 + bi), op=ALU.mult), reads=["rr", "gates"], writes=["rg"])
                            op("vector", lambda e, bi=bi: e.tensor_tensor(out=btmp2[bi][:], in0=psT[:, :, 0:64], in1=rg[:, 1 + bi, :].unsqueeze(2).to_broadcast([128, 4, 64]), op=ALU.mult),
                               reads=["B6", "rg"], writes=["btmp%d" % bi])
                            G(lambda e, bi=bi: e.tensor_tensor(out=bfl[:, k * 256:(k + 1) * 256], in0=bfl[:, k * 256:(k + 1) * 256], in1=btmp2[bi][:].rearrange("p g d -> p (g d)"), op=ALU.add),
                              reads=[bfk, "btmp%d" % bi], writes=[bfk])

                    steps = [(i, k) for i in range(NT) for k in range(2)]
                    adv(stageA(steps[0][0], steps[0][1], 0), 1000)
                    for n, (i, k) in enumerate(steps):
                        gnext = stageA(steps[n + 1][0], steps[n + 1][1], n + 1) if n + 1 < len(steps) else iter(())
                        stageB(i, k, n, gnext)
                        if k == 1:
                            bfl = bfull2[i % 2]
                            bfk = "bfull%d" % (i % 2)
                            rms_stats(bfl[:], 512, bfk, junkb[:], ssb, rsb, "b")
                            mB = mixB[i % 2]
                            mk = "mixB%d" % (i % 2)
                            op("vector", lambda e, mB=mB, bfl=bfl: e.scalar_tensor_tensor(out=mB[:], in0=bfl[:], scalar=rsb[:, 0:1], in1=onbw[:], op0=ALU.mult, op1=ALU.mult),
                               reads=[bfk, "rstdb", "onbw"], writes=[mk])
                            P.dma("gpsimd", mix_d[i * 128:(i + 1) * 128, 512:1024], mB[:], reads=[mk], writes=["mix_d"])
                    P.barrier()
            if debug == "p3":
                return
            with ExitStack() as s4:
                wo_b = T("wo_b", [128, 8, D], BF16, s4)
                wg_b = T("wg_b", [128, 8, DFF], BF16, s4)
                wu_b = T("wu_b", [128, 8, DFF], BF16, s4)
                wd_b = T("wd_b", [128, NFC, D], BF16, s4)
                nfw = T("nfw", [128, D], F32, s4)
                fnw = T("fnw", [128, D], F32, s4)
                P.dma("sync", nfw[:], norm_ffn_w[l].partition_broadcast(128), writes=["nfw"])
                P.dma("sync", fnw[:], final_norm_w.partition_broadcast(128), writes=["fnw"])
                s4a = ExitStack()
                stg = [T("stg%d" % i, [128, DFF], F32, s4a) for i in range(2)]
                si = 0
                for (wsrc, wdst, nck, wid, key) in ((w_o, wo_b, 8, D, "wo_b"), (w_gate, wg_b, 8, DFF, "wg_b"), (w_up, wu_b, 8, DFF, "wu_b"), (w_down, wd_b, NFC, D, "wd_b")):
                    for c in range(nck):
                        sg = stg[si % 2]
                        sk = "stg%d" % (si % 2)
                        P.dma("sync" if si % 2 == 0 else "scalar", sg[:, 0:wid], wsrc[l, c * 128:(c + 1) * 128, :], writes=[sk])
                        eng = ("gpsimd", "vector")[si % 2]
                        op(eng, lambda e, wdst=wdst, c=c, sg=sg, wid=wid: e.tensor_copy(wdst[:, c, :], sg[:, 0:wid]), reads=[sk], writes=[key])
                        si += 1
                P.barrier()
                s4a.close()
                mixt = [T("mixt%d" % i, [128, D], BF16, s4) for i in range(2)]
                mixT = T("mixT", [128, 8, 128], BF16, s4)
                x1 = T("x1", [128, 2, D], F32, s4)
                junk4 = T("junk4", [128, D], BF16, s4)
                ss4 = T("ss4", [128, 1], F32, s4)
                rs4 = T("rs4", [128, 1], F32, s4)
                h2 = T("h2", [128, D], BF16, s4)
                h2T = T("h2T", [128, 8, 256], BF16, s4)
                sgt = [T("sgt%d" % i, [128, 256], F32, s4) for i in range(2)]
                actT = T("actT", [128, NFC, 256], BF16, s4)
                x2 = [T("x2_%d" % i, [128, D], F32, s4) for i in range(1)]
                ss5 = T("ss5", [128, 1], F32, s4)
                rs5 = T("rs5", [128, 1], F32, s4)
                for TT in range(16):
                    for tt in range(2):
                        i = TT * 2 + tt
                        mt = mixt[i % 2]
                        mtk = "mixt%d" % (i % 2)
                        P.dma("sync", mt[:], mix_d[i * 128:(i + 1) * 128, :], reads=["mix_d"], writes=[mtk])
                        P.dma("scalar", x1[:, tt, :], xsrc[i * 128:(i + 1) * 128, :], reads=["xres_d"], writes=["x1"])
                        for c in range(8):
                            op("tensor", lambda e, c=c, mt=mt: e.transpose(BT[:, c, :], mt[:, c * 128:(c + 1) * 128], ident_b[:]), reads=[mtk, "ident_b"], writes=["BT"], signal=(c == 7))
                        op("vector", lambda e: e.tensor_copy(mixT[:], BT[:]), reads=["BT"], writes=["mixT"])
                        for half in range(2):
                            for c in range(8):
                                op("tensor", lambda e, half=half, c=c: e.matmul(B[half][:], lhsT=mixT[:, c, :], rhs=wo_b[:, c, half * 512:(half + 1) * 512], start=(c == 0), stop=(c == 7)),
                                   reads=["mixT", "wo_b"], writes=["B%d" % half], signal=(c == 7))
                            op("vector", lambda e, half=half, tt=tt: e.tensor_tensor(out=x1[:, tt, half * 512:(half + 1) * 512], in0=B[half][:], in1=x1[:, tt, half * 512:(half + 1) * 512], op=ALU.add),
                               reads=["B%d" % half, "x1"], writes=["x1"])
                        rms_stats(x1[:, tt, :], D, "x1", junk4[:], ss4, rs4, "4")
                        op("vector", lambda e, tt=tt: e.scalar_tensor_tensor(out=h2[:], in0=x1[:, tt, :], scalar=rs4[:, 0:1], in1=nfw[:], op0=ALU.mult, op1=ALU.mult),
                           reads=["x1", "rstd4", "nfw"], writes=["h2"])
                        for c in range(8):
                            op("tensor", lambda e, c=c: e.transpose(BT[:, c, :], h2[:, c * 128:(c + 1) * 128], ident_b[:]), reads=["h2", "ident_b"], writes=["BT"], signal=(c == 7))
                        op("vector", lambda e, tt=tt: e.tensor_copy(h2T[:, :, tt * 128:(tt + 1) * 128], BT[:]), reads=["BT"], writes=["h2T"])
                    for fc in range(NFC):
                        pg = B[2 + 2 * (fc % 2)]
                        pu = B[3 + 2 * (fc % 2)]
                        pgk = "B%d" % (2 + 2 * (fc % 2))
                        puk = "B%d" % (3 + 2 * (fc % 2))
                        for c in range(8):
                            op("tensor", lambda e, pg=pg, c=c, fc=fc: e.matmul(pg[:, 0:256], lhsT=wg_b[:, c, fc * 128:(fc + 1) * 128], rhs=h2T[:, c, :], start=(c == 0), stop=(c == 7)),
                               reads=["wg_b", "h2T"], writes=[pgk], signal=(c == 7))
                        for c in range(8):
                            op("tensor", lambda e, pu=pu, c=c, fc=fc: e.matmul(pu[:, 0:256], lhsT=wu_b[:, c, fc * 128:(fc + 1) * 128], rhs=h2T[:, c, :], start=(c == 0), stop=(c == 7)),
                               reads=["wu_b", "h2T"], writes=[puk], signal=(c == 7))
                        sg_ = sgt[fc % 2]
                        sgk = "sgt%d" % (fc % 2)
                        op("scalar", lambda e, sg_=sg_, pg=pg: e.activation(out=sg_[:], in_=pg[:, 0:256], func=AF.Silu), reads=[pgk], writes=[sgk])
                        op("vector", lambda e, sg_=sg_, pu=pu, fc=fc: e.tensor_tensor(out=actT[:, fc, :], in0=pu[:, 0:256], in1=sg_[:], op=ALU.mult), reads=[puk, sgk], writes=["actT"])
                    for tt in range(2):
                        i = TT * 2 + tt
                        x2i = x2[0]
                        x2k = "x2_0"
                        for half in range(2):
                            pd = B[(0, 6)[half]]
                            pdk = "B%d" % ((0, 6)[half])
                            for fc in range(NFC):
                                op("tensor", lambda e, pd=pd, fc=fc, tt=tt, half=half: e.matmul(pd[:], lhsT=actT[:, fc, tt * 128:(tt + 1) * 128], rhs=wd_b[:, fc, half * 512:(half + 1) * 512],
                                                                                      start=(fc == 0), stop=(fc == NFC - 1)),
                                   reads=["actT", "wd_b"], writes=[pdk], signal=(fc == NFC - 1))
                            op("vector", lambda e, pd=pd, half=half, tt=tt, x2i=x2i: e.tensor_tensor(out=x2i[:, half * 512:(half + 1) * 512], in0=pd[:], in1=x1[:, tt, half * 512:(half + 1) * 512], op=ALU.add),
                               reads=[pdk, "x1"], writes=[x2k])
                        if not last:
                            P.dma("gpsimd", xres_d[i * 128:(i + 1) * 128, :], x2i[:], reads=[x2k], writes=["xres_d"])
                        else:
                            rms_stats(x2i[:], D, x2k, junk4[:], ss5, rs5, "5")
                            op("vector", lambda e, x2i=x2i: e.scalar_tensor_tensor(out=x2i[:], in0=x2i[:], scalar=rs5[:, 0:1], in1=fnw[:], op0=ALU.mult, op1=ALU.mult),
                               reads=[x2k, "rstd5", "fnw"], writes=[x2k])
                            P.dma("gpsimd", out[i * 128:(i + 1) * 128, :], x2i[:], reads=[x2k], writes=["out"])
                P.barrier()
        for l_ in range(nlayers):
            layer(l_)
        P.finish("gpsimd", ["out", "mix_d", "xres_d"])
        P.barrier()
        with nc.Block() as block:
            P.emit(block)
    print("instructions:", P.n_inst)
    return nc


_NAMES = ["norm_mix_w", "w_in", "gmlp_norm_w", "gmlp_ws", "gmlp_bs", "cmp_pos_k", "cmp_pos_v", "cmp_k_w1", "cmp_k_w2",
          "cmp_v_w1", "cmp_v_w2", "gate_b", "out_norm_a_w", "out_norm_b_w", "w_o", "norm_ffn_w", "w_gate", "w_up",
          "w_down", "final_norm_w"]


def kernel(**inputs):
    x = np.ascontiguousarray(np.asarray(inputs["x"], dtype=np.float32))
    shared = {n: np.ascontiguousarray(np.asarray(inputs[n], dtype=np.float32)) for n in _NAMES}
    nc = build()
    in_maps = [dict(shared, x=x[b]) for b in range(8)]
    res = run_bass_kernel_spmd(nc, in_maps, core_ids=list(range(8)))
    return np.stack([np.asarray(r["out"], dtype=np.float32) for r in res.results], axis=0)
```

```python
import numpy as np
import concourse.bass as bass
import concourse.mybir as mybir
from concourse.bass_utils import run_bass_kernel_spmd

F32 = mybir.dt.float32
BF16 = mybir.dt.bfloat16
AF = mybir.ActivationFunctionType
ALU = mybir.AluOpType
AX = mybir.AxisListType


class Prog:
    ENGS = ("sync", "scalar", "vector", "gpsimd", "tensor")
    NDMA = 8
    R = 8

    def __init__(self, nc, stack):
        self.nc = nc
        self.q = {e: [] for e in self.ENGS}
        self.sem = {e: [stack.enter_context(nc.semaphore("s_%s%d" % (e, i))) for i in range(self.R)]
                    for e in self.ENGS}
        self.cnt = {e: 0 for e in self.ENGS}
        self.dsem = {e: [stack.enter_context(nc.semaphore("d_%s%d" % (e, i))) for i in range(self.NDMA)]
                     for e in ("sync", "scalar", "gpsimd")}
        self.dcnt = {e: 0 for e in self.dsem}
        self.semobj = {}
        for e in self.ENGS:
            for i in range(self.R):
                self.semobj[("c", e, i)] = self.sem[e][i]
        for e in self.dsem:
            for i in range(self.NDMA):
                self.semobj[("d", e, i)] = self.dsem[e][i]
        self.waited = {e: {} for e in self.ENGS}
        self.lastw = {}
        self.readers = {}
        self.n_inst = 0

    def _waits(self, eng, deps):
        need = {}
        for (sid, val) in deps:
            if sid[0] == "c" and sid[1] == eng and (val - 1) * self.R + sid[2] + 1 > self.cnt[eng]:
                continue
            if self.waited[eng].get(sid, 0) >= val:
                continue
            if need.get(sid, 0) < val:
                need[sid] = val
        out = []
        for sid, val in need.items():
            self.waited[eng][sid] = val
            out.append((self.semobj[sid], val))
        return out

    def _deps(self, reads, writes):
        deps = []
        for k in reads:
            if k in self.lastw:
                deps.append(self.lastw[k])
        for k in writes:
            if k in self.lastw:
                deps.append(self.lastw[k])
            deps.extend(self.readers.get(k, ()))
        return deps

    def _commit(self, tok, reads, writes):
        for k in reads:
            self.readers.setdefault(k, []).append(tok)
        for k in writes:
            self.lastw[k] = tok
            self.readers[k] = []

    def op(self, eng, fn, reads=(), writes=(), signal=True):
        waits = self._waits(eng, self._deps(reads, writes))
        n = self.cnt[eng]
        tok = (("c", eng, n % self.R), n // self.R + 1)
        if signal:
            self.cnt[eng] += 1
        sem = self.sem[eng][n % self.R]

        def run(e, fn=fn, waits=waits, signal=signal, sem=sem):
            for (s, v) in waits:
                e.wait_ge(s, v)
            ins = fn(e)
            if signal:
                ins.then_inc(sem, 1)

        self.q[eng].append(run)
        self._commit(tok, reads, writes)
        self.n_inst += 1

    def dma(self, eng, out, in_, reads=(), writes=(), **kw):
        n = self.dcnt[eng]
        self.dcnt[eng] += 1
        slot = n % self.NDMA
        val = 16 * (n // self.NDMA + 1)
        sid = ("d", eng, slot)
        deps = self._deps(reads, writes)
        if val > 16:
            deps.append((sid, val - 16))
        waits = self._waits(eng, deps)
        sem = self.dsem[eng][slot]

        def run(e, waits=waits, sem=sem, out=out, in_=in_, kw=kw):
            for (s, v) in waits:
                e.wait_ge(s, v)
            e.dma_start(out=out, in_=in_, **kw).then_inc(sem, 16)

        self.q[eng].append(run)
        self._commit((sid, val), reads, writes)
        self.n_inst += 1

    def barrier(self):
        deps = []
        for e in self.ENGS:
            n = self.cnt[e]
            for i in range(self.R):
                if n >= i + 1:
                    deps.append((("c", e, i), (n - 1 - i) // self.R + 1))
        for e in self.dsem:
            n = self.dcnt[e]
            for i in range(self.NDMA):
                if n >= i + 1:
                    deps.append((("d", e, i), 16 * ((n - 1 - i) // self.NDMA + 1)))
        for e in self.ENGS:
            waits = self._waits(e, deps)

            def run(en, waits=waits):
                for (s, v) in waits:
                    en.wait_ge(s, v)

            self.q[e].append(run)

    def finish(self, eng, keys):
        waits = self._waits(eng, self._deps(keys, ()))

        def run(e, waits=waits):
            for (s, v) in waits:
                e.wait_ge(s, v)

        self.q[eng].append(run)

    def emit(self, block):
        q = self.q

        @block.sync
        def _(e):
            for f in q["sync"]:
                f(e)

        @block.scalar
        def _(e):
            for f in q["scalar"]:
                f(e)

        @block.vector
        def _(e):
            for f in q["vector"]:
                f(e)

        @block.gpsimd
        def _(e):
            for f in q["gpsimd"]:
                f(e)

        @block.tensor
        def _(e):
            for f in q["tensor"]:
                f(e)


S = 4096
D = 1024
NT = 32
DFF = 2816
NFC = 22
NEG = -30000.0
L = 2


def build(debug=None, nlayers=L):
    from contextlib import ExitStack
    nc = bass.Bass("TRN2", target_bir_lowering=False)
    dt_in = lambda name, shape: nc.dram_tensor(name, shape, F32, kind="ExternalInput").ap()
    x_in = dt_in("x", [S, D])
    norm_mix_w = dt_in("norm_mix_w", [L, D])
    w_in = dt_in("w_in", [L, D, 2328])
    gmlp_norm_w = dt_in("gmlp_norm_w", [L, 512])
    gmlp_ws = dt_in("gmlp_ws", [L, 8, 128, 128])
    gmlp_bs = dt_in("gmlp_bs", [L, 8, 128])
    cmp_pos = {"k": dt_in("cmp_pos_k", [L, 32, 64]), "v": dt_in("cmp_pos_v", [L, 32, 64])}
    cmp_w1 = {"k": dt_in("cmp_k_w1", [L, 2048, 256]), "v": dt_in("cmp_v_w1", [L, 2048, 256])}
    cmp_w2 = {"k": dt_in("cmp_k_w2", [L, 256, 64]), "v": dt_in("cmp_v_w2", [L, 256, 64])}
    gate_b = dt_in("gate_b", [L, 24])
    out_norm_a_w = dt_in("out_norm_a_w", [L, 512])
    out_norm_b_w = dt_in("out_norm_b_w", [L, 512])
    w_o = dt_in("w_o", [L, D, D])
    norm_ffn_w = dt_in("norm_ffn_w", [L, D])
    w_gate = dt_in("w_gate", [L, D, DFF])
    w_up = dt_in("w_up", [L, D, DFF])
    w_down = dt_in("w_down", [L, DFF, D])
    final_norm_w = dt_in("final_norm_w", [D])
    out = nc.dram_tensor("out", [S, D], F32, kind="ExternalOutput").ap()
    dbg = debug is not None
    mix_d = nc.dram_tensor("mix_d", [S, D], BF16, kind="ExternalOutput" if dbg else "Internal").ap()
    xres_d = nc.dram_tensor("xres_d", [S, D], F32, kind="ExternalOutput" if dbg else "Internal").ap()

    with ExitStack() as st:
        P = Prog(nc, st)
        op = P.op

        sfx = [""]

        def T(name, shape, dt, stack=None):
            return (stack or st).enter_context(nc.sbuf_tensor(name + sfx[0], shape, dt))

        B = [st.enter_context(nc.psum_tensor("B%d" % i, [128, 512], F32)) for i in range(7)]
        BT = st.enter_context(nc.psum_tensor("BT", [128, 8, 128], BF16))

        ident_b = T("ident_b", [128, 128], BF16)
        ident_f = T("ident_f", [128, 128], F32)
        tri_f = T("tri_f", [128, 128], F32)
        tri_b = T("tri_b", [128, 128], BF16)
        ntri_b = T("ntri_b", [128, 128], BF16)
        ones_f = T("ones_f", [128, 128], F32)
        ind8 = T("ind8", [8, 512], F32)
        A_big = T("A_big", [16, 512], BF16)
        Bm4 = T("Bm4", [16, 4, 128], BF16)
        VM = T("VM", [128, 128], F32)
        AMk = T("AMk", [128, 128], F32)
        tmpc = T("tmpc", [128, 512], F32)
        tmpc2 = T("tmpc2", [128, 512], F32)

        def G(fn, reads=(), writes=()):
            op("gpsimd", fn, reads=reads, writes=writes)

        def asel(t, pattern, cmp_, fill, base, cm, key):
            G(lambda e: e.affine_select(out=t, in_=t, pattern=pattern, compare_op=cmp_, fill=fill,
                                        base=base, channel_multiplier=cm), reads=[key], writes=[key])

        G(lambda e: e.memset(ones_f[:], 1.0), writes=["ones_f"])
        G(lambda e: e.memset(ident_f[:], 1.0), writes=["ident_f"])
        asel(ident_f[:], [[-1, 128]], ALU.is_equal, 0.0, 0, 1, "ident_f")
        G(lambda e: e.tensor_copy(ident_b[:], ident_f[:]), reads=["ident_f"], writes=["ident_b"])
        G(lambda e: e.memset(tri_f[:], 1.0), writes=["tri_f"])
        asel(tri_f[:], [[1, 128]], ALU.is_ge, 0.0, 0, -1, "tri_f")
        G(lambda e: e.tensor_copy(tri_b[:], tri_f[:]), reads=["tri_f"], writes=["tri_b"])
        G(lambda e: e.memset(tmpc[:, 0:128], 1.0), writes=["tmpc"])
        asel(tmpc[:, 0:128], [[-1, 128]], ALU.is_gt, 0.0, 0, 1, "tmpc")
        G(lambda e: e.tensor_copy(ntri_b[:], tmpc[:, 0:128]), reads=["tmpc"], writes=["ntri_b"])
        G(lambda e: e.memset(ind8[:], 1.0), writes=["ind8"])
        asel(ind8[:].rearrange("p (h d) -> p h d", h=8), [[1, 8], [0, 64]], ALU.is_equal, 0.0, 0, -1, "ind8")
        G(lambda e: e.memset(tmpc[0:16, :], 1.0), writes=["tmpc"])
        asel(tmpc[0:16, :], [[1, 512]], ALU.is_equal, 0.0, -255, -1, "tmpc")
        G(lambda e: e.memset(tmpc2[0:16, :], 1.0), writes=["tmpc2"])
        asel(tmpc2[0:16, :], [[1, 512]], ALU.is_ge, 0.0, -263, 0, "tmpc2")
        asel(tmpc2[0:16, :], [[0, 512]], ALU.is_equal, 0.0, -8, 1, "tmpc2")
        asel(tmpc[0:16, :], [[0, 512]], ALU.is_ge, 0.0, 7, -1, "tmpc")
        G(lambda e: e.tensor_tensor(out=A_big[:], in0=tmpc[0:16, :], in1=tmpc2[0:16, :], op=ALU.add),
          reads=["tmpc", "tmpc2"], writes=["A_big"])
        G(lambda e: e.memset(tmpc[0:16, :], NEG), writes=["tmpc"])
        asel(tmpc[0:16, :].rearrange("p (g t) -> p g t", g=4), [[0, 4], [-1, 128]], ALU.is_gt, 0.0, 15, 16, "tmpc")
        asel(tmpc[0:16, :], [[0, 512]], ALU.is_ge, 0.0, 7, -1, "tmpc")
        G(lambda e: e.memset(tmpc2[0:16, :], NEG), writes=["tmpc2"])
        asel(tmpc2[0:16, :], [[0, 512]], ALU.is_equal, 0.0, -8, 1, "tmpc2")
        G(lambda e: e.tensor_tensor(out=Bm4[:].rearrange("p g t -> p (g t)"), in0=tmpc[0:16, :], in1=tmpc2[0:16, :], op=ALU.add),
          reads=["tmpc", "tmpc2"], writes=["Bm4"])
        G(lambda e: e.memset(VM[:], 1.0), writes=["VM"])
        asel(VM[:], [[-64, 128]], ALU.is_ge, 0.0, 64 * 64 - 128, 1, "VM")
        G(lambda e: e.memset(AMk[:], 10000.0), writes=["AMk"])
        asel(AMk[:], [[-64, 128]], ALU.is_ge, 0.0, 64 * 64, 1, "AMk")
        asel(AMk[:], [[64, 128]], ALU.is_ge, 0.0, -64 * 64 + 63, -1, "AMk")
        G(lambda e: e.memset(tmpc[:, 0:128], 10001.0), writes=["tmpc"])
        asel(tmpc[:, 0:128], [[-64, 128]], ALU.is_ge, 0.0, 64 * 64 - 64, 1, "tmpc")
        asel(tmpc[:, 0:128], [[64, 128]], ALU.is_ge, 0.0, -64 * 64 + 64 + 63, -1, "tmpc")
        G(lambda e: e.tensor_tensor(out=AMk[:], in0=AMk[:], in1=tmpc[:, 0:128], op=ALU.add), reads=["AMk", "tmpc"], writes=["AMk"])
        G(lambda e: e.memset(tmpc2[:, 0:128], -10000.0), writes=["tmpc2"])
        asel(tmpc2[:, 0:128], [[64, 128]], ALU.is_ge, 0.0, -64 * 64 - 1, -1, "tmpc2")
        G(lambda e: e.tensor_tensor(out=AMk[:], in0=AMk[:], in1=tmpc2[:, 0:128], op=ALU.add), reads=["AMk", "tmpc2"], writes=["AMk"])


        def rms_stats(src_ap, width, key_src, junk, ssum, rstd, tag):
            sc = float(width) ** -0.5
            op("scalar", lambda e: e.activation(out=junk, in_=src_ap, func=AF.Square, scale=sc, accum_out=ssum[:, 0:1]),
               reads=[key_src], writes=["junk" + tag, "ss" + tag])
            op("vector", lambda e: e.tensor_scalar(rstd[:, 0:1], ssum[:, 0:1], 1e-6, None, ALU.add), reads=["ss" + tag], writes=["rstd" + tag])
            op("scalar", lambda e: e.activation(out=rstd[:, 0:1], in_=rstd[:, 0:1], func=AF.Sqrt), reads=["rstd" + tag], writes=["rstd" + tag])
            op("vector", lambda e: e.reciprocal(rstd[:, 0:1], rstd[:, 0:1]), reads=["rstd" + tag], writes=["rstd" + tag])

        def layer(l):
            sfx[0] = "_L%d" % l
            xsrc = x_in if l == 0 else xres_d
            last = (l == nlayers - 1)
            P.barrier()
            with ExitStack() as s1:
                nmw = T("nmw", [128, D], F32, s1)
                gnw = T("gnw", [128, 512], F32, s1)
                onaw = T("onaw", [128, 512], F32, s1)
                onbw = T("onbw", [128, 512], F32, s1)
                gbt = T("gbt", [128, 24], F32, s1)
                bs8 = T("bs8", [8, 128], F32, s1)
                gates = T("gates", [128, NT, 24], F32, s1)
                P.dma("sync", nmw[:], norm_mix_w[l].partition_broadcast(128), writes=["nmw"])
                P.dma("sync", gnw[:], gmlp_norm_w[l].partition_broadcast(128), writes=["gnw"])
                P.dma("sync", onaw[:], out_norm_a_w[l].partition_broadcast(128), writes=["onaw"])
                P.dma("sync", onbw[:], out_norm_b_w[l].partition_broadcast(128), writes=["onbw"])
                P.dma("sync", gbt[:], gate_b[l].partition_broadcast(128), writes=["gbt"])
                P.dma("sync", bs8[:], gmlp_bs[l], writes=["bs8"])

                wtm = T("wtm", [128, 8, 1304], BF16, s1)
                wfm = T("wfm", [128, 8, 1024], BF16, s1)
                wsT = T("wsT", [128, 8, 128], BF16, s1)
                kTE = [T("kTE%d" % k, [128, S], BF16, s1) for k in range(2)]
                kTw = T("kTw", [128, S], BF16, s1)
                kcT = T("kcT", [128, S], BF16, s1)
                vcT = T("vcT", [128, S], BF16, s1)
                vs_aug = T("vs_aug", [128, NT, 2, 65], BF16, s1)
                vw_aug = T("vw_aug", [128, NT, 2, 65], BF16, s1)
                qT_all = T("qT_all", [128, 4, S], BF16, s1)
                w2k_pad = T("w2k_pad", [128, 2, 2, 128], BF16, s1)
                w2v = T("w2v", [128, 2, 64], BF16, s1)
                hidT = T("hidT", [128, 2, 256], BF16, s1)
                kcmp = T("kcmp", [128, 256], BF16, s1)
                R_cmp = T("R_cmp", [128, 2, 2, 129], BF16, s1)

                with ExitStack() as s0:
                    stage = [T("stage%d" % i, [128, 2328], F32, s0) for i in range(2)]
                    for c in range(8):
                        sg = stage[c % 2]
                        sk = "stage%d" % (c % 2)
                        P.dma("sync", sg[:], w_in[l, c * 128:(c + 1) * 128, :], writes=[sk])
                        cp = [
                            (wtm[:, c, 0:1024], sg[:, 0:1024]),
                            (wtm[:, c, 1024:1152], sg[:, 1920:2048]),
                            (wtm[:, c, 1152:1280], sg[:, 2176:2304]),
                            (wtm[:, c, 1280:1304], sg[:, 2304:2328]),
                            (wfm[:, c, 0:512].rearrange("p (g k d) -> p g k d", g=4, k=2),
                             sg[:, 1024:1536].rearrange("p (k g d) -> p g k d", k=2, g=4)),
                            (wfm[:, c, 512:640], sg[:, 1536:1664]),
                            (wfm[:, c, 640:768], sg[:, 1664:1792]),
                            (wfm[:, c, 768:896], sg[:, 1792:1920]),
                            (wfm[:, c, 896:1024], sg[:, 2048:2176]),
                        ]
                        for ci, (o_, i_) in enumerate(cp):
                            eng = "gpsimd" if ci % 2 == 0 else "vector"
                            op(eng, lambda e, o_=o_, i_=i_: e.tensor_copy(o_, i_), reads=[sk], writes=["wtm" if ci < 4 else "wfm"])
                    wsf = T("wsf", [128, 8, 128], F32, s0)
                    P.dma("sync", wsf[:], gmlp_ws[l].rearrange("h t s -> t h s"), writes=["wsf"])
                    for h in range(8):
                        bk = B[h % 2]
                        op("tensor", lambda e, h=h, bk=bk: e.transpose(bk[:, 0:128], wsf[:, h, :], ident_f[:]),
                           reads=["wsf", "ident_f"], writes=["B%d" % (h % 2)])
                        op("vector", lambda e, h=h, bk=bk: e.tensor_tensor(out=wsT[:, h, :], in0=bk[:, 0:128], in1=tri_f[:], op=ALU.mult),
                           reads=["B%d" % (h % 2), "tri_f"], writes=["wsT"])
                    for nt_ in range(2):
                        G(lambda e: e.memset(tmpc[:, 0:64], 1.0), writes=["tmpc"])
                        asel(tmpc[:, 0:64], [[-64, 64]], ALU.is_ge, 0.0, 2048 * nt_ + 31, 16, "tmpc")
                        asel(tmpc[:, 0:64], [[64, 64]], ALU.is_ge, 0.0, 63 - 2048 * nt_, -16, "tmpc")
                        for k in range(2):
                            G(lambda e, nt_=nt_, k=k: e.tensor_copy(R_cmp[:, nt_, k, 64:128], tmpc[:, 0:64]), reads=["tmpc"], writes=["R_cmp"])
                    G(lambda e: e.memset(R_cmp[:, :, :, 128:129], 1.0), writes=["R_cmp"])
                    G(lambda e: e.memset(vs_aug[:, :, :, 64:65], 1.0), writes=["vs_aug"])
                    G(lambda e: e.memset(vw_aug[:, :, :, 64:65], 1.0), writes=["vw_aug"])
                    G(lambda e: e.memset(kTE[0][:], 1.0), writes=["kTE0"])
                    asel(kTE[0][:].rearrange("p (j r) -> p j r", j=64), [[1, 64], [0, 64]], ALU.is_equal, 0.0, 64, -1, "kTE0")
                    G(lambda e: e.memset(kTE[1][:], 1.0), writes=["kTE1"])
                    asel(kTE[1][:].rearrange("p (j r) -> p j r", j=64), [[1, 64], [0, 64]], ALU.is_equal, 0.0, 0, -1, "kTE1")
                    P.barrier()

                with ExitStack() as s2:
                    xt = [T("xt%d" % i, [128, D], F32, s2) for i in range(2)]
                    junk = T("junk", [128, D], BF16, s2)
                    ssx = T("ssx", [128, 1], F32, s2)
                    rsx = T("rsx", [128, 1], F32, s2)
                    hb = T("hb", [128, D], BF16, s2)
                    hT = [T("hT%d" % i, [128, 8, 512], BF16, s2) for i in range(2)]
                    gu = T("gu", [128, 512], F32, s2)
                    gv = T("gv", [128, 512], F32, s2)
                    ssv = T("ssv", [128, 1], F32, s2)
                    rsv = T("rsv", [128, 1], F32, s2)
                    vn = T("vn", [128, 512], BF16, s2)
                    a_t = T("a_t", [128, 512], F32, s2)
                    ssa = T("ssa", [128, 1], F32, s2)
                    rsa = T("rsa", [128, 1], F32, s2)
                    mixA = [T("mixA%d" % i, [128, 512], BF16, s2) for i in range(2)]
                    gpre = T("gpre", [128, 24], F32, s2)
                    for TT in range(8):
                        hTt = hT[TT % 2]
                        hk = "hT%d" % (TT % 2)
                        for tt in range(4):
                            i = TT * 4 + tt
                            xti = xt[i % 2]
                            xk = "xt%d" % (i % 2)
                            P.dma("sync", xti[:], xsrc[i * 128:(i + 1) * 128, :], writes=[xk])
                            rms_stats(xti[:], D, xk, junk[:], ssx, rsx, "x")
                            op("vector", lambda e, xti=xti: e.scalar_tensor_tensor(out=hb[:], in0=xti[:], scalar=rsx[:, 0:1], in1=nmw[:], op0=ALU.mult, op1=ALU.mult),
                               reads=[xk, "rstdx", "nmw"], writes=["hb"])
                            for c in range(8):
                                op("tensor", lambda e, c=c: e.transpose(BT[:, c, :], hb[:, c * 128:(c + 1) * 128], ident_b[:]),
                                   reads=["hb", "ident_b"], writes=["BT"], signal=(c == 7))
                            op("vector", lambda e, hTt=hTt, tt=tt: e.tensor_copy(hTt[:, :, tt * 128:(tt + 1) * 128], BT[:]), reads=["BT"], writes=[hk])
                            for cb, (c0, c1) in enumerate(((0, 512), (512, 1024), (1024, 1304))):
                                for c in range(8):
                                    op("tensor", lambda e, cb=cb, c=c, c0=c0, c1=c1, hTt=hTt, tt=tt: e.matmul(B[cb][:, 0:c1 - c0], lhsT=hTt[:, c, tt * 128:(tt + 1) * 128], rhs=wtm[:, c, c0:c1],
                                                                                                      start=(c == 0), stop=(c == 7)),
                                       reads=[hk, "wtm"], writes=["B%d" % cb], signal=(c == 7))
                            op("scalar", lambda e: e.activation(out=gu[:], in_=B[0][:], func=AF.Gelu_apprx_tanh), reads=["B0"], writes=["gu"])
                            op("scalar", lambda e: e.activation(out=gv[:], in_=B[1][:], func=AF.Gelu_apprx_tanh), reads=["B1"], writes=["gv"])
                            rms_stats(gv[:], 512, "gv", junk[:, 0:512], ssv, rsv, "v")
                            op("vector", lambda e: e.scalar_tensor_tensor(out=vn[:], in0=gv[:], scalar=rsv[:, 0:1], in1=gnw[:], op0=ALU.mult, op1=ALU.mult),
                               reads=["gv", "rstdv", "gnw"], writes=["vn"])
                            op("vector", lambda e, i=i: e.tensor_copy(vs_aug[:, i, :, 0:64], B[2][:, 0:128].rearrange("p (k d) -> p k d", k=2)), reads=["B2"], writes=["vs_aug"])
                            op("vector", lambda e, i=i: e.tensor_copy(vw_aug[:, i, :, 0:64], B[2][:, 128:256].rearrange("p (k d) -> p k d", k=2)), reads=["B2"], writes=["vw_aug"])
                            op("vector", lambda e: e.tensor_tensor(out=gpre[:], in0=B[2][:, 256:280], in1=gbt[:], op=ALU.add), reads=["B2", "gbt"], writes=["gpre"])
                            op("scalar", lambda e, i=i: e.activation(out=gates[:, i, :], in_=gpre[:], func=AF.Sigmoid), reads=["gpre"], writes=["gates"])
                            op("tensor", lambda e: e.matmul(B[3][:], lhsT=bs8[:], rhs=ind8[:], start=True, stop=False), reads=["bs8", "ind8"], writes=["B3"], signal=False)
                            for h in range(8):
                                op("tensor", lambda e, h=h: e.matmul(B[3][:, h * 64:(h + 1) * 64], lhsT=wsT[:, h, :], rhs=vn[:, h * 64:(h + 1) * 64], start=False, stop=(h == 7)),
                                   reads=["wsT", "vn"], writes=["B3"], signal=(h == 7))
                            op("vector", lambda e: e.tensor_tensor(out=a_t[:], in0=B[3][:], in1=gu[:], op=ALU.mult), reads=["B3", "gu"], writes=["a_t"])
                            rms_stats(a_t[:], 512, "a_t", junk[:, 512:1024], ssa, rsa, "a")
                            mA = mixA[i % 2]
                            mk = "mixA%d" % (i % 2)
                            op("vector", lambda e, mA=mA: e.scalar_tensor_tensor(out=mA[:], in0=a_t[:], scalar=rsa[:, 0:1], in1=onaw[:], op0=ALU.mult, op1=ALU.mult),
                               reads=["a_t", "rstda", "onaw"], writes=[mk])
                            P.dma("gpsimd", mix_d[i * 128:(i + 1) * 128, 0:512], mA[:], reads=[mk], writes=["mix_d"])
                        tok = slice(TT * 512, (TT + 1) * 512)
                        for ch in range(8):
                            bk = B[4 + ch % 2]
                            bkk = "B%d" % (4 + ch % 2)
                            for c in range(8):
                                op("tensor", lambda e, bk=bk, ch=ch, c=c, hTt=hTt: e.matmul(bk[:], lhsT=wfm[:, c, ch * 128:(ch + 1) * 128], rhs=hTt[:, c, :], start=(c == 0), stop=(c == 7)),
                                   reads=[hk, "wfm"], writes=[bkk], signal=(c == 7))
                            if ch < 4:
                                op("scalar", lambda e, bk=bk, ch=ch, tok=tok: e.activation(out=qT_all[:, ch, tok], in_=bk[:], func=AF.Copy, scale=0.125), reads=[bkk], writes=["qT_all"])
                            elif ch == 4:
                                op("vector", lambda e, bk=bk, tok=tok: e.tensor_copy(kcT[:, tok], bk[:]), reads=[bkk], writes=["kcT"])
                            elif ch == 5:
                                op("vector", lambda e, bk=bk, tok=tok: e.tensor_copy(vcT[:, tok], bk[:]), reads=[bkk], writes=["vcT"])
                            elif ch == 6:
                                op("vector", lambda e, bk=bk, tok=tok: e.tensor_copy(kTE[0][0:64, tok], bk[0:64, :]), reads=[bkk], writes=["kTE0"])
                                op("vector", lambda e, bk=bk, tok=tok: e.tensor_copy(kTE[1][64:128, tok], bk[64:128, :]), reads=[bkk], writes=["kTE1"])
                            else:
                                op("vector", lambda e, bk=bk, tok=tok: e.tensor_copy(kTw[:, tok], bk[:]), reads=[bkk], writes=["kTw"])
                    P.barrier()

                with ExitStack() as s25:
                    w1A = {kv: T("w1A" + kv, [128, 32, 256], BF16, s25) for kv in "kv"}
                    posT = {kv: T("posT" + kv, [128, 32], BF16, s25) for kv in "kv"}
                    bcol = {kv: T("bcol" + kv, [128, 2], F32, s25) for kv in "kv"}
                    w1f = T("w1f", [128, 8, 256], F32, s25)
                    posf = T("posf", [32, 64], F32, s25)
                    w2f = T("w2f", [128, 2, 64], F32, s25)
                    G(lambda e: e.memset(w2k_pad[:], 0.0), writes=["w2k_pad"])
                    for kv in "kv":
                        src = cmp_w1[kv][l].rearrange("(l d) n -> d l n", d=64)
                        for q4 in range(4):
                            P.dma("sync", w1f[0:64], src[:, q4 * 8:(q4 + 1) * 8, :], writes=["w1f"])
                            P.dma("sync", w1f[64:128], src[:, q4 * 8:(q4 + 1) * 8, :], writes=["w1f"])
                            G(lambda e, kv=kv, q4=q4: e.tensor_copy(w1A[kv][:, q4 * 8:(q4 + 1) * 8, :], w1f[:]), reads=["w1f"], writes=["w1A" + kv])
                        P.dma("sync", posf[:], cmp_pos[kv][l], writes=["posf"])
                        op("tensor", lambda e: e.transpose(B[2][0:64, 0:32], posf[:, :], ident_f[0:32, 0:32]), reads=["posf", "ident_f"], writes=["B2"])
                        op("vector", lambda e, kv=kv: e.tensor_copy(posT[kv][0:64, :], B[2][0:64, 0:32]), reads=["B2"], writes=["posT" + kv])
                        for hc in range(2):
                            for ll in range(32):
                                op("tensor", lambda e, kv=kv, hc=hc, ll=ll: e.matmul(B[3][:, hc:hc + 1], lhsT=w1A[kv][0:64, ll, hc * 128:(hc + 1) * 128],
                                                                                 rhs=posT[kv][0:64, ll:ll + 1], start=(ll == 0), stop=(ll == 31)),
                                   reads=["w1A" + kv, "posT" + kv], writes=["B3"], signal=(ll == 31))
                        op("vector", lambda e, kv=kv: e.tensor_copy(bcol[kv][:], B[3][:, 0:2]), reads=["B3"], writes=["bcol" + kv])
                        P.dma("sync", w2f[:], cmp_w2[kv][l].rearrange("(c p) d -> p c d", p=128), writes=["w2f"])
                        if kv == "k":
                            for k in range(2):
                                G(lambda e, k=k: e.tensor_copy(w2k_pad[:, :, k, 64 * k:64 * k + 64], w2f[:]), reads=["w2f"], writes=["w2k_pad"])
                        else:
                            G(lambda e: e.tensor_copy(w2v[:], w2f[:]), reads=["w2f"], writes=["w2v"])
                    for kv, srcT in (("k", kcT), ("v", vcT)):
                        for k in range(2):
                            pr = slice(64 * k, 64 * k + 64)
                            for hc in range(2):
                                bk = B[hc]
                                for ll in range(32):
                                    op("tensor", lambda e, kv=kv, hc=hc, ll=ll, bk=bk, pr=pr, srcT=srcT: e.matmul(bk[:, 0:255], lhsT=w1A[kv][pr, ll, hc * 128:(hc + 1) * 128],
                                                                                                       rhs=srcT[pr, ll:ll + 16 * 254 + 1:16], start=(ll == 0), stop=(ll == 31)),
                                       reads=["w1A" + kv, "kcT", "vcT"], writes=["B%d" % hc], signal=(ll == 31))
                                op("scalar", lambda e, kv=kv, hc=hc, bk=bk: e.activation(out=hidT[:, hc, 0:255], in_=bk[:, 0:255], func=AF.Gelu_apprx_tanh, bias=bcol[kv][:, hc:hc + 1]),
                                   reads=["B%d" % hc, "bcol" + kv], writes=["hidT"])
                            if kv == "k":
                                for hc in range(2):
                                    op("tensor", lambda e, hc=hc, k=k: e.matmul(B[2][:, 0:255], lhsT=w2k_pad[:, hc, k, :], rhs=hidT[:, hc, 0:255], start=(hc == 0), stop=(hc == 1)),
                                       reads=["w2k_pad", "hidT"], writes=["B2"], signal=(hc == 1))
                                op("vector", lambda e, pr=pr: e.tensor_copy(kcmp[pr, 0:255], B[2][pr, 0:255]), reads=["B2"], writes=["kcmp"])
                            else:
                                for nt_, M in ((0, 128), (1, 127)):
                                    for hc in range(2):
                                        op("tensor", lambda e, hc=hc, nt_=nt_, M=M: e.matmul(B[3][0:M, 0:64], lhsT=hidT[:, hc, nt_ * 128:nt_ * 128 + M], rhs=w2v[:, hc, :], start=(hc == 0), stop=(hc == 1)),
                                           reads=["w2v", "hidT"], writes=["B3"], signal=(hc == 1))
                                    op("vector", lambda e, nt_=nt_, M=M, k=k: e.tensor_copy(R_cmp[0:M, nt_, k, 0:64], B[3][0:M, 0:64]), reads=["B3"], writes=["R_cmp"])
                    P.barrier()

                with ExitStack() as s3:
                    qB = [T("qB%d" % k, [128, 4, 128], BF16, s3) for k in range(2)]
                    PTc = T("PTc", [128, 2, 512], BF16, s3)
                    PT = [T("PT%d" % i, [128, 512], BF16, s3) for i in range(4)]
                    oc2 = [T("oc%d" % j, [128, 4, 129], F32, s3) for j in range(2)]
                    rc2 = [T("rc%d" % j, [128, 4], F32, s3) for j in range(2)]
                    imp = T("imp", [128, 64], F32, s3)
                    score = T("score", [128, 64], F32, s3)
                    sc2 = T("sc2", [128, 64], F32, s3)
                    m8 = T("m8", [128, 16], F32, s3)
                    biasP = [T("biasP%d" % k, [128, 128], BF16, s3) for k in range(2)]
                    oT_sb = T("oT_sb", [128, 2, 512], F32, s3)
                    rr = T("rr", [128, 2, 4], F32, s3)
                    rg = T("rg", [128, 3, 4], F32, s3)
                    bfull2 = [T("bfull%d" % j, [128, 512], F32, s3) for j in range(2)]
                    btmp2 = [T("btmp%d" % j, [128, 4, 64], F32, s3) for j in range(2)]
                    ssb = T("ssb", [128, 1], F32, s3)
                    rsb = T("rsb", [128, 1], F32, s3)
                    junkb = T("junkb", [128, 512], F32, s3)
                    mixB = [T("mixB%d" % i, [128, 512], BF16, s3) for i in range(2)]
                    for k in range(2):
                        G(lambda e, k=k: e.memset(qB[k][:], 0.0), writes=["qB%d" % k])
                        G(lambda e, k=k: e.memset(biasP[k][:], 0.0), writes=["biasP%d" % k])
                    psC = [B[6][:, 260:389], B[5][:, 0:129], B[5][:, 129:258], B[5][:, 258:387]]
                    psCk = ["B6", "B5", "B5", "B5"]
                    psB = BT[:, 0, :]
                    psT = B[6][:, 0:260].rearrange("p (g d) -> p g d", g=4)
                    sidx = [0]
                    pidx = [0]

                    def stageA(i, k, n):
                        pr = slice(64 * k, 64 * k + 64)
                        br_ = slice(64, 128) if k == 0 else slice(0, 64)
                        qk = "qB%d" % k
                        ocn, rcn = oc2[n % 2], rc2[n % 2]
                        ock, rck = "oc%d" % (n % 2), "rc%d" % (n % 2)
                        G(lambda e: e.tensor_copy(qB[k][pr, :, :], qT_all[pr, :, i * 128:(i + 1) * 128]), reads=["qT_all"], writes=[qk])
                        yield
                        n_tiles = 1 if i <= 15 else 2
                        for nt_ in range(n_tiles):
                            M = 128 if nt_ == 0 else 127
                            bs_ = B[5]
                            bsk = "B5"
                            a0 = 256 - 8 * i + nt_ * 128
                            op("tensor", lambda e, bs_=bs_, M=M, nt_=nt_: e.matmul(bs_[0:M, :], lhsT=kcmp[pr, nt_ * 128:nt_ * 128 + M], rhs=qT_all[pr, :, i * 128:(i + 1) * 128], start=True, stop=False),
                               reads=["kcmp", "qT_all"], writes=[bsk], signal=False)
                            op("tensor", lambda e, bs_=bs_, M=M, a0=a0: e.matmul(bs_[0:M, :], lhsT=A_big[0:9, a0:a0 + M], rhs=Bm4[0:9, :, :], start=False, stop=True),
                               reads=["A_big", "Bm4"], writes=[bsk])
                            yield
                            op("scalar", lambda e, bs_=bs_, M=M, nt_=nt_: e.activation(out=PTc[0:M, nt_, :], in_=bs_[0:M, :], func=AF.Exp), reads=[bsk], writes=["PTc"])
                            yield
                        for g in range(4):
                            for nt_ in range(n_tiles):
                                M = 128 if nt_ == 0 else 127
                                op("tensor", lambda e, g=g, nt_=nt_, M=M: e.matmul(psC[g], lhsT=PTc[0:M, nt_, g * 128:(g + 1) * 128], rhs=R_cmp[0:M, nt_, k, :],
                                                                                start=(nt_ == 0), stop=(nt_ == n_tiles - 1)),
                                   reads=["PTc", "R_cmp"], writes=[psCk[g]], signal=(nt_ == n_tiles - 1))
                            if g % 2 == 1:
                                yield
                        for g in range(4):
                            op("vector", lambda e, g=g: e.tensor_copy(ocn[:, g, :], psC[g]), reads=[psCk[g]], writes=[ock])
                            if g % 2 == 1:
                                yield
                        op("vector", lambda e: e.tensor_scalar(rcn[:], ocn[:, :, 128], 1e-30, None, ALU.max), reads=[ock], writes=[rck])
                        yield
                        op("vector", lambda e: e.reciprocal(rcn[:], rcn[:]), reads=[rck], writes=[rck])
                        yield
                        if i >= 8:
                            op("vector", lambda e: e.tensor_scalar(imp[:], ocn[:, 0, 64:128], rcn[:, 0:1], None, ALU.mult), reads=[ock, rck], writes=["imp"])
                            yield
                            for g in range(1, 4):
                                op("vector", lambda e, g=g: e.scalar_tensor_tensor(out=imp[:], in0=ocn[:, g, 64:128], scalar=rcn[:, g:g + 1], in1=imp[:], op0=ALU.mult, op1=ALU.add),
                                   reads=[ock, rck, "imp"], writes=["imp"])
                                yield
                            c0 = 64 - 2 * i
                            op("vector", lambda e: e.tensor_tensor(out=score[:], in0=imp[:], in1=VM[:, c0:c0 + 64], op=ALU.mult), reads=["imp", "VM"], writes=["score"])
                            yield
                            op("vector", lambda e: e.tensor_tensor(out=score[:], in0=score[:], in1=AMk[:, c0:c0 + 64], op=ALU.add), reads=["score", "AMk"], writes=["score"])
                            yield
                            op("vector", lambda e: e.memset(score[:, 0:1], 10002.0), reads=["score"], writes=["score"])
                            yield
                            op("vector", lambda e: e.max(out=m8[:, 0:8], in_=score[:]), reads=["score"], writes=["m8"])
                            yield
                            op("vector", lambda e: e.match_replace(out=sc2[:], in_to_replace=m8[:, 0:8], in_values=score[:], imm_value=NEG), reads=["score", "m8"], writes=["sc2"])
                            yield
                            op("vector", lambda e: e.max(out=m8[:, 8:16], in_=sc2[:]), reads=["sc2"], writes=["m8"])
                            yield
                            bo = 64 if k == 0 else 0
                            op("vector", lambda e: e.tensor_scalar(biasP[k][:, bo:bo + 64], score[:], m8[:, 15:16], NEG, ALU.is_lt, ALU.mult),
                               reads=["score", "m8"], writes=["biasP%d" % k])
                            yield
                            op("tensor", lambda e: e.transpose(psB, biasP[k][:], ident_b[:]), reads=["biasP%d" % k, "ident_b"], writes=["BT"])
                            yield
                            op("vector", lambda e: e.tensor_copy(qB[k][br_, :, :], BT[br_, 0, :].unsqueeze(1).to_broadcast([64, 4, 128])),
                               reads=["BT"], writes=[qk])
                            yield

                    def adv(gen, cnt):
                        for _ in range(cnt):
                            try:
                                next(gen)
                            except StopIteration:
                                return

                    def makeB(i, k, n):
                        pr = slice(64 * k, 64 * k + 64)
                        qk = "qB%d" % k
                        ocn, rcn = oc2[n % 2], rc2[n % 2]
                        ock, rck = "oc%d" % (n % 2), "rc%d" % (n % 2)
                        bfl = bfull2[i % 2]
                        bfk = "bfull%d" % (i % 2)
                        cfg = ((kTE[k], vs_aug, list(range(0, i + 1))), (kTw, vw_aug, list(range(max(0, i - 4), i + 1))))
                        stp = [(bi, ji, jt, len(cfg[bi][2])) for bi in range(2) for ji, jt in enumerate(cfg[bi][2])]
                        banks = {}

                        def emitS(idx):
                            bi, ji, jt, nj = stp[idx]
                            cache = cfg[bi][0]
                            ck = ("kTE%d" % k) if bi == 0 else "kTw"
                            bnum = (0, 1, 4)[sidx[0] % 3]
                            bs_ = B[bnum]
                            bsk = "B%d" % bnum
                            sidx[0] += 1
                            banks[idx] = (bs_, bsk)
                            if bi == 0:
                                op("tensor", lambda e: e.matmul(bs_[:], lhsT=cache[:, jt * 128:(jt + 1) * 128], rhs=qB[k][:, :, :], start=True, stop=True),
                                   reads=[ck, qk], writes=[bsk])
                            else:
                                op("tensor", lambda e: e.matmul(bs_[:], lhsT=cache[pr, jt * 128:(jt + 1) * 128], rhs=qT_all[pr, :, i * 128:(i + 1) * 128], start=True, stop=True),
                                   reads=[ck, "qT_all"], writes=[bsk])

                        def emitPV(idx):
                            bi, ji, jt, nj = stp[idx]
                            vaug = cfg[bi][1]
                            pso = B[2 + bi]
                            psok = "B%d" % (2 + bi)
                            bs_, bsk = banks[idx]
                            pt = PT[pidx[0] % 4]
                            ptk = "PT%d" % (pidx[0] % 4)
                            pidx[0] += 1
                            op("scalar", lambda e: e.activation(out=pt[:], in_=bs_[:], func=AF.Exp), reads=[bsk], writes=[ptk])
                            if jt == i:
                                G(lambda e: e.tensor_tensor(out=pt[:].rearrange("p (g t) -> p g t", g=4), in0=pt[:].rearrange("p (g t) -> p g t", g=4),
                                                            in1=tri_b[:, :].unsqueeze(1).to_broadcast([128, 4, 128]), op=ALU.mult), reads=[ptk, "tri_b"], writes=[ptk])
                            if bi == 1 and jt == i - 4:
                                G(lambda e: e.tensor_tensor(out=pt[:].rearrange("p (g t) -> p g t", g=4), in0=pt[:].rearrange("p (g t) -> p g t", g=4),
                                                            in1=ntri_b[:, :].unsqueeze(1).to_broadcast([128, 4, 128]), op=ALU.mult), reads=[ptk, "ntri_b"], writes=[ptk])
                            op("tensor", lambda e: e.matmul(pso[0:65, :], lhsT=vaug[:, jt, k, :], rhs=pt[:], start=(ji == 0), stop=(ji == nj - 1)),
                               reads=[ptk, "vs_aug", "vw_aug"], writes=[psok], signal=(ji == nj - 1))
                            if ji == nj - 1:
                                op("vector", lambda e: e.tensor_copy(oT_sb[0:65, bi, :], pso[0:65, :]), reads=[psok], writes=["oT_sb"])

                        def begin():
                            emitS(0)
                            if len(stp) > 1:
                                emitS(1)

                        def loop(gnext):
                            for idx in range(len(stp)):
                                if idx + 2 < len(stp):
                                    emitS(idx + 2)
                                emitPV(idx)
                                adv(gnext, 3)
                            adv(gnext, 1000)

                        return begin, loop

                    def makeFin(i, k, n):
                        ocn, rcn = oc2[n % 2], rc2[n % 2]
                        ock, rck = "oc%d" % (n % 2), "rc%d" % (n % 2)
                        bfl = bfull2[i % 2]
                        bfk = "bfull%d" % (i % 2)
                        gsl = lambda br: gates[:, i, br * 8 + k * 4: br * 8 + k * 4 + 4]
                        op("vector", lambda e: e.tensor_tensor(out=rg[:, 0, :], in0=rcn[:], in1=gsl(0), op=ALU.mult), reads=[rck, "gates"], writes=["rg"])
                        op("vector", lambda e: e.tensor_tensor(out=bfl[:, k * 256:(k + 1) * 256].rearrange("p (g d) -> p g d", g=4), in0=ocn[:, :, 0:64],
                                                               in1=rg[:, 0, :].unsqueeze(2).to_broadcast([128, 4, 64]), op=ALU.mult), reads=[ock, "rg"], writes=[bfk])
                        for bi in range(2):
                            for g in range(4):
                                op("tensor", lambda e, bi=bi, g=g: e.transpose(psT[:, g, :], oT_sb[0:65, bi, g * 128:(g + 1) * 128], ident_f[0:65, 0:65]),
                                   reads=["oT_sb", "ident_f"], writes=["B6"], signal=(g == 3))
                            op("vector", lambda e, bi=bi: e.tensor_scalar(rr[:, bi, :], psT[:, :, 64], 1e-30, None, ALU.max), reads=["B6"], writes=["rr"])
                            op("vector", lambda e, bi=bi: e.reciprocal(rr[:, bi, :], rr[:, bi, :]), reads=["rr"], writes=["rr"])
                            op("vector", lambda e, bi=bi: e.tensor_tensor(out=rg[:, 1 + bi, :], in0=rr[:, bi, :], in1=gsl(1 + bi), op=ALU.mult), reads=["rr", "gates"], writes=["rg"])
                            for g in range(4):
                                cs = slice(k * 256 + g * 64, k * 256 + g * 64 + 64)
                                op("vector", lambda e, bi=bi, g=g, cs=cs: e.scalar_tensor_tensor(out=bfl[:, cs], in0=psT[:, g, 0:64], scalar=rg[:, 1 + bi, g:g + 1], in1=bfl[:, cs], op0=ALU.mult, op1=ALU.add),
                                   reads=["B6", "rg", bfk], writes=[bfk])

                    steps = [(i, k) for i in range(NT) for k in range(2)]
                    adv(stageA(steps[0][0], steps[0][1], 0), 1000)
                    Bcur = makeB(steps[0][0], steps[0][1], 0)
                    Bcur[0]()
                    for n, (i, k) in enumerate(steps):
                        gnext = stageA(steps[n + 1][0], steps[n + 1][1], n + 1) if n + 1 < len(steps) else iter(())
                        Bcur[1](gnext)
                        if n + 1 < len(steps):
                            Bnext = makeB(steps[n + 1][0], steps[n + 1][1], n + 1)
                            Bnext[0]()
                        makeFin(i, k, n)
                        if n + 1 < len(steps):
                            Bcur = Bnext
                        if k == 1:
                            bfl = bfull2[i % 2]
                            bfk = "bfull%d" % (i % 2)
                            rms_stats(bfl[:], 512, bfk, junkb[:], ssb, rsb, "b")
                            mB = mixB[i % 2]
                            mk = "mixB%d" % (i % 2)
                            op("vector", lambda e, mB=mB, bfl=bfl: e.scalar_tensor_tensor(out=mB[:], in0=bfl[:], scalar=rsb[:, 0:1], in1=onbw[:], op0=ALU.mult, op1=ALU.mult),
                               reads=[bfk, "rstdb", "onbw"], writes=[mk])
                            P.dma("gpsimd", mix_d[i * 128:(i + 1) * 128, 512:1024], mB[:], reads=[mk], writes=["mix_d"])
                    P.barrier()
            if debug == "p3":
                return
            with ExitStack() as s4:
                wo_b = T("wo_b", [128, 8, D], BF16, s4)
                wg_b = T("wg_b", [128, 8, DFF], BF16, s4)
                wu_b = T("wu_b", [128, 8, DFF], BF16, s4)
                wd_b = T("wd_b", [128, NFC, D], BF16, s4)
                nfw = T("nfw", [128, D], F32, s4)
                fnw = T("fnw", [128, D], F32, s4)
                P.dma("sync", nfw[:], norm_ffn_w[l].partition_broadcast(128), writes=["nfw"])
                P.dma("sync", fnw[:], final_norm_w.partition_broadcast(128), writes=["fnw"])
                s4a = ExitStack()
                stg = [T("stg%d" % i, [128, DFF], F32, s4a) for i in range(2)]
                si = 0
                for (wsrc, wdst, nck, wid, key) in ((w_o, wo_b, 8, D, "wo_b"), (w_gate, wg_b, 8, DFF, "wg_b"), (w_up, wu_b, 8, DFF, "wu_b"), (w_down, wd_b, NFC, D, "wd_b")):
                    for c in range(nck):
                        sg = stg[si % 2]
                        sk = "stg%d" % (si % 2)
                        P.dma("sync" if si % 2 == 0 else "scalar", sg[:, 0:wid], wsrc[l, c * 128:(c + 1) * 128, :], writes=[sk])
                        eng = ("gpsimd", "vector")[si % 2]
                        op(eng, lambda e, wdst=wdst, c=c, sg=sg, wid=wid: e.tensor_copy(wdst[:, c, :], sg[:, 0:wid]), reads=[sk], writes=[key])
                        si += 1
                P.barrier()
                s4a.close()
                mixt = [T("mixt%d" % i, [128, D], BF16, s4) for i in range(2)]
                mixT = T("mixT", [128, 8, 128], BF16, s4)
                x1 = T("x1", [128, 2, D], F32, s4)
                junk4 = T("junk4", [128, D], BF16, s4)
                ss4 = T("ss4", [128, 1], F32, s4)
                rs4 = T("rs4", [128, 1], F32, s4)
                h2 = T("h2", [128, D], BF16, s4)
                h2T = T("h2T", [128, 8, 256], BF16, s4)
                sgt = [T("sgt%d" % i, [128, 256], F32, s4) for i in range(2)]
                actT = T("actT", [128, NFC, 256], BF16, s4)
                x2 = [T("x2_%d" % i, [128, D], F32, s4) for i in range(1)]
                ss5 = T("ss5", [128, 1], F32, s4)
                rs5 = T("rs5", [128, 1], F32, s4)
                for TT in range(16):
                    for tt in range(2):
                        i = TT * 2 + tt
                        mt = mixt[i % 2]
                        mtk = "mixt%d" % (i % 2)
                        P.dma("sync", mt[:], mix_d[i * 128:(i + 1) * 128, :], reads=["mix_d"], writes=[mtk])
                        P.dma("scalar", x1[:, tt, :], xsrc[i * 128:(i + 1) * 128, :], reads=["xres_d"], writes=["x1"])
                        for c in range(8):
                            op("tensor", lambda e, c=c, mt=mt: e.transpose(BT[:, c, :], mt[:, c * 128:(c + 1) * 128], ident_b[:]), reads=[mtk, "ident_b"], writes=["BT"], signal=(c == 7))
                        op("vector", lambda e: e.tensor_copy(mixT[:], BT[:]), reads=["BT"], writes=["mixT"])
                        for half in range(2):
                            for c in range(8):
                                op("tensor", lambda e, half=half, c=c: e.matmul(B[half][:], lhsT=mixT[:, c, :], rhs=wo_b[:, c, half * 512:(half + 1) * 512], start=(c == 0), stop=(c == 7)),
                                   reads=["mixT", "wo_b"], writes=["B%d" % half], signal=(c == 7))
                            op("vector", lambda e, half=half, tt=tt: e.tensor_tensor(out=x1[:, tt, half * 512:(half + 1) * 512], in0=B[half][:], in1=x1[:, tt, half * 512:(half + 1) * 512], op=ALU.add),
                               reads=["B%d" % half, "x1"], writes=["x1"])
                        rms_stats(x1[:, tt, :], D, "x1", junk4[:], ss4, rs4, "4")
                        op("vector", lambda e, tt=tt: e.scalar_tensor_tensor(out=h2[:], in0=x1[:, tt, :], scalar=rs4[:, 0:1], in1=nfw[:], op0=ALU.mult, op1=ALU.mult),
                           reads=["x1", "rstd4", "nfw"], writes=["h2"])
                        for c in range(8):
                            op("tensor", lambda e, c=c: e.transpose(BT[:, c, :], h2[:, c * 128:(c + 1) * 128], ident_b[:]), reads=["h2", "ident_b"], writes=["BT"], signal=(c == 7))
                        op("vector", lambda e, tt=tt: e.tensor_copy(h2T[:, :, tt * 128:(tt + 1) * 128], BT[:]), reads=["BT"], writes=["h2T"])
                    for fc in range(NFC):
                        pg = B[2 + 2 * (fc % 2)]
                        pu = B[3 + 2 * (fc % 2)]
                        pgk = "B%d" % (2 + 2 * (fc % 2))
                        puk = "B%d" % (3 + 2 * (fc % 2))
                        for c in range(8):
                            op("tensor", lambda e, pg=pg, c=c, fc=fc: e.matmul(pg[:, 0:256], lhsT=wg_b[:, c, fc * 128:(fc + 1) * 128], rhs=h2T[:, c, :], start=(c == 0), stop=(c == 7)),
                               reads=["wg_b", "h2T"], writes=[pgk], signal=(c == 7))
                        for c in range(8):
                            op("tensor", lambda e, pu=pu, c=c, fc=fc: e.matmul(pu[:, 0:256], lhsT=wu_b[:, c, fc * 128:(fc + 1) * 128], rhs=h2T[:, c, :], start=(c == 0), stop=(c == 7)),
                               reads=["wu_b", "h2T"], writes=[puk], signal=(c == 7))
                        sg_ = sgt[fc % 2]
                        sgk = "sgt%d" % (fc % 2)
                        op("scalar", lambda e, sg_=sg_, pg=pg: e.activation(out=sg_[:], in_=pg[:, 0:256], func=AF.Silu), reads=[pgk], writes=[sgk])
                        op("vector", lambda e, sg_=sg_, pu=pu, fc=fc: e.tensor_tensor(out=actT[:, fc, :], in0=pu[:, 0:256], in1=sg_[:], op=ALU.mult), reads=[puk, sgk], writes=["actT"])
                    for tt in range(2):
                        i = TT * 2 + tt
                        x2i = x2[0]
                        x2k = "x2_0"
                        for half in range(2):
                            pd = B[(0, 6)[half]]
                            pdk = "B%d" % ((0, 6)[half])
                            for fc in range(NFC):
                                op("tensor", lambda e, pd=pd, fc=fc, tt=tt, half=half: e.matmul(pd[:], lhsT=actT[:, fc, tt * 128:(tt + 1) * 128], rhs=wd_b[:, fc, half * 512:(half + 1) * 512],
                                                                                      start=(fc == 0), stop=(fc == NFC - 1)),
                                   reads=["actT", "wd_b"], writes=[pdk], signal=(fc == NFC - 1))
                            op("vector", lambda e, pd=pd, half=half, tt=tt, x2i=x2i: e.tensor_tensor(out=x2i[:, half * 512:(half + 1) * 512], in0=pd[:], in1=x1[:, tt, half * 512:(half + 1) * 512], op=ALU.add),
                               reads=[pdk, "x1"], writes=[x2k])
                        if not last:
                            P.dma("gpsimd", xres_d[i * 128:(i + 1) * 128, :], x2i[:], reads=[x2k], writes=["xres_d"])
                        else:
                            rms_stats(x2i[:], D, x2k, junk4[:], ss5, rs5, "5")
                            op("vector", lambda e, x2i=x2i: e.scalar_tensor_tensor(out=x2i[:], in0=x2i[:], scalar=rs5[:, 0:1], in1=fnw[:], op0=ALU.mult, op1=ALU.mult),
                               reads=[x2k, "rstd5", "fnw"], writes=[x2k])
                            P.dma("gpsimd", out[i * 128:(i + 1) * 128, :], x2i[:], reads=[x2k], writes=["out"])
                P.barrier()
        for l_ in range(nlayers):
            layer(l_)
        P.finish("gpsimd", ["out", "mix_d", "xres_d"])
        P.barrier()
        with nc.Block() as block:
            P.emit(block)
    print("instructions:", P.n_inst)
    return nc


_NAMES = ["norm_mix_w", "w_in", "gmlp_norm_w", "gmlp_ws", "gmlp_bs", "cmp_pos_k", "cmp_pos_v", "cmp_k_w1", "cmp_k_w2",
          "cmp_v_w1", "cmp_v_w2", "gate_b", "out_norm_a_w", "out_norm_b_w", "w_o", "norm_ffn_w", "w_gate", "w_up",
          "w_down", "final_norm_w"]


def kernel(**inputs):
    x = np.ascontiguousarray(np.asarray(inputs["x"], dtype=np.float32))
    shared = {n: np.ascontiguousarray(np.asarray(inputs[n], dtype=np.float32)) for n in _NAMES}
    nc = build()
    in_maps = [dict(shared, x=x[b]) for b in range(8)]
    res = run_bass_kernel_spmd(nc, in_maps, core_ids=list(range(8)))
    return np.stack([np.asarray(r["out"], dtype=np.float32) for r in res.results], axis=0)
```

```python
import numpy as np
import concourse.bass as bass
import concourse.mybir as mybir
from concourse.bass_utils import run_bass_kernel_spmd

F32 = mybir.dt.float32
BF16 = mybir.dt.bfloat16
AF = mybir.ActivationFunctionType
ALU = mybir.AluOpType
AX = mybir.AxisListType


class Prog:
    ENGS = ("sync", "scalar", "vector", "gpsimd", "tensor")
    NDMA = 8
    R = 8

    def __init__(self, nc, stack):
        self.nc = nc
        self.q = {e: [] for e in self.ENGS}
        self.sem = {e: [stack.enter_context(nc.semaphore("s_%s%d" % (e, i))) for i in range(self.R)]
                    for e in self.ENGS}
        self.cnt = {e: 0 for e in self.ENGS}
        self.dsem = {e: [stack.enter_context(nc.semaphore("d_%s%d" % (e, i))) for i in range(self.NDMA)]
                     for e in ("sync", "scalar", "gpsimd")}
        self.dcnt = {e: 0 for e in self.dsem}
        self.semobj = {}
        for e in self.ENGS:
            for i in range(self.R):
                self.semobj[("c", e, i)] = self.sem[e][i]
        for e in self.dsem:
            for i in range(self.NDMA):
                self.semobj[("d", e, i)] = self.dsem[e][i]
        self.waited = {e: {} for e in self.ENGS}
        self.lastw = {}
        self.readers = {}
        self.n_inst = 0

    def _waits(self, eng, deps):
        need = {}
        for (sid, val) in deps:
            if sid[0] == "c" and sid[1] == eng and (val - 1) * self.R + sid[2] + 1 > self.cnt[eng]:
                continue
            if self.waited[eng].get(sid, 0) >= val:
                continue
            if need.get(sid, 0) < val:
                need[sid] = val
        out = []
        for sid, val in need.items():
            self.waited[eng][sid] = val
            out.append((self.semobj[sid], val))
        return out

    def _deps(self, reads, writes):
        deps = []
        for k in reads:
            if k in self.lastw:
                deps.append(self.lastw[k])
        for k in writes:
            if k in self.lastw:
                deps.append(self.lastw[k])
            deps.extend(self.readers.get(k, ()))
        return deps

    def _commit(self, tok, reads, writes):
        for k in reads:
            self.readers.setdefault(k, []).append(tok)
        for k in writes:
            self.lastw[k] = tok
            self.readers[k] = []

    def op(self, eng, fn, reads=(), writes=(), signal=True):
        waits = self._waits(eng, self._deps(reads, writes))
        n = self.cnt[eng]
        tok = (("c", eng, n % self.R), n // self.R + 1)
        if signal:
            self.cnt[eng] += 1
        sem = self.sem[eng][n % self.R]

        def run(e, fn=fn, waits=waits, signal=signal, sem=sem):
            for (s, v) in waits:
                e.wait_ge(s, v)
            ins = fn(e)
            if signal:
                ins.then_inc(sem, 1)

        self.q[eng].append(run)
        self._commit(tok, reads, writes)
        self.n_inst += 1

    def dma(self, eng, out, in_, reads=(), writes=(), **kw):
        n = self.dcnt[eng]
        self.dcnt[eng] += 1
        slot = n % self.NDMA
        val = 16 * (n // self.NDMA + 1)
        sid = ("d", eng, slot)
        deps = self._deps(reads, writes)
        if val > 16:
            deps.append((sid, val - 16))
        waits = self._waits(eng, deps)
        sem = self.dsem[eng][slot]

        def run(e, waits=waits, sem=sem, out=out, in_=in_, kw=kw):
            for (s, v) in waits:
                e.wait_ge(s, v)
            e.dma_start(out=out, in_=in_, **kw).then_inc(sem, 16)

        self.q[eng].append(run)
        self._commit((sid, val), reads, writes)
        self.n_inst += 1

    def barrier(self):
        deps = []
        for e in self.ENGS:
            n = self.cnt[e]
            for i in range(self.R):
                if n >= i + 1:
                    deps.append((("c", e, i), (n - 1 - i) // self.R + 1))
        for e in self.dsem:
            n = self.dcnt[e]
            for i in range(self.NDMA):
                if n >= i + 1:
                    deps.append((("d", e, i), 16 * ((n - 1 - i) // self.NDMA + 1)))
        for e in self.ENGS:
            waits = self._waits(e, deps)

            def run(en, waits=waits):
                for (s, v) in waits:
                    en.wait_ge(s, v)

            self.q[e].append(run)

    def finish(self, eng, keys):
        waits = self._waits(eng, self._deps(keys, ()))

        def run(e, waits=waits):
            for (s, v) in waits:
                e.wait_ge(s, v)

        self.q[eng].append(run)

    def emit(self, block):
        q = self.q

        @block.sync
        def _(e):
            for f in q["sync"]:
                f(e)

        @block.scalar
        def _(e):
            for f in q["scalar"]:
                f(e)

        @block.vector
        def _(e):
            for f in q["vector"]:
                f(e)

        @block.gpsimd
        def _(e):
            for f in q["gpsimd"]:
                f(e)

        @block.tensor
        def _(e):
            for f in q["tensor"]:
                f(e)


S = 4096
D = 1024
NT = 32
DFF = 2816
NFC = 22
NEG = -30000.0
L = 2


def build(debug=None, nlayers=L):
    from contextlib import ExitStack
    nc = bass.Bass("TRN2", target_bir_lowering=False)
    dt_in = lambda name, shape: nc.dram_tensor(name, shape, F32, kind="ExternalInput").ap()
    x_in = dt_in("x", [S, D])
    norm_mix_w = dt_in("norm_mix_w", [L, D])
    w_in = dt_in("w_in", [L, D, 2328])
    gmlp_norm_w = dt_in("gmlp_norm_w", [L, 512])
    gmlp_ws = dt_in("gmlp_ws", [L, 8, 128, 128])
    gmlp_bs = dt_in("gmlp_bs", [L, 8, 128])
    cmp_pos = {"k": dt_in("cmp_pos_k", [L, 32, 64]), "v": dt_in("cmp_pos_v", [L, 32, 64])}
    cmp_w1 = {"k": dt_in("cmp_k_w1", [L, 2048, 256]), "v": dt_in("cmp_v_w1", [L, 2048, 256])}
    cmp_w2 = {"k": dt_in("cmp_k_w2", [L, 256, 64]), "v": dt_in("cmp_v_w2", [L, 256, 64])}
    gate_b = dt_in("gate_b", [L, 24])
    out_norm_a_w = dt_in("out_norm_a_w", [L, 512])
    out_norm_b_w = dt_in("out_norm_b_w", [L, 512])
    w_o = dt_in("w_o", [L, D, D])
    norm_ffn_w = dt_in("norm_ffn_w", [L, D])
    w_gate = dt_in("w_gate", [L, D, DFF])
    w_up = dt_in("w_up", [L, D, DFF])
    w_down = dt_in("w_down", [L, DFF, D])
    final_norm_w = dt_in("final_norm_w", [D])
    out = nc.dram_tensor("out", [S, D], F32, kind="ExternalOutput").ap()
    dbg = debug is not None
    mix_d = nc.dram_tensor("mix_d", [S, D], BF16, kind="ExternalOutput" if dbg else "Internal").ap()
    xres_d = nc.dram_tensor("xres_d", [S, D], F32, kind="ExternalOutput" if dbg else "Internal").ap()

    with ExitStack() as st:
        P = Prog(nc, st)
        op = P.op

        sfx = [""]

        def T(name, shape, dt, stack=None):
            return (stack or st).enter_context(nc.sbuf_tensor(name + sfx[0], shape, dt))

        B = [st.enter_context(nc.psum_tensor("B%d" % i, [128, 512], F32)) for i in range(7)]
        BT = st.enter_context(nc.psum_tensor("BT", [128, 8, 128], BF16))

        ident_b = T("ident_b", [128, 128], BF16)
        ident_f = T("ident_f", [128, 128], F32)
        tri_f = T("tri_f", [128, 128], F32)
        tri_b = T("tri_b", [128, 128], BF16)
        ntri_b = T("ntri_b", [128, 128], BF16)
        ones_f = T("ones_f", [128, 128], F32)
        ind8 = T("ind8", [8, 512], F32)
        A_big = T("A_big", [16, 512], BF16)
        Bm4 = T("Bm4", [16, 4, 128], BF16)
        VM = T("VM", [128, 128], F32)
        AMk = T("AMk", [128, 128], F32)
        tmpc = T("tmpc", [128, 512], F32)
        tmpc2 = T("tmpc2", [128, 512], F32)

        def G(fn, reads=(), writes=()):
            op("gpsimd", fn, reads=reads, writes=writes)

        def asel(t, pattern, cmp_, fill, base, cm, key):
            G(lambda e: e.affine_select(out=t, in_=t, pattern=pattern, compare_op=cmp_, fill=fill,
                                        base=base, channel_multiplier=cm), reads=[key], writes=[key])

        G(lambda e: e.memset(ones_f[:], 1.0), writes=["ones_f"])
        G(lambda e: e.memset(ident_f[:], 1.0), writes=["ident_f"])
        asel(ident_f[:], [[-1, 128]], ALU.is_equal, 0.0, 0, 1, "ident_f")
        G(lambda e: e.tensor_copy(ident_b[:], ident_f[:]), reads=["ident_f"], writes=["ident_b"])
        G(lambda e: e.memset(tri_f[:], 1.0), writes=["tri_f"])
        asel(tri_f[:], [[1, 128]], ALU.is_ge, 0.0, 0, -1, "tri_f")
        G(lambda e: e.tensor_copy(tri_b[:], tri_f[:]), reads=["tri_f"], writes=["tri_b"])
        G(lambda e: e.memset(tmpc[:, 0:128], 1.0), writes=["tmpc"])
        asel(tmpc[:, 0:128], [[-1, 128]], ALU.is_gt, 0.0, 0, 1, "tmpc")
        G(lambda e: e.tensor_copy(ntri_b[:], tmpc[:, 0:128]), reads=["tmpc"], writes=["ntri_b"])
        G(lambda e: e.memset(ind8[:], 1.0), writes=["ind8"])
        asel(ind8[:].rearrange("p (h d) -> p h d", h=8), [[1, 8], [0, 64]], ALU.is_equal, 0.0, 0, -1, "ind8")
        G(lambda e: e.memset(tmpc[0:16, :], 1.0), writes=["tmpc"])
        asel(tmpc[0:16, :], [[1, 512]], ALU.is_equal, 0.0, -255, -1, "tmpc")
        G(lambda e: e.memset(tmpc2[0:16, :], 1.0), writes=["tmpc2"])
        asel(tmpc2[0:16, :], [[1, 512]], ALU.is_ge, 0.0, -263, 0, "tmpc2")
        asel(tmpc2[0:16, :], [[0, 512]], ALU.is_equal, 0.0, -8, 1, "tmpc2")
        asel(tmpc[0:16, :], [[0, 512]], ALU.is_ge, 0.0, 7, -1, "tmpc")
        G(lambda e: e.tensor_tensor(out=A_big[:], in0=tmpc[0:16, :], in1=tmpc2[0:16, :], op=ALU.add),
          reads=["tmpc", "tmpc2"], writes=["A_big"])
        G(lambda e: e.memset(tmpc[0:16, :], NEG), writes=["tmpc"])
        asel(tmpc[0:16, :].rearrange("p (g t) -> p g t", g=4), [[0, 4], [-1, 128]], ALU.is_gt, 0.0, 15, 16, "tmpc")
        asel(tmpc[0:16, :], [[0, 512]], ALU.is_ge, 0.0, 7, -1, "tmpc")
        G(lambda e: e.memset(tmpc2[0:16, :], NEG), writes=["tmpc2"])
        asel(tmpc2[0:16, :], [[0, 512]], ALU.is_equal, 0.0, -8, 1, "tmpc2")
        G(lambda e: e.tensor_tensor(out=Bm4[:].rearrange("p g t -> p (g t)"), in0=tmpc[0:16, :], in1=tmpc2[0:16, :], op=ALU.add),
          reads=["tmpc", "tmpc2"], writes=["Bm4"])
        G(lambda e: e.memset(VM[:], 1.0), writes=["VM"])
        asel(VM[:], [[-64, 128]], ALU.is_ge, 0.0, 64 * 64 - 128, 1, "VM")
        G(lambda e: e.memset(AMk[:], 10000.0), writes=["AMk"])
        asel(AMk[:], [[-64, 128]], ALU.is_ge, 0.0, 64 * 64, 1, "AMk")
        asel(AMk[:], [[64, 128]], ALU.is_ge, 0.0, -64 * 64 + 63, -1, "AMk")
        G(lambda e: e.memset(tmpc[:, 0:128], 10001.0), writes=["tmpc"])
        asel(tmpc[:, 0:128], [[-64, 128]], ALU.is_ge, 0.0, 64 * 64 - 64, 1, "tmpc")
        asel(tmpc[:, 0:128], [[64, 128]], ALU.is_ge, 0.0, -64 * 64 + 64 + 63, -1, "tmpc")
        G(lambda e: e.tensor_tensor(out=AMk[:], in0=AMk[:], in1=tmpc[:, 0:128], op=ALU.add), reads=["AMk", "tmpc"], writes=["AMk"])
        G(lambda e: e.memset(tmpc2[:, 0:128], -10000.0), writes=["tmpc2"])
        asel(tmpc2[:, 0:128], [[64, 128]], ALU.is_ge, 0.0, -64 * 64 - 1, -1, "tmpc2")
        G(lambda e: e.tensor_tensor(out=AMk[:], in0=AMk[:], in1=tmpc2[:, 0:128], op=ALU.add), reads=["AMk", "tmpc2"], writes=["AMk"])


        def rms_stats(src_ap, width, key_src, junk, ssum, rstd, tag):
            sc = float(width) ** -0.5
            op("scalar", lambda e: e.activation(out=junk, in_=src_ap, func=AF.Square, scale=sc, accum_out=ssum[:, 0:1]),
               reads=[key_src], writes=["junk" + tag, "ss" + tag])
            op("vector", lambda e: e.tensor_scalar(rstd[:, 0:1], ssum[:, 0:1], 1e-6, None, ALU.add), reads=["ss" + tag], writes=["rstd" + tag])
            op("scalar", lambda e: e.activation(out=rstd[:, 0:1], in_=rstd[:, 0:1], func=AF.Sqrt), reads=["rstd" + tag], writes=["rstd" + tag])
            op("vector", lambda e: e.reciprocal(rstd[:, 0:1], rstd[:, 0:1]), reads=["rstd" + tag], writes=["rstd" + tag])

        def layer(l):
            sfx[0] = "_L%d" % l
            xsrc = x_in if l == 0 else xres_d
            last = (l == nlayers - 1)
            P.barrier()
            with ExitStack() as s1:
                nmw = T("nmw", [128, D], F32, s1)
                gnw = T("gnw", [128, 512], F32, s1)
                onaw = T("onaw", [128, 512], F32, s1)
                onbw = T("onbw", [128, 512], F32, s1)
                gbt = T("gbt", [128, 24], F32, s1)
                bs8 = T("bs8", [8, 128], F32, s1)
                gates = T("gates", [128, NT, 24], F32, s1)
                P.dma("sync", nmw[:], norm_mix_w[l].partition_broadcast(128), writes=["nmw"])
                P.dma("sync", gnw[:], gmlp_norm_w[l].partition_broadcast(128), writes=["gnw"])
                P.dma("sync", onaw[:], out_norm_a_w[l].partition_broadcast(128), writes=["onaw"])
                P.dma("sync", onbw[:], out_norm_b_w[l].partition_broadcast(128), writes=["onbw"])
                P.dma("sync", gbt[:], gate_b[l].partition_broadcast(128), writes=["gbt"])
                P.dma("sync", bs8[:], gmlp_bs[l], writes=["bs8"])

                wtm = T("wtm", [128, 8, 1304], BF16, s1)
                wfm = T("wfm", [128, 8, 1024], BF16, s1)
                wsT = T("wsT", [128, 8, 128], BF16, s1)
                kTE = [T("kTE%d" % k, [128, S], BF16, s1) for k in range(2)]
                kTw = T("kTw", [128, S], BF16, s1)
                kcT = T("kcT", [128, S], BF16, s1)
                vcT = T("vcT", [128, S], BF16, s1)
                vs_aug = T("vs_aug", [128, NT, 2, 65], BF16, s1)
                vw_aug = T("vw_aug", [128, NT, 2, 65], BF16, s1)
                qT_all = T("qT_all", [128, 4, S], BF16, s1)
                w2k_pad = T("w2k_pad", [128, 2, 2, 128], BF16, s1)
                w2v = T("w2v", [128, 2, 64], BF16, s1)
                hidT = T("hidT", [128, 2, 256], BF16, s1)
                kcmp = T("kcmp", [128, 256], BF16, s1)
                R_cmp = T("R_cmp", [128, 2, 2, 129], BF16, s1)

                with ExitStack() as s0:
                    stage = [T("stage%d" % i, [128, 2328], F32, s0) for i in range(2)]
                    for c in range(8):
                        sg = stage[c % 2]
                        sk = "stage%d" % (c % 2)
                        P.dma("sync", sg[:], w_in[l, c * 128:(c + 1) * 128, :], writes=[sk])
                        cp = [
                            (wtm[:, c, 0:1024], sg[:, 0:1024]),
                            (wtm[:, c, 1024:1152], sg[:, 1920:2048]),
                            (wtm[:, c, 1152:1280], sg[:, 2176:2304]),
                            (wtm[:, c, 1280:1304], sg[:, 2304:2328]),
                            (wfm[:, c, 0:512].rearrange("p (g k d) -> p g k d", g=4, k=2),
                             sg[:, 1024:1536].rearrange("p (k g d) -> p g k d", k=2, g=4)),
                            (wfm[:, c, 512:640], sg[:, 1536:1664]),
                            (wfm[:, c, 640:768], sg[:, 1664:1792]),
                            (wfm[:, c, 768:896], sg[:, 1792:1920]),
                            (wfm[:, c, 896:1024], sg[:, 2048:2176]),
                        ]
                        for ci, (o_, i_) in enumerate(cp):
                            eng = "gpsimd" if ci % 2 == 0 else "vector"
                            op(eng, lambda e, o_=o_, i_=i_: e.tensor_copy(o_, i_), reads=[sk], writes=["wtm" if ci < 4 else "wfm"])
                    wsf = T("wsf", [128, 8, 128], F32, s0)
                    P.dma("sync", wsf[:], gmlp_ws[l].rearrange("h t s -> t h s"), writes=["wsf"])
                    for h in range(8):
                        bk = B[h % 2]
                        op("tensor", lambda e, h=h, bk=bk: e.transpose(bk[:, 0:128], wsf[:, h, :], ident_f[:]),
                           reads=["wsf", "ident_f"], writes=["B%d" % (h % 2)])
                        op("vector", lambda e, h=h, bk=bk: e.tensor_tensor(out=wsT[:, h, :], in0=bk[:, 0:128], in1=tri_f[:], op=ALU.mult),
                           reads=["B%d" % (h % 2), "tri_f"], writes=["wsT"])
                    for nt_ in range(2):
                        G(lambda e: e.memset(tmpc[:, 0:64], 1.0), writes=["tmpc"])
                        asel(tmpc[:, 0:64], [[-64, 64]], ALU.is_ge, 0.0, 2048 * nt_ + 31, 16, "tmpc")
                        asel(tmpc[:, 0:64], [[64, 64]], ALU.is_ge, 0.0, 63 - 2048 * nt_, -16, "tmpc")
                        for k in range(2):
                            G(lambda e, nt_=nt_, k=k: e.tensor_copy(R_cmp[:, nt_, k, 64:128], tmpc[:, 0:64]), reads=["tmpc"], writes=["R_cmp"])
                    G(lambda e: e.memset(R_cmp[:, :, :, 128:129], 1.0), writes=["R_cmp"])
                    G(lambda e: e.memset(vs_aug[:, :, :, 64:65], 1.0), writes=["vs_aug"])
                    G(lambda e: e.memset(vw_aug[:, :, :, 64:65], 1.0), writes=["vw_aug"])
                    G(lambda e: e.memset(kTE[0][:], 1.0), writes=["kTE0"])
                    asel(kTE[0][:].rearrange("p (j r) -> p j r", j=64), [[1, 64], [0, 64]], ALU.is_equal, 0.0, 64, -1, "kTE0")
                    G(lambda e: e.memset(kTE[1][:], 1.0), writes=["kTE1"])
                    asel(kTE[1][:].rearrange("p (j r) -> p j r", j=64), [[1, 64], [0, 64]], ALU.is_equal, 0.0, 0, -1, "kTE1")
                    P.barrier()

                with ExitStack() as s2:
                    xt = [T("xt%d" % i, [128, D], F32, s2) for i in range(2)]
                    junk = T("junk", [128, D], BF16, s2)
                    ssx = T("ssx", [128, 1], F32, s2)
                    rsx = T("rsx", [128, 1], F32, s2)
                    hb = T("hb", [128, D], BF16, s2)
                    hT = [T("hT%d" % i, [128, 8, 512], BF16, s2) for i in range(2)]
                    gu = T("gu", [128, 512], F32, s2)
                    gv = T("gv", [128, 512], F32, s2)
                    ssv = T("ssv", [128, 1], F32, s2)
                    rsv = T("rsv", [128, 1], F32, s2)
                    vn = T("vn", [128, 512], BF16, s2)
                    a_t = T("a_t", [128, 512], F32, s2)
                    ssa = T("ssa", [128, 1], F32, s2)
                    rsa = T("rsa", [128, 1], F32, s2)
                    mixA = [T("mixA%d" % i, [128, 512], BF16, s2) for i in range(2)]
                    gpre = T("gpre", [128, 24], F32, s2)
                    for TT in range(8):
                        hTt = hT[TT % 2]
                        hk = "hT%d" % (TT % 2)
                        for tt in range(4):
                            i = TT * 4 + tt
                            xti = xt[i % 2]
                            xk = "xt%d" % (i % 2)
                            P.dma("sync", xti[:], xsrc[i * 128:(i + 1) * 128, :], writes=[xk])
                            rms_stats(xti[:], D, xk, junk[:], ssx, rsx, "x")
                            op("vector", lambda e, xti=xti: e.scalar_tensor_tensor(out=hb[:], in0=xti[:], scalar=rsx[:, 0:1], in1=nmw[:], op0=ALU.mult, op1=ALU.mult),
                               reads=[xk, "rstdx", "nmw"], writes=["hb"])
                            for c in range(8):
                                op("tensor", lambda e, c=c: e.transpose(BT[:, c, :], hb[:, c * 128:(c + 1) * 128], ident_b[:]),
                                   reads=["hb", "ident_b"], writes=["BT"], signal=(c == 7))
                            op("vector", lambda e, hTt=hTt, tt=tt: e.tensor_copy(hTt[:, :, tt * 128:(tt + 1) * 128], BT[:]), reads=["BT"], writes=[hk])
                            for cb, (c0, c1) in enumerate(((0, 512), (512, 1024), (1024, 1304))):
                                for c in range(8):
                                    op("tensor", lambda e, cb=cb, c=c, c0=c0, c1=c1, hTt=hTt, tt=tt: e.matmul(B[cb][:, 0:c1 - c0], lhsT=hTt[:, c, tt * 128:(tt + 1) * 128], rhs=wtm[:, c, c0:c1],
                                                                                                      start=(c == 0), stop=(c == 7)),
                                       reads=[hk, "wtm"], writes=["B%d" % cb], signal=(c == 7))
                            op("scalar", lambda e: e.activation(out=gu[:], in_=B[0][:], func=AF.Gelu_apprx_tanh), reads=["B0"], writes=["gu"])
                            op("scalar", lambda e: e.activation(out=gv[:], in_=B[1][:], func=AF.Gelu_apprx_tanh), reads=["B1"], writes=["gv"])
                            rms_stats(gv[:], 512, "gv", junk[:, 0:512], ssv, rsv, "v")
                            op("vector", lambda e: e.scalar_tensor_tensor(out=vn[:], in0=gv[:], scalar=rsv[:, 0:1], in1=gnw[:], op0=ALU.mult, op1=ALU.mult),
                               reads=["gv", "rstdv", "gnw"], writes=["vn"])
                            op("vector", lambda e, i=i: e.tensor_copy(vs_aug[:, i, :, 0:64], B[2][:, 0:128].rearrange("p (k d) -> p k d", k=2)), reads=["B2"], writes=["vs_aug"])
                            op("vector", lambda e, i=i: e.tensor_copy(vw_aug[:, i, :, 0:64], B[2][:, 128:256].rearrange("p (k d) -> p k d", k=2)), reads=["B2"], writes=["vw_aug"])
                            op("vector", lambda e: e.tensor_tensor(out=gpre[:], in0=B[2][:, 256:280], in1=gbt[:], op=ALU.add), reads=["B2", "gbt"], writes=["gpre"])
                            op("scalar", lambda e, i=i: e.activation(out=gates[:, i, :], in_=gpre[:], func=AF.Sigmoid), reads=["gpre"], writes=["gates"])
                            op("tensor", lambda e: e.matmul(B[3][:], lhsT=bs8[:], rhs=ind8[:], start=True, stop=False), reads=["bs8", "ind8"], writes=["B3"], signal=False)
                            for h in range(8):
                                op("tensor", lambda e, h=h: e.matmul(B[3][:, h * 64:(h + 1) * 64], lhsT=wsT[:, h, :], rhs=vn[:, h * 64:(h + 1) * 64], start=False, stop=(h == 7)),
                                   reads=["wsT", "vn"], writes=["B3"], signal=(h == 7))
                            op("vector", lambda e: e.tensor_tensor(out=a_t[:], in0=B[3][:], in1=gu[:], op=ALU.mult), reads=["B3", "gu"], writes=["a_t"])
                            rms_stats(a_t[:], 512, "a_t", junk[:, 512:1024], ssa, rsa, "a")
                            mA = mixA[i % 2]
                            mk = "mixA%d" % (i % 2)
                            op("vector", lambda e, mA=mA: e.scalar_tensor_tensor(out=mA[:], in0=a_t[:], scalar=rsa[:, 0:1], in1=onaw[:], op0=ALU.mult, op1=ALU.mult),
                               reads=["a_t", "rstda", "onaw"], writes=[mk])
                            P.dma("gpsimd", mix_d[i * 128:(i + 1) * 128, 0:512], mA[:], reads=[mk], writes=["mix_d"])
                        tok = slice(TT * 512, (TT + 1) * 512)
                        for ch in range(8):
                            bk = B[4 + ch % 2]
                            bkk = "B%d" % (4 + ch % 2)
                            for c in range(8):
                                op("tensor", lambda e, bk=bk, ch=ch, c=c, hTt=hTt: e.matmul(bk[:], lhsT=wfm[:, c, ch * 128:(ch + 1) * 128], rhs=hTt[:, c, :], start=(c == 0), stop=(c == 7)),
                                   reads=[hk, "wfm"], writes=[bkk], signal=(c == 7))
                            if ch < 4:
                                op("scalar", lambda e, bk=bk, ch=ch, tok=tok: e.activation(out=qT_all[:, ch, tok], in_=bk[:], func=AF.Copy, scale=0.125), reads=[bkk], writes=["qT_all"])
                            elif ch == 4:
                                op("vector", lambda e, bk=bk, tok=tok: e.tensor_copy(kcT[:, tok], bk[:]), reads=[bkk], writes=["kcT"])
                            elif ch == 5:
                                op("vector", lambda e, bk=bk, tok=tok: e.tensor_copy(vcT[:, tok], bk[:]), reads=[bkk], writes=["vcT"])
                            elif ch == 6:
                                op("vector", lambda e, bk=bk, tok=tok: e.tensor_copy(kTE[0][0:64, tok], bk[0:64, :]), reads=[bkk], writes=["kTE0"])
                                op("vector", lambda e, bk=bk, tok=tok: e.tensor_copy(kTE[1][64:128, tok], bk[64:128, :]), reads=[bkk], writes=["kTE1"])
                            else:
                                op("vector", lambda e, bk=bk, tok=tok: e.tensor_copy(kTw[:, tok], bk[:]), reads=[bkk], writes=["kTw"])
                    P.barrier()

                with ExitStack() as s25:
                    w1A = {kv: T("w1A" + kv, [128, 32, 256], BF16, s25) for kv in "kv"}
                    posT = {kv: T("posT" + kv, [128, 32], BF16, s25) for kv in "kv"}
                    bcol = {kv: T("bcol" + kv, [128, 2], F32, s25) for kv in "kv"}
                    w1f = T("w1f", [128, 8, 256], F32, s25)
                    posf = T("posf", [32, 64], F32, s25)
                    w2f = T("w2f", [128, 2, 64], F32, s25)
                    G(lambda e: e.memset(w2k_pad[:], 0.0), writes=["w2k_pad"])
                    for kv in "kv":
                        src = cmp_w1[kv][l].rearrange("(l d) n -> d l n", d=64)
                        for q4 in range(4):
                            P.dma("sync", w1f[0:64], src[:, q4 * 8:(q4 + 1) * 8, :], writes=["w1f"])
                            P.dma("sync", w1f[64:128], src[:, q4 * 8:(q4 + 1) * 8, :], writes=["w1f"])
                            G(lambda e, kv=kv, q4=q4: e.tensor_copy(w1A[kv][:, q4 * 8:(q4 + 1) * 8, :], w1f[:]), reads=["w1f"], writes=["w1A" + kv])
                        P.dma("sync", posf[:], cmp_pos[kv][l], writes=["posf"])
                        op("tensor", lambda e: e.transpose(B[2][0:64, 0:32], posf[:, :], ident_f[0:32, 0:32]), reads=["posf", "ident_f"], writes=["B2"])
                        op("vector", lambda e, kv=kv: e.tensor_copy(posT[kv][0:64, :], B[2][0:64, 0:32]), reads=["B2"], writes=["posT" + kv])
                        for hc in range(2):
                            for ll in range(32):
                                op("tensor", lambda e, kv=kv, hc=hc, ll=ll: e.matmul(B[3][:, hc:hc + 1], lhsT=w1A[kv][0:64, ll, hc * 128:(hc + 1) * 128],
                                                                                 rhs=posT[kv][0:64, ll:ll + 1], start=(ll == 0), stop=(ll == 31)),
                                   reads=["w1A" + kv, "posT" + kv], writes=["B3"], signal=(ll == 31))
                        op("vector", lambda e, kv=kv: e.tensor_copy(bcol[kv][:], B[3][:, 0:2]), reads=["B3"], writes=["bcol" + kv])
                        P.dma("sync", w2f[:], cmp_w2[kv][l].rearrange("(c p) d -> p c d", p=128), writes=["w2f"])
                        if kv == "k":
                            for k in range(2):
                                G(lambda e, k=k: e.tensor_copy(w2k_pad[:, :, k, 64 * k:64 * k + 64], w2f[:]), reads=["w2f"], writes=["w2k_pad"])
                        else:
                            G(lambda e: e.tensor_copy(w2v[:], w2f[:]), reads=["w2f"], writes=["w2v"])
                    for kv, srcT in (("k", kcT), ("v", vcT)):
                        for k in range(2):
                            pr = slice(64 * k, 64 * k + 64)
                            for hc in range(2):
                                bk = B[hc]
                                for ll in range(32):
                                    op("tensor", lambda e, kv=kv, hc=hc, ll=ll, bk=bk, pr=pr, srcT=srcT: e.matmul(bk[:, 0:255], lhsT=w1A[kv][pr, ll, hc * 128:(hc + 1) * 128],
                                                                                                       rhs=srcT[pr, ll:ll + 16 * 254 + 1:16], start=(ll == 0), stop=(ll == 31)),
                                       reads=["w1A" + kv, "kcT", "vcT"], writes=["B%d" % hc], signal=(ll == 31))
                                op("scalar", lambda e, kv=kv, hc=hc, bk=bk: e.activation(out=hidT[:, hc, 0:255], in_=bk[:, 0:255], func=AF.Gelu_apprx_tanh, bias=bcol[kv][:, hc:hc + 1]),
                                   reads=["B%d" % hc, "bcol" + kv], writes=["hidT"])
                            if kv == "k":
                                for hc in range(2):
                                    op("tensor", lambda e, hc=hc, k=k: e.matmul(B[2][:, 0:255], lhsT=w2k_pad[:, hc, k, :], rhs=hidT[:, hc, 0:255], start=(hc == 0), stop=(hc == 1)),
                                       reads=["w2k_pad", "hidT"], writes=["B2"], signal=(hc == 1))
                                op("vector", lambda e, pr=pr: e.tensor_copy(kcmp[pr, 0:255], B[2][pr, 0:255]), reads=["B2"], writes=["kcmp"])
                            else:
                                for nt_, M in ((0, 128), (1, 127)):
                                    for hc in range(2):
                                        op("tensor", lambda e, hc=hc, nt_=nt_, M=M: e.matmul(B[3][0:M, 0:64], lhsT=hidT[:, hc, nt_ * 128:nt_ * 128 + M], rhs=w2v[:, hc, :], start=(hc == 0), stop=(hc == 1)),
                                           reads=["w2v", "hidT"], writes=["B3"], signal=(hc == 1))
                                    op("vector", lambda e, nt_=nt_, M=M, k=k: e.tensor_copy(R_cmp[0:M, nt_, k, 0:64], B[3][0:M, 0:64]), reads=["B3"], writes=["R_cmp"])
                    P.barrier()

                with ExitStack() as s3:
                    qB = [T("qB%d" % k, [128, 4, 128], BF16, s3) for k in range(2)]
                    PTc = T("PTc", [128, 2, 512], BF16, s3)
                    PT = [T("PT%d" % i, [128, 512], BF16, s3) for i in range(4)]
                    oc2 = [T("oc%d" % j, [128, 4, 129], F32, s3) for j in range(2)]
                    rc2 = [T("rc%d" % j, [128, 4], F32, s3) for j in range(2)]
                    imp = T("imp", [128, 64], F32, s3)
                    score = T("score", [128, 64], F32, s3)
                    sc2 = T("sc2", [128, 64], F32, s3)
                    m8 = T("m8", [128, 16], F32, s3)
                    biasP = [T("biasP%d" % k, [128, 128], BF16, s3) for k in range(2)]
                    oT_sb = T("oT_sb", [128, 2, 512], F32, s3)
                    rr = T("rr", [128, 2, 4], F32, s3)
                    rg = T("rg", [128, 3, 4], F32, s3)
                    bfull2 = [T("bfull%d" % j, [128, 512], F32, s3) for j in range(2)]
                    btmp2 = [T("btmp%d" % j, [128, 4, 64], F32, s3) for j in range(2)]
                    ssb = T("ssb", [128, 1], F32, s3)
                    rsb = T("rsb", [128, 1], F32, s3)
                    junkb = T("junkb", [128, 512], F32, s3)
                    mixB = [T("mixB%d" % i, [128, 512], BF16, s3) for i in range(2)]
                    for k in range(2):
                        G(lambda e, k=k: e.memset(qB[k][:], 0.0), writes=["qB%d" % k])
                        G(lambda e, k=k: e.memset(biasP[k][:], 0.0), writes=["biasP%d" % k])
                    psC = [B[6][:, 260:389], B[5][:, 0:129], B[5][:, 129:258], B[5][:, 258:387]]
                    psCk = ["B6", "B5", "B5", "B5"]
                    psB = BT[:, 0, :]
                    psT = B[6][:, 0:260].rearrange("p (g d) -> p g d", g=4)
                    sidx = [0]
                    pidx = [0]

                    def stageA(i, k, n):
                        pr = slice(64 * k, 64 * k + 64)
                        br_ = slice(64, 128) if k == 0 else slice(0, 64)
                        qk = "qB%d" % k
                        ocn, rcn = oc2[n % 2], rc2[n % 2]
                        ock, rck = "oc%d" % (n % 2), "rc%d" % (n % 2)
                        G(lambda e: e.tensor_copy(qB[k][pr, :, :], qT_all[pr, :, i * 128:(i + 1) * 128]), reads=["qT_all"], writes=[qk])
                        n_tiles = 1 if i <= 15 else 2
                        for nt_ in range(n_tiles):
                            M = 128 if nt_ == 0 else 127
                            bs_ = B[5]
                            bsk = "B5"
                            a0 = 256 - 8 * i + nt_ * 128
                            op("tensor", lambda e, bs_=bs_, M=M, nt_=nt_: e.matmul(bs_[0:M, :], lhsT=kcmp[pr, nt_ * 128:nt_ * 128 + M], rhs=qT_all[pr, :, i * 128:(i + 1) * 128], start=True, stop=False),
                               reads=["kcmp", "qT_all"], writes=[bsk], signal=False)
                            op("tensor", lambda e, bs_=bs_, M=M, a0=a0: e.matmul(bs_[0:M, :], lhsT=A_big[0:9, a0:a0 + M], rhs=Bm4[0:9, :, :], start=False, stop=True),
                               reads=["A_big", "Bm4"], writes=[bsk])
                            yield
                            yield
                            op("scalar", lambda e, bs_=bs_, M=M, nt_=nt_: e.activation(out=PTc[0:M, nt_, :], in_=bs_[0:M, :], func=AF.Exp), reads=[bsk], writes=["PTc"])
                            yield
                            yield
                        for g in range(4):
                            for nt_ in range(n_tiles):
                                M = 128 if nt_ == 0 else 127
                                op("tensor", lambda e, g=g, nt_=nt_, M=M: e.matmul(psC[g], lhsT=PTc[0:M, nt_, g * 128:(g + 1) * 128], rhs=R_cmp[0:M, nt_, k, :],
                                                                                start=(nt_ == 0), stop=(nt_ == n_tiles - 1)),
                                   reads=["PTc", "R_cmp"], writes=[psCk[g]], signal=(nt_ == n_tiles - 1))
                        yield
                        yield
                        for g in range(4):
                            op("vector", lambda e, g=g: e.tensor_copy(ocn[:, g, :], psC[g]), reads=[psCk[g]], writes=[ock])
                        op("vector", lambda e: e.tensor_scalar(rcn[:], ocn[:, :, 128], 1e-30, None, ALU.max), reads=[ock], writes=[rck])
                        op("vector", lambda e: e.reciprocal(rcn[:], rcn[:]), reads=[rck], writes=[rck])
                        yield
                        if i >= 8:
                            op("vector", lambda e: e.tensor_scalar(imp[:], ocn[:, 0, 64:128], rcn[:, 0:1], None, ALU.mult), reads=[ock, rck], writes=["imp"])
                            for g in range(1, 4):
                                op("vector", lambda e, g=g: e.scalar_tensor_tensor(out=imp[:], in0=ocn[:, g, 64:128], scalar=rcn[:, g:g + 1], in1=imp[:], op0=ALU.mult, op1=ALU.add),
                                   reads=[ock, rck, "imp"], writes=["imp"])
                            c0 = 64 - 2 * i
                            op("vector", lambda e: e.tensor_tensor(out=score[:], in0=imp[:], in1=VM[:, c0:c0 + 64], op=ALU.mult), reads=["imp", "VM"], writes=["score"])
                            op("vector", lambda e: e.tensor_tensor(out=score[:], in0=score[:], in1=AMk[:, c0:c0 + 64], op=ALU.add), reads=["score", "AMk"], writes=["score"])
                            op("vector", lambda e: e.memset(score[:, 0:1], 10002.0), reads=["score"], writes=["score"])
                            yield
                            op("vector", lambda e: e.max(out=m8[:, 0:8], in_=score[:]), reads=["score"], writes=["m8"])
                            op("vector", lambda e: e.match_replace(out=sc2[:], in_to_replace=m8[:, 0:8], in_values=score[:], imm_value=NEG), reads=["score", "m8"], writes=["sc2"])
                            op("vector", lambda e: e.max(out=m8[:, 8:16], in_=sc2[:]), reads=["sc2"], writes=["m8"])
                            bo = 64 if k == 0 else 0
                            op("vector", lambda e: e.tensor_scalar(biasP[k][:, bo:bo + 64], score[:], m8[:, 15:16], NEG, ALU.is_lt, ALU.mult),
                               reads=["score", "m8"], writes=["biasP%d" % k])
                            yield
                            yield
                            yield
                            yield
                            op("tensor", lambda e: e.transpose(psB, biasP[k][:], ident_b[:]), reads=["biasP%d" % k, "ident_b"], writes=["BT"])
                            yield
                            yield
                            op("vector", lambda e: e.tensor_copy(qB[k][br_, :, :], BT[br_, 0, :].unsqueeze(1).to_broadcast([64, 4, 128])),
                               reads=["BT"], writes=[qk])
                            yield

                    def adv(gen, cnt):
                        for _ in range(cnt):
                            try:
                                next(gen)
                            except StopIteration:
                                return

                    def makeB(i, k, n):
                        pr = slice(64 * k, 64 * k + 64)
                        qk = "qB%d" % k
                        ocn, rcn = oc2[n % 2], rc2[n % 2]
                        ock, rck = "oc%d" % (n % 2), "rc%d" % (n % 2)
                        bfl = bfull2[i % 2]
                        bfk = "bfull%d" % (i % 2)
                        cfg = ((kTE[k], vs_aug, list(range(0, i + 1))), (kTw, vw_aug, list(range(max(0, i - 4), i + 1))))
                        stp = [(bi, ji, jt, len(cfg[bi][2])) for bi in range(2) for ji, jt in enumerate(cfg[bi][2])]
                        banks = {}

                        def emitS(idx):
                            bi, ji, jt, nj = stp[idx]
                            cache = cfg[bi][0]
                            ck = ("kTE%d" % k) if bi == 0 else "kTw"
                            bnum = (0, 1, 4)[sidx[0] % 3]
                            bs_ = B[bnum]
                            bsk = "B%d" % bnum
                            sidx[0] += 1
                            banks[idx] = (bs_, bsk)
                            if bi == 0:
                                op("tensor", lambda e: e.matmul(bs_[:], lhsT=cache[:, jt * 128:(jt + 1) * 128], rhs=qB[k][:, :, :], start=True, stop=True),
                                   reads=[ck, qk], writes=[bsk])
                            else:
                                op("tensor", lambda e: e.matmul(bs_[:], lhsT=cache[pr, jt * 128:(jt + 1) * 128], rhs=qT_all[pr, :, i * 128:(i + 1) * 128], start=True, stop=True),
                                   reads=[ck, "qT_all"], writes=[bsk])

                        def emitPV(idx):
                            bi, ji, jt, nj = stp[idx]
                            vaug = cfg[bi][1]
                            pso = B[2 + bi]
                            psok = "B%d" % (2 + bi)
                            bs_, bsk = banks[idx]
                            pt = PT[pidx[0] % 4]
                            ptk = "PT%d" % (pidx[0] % 4)
                            pidx[0] += 1
                            op("scalar", lambda e: e.activation(out=pt[:], in_=bs_[:], func=AF.Exp), reads=[bsk], writes=[ptk])
                            if jt == i:
                                G(lambda e: e.tensor_tensor(out=pt[:].rearrange("p (g t) -> p g t", g=4), in0=pt[:].rearrange("p (g t) -> p g t", g=4),
                                                            in1=tri_b[:, :].unsqueeze(1).to_broadcast([128, 4, 128]), op=ALU.mult), reads=[ptk, "tri_b"], writes=[ptk])
                            if bi == 1 and jt == i - 4:
                                G(lambda e: e.tensor_tensor(out=pt[:].rearrange("p (g t) -> p g t", g=4), in0=pt[:].rearrange("p (g t) -> p g t", g=4),
                                                            in1=ntri_b[:, :].unsqueeze(1).to_broadcast([128, 4, 128]), op=ALU.mult), reads=[ptk, "ntri_b"], writes=[ptk])
                            op("tensor", lambda e: e.matmul(pso[0:65, :], lhsT=vaug[:, jt, k, :], rhs=pt[:], start=(ji == 0), stop=(ji == nj - 1)),
                               reads=[ptk, "vs_aug", "vw_aug"], writes=[psok], signal=(ji == nj - 1))
                            if ji == nj - 1:
                                op("vector", lambda e: e.tensor_copy(oT_sb[0:65, bi, :], pso[0:65, :]), reads=[psok], writes=["oT_sb"])

                        def begin():
                            emitS(0)
                            if len(stp) > 1:
                                emitS(1)

                        def loop(gnext):
                            for idx in range(len(stp)):
                                if idx + 2 < len(stp):
                                    emitS(idx + 2)
                                emitPV(idx)
                                adv(gnext, 1)
                            adv(gnext, 1000)

                        return begin, loop

                    def makeFin(i, k, n):
                        ocn, rcn = oc2[n % 2], rc2[n % 2]
                        ock, rck = "oc%d" % (n % 2), "rc%d" % (n % 2)
                        bfl = bfull2[i % 2]
                        bfk = "bfull%d" % (i % 2)
                        gsl = lambda br: gates[:, i, br * 8 + k * 4: br * 8 + k * 4 + 4]
                        op("vector", lambda e: e.tensor_tensor(out=rg[:, 0, :], in0=rcn[:], in1=gsl(0), op=ALU.mult), reads=[rck, "gates"], writes=["rg"])
                        op("vector", lambda e: e.tensor_tensor(out=bfl[:, k * 256:(k + 1) * 256].rearrange("p (g d) -> p g d", g=4), in0=ocn[:, :, 0:64],
                                                               in1=rg[:, 0, :].unsqueeze(2).to_broadcast([128, 4, 64]), op=ALU.mult), reads=[ock, "rg"], writes=[bfk])
                        for bi in range(2):
                            for g in range(4):
                                op("tensor", lambda e, bi=bi, g=g: e.transpose(psT[:, g, :], oT_sb[0:65, bi, g * 128:(g + 1) * 128], ident_f[0:65, 0:65]),
                                   reads=["oT_sb", "ident_f"], writes=["B6"], signal=(g == 3))
                            op("vector", lambda e, bi=bi: e.tensor_scalar(rr[:, bi, :], psT[:, :, 64], 1e-30, None, ALU.max), reads=["B6"], writes=["rr"])
                            op("vector", lambda e, bi=bi: e.reciprocal(rr[:, bi, :], rr[:, bi, :]), reads=["rr"], writes=["rr"])
                            op("vector", lambda e, bi=bi: e.tensor_tensor(out=rg[:, 1 + bi, :], in0=rr[:, bi, :], in1=gsl(1 + bi), op=ALU.mult), reads=["rr", "gates"], writes=["rg"])
                            for g in range(4):
                                cs = slice(k * 256 + g * 64, k * 256 + g * 64 + 64)
                                op("vector", lambda e, bi=bi, g=g, cs=cs: e.scalar_tensor_tensor(out=bfl[:, cs], in0=psT[:, g, 0:64], scalar=rg[:, 1 + bi, g:g + 1], in1=bfl[:, cs], op0=ALU.mult, op1=ALU.add),
                                   reads=["B6", "rg", bfk], writes=[bfk])

                    steps = [(i, k) for i in range(NT) for k in range(2)]
                    adv(stageA(steps[0][0], steps[0][1], 0), 1000)
                    Bcur = makeB(steps[0][0], steps[0][1], 0)
                    Bcur[0]()
                    for n, (i, k) in enumerate(steps):
                        gnext = stageA(steps[n + 1][0], steps[n + 1][1], n + 1) if n + 1 < len(steps) else iter(())
                        Bcur[1](gnext)
                        if n + 1 < len(steps):
                            Bnext = makeB(steps[n + 1][0], steps[n + 1][1], n + 1)
                            Bnext[0]()
                        makeFin(i, k, n)
                        if n + 1 < len(steps):
                            Bcur = Bnext
                        if k == 1:
                            bfl = bfull2[i % 2]
                            bfk = "bfull%d" % (i % 2)
                            rms_stats(bfl[:], 512, bfk, junkb[:], ssb, rsb, "b")
                            mB = mixB[i % 2]
                            mk = "mixB%d" % (i % 2)
                            op("vector", lambda e, mB=mB, bfl=bfl: e.scalar_tensor_tensor(out=mB[:], in0=bfl[:], scalar=rsb[:, 0:1], in1=onbw[:], op0=ALU.mult, op1=ALU.mult),
                               reads=[bfk, "rstdb", "onbw"], writes=[mk])
                            P.dma("gpsimd", mix_d[i * 128:(i + 1) * 128, 512:1024], mB[:], reads=[mk], writes=["mix_d"])
                    P.barrier()
            if debug == "p3":
                return
            with ExitStack() as s4:
                wo_b = T("wo_b", [128, 8, D], BF16, s4)
                wg_b = T("wg_b", [128, 8, DFF], BF16, s4)
                wu_b = T("wu_b", [128, 8, DFF], BF16, s4)
                wd_b = T("wd_b", [128, NFC, D], BF16, s4)
                nfw = T("nfw", [128, D], F32, s4)
                fnw = T("fnw", [128, D], F32, s4)
                P.dma("sync", nfw[:], norm_ffn_w[l].partition_broadcast(128), writes=["nfw"])
                P.dma("sync", fnw[:], final_norm_w.partition_broadcast(128), writes=["fnw"])
                s4a = ExitStack()
                stg = [T("stg%d" % i, [128, DFF], F32, s4a) for i in range(2)]
                si = 0
                for (wsrc, wdst, nck, wid, key) in ((w_o, wo_b, 8, D, "wo_b"), (w_gate, wg_b, 8, DFF, "wg_b"), (w_up, wu_b, 8, DFF, "wu_b"), (w_down, wd_b, NFC, D, "wd_b")):
                    for c in range(nck):
                        sg = stg[si % 2]
                        sk = "stg%d" % (si % 2)
                        P.dma("sync" if si % 2 == 0 else "scalar", sg[:, 0:wid], wsrc[l, c * 128:(c + 1) * 128, :], writes=[sk])
                        eng = ("gpsimd", "vector")[si % 2]
                        op(eng, lambda e, wdst=wdst, c=c, sg=sg, wid=wid: e.tensor_copy(wdst[:, c, :], sg[:, 0:wid]), reads=[sk], writes=[key])
                        si += 1
                P.barrier()
                s4a.close()
                mixt = [T("mixt%d" % i, [128, D], BF16, s4) for i in range(2)]
                mixT = T("mixT", [128, 8, 128], BF16, s4)
                x1 = T("x1", [128, 2, D], F32, s4)
                junk4 = T("junk4", [128, D], BF16, s4)
                ss4 = T("ss4", [128, 1], F32, s4)
                rs4 = T("rs4", [128, 1], F32, s4)
                h2 = T("h2", [128, D], BF16, s4)
                h2T = T("h2T", [128, 8, 256], BF16, s4)
                sgt = [T("sgt%d" % i, [128, 256], F32, s4) for i in range(2)]
                actT = T("actT", [128, NFC, 256], BF16, s4)
                x2 = [T("x2_%d" % i, [128, D], F32, s4) for i in range(1)]
                ss5 = T("ss5", [128, 1], F32, s4)
                rs5 = T("rs5", [128, 1], F32, s4)
                for TT in range(16):
                    for tt in range(2):
                        i = TT * 2 + tt
                        mt = mixt[i % 2]
                        mtk = "mixt%d" % (i % 2)
                        P.dma("sync", mt[:], mix_d[i * 128:(i + 1) * 128, :], reads=["mix_d"], writes=[mtk])
                        P.dma("scalar", x1[:, tt, :], xsrc[i * 128:(i + 1) * 128, :], reads=["xres_d"], writes=["x1"])
                        for c in range(8):
                            op("tensor", lambda e, c=c, mt=mt: e.transpose(BT[:, c, :], mt[:, c * 128:(c + 1) * 128], ident_b[:]), reads=[mtk, "ident_b"], writes=["BT"], signal=(c == 7))
                        op("vector", lambda e: e.tensor_copy(mixT[:], BT[:]), reads=["BT"], writes=["mixT"])
                        for half in range(2):
                            for c in range(8):
                                op("tensor", lambda e, half=half, c=c: e.matmul(B[half][:], lhsT=mixT[:, c, :], rhs=wo_b[:, c, half * 512:(half + 1) * 512], start=(c == 0), stop=(c == 7)),
                                   reads=["mixT", "wo_b"], writes=["B%d" % half], signal=(c == 7))
                            op("vector", lambda e, half=half, tt=tt: e.tensor_tensor(out=x1[:, tt, half * 512:(half + 1) * 512], in0=B[half][:], in1=x1[:, tt, half * 512:(half + 1) * 512], op=ALU.add),
                               reads=["B%d" % half, "x1"], writes=["x1"])
                        rms_stats(x1[:, tt, :], D, "x1", junk4[:], ss4, rs4, "4")
                        op("vector", lambda e, tt=tt: e.scalar_tensor_tensor(out=h2[:], in0=x1[:, tt, :], scalar=rs4[:, 0:1], in1=nfw[:], op0=ALU.mult, op1=ALU.mult),
                           reads=["x1", "rstd4", "nfw"], writes=["h2"])
                        for c in range(8):
                            op("tensor", lambda e, c=c: e.transpose(BT[:, c, :], h2[:, c * 128:(c + 1) * 128], ident_b[:]), reads=["h2", "ident_b"], writes=["BT"], signal=(c == 7))
                        op("vector", lambda e, tt=tt: e.tensor_copy(h2T[:, :, tt * 128:(tt + 1) * 128], BT[:]), reads=["BT"], writes=["h2T"])
                    for fc in range(NFC):
                        pg = B[2 + 2 * (fc % 2)]
                        pu = B[3 + 2 * (fc % 2)]
                        pgk = "B%d" % (2 + 2 * (fc % 2))
                        puk = "B%d" % (3 + 2 * (fc % 2))
                        for c in range(8):
                            op("tensor", lambda e, pg=pg, c=c, fc=fc: e.matmul(pg[:, 0:256], lhsT=wg_b[:, c, fc * 128:(fc + 1) * 128], rhs=h2T[:, c, :], start=(c == 0), stop=(c == 7)),
                               reads=["wg_b", "h2T"], writes=[pgk], signal=(c == 7))
                        for c in range(8):
                            op("tensor", lambda e, pu=pu, c=c, fc=fc: e.matmul(pu[:, 0:256], lhsT=wu_b[:, c, fc * 128:(fc + 1) * 128], rhs=h2T[:, c, :], start=(c == 0), stop=(c == 7)),
                               reads=["wu_b", "h2T"], writes=[puk], signal=(c == 7))
                        sg_ = sgt[fc % 2]
                        sgk = "sgt%d" % (fc % 2)
                        op("scalar", lambda e, sg_=sg_, pg=pg: e.activation(out=sg_[:], in_=pg[:, 0:256], func=AF.Silu), reads=[pgk], writes=[sgk])
                        op("vector", lambda e, sg_=sg_, pu=pu, fc=fc: e.tensor_tensor(out=actT[:, fc, :], in0=pu[:, 0:256], in1=sg_[:], op=ALU.mult), reads=[puk, sgk], writes=["actT"])
                    for tt in range(2):
                        i = TT * 2 + tt
                        x2i = x2[0]
                        x2k = "x2_0"
                        for half in range(2):
                            pd = B[(0, 6)[half]]
                            pdk = "B%d" % ((0, 6)[half])
                            for fc in range(NFC):
                                op("tensor", lambda e, pd=pd, fc=fc, tt=tt, half=half: e.matmul(pd[:], lhsT=actT[:, fc, tt * 128:(tt + 1) * 128], rhs=wd_b[:, fc, half * 512:(half + 1) * 512],
                                                                                      start=(fc == 0), stop=(fc == NFC - 1)),
                                   reads=["actT", "wd_b"], writes=[pdk], signal=(fc == NFC - 1))
                            op("vector", lambda e, pd=pd, half=half, tt=tt, x2i=x2i: e.tensor_tensor(out=x2i[:, half * 512:(half + 1) * 512], in0=pd[:], in1=x1[:, tt, half * 512:(half + 1) * 512], op=ALU.add),
                               reads=[pdk, "x1"], writes=[x2k])
                        if not last:
                            P.dma("gpsimd", xres_d[i * 128:(i + 1) * 128, :], x2i[:], reads=[x2k], writes=["xres_d"])
                        else:
                            rms_stats(x2i[:], D, x2k, junk4[:], ss5, rs5, "5")
                            op("vector", lambda e, x2i=x2i: e.scalar_tensor_tensor(out=x2i[:], in0=x2i[:], scalar=rs5[:, 0:1], in1=fnw[:], op0=ALU.mult, op1=ALU.mult),
                               reads=[x2k, "rstd5", "fnw"], writes=[x2k])
                            P.dma("gpsimd", out[i * 128:(i + 1) * 128, :], x2i[:], reads=[x2k], writes=["out"])
                P.barrier()
        for l_ in range(nlayers):
            layer(l_)
        P.finish("gpsimd", ["out", "mix_d", "xres_d"])
        P.barrier()
        with nc.Block() as block:
            P.emit(block)
    print("instructions:", P.n_inst)
    return nc


_NAMES = ["norm_mix_w", "w_in", "gmlp_norm_w", "gmlp_ws", "gmlp_bs", "cmp_pos_k", "cmp_pos_v", "cmp_k_w1", "cmp_k_w2",
          "cmp_v_w1", "cmp_v_w2", "gate_b", "out_norm_a_w", "out_norm_b_w", "w_o", "norm_ffn_w", "w_gate", "w_up",
          "w_down", "final_norm_w"]


def kernel(**inputs):
    x = np.ascontiguousarray(np.asarray(inputs["x"], dtype=np.float32))
    shared = {n: np.ascontiguousarray(np.asarray(inputs[n], dtype=np.float32)) for n in _NAMES}
    nc = build()
    in_maps = [dict(shared, x=x[b]) for b in range(8)]
    res = run_bass_kernel_spmd(nc, in_maps, core_ids=list(range(8)))
    return np.stack([np.asarray(r["out"], dtype=np.float32) for r in res.results], axis=0)
```

```python
import numpy as np
import concourse.bass as bass
import concourse.mybir as mybir
from concourse.bass_utils import run_bass_kernel_spmd

F32 = mybir.dt.float32
BF16 = mybir.dt.bfloat16
AF = mybir.ActivationFunctionType
ALU = mybir.AluOpType
AX = mybir.AxisListType


class Prog:
    ENGS = ("sync", "scalar", "vector", "gpsimd", "tensor")
    NDMA = 8
    R = 8

    def __init__(self, nc, stack):
        self.nc = nc
        self.q = {e: [] for e in self.ENGS}
        self.sem = {e: [stack.enter_context(nc.semaphore("s_%s%d" % (e, i))) for i in range(self.R)]
                    for e in self.ENGS}
        self.cnt = {e: 0 for e in self.ENGS}
        self.dsem = {e: [stack.enter_context(nc.semaphore("d_%s%d" % (e, i))) for i in range(self.NDMA)]
                     for e in ("sync", "scalar", "gpsimd")}
        self.dcnt = {e: 0 for e in self.dsem}
        self.semobj = {}
        for e in self.ENGS:
            for i in range(self.R):
                self.semobj[("c", e, i)] = self.sem[e][i]
        for e in self.dsem:
            for i in range(self.NDMA):
                self.semobj[("d", e, i)] = self.dsem[e][i]
        self.waited = {e: {} for e in self.ENGS}
        self.lastw = {}
        self.readers = {}
        self.n_inst = 0

    def _waits(self, eng, deps):
        need = {}
        for (sid, val) in deps:
            if sid[0] == "c" and sid[1] == eng and (val - 1) * self.R + sid[2] + 1 > self.cnt[eng]:
                continue
            if self.waited[eng].get(sid, 0) >= val:
                continue
            if need.get(sid, 0) < val:
                need[sid] = val
        out = []
        for sid, val in need.items():
            self.waited[eng][sid] = val
            out.append((self.semobj[sid], val))
        return out

    def _deps(self, reads, writes):
        deps = []
        for k in reads:
            if k in self.lastw:
                deps.append(self.lastw[k])
        for k in writes:
            if k in self.lastw:
                deps.append(self.lastw[k])
            deps.extend(self.readers.get(k, ()))
        return deps

    def _commit(self, tok, reads, writes):
        for k in reads:
            self.readers.setdefault(k, []).append(tok)
        for k in writes:
            self.lastw[k] = tok
            self.readers[k] = []

    def op(self, eng, fn, reads=(), writes=(), signal=True):
        waits = self._waits(eng, self._deps(reads, writes))
        n = self.cnt[eng]
        tok = (("c", eng, n % self.R), n // self.R + 1)
        if signal:
            self.cnt[eng] += 1
        sem = self.sem[eng][n % self.R]

        def run(e, fn=fn, waits=waits, signal=signal, sem=sem):
            for (s, v) in waits:
                e.wait_ge(s, v)
            ins = fn(e)
            if signal:
                ins.then_inc(sem, 1)

        self.q[eng].append(run)
        self._commit(tok, reads, writes)
        self.n_inst += 1

    def dma(self, eng, out, in_, reads=(), writes=(), **kw):
        n = self.dcnt[eng]
        self.dcnt[eng] += 1
        slot = n % self.NDMA
        val = 16 * (n // self.NDMA + 1)
        sid = ("d", eng, slot)
        deps = self._deps(reads, writes)
        if val > 16:
            deps.append((sid, val - 16))
        waits = self._waits(eng, deps)
        sem = self.dsem[eng][slot]

        def run(e, waits=waits, sem=sem, out=out, in_=in_, kw=kw):
            for (s, v) in waits:
                e.wait_ge(s, v)
            e.dma_start(out=out, in_=in_, **kw).then_inc(sem, 16)

        self.q[eng].append(run)
        self._commit((sid, val), reads, writes)
        self.n_inst += 1

    def barrier(self):
        deps = []
        for e in self.ENGS:
            n = self.cnt[e]
            for i in range(self.R):
                if n >= i + 1:
                    deps.append((("c", e, i), (n - 1 - i) // self.R + 1))
        for e in self.dsem:
            n = self.dcnt[e]
            for i in range(self.NDMA):
                if n >= i + 1:
                    deps.append((("d", e, i), 16 * ((n - 1 - i) // self.NDMA + 1)))
        for e in self.ENGS:
            waits = self._waits(e, deps)

            def run(en, waits=waits):
                for (s, v) in waits:
                    en.wait_ge(s, v)

            self.q[e].append(run)

    def finish(self, eng, keys):
        waits = self._waits(eng, self._deps(keys, ()))

        def run(e, waits=waits):
            for (s, v) in waits:
                e.wait_ge(s, v)

        self.q[eng].append(run)

    def emit(self, block):
        q = self.q

        @block.sync
        def _(e):
            for f in q["sync"]:
                f(e)

        @block.scalar
        def _(e):
            for f in q["scalar"]:
                f(e)

        @block.vector
        def _(e):
            for f in q["vector"]:
                f(e)

        @block.gpsimd
        def _(e):
            for f in q["gpsimd"]:
                f(e)

        @block.tensor
        def _(e):
            for f in q["tensor"]:
                f(e)


S = 4096
D = 1024
NT = 32
DFF = 2816
NFC = 22
NEG = -30000.0
L = 2


def build(debug=None, nlayers=L):
    from contextlib import ExitStack
    nc = bass.Bass("TRN2", target_bir_lowering=False)
    dt_in = lambda name, shape: nc.dram_tensor(name, shape, F32, kind="ExternalInput").ap()
    x_in = dt_in("x", [S, D])
    norm_mix_w = dt_in("norm_mix_w", [L, D])
    w_in = dt_in("w_in", [L, D, 2328])
    gmlp_norm_w = dt_in("gmlp_norm_w", [L, 512])
    gmlp_ws = dt_in("gmlp_ws", [L, 8, 128, 128])
    gmlp_bs = dt_in("gmlp_bs", [L, 8, 128])
    cmp_pos = {"k": dt_in("cmp_pos_k", [L, 32, 64]), "v": dt_in("cmp_pos_v", [L, 32, 64])}
    cmp_w1 = {"k": dt_in("cmp_k_w1", [L, 2048, 256]), "v": dt_in("cmp_v_w1", [L, 2048, 256])}
    cmp_w2 = {"k": dt_in("cmp_k_w2", [L, 256, 64]), "v": dt_in("cmp_v_w2", [L, 256, 64])}
    gate_b = dt_in("gate_b", [L, 24])
    out_norm_a_w = dt_in("out_norm_a_w", [L, 512])
    out_norm_b_w = dt_in("out_norm_b_w", [L, 512])
    w_o = dt_in("w_o", [L, D, D])
    norm_ffn_w = dt_in("norm_ffn_w", [L, D])
    w_gate = dt_in("w_gate", [L, D, DFF])
    w_up = dt_in("w_up", [L, D, DFF])
    w_down = dt_in("w_down", [L, DFF, D])
    final_norm_w = dt_in("final_norm_w", [D])
    out = nc.dram_tensor("out", [S, D], F32, kind="ExternalOutput").ap()
    dbg = debug is not None
    mix_d = nc.dram_tensor("mix_d", [S, D], BF16, kind="ExternalOutput" if dbg else "Internal").ap()
    xres_d = nc.dram_tensor("xres_d", [S, D], F32, kind="ExternalOutput" if dbg else "Internal").ap()

    with ExitStack() as st:
        P = Prog(nc, st)
        op = P.op

        sfx = [""]

        def T(name, shape, dt, stack=None):
            return (stack or st).enter_context(nc.sbuf_tensor(name + sfx[0], shape, dt))

        B = [st.enter_context(nc.psum_tensor("B%d" % i, [128, 512], F32)) for i in range(7)]
        BT = st.enter_context(nc.psum_tensor("BT", [128, 8, 128], BF16))

        ident_b = T("ident_b", [128, 128], BF16)
        ident_f = T("ident_f", [128, 128], F32)
        tri_f = T("tri_f", [128, 128], F32)
        tri_b = T("tri_b", [128, 128], BF16)
        ntri_b = T("ntri_b", [128, 128], BF16)
        ones_f = T("ones_f", [128, 128], F32)
        ind8 = T("ind8", [8, 512], F32)
        A_big = T("A_big", [16, 512], BF16)
        Bm4 = T("Bm4", [16, 4, 128], BF16)
        VM = T("VM", [128, 128], F32)
        AMk = T("AMk", [128, 128], F32)
        tmpc = T("tmpc", [128, 512], F32)
        tmpc2 = T("tmpc2", [128, 512], F32)

        def G(fn, reads=(), writes=()):
            op("gpsimd", fn, reads=reads, writes=writes)

        def asel(t, pattern, cmp_, fill, base, cm, key):
            G(lambda e: e.affine_select(out=t, in_=t, pattern=pattern, compare_op=cmp_, fill=fill,
                                        base=base, channel_multiplier=cm), reads=[key], writes=[key])

        G(lambda e: e.memset(ones_f[:], 1.0), writes=["ones_f"])
        G(lambda e: e.memset(ident_f[:], 1.0), writes=["ident_f"])
        asel(ident_f[:], [[-1, 128]], ALU.is_equal, 0.0, 0, 1, "ident_f")
        G(lambda e: e.tensor_copy(ident_b[:], ident_f[:]), reads=["ident_f"], writes=["ident_b"])
        G(lambda e: e.memset(tri_f[:], 1.0), writes=["tri_f"])
        asel(tri_f[:], [[1, 128]], ALU.is_ge, 0.0, 0, -1, "tri_f")
        G(lambda e: e.tensor_copy(tri_b[:], tri_f[:]), reads=["tri_f"], writes=["tri_b"])
        G(lambda e: e.memset(tmpc[:, 0:128], 1.0), writes=["tmpc"])
        asel(tmpc[:, 0:128], [[-1, 128]], ALU.is_gt, 0.0, 0, 1, "tmpc")
        G(lambda e: e.tensor_copy(ntri_b[:], tmpc[:, 0:128]), reads=["tmpc"], writes=["ntri_b"])
        G(lambda e: e.memset(ind8[:], 1.0), writes=["ind8"])
        asel(ind8[:].rearrange("p (h d) -> p h d", h=8), [[1, 8], [0, 64]], ALU.is_equal, 0.0, 0, -1, "ind8")
        G(lambda e: e.memset(tmpc[0:16, :], 1.0), writes=["tmpc"])
        asel(tmpc[0:16, :], [[1, 512]], ALU.is_equal, 0.0, -255, -1, "tmpc")
        G(lambda e: e.memset(tmpc2[0:16, :], 1.0), writes=["tmpc2"])
        asel(tmpc2[0:16, :], [[1, 512]], ALU.is_ge, 0.0, -263, 0, "tmpc2")
        asel(tmpc2[0:16, :], [[0, 512]], ALU.is_equal, 0.0, -8, 1, "tmpc2")
        asel(tmpc[0:16, :], [[0, 512]], ALU.is_ge, 0.0, 7, -1, "tmpc")
        G(lambda e: e.tensor_tensor(out=A_big[:], in0=tmpc[0:16, :], in1=tmpc2[0:16, :], op=ALU.add),
          reads=["tmpc", "tmpc2"], writes=["A_big"])
        G(lambda e: e.memset(tmpc[0:16, :], NEG), writes=["tmpc"])
        asel(tmpc[0:16, :].rearrange("p (g t) -> p g t", g=4), [[0, 4], [-1, 128]], ALU.is_gt, 0.0, 15, 16, "tmpc")
        asel(tmpc[0:16, :], [[0, 512]], ALU.is_ge, 0.0, 7, -1, "tmpc")
        G(lambda e: e.memset(tmpc2[0:16, :], NEG), writes=["tmpc2"])
        asel(tmpc2[0:16, :], [[0, 512]], ALU.is_equal, 0.0, -8, 1, "tmpc2")
        G(lambda e: e.tensor_tensor(out=Bm4[:].rearrange("p g t -> p (g t)"), in0=tmpc[0:16, :], in1=tmpc2[0:16, :], op=ALU.add),
          reads=["tmpc", "tmpc2"], writes=["Bm4"])
        G(lambda e: e.memset(VM[:], 1.0), writes=["VM"])
        asel(VM[:], [[-64, 128]], ALU.is_ge, 0.0, 64 * 64 - 128, 1, "VM")
        G(lambda e: e.memset(AMk[:], 10000.0), writes=["AMk"])
        asel(AMk[:], [[-64, 128]], ALU.is_ge, 0.0, 64 * 64, 1, "AMk")
        asel(AMk[:], [[64, 128]], ALU.is_ge, 0.0, -64 * 64 + 63, -1, "AMk")
        G(lambda e: e.memset(tmpc[:, 0:128], 10001.0), writes=["tmpc"])
        asel(tmpc[:, 0:128], [[-64, 128]], ALU.is_ge, 0.0, 64 * 64 - 64, 1, "tmpc")
        asel(tmpc[:, 0:128], [[64, 128]], ALU.is_ge, 0.0, -64 * 64 + 64 + 63, -1, "tmpc")
        G(lambda e: e.tensor_tensor(out=AMk[:], in0=AMk[:], in1=tmpc[:, 0:128], op=ALU.add), reads=["AMk", "tmpc"], writes=["AMk"])
        G(lambda e: e.memset(tmpc2[:, 0:128], -10000.0), writes=["tmpc2"])
        asel(tmpc2[:, 0:128], [[64, 128]], ALU.is_ge, 0.0, -64 * 64 - 1, -1, "tmpc2")
        G(lambda e: e.tensor_tensor(out=AMk[:], in0=AMk[:], in1=tmpc2[:, 0:128], op=ALU.add), reads=["AMk", "tmpc2"], writes=["AMk"])


        def rms_stats(src_ap, width, key_src, junk, ssum, rstd, tag):
            sc = float(width) ** -0.5
            op("scalar", lambda e: e.activation(out=junk, in_=src_ap, func=AF.Square, scale=sc, accum_out=ssum[:, 0:1]),
               reads=[key_src], writes=["junk" + tag, "ss" + tag])
            op("vector", lambda e: e.tensor_scalar(rstd[:, 0:1], ssum[:, 0:1], 1e-6, None, ALU.add), reads=["ss" + tag], writes=["rstd" + tag])
            op("scalar", lambda e: e.activation(out=rstd[:, 0:1], in_=rstd[:, 0:1], func=AF.Sqrt), reads=["rstd" + tag], writes=["rstd" + tag])
            op("vector", lambda e: e.reciprocal(rstd[:, 0:1], rstd[:, 0:1]), reads=["rstd" + tag], writes=["rstd" + tag])

        def layer(l):
            sfx[0] = "_L%d" % l
            xsrc = x_in if l == 0 else xres_d
            last = (l == nlayers - 1)
            P.barrier()
            with ExitStack() as s1:
                nmw = T("nmw", [128, D], F32, s1)
                gnw = T("gnw", [128, 512], F32, s1)
                onaw = T("onaw", [128, 512], F32, s1)
                onbw = T("onbw", [128, 512], F32, s1)
                gbt = T("gbt", [128, 24], F32, s1)
                bs8 = T("bs8", [8, 128], F32, s1)
                gates = T("gates", [128, NT, 24], F32, s1)
                P.dma("sync", nmw[:], norm_mix_w[l].partition_broadcast(128), writes=["nmw"])
                P.dma("sync", gnw[:], gmlp_norm_w[l].partition_broadcast(128), writes=["gnw"])
                P.dma("sync", onaw[:], out_norm_a_w[l].partition_broadcast(128), writes=["onaw"])
                P.dma("sync", onbw[:], out_norm_b_w[l].partition_broadcast(128), writes=["onbw"])
                P.dma("sync", gbt[:], gate_b[l].partition_broadcast(128), writes=["gbt"])
                P.dma("sync", bs8[:], gmlp_bs[l], writes=["bs8"])

                wtm = T("wtm", [128, 8, 1304], BF16, s1)
                wfm = T("wfm", [128, 8, 1024], BF16, s1)
                wsT = T("wsT", [128, 8, 128], BF16, s1)
                kTE = [T("kTE%d" % k, [128, S], BF16, s1) for k in range(2)]
                kTw = T("kTw", [128, S], BF16, s1)
                kcT = T("kcT", [128, S], BF16, s1)
                vcT = T("vcT", [128, S], BF16, s1)
                vs_aug = T("vs_aug", [128, NT, 2, 65], BF16, s1)
                vw_aug = T("vw_aug", [128, NT, 2, 65], BF16, s1)
                qT_all = T("qT_all", [128, 4, S], BF16, s1)
                w2k_pad = T("w2k_pad", [128, 2, 2, 128], BF16, s1)
                w2v = T("w2v", [128, 2, 64], BF16, s1)
                hidT = T("hidT", [128, 2, 256], BF16, s1)
                kcmp = T("kcmp", [128, 256], BF16, s1)
                R_cmp = T("R_cmp", [128, 2, 2, 129], BF16, s1)

                with ExitStack() as s0:
                    stage = [T("stage%d" % i, [128, 2328], F32, s0) for i in range(2)]
                    for c in range(8):
                        sg = stage[c % 2]
                        sk = "stage%d" % (c % 2)
                        P.dma("sync", sg[:], w_in[l, c * 128:(c + 1) * 128, :], writes=[sk])
                        cp = [
                            (wtm[:, c, 0:1024], sg[:, 0:1024]),
                            (wtm[:, c, 1024:1152], sg[:, 1920:2048]),
                            (wtm[:, c, 1152:1280], sg[:, 2176:2304]),
                            (wtm[:, c, 1280:1304], sg[:, 2304:2328]),
                            (wfm[:, c, 0:512].rearrange("p (g k d) -> p g k d", g=4, k=2),
                             sg[:, 1024:1536].rearrange("p (k g d) -> p g k d", k=2, g=4)),
                            (wfm[:, c, 512:640], sg[:, 1536:1664]),
                            (wfm[:, c, 640:768], sg[:, 1664:1792]),
                            (wfm[:, c, 768:896], sg[:, 1792:1920]),
                            (wfm[:, c, 896:1024], sg[:, 2048:2176]),
                        ]
                        for ci, (o_, i_) in enumerate(cp):
                            if ci % 2 == 0:
                                op("scalar", lambda e, o_=o_, i_=i_: e.copy(o_, i_), reads=[sk], writes=["wtm" if ci < 4 else "wfm"])
                            else:
                                op("vector", lambda e, o_=o_, i_=i_: e.tensor_copy(o_, i_), reads=[sk], writes=["wtm" if ci < 4 else "wfm"])
                    wsf = T("wsf", [128, 8, 128], F32, s0)
                    P.dma("sync", wsf[:], gmlp_ws[l].rearrange("h t s -> t h s"), writes=["wsf"])
                    for h in range(8):
                        bk = B[h % 2]
                        op("tensor", lambda e, h=h, bk=bk: e.transpose(bk[:, 0:128], wsf[:, h, :], ident_f[:]),
                           reads=["wsf", "ident_f"], writes=["B%d" % (h % 2)])
                        op("vector", lambda e, h=h, bk=bk: e.tensor_tensor(out=wsT[:, h, :], in0=bk[:, 0:128], in1=tri_f[:], op=ALU.mult),
                           reads=["B%d" % (h % 2), "tri_f"], writes=["wsT"])
                    for nt_ in range(2):
                        G(lambda e: e.memset(tmpc[:, 0:64], 1.0), writes=["tmpc"])
                        asel(tmpc[:, 0:64], [[-64, 64]], ALU.is_ge, 0.0, 2048 * nt_ + 31, 16, "tmpc")
                        asel(tmpc[:, 0:64], [[64, 64]], ALU.is_ge, 0.0, 63 - 2048 * nt_, -16, "tmpc")
                        for k in range(2):
                            G(lambda e, nt_=nt_, k=k: e.tensor_copy(R_cmp[:, nt_, k, 64:128], tmpc[:, 0:64]), reads=["tmpc"], writes=["R_cmp"])
                    G(lambda e: e.memset(R_cmp[:, :, :, 128:129], 1.0), writes=["R_cmp"])
                    G(lambda e: e.memset(vs_aug[:, :, :, 64:65], 1.0), writes=["vs_aug"])
                    G(lambda e: e.memset(vw_aug[:, :, :, 64:65], 1.0), writes=["vw_aug"])
                    G(lambda e: e.memset(kTE[0][:], 1.0), writes=["kTE0"])
                    asel(kTE[0][:].rearrange("p (j r) -> p j r", j=64), [[1, 64], [0, 64]], ALU.is_equal, 0.0, 64, -1, "kTE0")
                    G(lambda e: e.memset(kTE[1][:], 1.0), writes=["kTE1"])
                    asel(kTE[1][:].rearrange("p (j r) -> p j r", j=64), [[1, 64], [0, 64]], ALU.is_equal, 0.0, 0, -1, "kTE1")
                    P.barrier()

                with ExitStack() as s2:
                    xt = [T("xt%d" % i, [128, D], F32, s2) for i in range(2)]
                    junk = T("junk", [128, D], BF16, s2)
                    ssx = T("ssx", [128, 1], F32, s2)
                    rsx = T("rsx", [128, 1], F32, s2)
                    hb = T("hb", [128, D], BF16, s2)
                    hT = [T("hT%d" % i, [128, 8, 512], BF16, s2) for i in range(2)]
                    gu = T("gu", [128, 512], F32, s2)
                    gv = T("gv", [128, 512], F32, s2)
                    ssv = T("ssv", [128, 1], F32, s2)
                    rsv = T("rsv", [128, 1], F32, s2)
                    vn = T("vn", [128, 512], BF16, s2)
                    a_t = T("a_t", [128, 512], F32, s2)
                    ssa = T("ssa", [128, 1], F32, s2)
                    rsa = T("rsa", [128, 1], F32, s2)
                    mixA = [T("mixA%d" % i, [128, 512], BF16, s2) for i in range(2)]
                    gpre = T("gpre", [128, 24], F32, s2)
                    for TT in range(8):
                        hTt = hT[TT % 2]
                        hk = "hT%d" % (TT % 2)
                        for tt in range(4):
                            i = TT * 4 + tt
                            xti = xt[i % 2]
                            xk = "xt%d" % (i % 2)
                            P.dma("sync", xti[:], xsrc[i * 128:(i + 1) * 128, :], writes=[xk])
                            rms_stats(xti[:], D, xk, junk[:], ssx, rsx, "x")
                            op("vector", lambda e, xti=xti: e.scalar_tensor_tensor(out=hb[:], in0=xti[:], scalar=rsx[:, 0:1], in1=nmw[:], op0=ALU.mult, op1=ALU.mult),
                               reads=[xk, "rstdx", "nmw"], writes=["hb"])
                            for c in range(8):
                                op("tensor", lambda e, c=c: e.transpose(BT[:, c, :], hb[:, c * 128:(c + 1) * 128], ident_b[:]),
                                   reads=["hb", "ident_b"], writes=["BT"], signal=(c == 7))
                            op("vector", lambda e, hTt=hTt, tt=tt: e.tensor_copy(hTt[:, :, tt * 128:(tt + 1) * 128], BT[:]), reads=["BT"], writes=[hk])
                            for cb, (c0, c1) in enumerate(((0, 512), (512, 1024), (1024, 1304))):
                                for c in range(8):
                                    op("tensor", lambda e, cb=cb, c=c, c0=c0, c1=c1, hTt=hTt, tt=tt: e.matmul(B[cb][:, 0:c1 - c0], lhsT=hTt[:, c, tt * 128:(tt + 1) * 128], rhs=wtm[:, c, c0:c1],
                                                                                                      start=(c == 0), stop=(c == 7)),
                                       reads=[hk, "wtm"], writes=["B%d" % cb], signal=(c == 7))
                            op("scalar", lambda e: e.activation(out=gu[:], in_=B[0][:], func=AF.Gelu_apprx_tanh), reads=["B0"], writes=["gu"])
                            op("scalar", lambda e: e.activation(out=gv[:], in_=B[1][:], func=AF.Gelu_apprx_tanh), reads=["B1"], writes=["gv"])
                            rms_stats(gv[:], 512, "gv", junk[:, 0:512], ssv, rsv, "v")
                            op("vector", lambda e: e.scalar_tensor_tensor(out=vn[:], in0=gv[:], scalar=rsv[:, 0:1], in1=gnw[:], op0=ALU.mult, op1=ALU.mult),
                               reads=["gv", "rstdv", "gnw"], writes=["vn"])
                            op("vector", lambda e, i=i: e.tensor_copy(vs_aug[:, i, :, 0:64], B[2][:, 0:128].rearrange("p (k d) -> p k d", k=2)), reads=["B2"], writes=["vs_aug"])
                            op("vector", lambda e, i=i: e.tensor_copy(vw_aug[:, i, :, 0:64], B[2][:, 128:256].rearrange("p (k d) -> p k d", k=2)), reads=["B2"], writes=["vw_aug"])
                            op("vector", lambda e: e.tensor_tensor(out=gpre[:], in0=B[2][:, 256:280], in1=gbt[:], op=ALU.add), reads=["B2", "gbt"], writes=["gpre"])
                            op("scalar", lambda e, i=i: e.activation(out=gates[:, i, :], in_=gpre[:], func=AF.Sigmoid), reads=["gpre"], writes=["gates"])
                            op("tensor", lambda e: e.matmul(B[3][:], lhsT=bs8[:], rhs=ind8[:], start=True, stop=False), reads=["bs8", "ind8"], writes=["B3"], signal=False)
                            for h in range(8):
                                op("tensor", lambda e, h=h: e.matmul(B[3][:, h * 64:(h + 1) * 64], lhsT=wsT[:, h, :], rhs=vn[:, h * 64:(h + 1) * 64], start=False, stop=(h == 7)),
                                   reads=["wsT", "vn"], writes=["B3"], signal=(h == 7))
                            op("vector", lambda e: e.tensor_tensor(out=a_t[:], in0=B[3][:], in1=gu[:], op=ALU.mult), reads=["B3", "gu"], writes=["a_t"])
                            rms_stats(a_t[:], 512, "a_t", junk[:, 512:1024], ssa, rsa, "a")
                            mA = mixA[i % 2]
                            mk = "mixA%d" % (i % 2)
                            op("vector", lambda e, mA=mA: e.scalar_tensor_tensor(out=mA[:], in0=a_t[:], scalar=rsa[:, 0:1], in1=onaw[:], op0=ALU.mult, op1=ALU.mult),
                               reads=["a_t", "rstda", "onaw"], writes=[mk])
                            P.dma("gpsimd", mix_d[i * 128:(i + 1) * 128, 0:512], mA[:], reads=[mk], writes=["mix_d"])
                        tok = slice(TT * 512, (TT + 1) * 512)
                        for ch in range(8):
                            bk = B[4 + ch % 2]
                            bkk = "B%d" % (4 + ch % 2)
                            for c in range(8):
                                op("tensor", lambda e, bk=bk, ch=ch, c=c, hTt=hTt: e.matmul(bk[:], lhsT=wfm[:, c, ch * 128:(ch + 1) * 128], rhs=hTt[:, c, :], start=(c == 0), stop=(c == 7)),
                                   reads=[hk, "wfm"], writes=[bkk], signal=(c == 7))
                            if ch < 4:
                                op("scalar", lambda e, bk=bk, ch=ch, tok=tok: e.activation(out=qT_all[:, ch, tok], in_=bk[:], func=AF.Copy, scale=0.125), reads=[bkk], writes=["qT_all"])
                            elif ch == 4:
                                op("vector", lambda e, bk=bk, tok=tok: e.tensor_copy(kcT[:, tok], bk[:]), reads=[bkk], writes=["kcT"])
                            elif ch == 5:
                                op("vector", lambda e, bk=bk, tok=tok: e.tensor_copy(vcT[:, tok], bk[:]), reads=[bkk], writes=["vcT"])
                            elif ch == 6:
                                op("vector", lambda e, bk=bk, tok=tok: e.tensor_copy(kTE[0][0:64, tok], bk[0:64, :]), reads=[bkk], writes=["kTE0"])
                                op("vector", lambda e, bk=bk, tok=tok: e.tensor_copy(kTE[1][64:128, tok], bk[64:128, :]), reads=[bkk], writes=["kTE1"])
                            else:
                                op("vector", lambda e, bk=bk, tok=tok: e.tensor_copy(kTw[:, tok], bk[:]), reads=[bkk], writes=["kTw"])
                    P.barrier()

                with ExitStack() as s25:
                    w1A = {kv: T("w1A" + kv, [128, 32, 256], BF16, s25) for kv in "kv"}
                    posT = {kv: T("posT" + kv, [128, 32], BF16, s25) for kv in "kv"}
                    bcol = {kv: T("bcol" + kv, [128, 2], F32, s25) for kv in "kv"}
                    w1f = T("w1f", [128, 8, 256], F32, s25)
                    posf = T("posf", [32, 64], F32, s25)
                    w2f = T("w2f", [128, 2, 64], F32, s25)
                    G(lambda e: e.memset(w2k_pad[:], 0.0), writes=["w2k_pad"])
                    for kv in "kv":
                        src = cmp_w1[kv][l].rearrange("(l d) n -> d l n", d=64)
                        for q4 in range(4):
                            P.dma("sync", w1f[0:64], src[:, q4 * 8:(q4 + 1) * 8, :], writes=["w1f"])
                            P.dma("sync", w1f[64:128], src[:, q4 * 8:(q4 + 1) * 8, :], writes=["w1f"])
                            if q4 % 2 == 0:
                                op("scalar", lambda e, kv=kv, q4=q4: e.copy(w1A[kv][:, q4 * 8:(q4 + 1) * 8, :], w1f[:]), reads=["w1f"], writes=["w1A" + kv])
                            else:
                                op("vector", lambda e, kv=kv, q4=q4: e.tensor_copy(w1A[kv][:, q4 * 8:(q4 + 1) * 8, :], w1f[:]), reads=["w1f"], writes=["w1A" + kv])
                        P.dma("sync", posf[:], cmp_pos[kv][l], writes=["posf"])
                        op("tensor", lambda e: e.transpose(B[2][0:64, 0:32], posf[:, :], ident_f[0:32, 0:32]), reads=["posf", "ident_f"], writes=["B2"])
                        op("vector", lambda e, kv=kv: e.tensor_copy(posT[kv][0:64, :], B[2][0:64, 0:32]), reads=["B2"], writes=["posT" + kv])
                        for hc in range(2):
                            for ll in range(32):
                                op("tensor", lambda e, kv=kv, hc=hc, ll=ll: e.matmul(B[3][:, hc:hc + 1], lhsT=w1A[kv][0:64, ll, hc * 128:(hc + 1) * 128],
                                                                                 rhs=posT[kv][0:64, ll:ll + 1], start=(ll == 0), stop=(ll == 31)),
                                   reads=["w1A" + kv, "posT" + kv], writes=["B3"], signal=(ll == 31))
                        op("vector", lambda e, kv=kv: e.tensor_copy(bcol[kv][:], B[3][:, 0:2]), reads=["B3"], writes=["bcol" + kv])
                        P.dma("sync", w2f[:], cmp_w2[kv][l].rearrange("(c p) d -> p c d", p=128), writes=["w2f"])
                        if kv == "k":
                            for k in range(2):
                                G(lambda e, k=k: e.tensor_copy(w2k_pad[:, :, k, 64 * k:64 * k + 64], w2f[:]), reads=["w2f"], writes=["w2k_pad"])
                        else:
                            G(lambda e: e.tensor_copy(w2v[:], w2f[:]), reads=["w2f"], writes=["w2v"])
                    for kv, srcT in (("k", kcT), ("v", vcT)):
                        for k in range(2):
                            pr = slice(64 * k, 64 * k + 64)
                            for hc in range(2):
                                bk = B[hc]
                                for ll in range(32):
                                    op("tensor", lambda e, kv=kv, hc=hc, ll=ll, bk=bk, pr=pr, srcT=srcT: e.matmul(bk[:, 0:255], lhsT=w1A[kv][pr, ll, hc * 128:(hc + 1) * 128],
                                                                                                       rhs=srcT[pr, ll:ll + 16 * 254 + 1:16], start=(ll == 0), stop=(ll == 31)),
                                       reads=["w1A" + kv, "kcT", "vcT"], writes=["B%d" % hc], signal=(ll == 31))
                                op("scalar", lambda e, kv=kv, hc=hc, bk=bk: e.activation(out=hidT[:, hc, 0:255], in_=bk[:, 0:255], func=AF.Gelu_apprx_tanh, bias=bcol[kv][:, hc:hc + 1]),
                                   reads=["B%d" % hc, "bcol" + kv], writes=["hidT"])
                            if kv == "k":
                                for hc in range(2):
                                    op("tensor", lambda e, hc=hc, k=k: e.matmul(B[2][:, 0:255], lhsT=w2k_pad[:, hc, k, :], rhs=hidT[:, hc, 0:255], start=(hc == 0), stop=(hc == 1)),
                                       reads=["w2k_pad", "hidT"], writes=["B2"], signal=(hc == 1))
                                op("vector", lambda e, pr=pr: e.tensor_copy(kcmp[pr, 0:255], B[2][pr, 0:255]), reads=["B2"], writes=["kcmp"])
                            else:
                                for nt_, M in ((0, 128), (1, 127)):
                                    for hc in range(2):
                                        op("tensor", lambda e, hc=hc, nt_=nt_, M=M: e.matmul(B[3][0:M, 0:64], lhsT=hidT[:, hc, nt_ * 128:nt_ * 128 + M], rhs=w2v[:, hc, :], start=(hc == 0), stop=(hc == 1)),
                                           reads=["w2v", "hidT"], writes=["B3"], signal=(hc == 1))
                                    op("vector", lambda e, nt_=nt_, M=M, k=k: e.tensor_copy(R_cmp[0:M, nt_, k, 0:64], B[3][0:M, 0:64]), reads=["B3"], writes=["R_cmp"])
                    P.barrier()

                with ExitStack() as s3:
                    qB = [T("qB%d" % k, [128, 4, 128], BF16, s3) for k in range(2)]
                    PTc = T("PTc", [128, 2, 512], BF16, s3)
                    PT = [T("PT%d" % i, [128, 512], BF16, s3) for i in range(4)]
                    oc2 = [T("oc%d" % j, [128, 4, 129], F32, s3) for j in range(2)]
                    rc2 = [T("rc%d" % j, [128, 4], F32, s3) for j in range(2)]
                    imp = T("imp", [128, 64], F32, s3)
                    score = T("score", [128, 64], F32, s3)
                    sc2 = T("sc2", [128, 64], F32, s3)
                    m8 = T("m8", [128, 16], F32, s3)
                    biasP = [T("biasP%d" % k, [128, 128], BF16, s3) for k in range(2)]
                    oT_sb = T("oT_sb", [128, 2, 512], F32, s3)
                    rr = T("rr", [128, 2, 4], F32, s3)
                    rg = T("rg", [128, 3, 4], F32, s3)
                    bfull2 = [T("bfull%d" % j, [128, 512], F32, s3) for j in range(2)]
                    btmp2 = [T("btmp%d" % j, [128, 4, 64], F32, s3) for j in range(2)]
                    ssb = T("ssb", [128, 1], F32, s3)
                    rsb = T("rsb", [128, 1], F32, s3)
                    junkb = T("junkb", [128, 512], F32, s3)
                    mixB = [T("mixB%d" % i, [128, 512], BF16, s3) for i in range(2)]
                    for k in range(2):
                        G(lambda e, k=k: e.memset(qB[k][:], 0.0), writes=["qB%d" % k])
                        G(lambda e, k=k: e.memset(biasP[k][:], 0.0), writes=["biasP%d" % k])
                    psC = [B[6][:, 260:389], B[5][:, 0:129], B[5][:, 129:258], B[5][:, 258:387]]
                    psCk = ["B6", "B5", "B5", "B5"]
                    psB = BT[:, 0, :]
                    psT = B[6][:, 0:260].rearrange("p (g d) -> p g d", g=4)
                    sidx = [0]
                    pidx = [0]

                    def stageA(i, k, n):
                        pr = slice(64 * k, 64 * k + 64)
                        br_ = slice(64, 128) if k == 0 else slice(0, 64)
                        qk = "qB%d" % k
                        ocn, rcn = oc2[n % 2], rc2[n % 2]
                        ock, rck = "oc%d" % (n % 2), "rc%d" % (n % 2)
                        G(lambda e: e.tensor_copy(qB[k][pr, :, :], qT_all[pr, :, i * 128:(i + 1) * 128]), reads=["qT_all"], writes=[qk])
                        n_tiles = 1 if i <= 15 else 2
                        for nt_ in range(n_tiles):
                            M = 128 if nt_ == 0 else 127
                            bs_ = B[5]
                            bsk = "B5"
                            a0 = 256 - 8 * i + nt_ * 128
                            op("tensor", lambda e, bs_=bs_, M=M, nt_=nt_: e.matmul(bs_[0:M, :], lhsT=kcmp[pr, nt_ * 128:nt_ * 128 + M], rhs=qT_all[pr, :, i * 128:(i + 1) * 128], start=True, stop=False),
                               reads=["kcmp", "qT_all"], writes=[bsk], signal=False)
                            op("tensor", lambda e, bs_=bs_, M=M, a0=a0: e.matmul(bs_[0:M, :], lhsT=A_big[0:9, a0:a0 + M], rhs=Bm4[0:9, :, :], start=False, stop=True),
                               reads=["A_big", "Bm4"], writes=[bsk])
                            yield
                            yield
                            op("scalar", lambda e, bs_=bs_, M=M, nt_=nt_: e.activation(out=PTc[0:M, nt_, :], in_=bs_[0:M, :], func=AF.Exp), reads=[bsk], writes=["PTc"])
                            yield
                            yield
                        for g in range(4):
                            for nt_ in range(n_tiles):
                                M = 128 if nt_ == 0 else 127
                                op("tensor", lambda e, g=g, nt_=nt_, M=M: e.matmul(psC[g], lhsT=PTc[0:M, nt_, g * 128:(g + 1) * 128], rhs=R_cmp[0:M, nt_, k, :],
                                                                                start=(nt_ == 0), stop=(nt_ == n_tiles - 1)),
                                   reads=["PTc", "R_cmp"], writes=[psCk[g]], signal=(nt_ == n_tiles - 1))
                        yield
                        yield
                        for g in range(4):
                            op("vector", lambda e, g=g: e.tensor_copy(ocn[:, g, :], psC[g]), reads=[psCk[g]], writes=[ock])
                        op("vector", lambda e: e.tensor_scalar(rcn[:], ocn[:, :, 128], 1e-30, None, ALU.max), reads=[ock], writes=[rck])
                        op("vector", lambda e: e.reciprocal(rcn[:], rcn[:]), reads=[rck], writes=[rck])
                        yield
                        if i >= 8:
                            op("vector", lambda e: e.tensor_scalar(imp[:], ocn[:, 0, 64:128], rcn[:, 0:1], None, ALU.mult), reads=[ock, rck], writes=["imp"])
                            for g in range(1, 4):
                                op("vector", lambda e, g=g: e.scalar_tensor_tensor(out=imp[:], in0=ocn[:, g, 64:128], scalar=rcn[:, g:g + 1], in1=imp[:], op0=ALU.mult, op1=ALU.add),
                                   reads=[ock, rck, "imp"], writes=["imp"])
                            c0 = 64 - 2 * i
                            op("vector", lambda e: e.tensor_tensor(out=score[:], in0=imp[:], in1=VM[:, c0:c0 + 64], op=ALU.mult), reads=["imp", "VM"], writes=["score"])
                            op("vector", lambda e: e.tensor_tensor(out=score[:], in0=score[:], in1=AMk[:, c0:c0 + 64], op=ALU.add), reads=["score", "AMk"], writes=["score"])
                            op("vector", lambda e: e.memset(score[:, 0:1], 10002.0), reads=["score"], writes=["score"])
                            yield
                            op("vector", lambda e: e.max(out=m8[:, 0:8], in_=score[:]), reads=["score"], writes=["m8"])
                            op("vector", lambda e: e.match_replace(out=sc2[:], in_to_replace=m8[:, 0:8], in_values=score[:], imm_value=NEG), reads=["score", "m8"], writes=["sc2"])
                            op("vector", lambda e: e.max(out=m8[:, 8:16], in_=sc2[:]), reads=["sc2"], writes=["m8"])
                            bo = 64 if k == 0 else 0
                            op("vector", lambda e: e.tensor_scalar(biasP[k][:, bo:bo + 64], score[:], m8[:, 15:16], NEG, ALU.is_lt, ALU.mult),
                               reads=["score", "m8"], writes=["biasP%d" % k])
                            yield
                            yield
                            yield
                            yield
                            op("tensor", lambda e: e.transpose(psB, biasP[k][:], ident_b[:]), reads=["biasP%d" % k, "ident_b"], writes=["BT"])
                            yield
                            yield
                            op("vector", lambda e: e.tensor_copy(qB[k][br_, :, :], BT[br_, 0, :].unsqueeze(1).to_broadcast([64, 4, 128])),
                               reads=["BT"], writes=[qk])
                            yield

                    def adv(gen, cnt):
                        for _ in range(cnt):
                            try:
                                next(gen)
                            except StopIteration:
                                return

                    def makeB(i, k, n):
                        pr = slice(64 * k, 64 * k + 64)
                        qk = "qB%d" % k
                        ocn, rcn = oc2[n % 2], rc2[n % 2]
                        ock, rck = "oc%d" % (n % 2), "rc%d" % (n % 2)
                        bfl = bfull2[i % 2]
                        bfk = "bfull%d" % (i % 2)
                        cfg = ((kTE[k], vs_aug, list(range(0, i + 1))), (kTw, vw_aug, list(range(max(0, i - 4), i + 1))))
                        stp = [(bi, ji, jt, len(cfg[bi][2])) for bi in range(2) for ji, jt in enumerate(cfg[bi][2])]
                        banks = {}

                        def emitS(idx):
                            bi, ji, jt, nj = stp[idx]
                            cache = cfg[bi][0]
                            ck = ("kTE%d" % k) if bi == 0 else "kTw"
                            bnum = (0, 1, 4)[sidx[0] % 3]
                            bs_ = B[bnum]
                            bsk = "B%d" % bnum
                            sidx[0] += 1
                            banks[idx] = (bs_, bsk)
                            if bi == 0:
                                op("tensor", lambda e: e.matmul(bs_[:], lhsT=cache[:, jt * 128:(jt + 1) * 128], rhs=qB[k][:, :, :], start=True, stop=True),
                                   reads=[ck, qk], writes=[bsk])
                            else:
                                op("tensor", lambda e: e.matmul(bs_[:], lhsT=cache[pr, jt * 128:(jt + 1) * 128], rhs=qT_all[pr, :, i * 128:(i + 1) * 128], start=True, stop=True),
                                   reads=[ck, "qT_all"], writes=[bsk])

                        def emitPV(idx):
                            bi, ji, jt, nj = stp[idx]
                            vaug = cfg[bi][1]
                            pso = B[2 + bi]
                            psok = "B%d" % (2 + bi)
                            bs_, bsk = banks[idx]
                            pt = PT[pidx[0] % 4]
                            ptk = "PT%d" % (pidx[0] % 4)
                            pidx[0] += 1
                            op("scalar", lambda e: e.activation(out=pt[:], in_=bs_[:], func=AF.Exp), reads=[bsk], writes=[ptk])
                            if jt == i:
                                G(lambda e: e.tensor_tensor(out=pt[:].rearrange("p (g t) -> p g t", g=4), in0=pt[:].rearrange("p (g t) -> p g t", g=4),
                                                            in1=tri_b[:, :].unsqueeze(1).to_broadcast([128, 4, 128]), op=ALU.mult), reads=[ptk, "tri_b"], writes=[ptk])
                            if bi == 1 and jt == i - 4:
                                G(lambda e: e.tensor_tensor(out=pt[:].rearrange("p (g t) -> p g t", g=4), in0=pt[:].rearrange("p (g t) -> p g t", g=4),
                                                            in1=ntri_b[:, :].unsqueeze(1).to_broadcast([128, 4, 128]), op=ALU.mult), reads=[ptk, "ntri_b"], writes=[ptk])
                            op("tensor", lambda e: e.matmul(pso[0:65, :], lhsT=vaug[:, jt, k, :], rhs=pt[:], start=(ji == 0), stop=(ji == nj - 1)),
                               reads=[ptk, "vs_aug", "vw_aug"], writes=[psok], signal=(ji == nj - 1))
                            if ji == nj - 1:
                                op("vector", lambda e: e.tensor_copy(oT_sb[0:65, bi, :], pso[0:65, :]), reads=[psok], writes=["oT_sb"])

                        def begin():
                            emitS(0)
                            if len(stp) > 1:
                                emitS(1)

                        def loop(gnext):
                            for idx in range(len(stp)):
                                if idx + 2 < len(stp):
                                    emitS(idx + 2)
                                emitPV(idx)
                                adv(gnext, 1)
                            adv(gnext, 1000)

                        return begin, loop

                    def makeFin(i, k, n):
                        ocn, rcn = oc2[n % 2], rc2[n % 2]
                        ock, rck = "oc%d" % (n % 2), "rc%d" % (n % 2)
                        bfl = bfull2[i % 2]
                        bfk = "bfull%d" % (i % 2)
                        gsl = lambda br: gates[:, i, br * 8 + k * 4: br * 8 + k * 4 + 4]
                        op("vector", lambda e: e.tensor_tensor(out=rg[:, 0, :], in0=rcn[:], in1=gsl(0), op=ALU.mult), reads=[rck, "gates"], writes=["rg"])
                        op("vector", lambda e: e.tensor_tensor(out=bfl[:, k * 256:(k + 1) * 256].rearrange("p (g d) -> p g d", g=4), in0=ocn[:, :, 0:64],
                                                               in1=rg[:, 0, :].unsqueeze(2).to_broadcast([128, 4, 64]), op=ALU.mult), reads=[ock, "rg"], writes=[bfk])
                        for bi in range(2):
                            for g in range(4):
                                op("tensor", lambda e, bi=bi, g=g: e.transpose(psT[:, g, :], oT_sb[0:65, bi, g * 128:(g + 1) * 128], ident_f[0:65, 0:65]),
                                   reads=["oT_sb", "ident_f"], writes=["B6"], signal=(g == 3))
                            op("vector", lambda e, bi=bi: e.tensor_scalar(rr[:, bi, :], psT[:, :, 64], 1e-30, None, ALU.max), reads=["B6"], writes=["rr"])
                            op("vector", lambda e, bi=bi: e.reciprocal(rr[:, bi, :], rr[:, bi, :]), reads=["rr"], writes=["rr"])
                            op("vector", lambda e, bi=bi: e.tensor_tensor(out=rg[:, 1 + bi, :], in0=rr[:, bi, :], in1=gsl(1 + bi), op=ALU.mult), reads=["rr", "gates"], writes=["rg"])
                            for g in range(4):
                                cs = slice(k * 256 + g * 64, k * 256 + g * 64 + 64)
                                op("vector", lambda e, bi=bi, g=g, cs=cs: e.scalar_tensor_tensor(out=bfl[:, cs], in0=psT[:, g, 0:64], scalar=rg[:, 1 + bi, g:g + 1], in1=bfl[:, cs], op0=ALU.mult, op1=ALU.add),
                                   reads=["B6", "rg", bfk], writes=[bfk])

                    steps = [(i, k) for i in range(NT) for k in range(2)]
                    adv(stageA(steps[0][0], steps[0][1], 0), 1000)
                    Bcur = makeB(steps[0][0], steps[0][1], 0)
                    Bcur[0]()
                    for n, (i, k) in enumerate(steps):
                        gnext = stageA(steps[n + 1][0], steps[n + 1][1], n + 1) if n + 1 < len(steps) else iter(())
                        Bcur[1](gnext)
                        if n + 1 < len(steps):
                            Bnext = makeB(steps[n + 1][0], steps[n + 1][1], n + 1)
                            Bnext[0]()
                        makeFin(i, k, n)
                        if n + 1 < len(steps):
                            Bcur = Bnext
                        if k == 1:
                            bfl = bfull2[i % 2]
                            bfk = "bfull%d" % (i % 2)
                            rms_stats(bfl[:], 512, bfk, junkb[:], ssb, rsb, "b")
                            mB = mixB[i % 2]
                            mk = "mixB%d" % (i % 2)
                            op("vector", lambda e, mB=mB, bfl=bfl: e.scalar_tensor_tensor(out=mB[:], in0=bfl[:], scalar=rsb[:, 0:1], in1=onbw[:], op0=ALU.mult, op1=ALU.mult),
                               reads=[bfk, "rstdb", "onbw"], writes=[mk])
                            P.dma("gpsimd", mix_d[i * 128:(i + 1) * 128, 512:1024], mB[:], reads=[mk], writes=["mix_d"])
                    P.barrier()
            if debug == "p3":
                return
            with ExitStack() as s4:
                wo_b = T("wo_b", [128, 8, D], BF16, s4)
                wg_b = T("wg_b", [128, 8, DFF], BF16, s4)
                wu_b = T("wu_b", [128, 8, DFF], BF16, s4)
                wd_b = T("wd_b", [128, NFC, D], BF16, s4)
                nfw = T("nfw", [128, D], F32, s4)
                fnw = T("fnw", [128, D], F32, s4)
                P.dma("sync", nfw[:], norm_ffn_w[l].partition_broadcast(128), writes=["nfw"])
                P.dma("sync", fnw[:], final_norm_w.partition_broadcast(128), writes=["fnw"])
                s4a = ExitStack()
                stg = [T("stg%d" % i, [128, DFF], F32, s4a) for i in range(3)]
                si = 0
                for (wsrc, wdst, nck, wid, key) in ((w_o, wo_b, 8, D, "wo_b"), (w_gate, wg_b, 8, DFF, "wg_b"), (w_up, wu_b, 8, DFF, "wu_b"), (w_down, wd_b, NFC, D, "wd_b")):
                    for c in range(nck):
                        sg = stg[si % 3]
                        sk = "stg%d" % (si % 3)
                        P.dma("sync" if si % 2 == 0 else "gpsimd", sg[:, 0:wid], wsrc[l, c * 128:(c + 1) * 128, :], writes=[sk])
                        if si % 2 == 0:
                            op("scalar", lambda e, wdst=wdst, c=c, sg=sg, wid=wid: e.copy(wdst[:, c, :], sg[:, 0:wid]), reads=[sk], writes=[key])
                        else:
                            op("vector", lambda e, wdst=wdst, c=c, sg=sg, wid=wid: e.tensor_copy(wdst[:, c, :], sg[:, 0:wid]), reads=[sk], writes=[key])
                        si += 1
                P.barrier()
                s4a.close()
                mixt = [T("mixt%d" % i, [128, D], BF16, s4) for i in range(2)]
                mixT = T("mixT", [128, 8, 128], BF16, s4)
                x1 = T("x1", [128, 2, D], F32, s4)
                junk4 = T("junk4", [128, D], BF16, s4)
                ss4 = T("ss4", [128, 1], F32, s4)
                rs4 = T("rs4", [128, 1], F32, s4)
                h2 = T("h2", [128, D], BF16, s4)
                h2T = T("h2T", [128, 8, 256], BF16, s4)
                sgt = [T("sgt%d" % i, [128, 256], F32, s4) for i in range(2)]
                actT = T("actT", [128, NFC, 256], BF16, s4)
                x2 = [T("x2_%d" % i, [128, D], F32, s4) for i in range(1)]
                ss5 = T("ss5", [128, 1], F32, s4)
                rs5 = T("rs5", [128, 1], F32, s4)
                for TT in range(16):
                    for tt in range(2):
                        i = TT * 2 + tt
                        mt = mixt[i % 2]
                        mtk = "mixt%d" % (i % 2)
                        P.dma("sync", mt[:], mix_d[i * 128:(i + 1) * 128, :], reads=["mix_d"], writes=[mtk])
                        P.dma("scalar", x1[:, tt, :], xsrc[i * 128:(i + 1) * 128, :], reads=["xres_d"], writes=["x1"])
                        for c in range(8):
                            op("tensor", lambda e, c=c, mt=mt: e.transpose(BT[:, c, :], mt[:, c * 128:(c + 1) * 128], ident_b[:]), reads=[mtk, "ident_b"], writes=["BT"], signal=(c == 7))
                        op("vector", lambda e: e.tensor_copy(mixT[:], BT[:]), reads=["BT"], writes=["mixT"])
                        for half in range(2):
                            for c in range(8):
                                op("tensor", lambda e, half=half, c=c: e.matmul(B[half][:], lhsT=mixT[:, c, :], rhs=wo_b[:, c, half * 512:(half + 1) * 512], start=(c == 0), stop=(c == 7)),
                                   reads=["mixT", "wo_b"], writes=["B%d" % half], signal=(c == 7))
                            op("vector", lambda e, half=half, tt=tt: e.tensor_tensor(out=x1[:, tt, half * 512:(half + 1) * 512], in0=B[half][:], in1=x1[:, tt, half * 512:(half + 1) * 512], op=ALU.add),
                               reads=["B%d" % half, "x1"], writes=["x1"])
                        rms_stats(x1[:, tt, :], D, "x1", junk4[:], ss4, rs4, "4")
                        op("vector", lambda e, tt=tt: e.scalar_tensor_tensor(out=h2[:], in0=x1[:, tt, :], scalar=rs4[:, 0:1], in1=nfw[:], op0=ALU.mult, op1=ALU.mult),
                           reads=["x1", "rstd4", "nfw"], writes=["h2"])
                        for c in range(8):
                            op("tensor", lambda e, c=c: e.transpose(BT[:, c, :], h2[:, c * 128:(c + 1) * 128], ident_b[:]), reads=["h2", "ident_b"], writes=["BT"], signal=(c == 7))
                        op("vector", lambda e, tt=tt: e.tensor_copy(h2T[:, :, tt * 128:(tt + 1) * 128], BT[:]), reads=["BT"], writes=["h2T"])
                    for fc in range(NFC):
                        pg = B[2 + 2 * (fc % 2)]
                        pu = B[3 + 2 * (fc % 2)]
                        pgk = "B%d" % (2 + 2 * (fc % 2))
                        puk = "B%d" % (3 + 2 * (fc % 2))
                        for c in range(8):
                            op("tensor", lambda e, pg=pg, c=c, fc=fc: e.matmul(pg[:, 0:256], lhsT=wg_b[:, c, fc * 128:(fc + 1) * 128], rhs=h2T[:, c, :], start=(c == 0), stop=(c == 7)),
                               reads=["wg_b", "h2T"], writes=[pgk], signal=(c == 7))
                        for c in range(8):
                            op("tensor", lambda e, pu=pu, c=c, fc=fc: e.matmul(pu[:, 0:256], lhsT=wu_b[:, c, fc * 128:(fc + 1) * 128], rhs=h2T[:, c, :], start=(c == 0), stop=(c == 7)),
                               reads=["wu_b", "h2T"], writes=[puk], signal=(c == 7))
                        sg_ = sgt[fc % 2]
                        sgk = "sgt%d" % (fc % 2)
                        op("scalar", lambda e, sg_=sg_, pg=pg: e.activation(out=sg_[:], in_=pg[:, 0:256], func=AF.Silu), reads=[pgk], writes=[sgk])
                        op("vector", lambda e, sg_=sg_, pu=pu, fc=fc: e.tensor_tensor(out=actT[:, fc, :], in0=pu[:, 0:256], in1=sg_[:], op=ALU.mult), reads=[puk, sgk], writes=["actT"])
                    for tt in range(2):
                        i = TT * 2 + tt
                        x2i = x2[0]
                        x2k = "x2_0"
                        for half in range(2):
                            pd = B[(0, 6)[half]]
                            pdk = "B%d" % ((0, 6)[half])
                            for fc in range(NFC):
                                op("tensor", lambda e, pd=pd, fc=fc, tt=tt, half=half: e.matmul(pd[:], lhsT=actT[:, fc, tt * 128:(tt + 1) * 128], rhs=wd_b[:, fc, half * 512:(half + 1) * 512],
                                                                                      start=(fc == 0), stop=(fc == NFC - 1)),
                                   reads=["actT", "wd_b"], writes=[pdk], signal=(fc == NFC - 1))
                            op("vector", lambda e, pd=pd, half=half, tt=tt, x2i=x2i: e.tensor_tensor(out=x2i[:, half * 512:(half + 1) * 512], in0=pd[:], in1=x1[:, tt, half * 512:(half + 1) * 512], op=ALU.add),
                               reads=[pdk, "x1"], writes=[x2k])
                        if not last:
                            P.dma("gpsimd", xres_d[i * 128:(i + 1) * 128, :], x2i[:], reads=[x2k], writes=["xres_d"])
                        else:
                            rms_stats(x2i[:], D, x2k, junk4[:], ss5, rs5, "5")
                            op("vector", lambda e, x2i=x2i: e.scalar_tensor_tensor(out=x2i[:], in0=x2i[:], scalar=rs5[:, 0:1], in1=fnw[:], op0=ALU.mult, op1=ALU.mult),
                               reads=[x2k, "rstd5", "fnw"], writes=[x2k])
                            P.dma("gpsimd", out[i * 128:(i + 1) * 128, :], x2i[:], reads=[x2k], writes=["out"])
                P.barrier()
        for l_ in range(nlayers):
            layer(l_)
        P.finish("gpsimd", ["out", "mix_d", "xres_d"])
        P.barrier()
        with nc.Block() as block:
            P.emit(block)
    print("instructions:", P.n_inst)
    return nc


_NAMES = ["norm_mix_w", "w_in", "gmlp_norm_w", "gmlp_ws", "gmlp_bs", "cmp_pos_k", "cmp_pos_v", "cmp_k_w1", "cmp_k_w2",
          "cmp_v_w1", "cmp_v_w2", "gate_b", "out_norm_a_w", "out_norm_b_w", "w_o", "norm_ffn_w", "w_gate", "w_up",
          "w_down", "final_norm_w"]


def kernel(**inputs):
    x = np.ascontiguousarray(np.asarray(inputs["x"], dtype=np.float32))
    shared = {n: np.ascontiguousarray(np.asarray(inputs[n], dtype=np.float32)) for n in _NAMES}
    nc = build()
    in_maps = [dict(shared, x=x[b]) for b in range(8)]
    res = run_bass_kernel_spmd(nc, in_maps, core_ids=list(range(8)))
    return np.stack([np.asarray(r["out"], dtype=np.float32) for r in res.results], axis=0)
```

```python
import numpy as np
import concourse.bass as bass
import concourse.mybir as mybir
from concourse.bass_utils import run_bass_kernel_spmd

F32 = mybir.dt.float32
BF16 = mybir.dt.bfloat16
AF = mybir.ActivationFunctionType
ALU = mybir.AluOpType
AX = mybir.AxisListType


class Prog:
    ENGS = ("sync", "scalar", "vector", "gpsimd", "tensor")
    NDMA = 8
    R = 8

    def __init__(self, nc, stack):
        self.nc = nc
        self.q = {e: [] for e in self.ENGS}
        self.sem = {e: [stack.enter_context(nc.semaphore("s_%s%d" % (e, i))) for i in range(self.R)]
                    for e in self.ENGS}
        self.cnt = {e: 0 for e in self.ENGS}
        self.dsem = {e: [stack.enter_context(nc.semaphore("d_%s%d" % (e, i))) for i in range(self.NDMA)]
                     for e in ("sync", "scalar", "gpsimd")}
        self.dcnt = {e: 0 for e in self.dsem}
        self.semobj = {}
        for e in self.ENGS:
            for i in range(self.R):
                self.semobj[("c", e, i)] = self.sem[e][i]
        for e in self.dsem:
            for i in range(self.NDMA):
                self.semobj[("d", e, i)] = self.dsem[e][i]
        self.waited = {e: {} for e in self.ENGS}
        self.lastw = {}
        self.readers = {}
        self.n_inst = 0

    def _waits(self, eng, deps):
        need = {}
        for (sid, val) in deps:
            if sid[0] == "c" and sid[1] == eng and (val - 1) * self.R + sid[2] + 1 > self.cnt[eng]:
                continue
            if self.waited[eng].get(sid, 0) >= val:
                continue
            if need.get(sid, 0) < val:
                need[sid] = val
        out = []
        for sid, val in need.items():
            self.waited[eng][sid] = val
            out.append((self.semobj[sid], val))
        return out

    def _deps(self, reads, writes):
        deps = []
        for k in reads:
            if k in self.lastw:
                deps.append(self.lastw[k])
        for k in writes:
            if k in self.lastw:
                deps.append(self.lastw[k])
            deps.extend(self.readers.get(k, ()))
        return deps

    def _commit(self, tok, reads, writes):
        for k in reads:
            self.readers.setdefault(k, []).append(tok)
        for k in writes:
            self.lastw[k] = tok
            self.readers[k] = []

    def op(self, eng, fn, reads=(), writes=(), signal=True):
        waits = self._waits(eng, self._deps(reads, writes))
        n = self.cnt[eng]
        tok = (("c", eng, n % self.R), n // self.R + 1)
        if signal:
            self.cnt[eng] += 1
        sem = self.sem[eng][n % self.R]

        def run(e, fn=fn, waits=waits, signal=signal, sem=sem):
            for (s, v) in waits:
                e.wait_ge(s, v)
            ins = fn(e)
            if signal:
                ins.then_inc(sem, 1)

        self.q[eng].append(run)
        self._commit(tok, reads, writes)
        self.n_inst += 1

    def dma(self, eng, out, in_, reads=(), writes=(), **kw):
        n = self.dcnt[eng]
        self.dcnt[eng] += 1
        slot = n % self.NDMA
        val = 16 * (n // self.NDMA + 1)
        sid = ("d", eng, slot)
        deps = self._deps(reads, writes)
        if val > 16:
            deps.append((sid, val - 16))
        waits = self._waits(eng, deps)
        sem = self.dsem[eng][slot]

        def run(e, waits=waits, sem=sem, out=out, in_=in_, kw=kw):
            for (s, v) in waits:
                e.wait_ge(s, v)
            e.dma_start(out=out, in_=in_, **kw).then_inc(sem, 16)

        self.q[eng].append(run)
        self._commit((sid, val), reads, writes)
        self.n_inst += 1

    def barrier(self):
        deps = []
        for e in self.ENGS:
            n = self.cnt[e]
            for i in range(self.R):
                if n >= i + 1:
                    deps.append((("c", e, i), (n - 1 - i) // self.R + 1))
        for e in self.dsem:
            n = self.dcnt[e]
            for i in range(self.NDMA):
                if n >= i + 1:
                    deps.append((("d", e, i), 16 * ((n - 1 - i) // self.NDMA + 1)))
        for e in self.ENGS:
            waits = self._waits(e, deps)

            def run(en, waits=waits):
                for (s, v) in waits:
                    en.wait_ge(s, v)

            self.q[e].append(run)

    def finish(self, eng, keys):
        waits = self._waits(eng, self._deps(keys, ()))

        def run(e, waits=waits):
            for (s, v) in waits:
                e.wait_ge(s, v)

        self.q[eng].append(run)

    def emit(self, block):
        q = self.q

        @block.sync
        def _(e):
            for f in q["sync"]:
                f(e)

        @block.scalar
        def _(e):
            for f in q["scalar"]:
                f(e)

        @block.vector
        def _(e):
            for f in q["vector"]:
                f(e)

        @block.gpsimd
        def _(e):
            for f in q["gpsimd"]:
                f(e)

        @block.tensor
        def _(e):
            for f in q["tensor"]:
                f(e)


S = 4096
D = 1024
NT = 32
DFF = 2816
NFC = 22
NEG = -30000.0
L = 2


def build(debug=None, nlayers=L):
    from contextlib import ExitStack
    nc = bass.Bass("TRN2", target_bir_lowering=False)
    dt_in = lambda name, shape: nc.dram_tensor(name, shape, F32, kind="ExternalInput").ap()
    x_in = dt_in("x", [S, D])
    norm_mix_w = dt_in("norm_mix_w", [L, D])
    w_in = dt_in("w_in", [L, D, 2328])
    gmlp_norm_w = dt_in("gmlp_norm_w", [L, 512])
    gmlp_ws = dt_in("gmlp_ws", [L, 8, 128, 128])
    gmlp_bs = dt_in("gmlp_bs", [L, 8, 128])
    cmp_pos = {"k": dt_in("cmp_pos_k", [L, 32, 64]), "v": dt_in("cmp_pos_v", [L, 32, 64])}
    cmp_w1 = {"k": dt_in("cmp_k_w1", [L, 2048, 256]), "v": dt_in("cmp_v_w1", [L, 2048, 256])}
    cmp_w2 = {"k": dt_in("cmp_k_w2", [L, 256, 64]), "v": dt_in("cmp_v_w2", [L, 256, 64])}
    gate_b = dt_in("gate_b", [L, 24])
    out_norm_a_w = dt_in("out_norm_a_w", [L, 512])
    out_norm_b_w = dt_in("out_norm_b_w", [L, 512])
    w_o = dt_in("w_o", [L, D, D])
    norm_ffn_w = dt_in("norm_ffn_w", [L, D])
    w_gate = dt_in("w_gate", [L, D, DFF])
    w_up = dt_in("w_up", [L, D, DFF])
    w_down = dt_in("w_down", [L, DFF, D])
    final_norm_w = dt_in("final_norm_w", [D])
    out = nc.dram_tensor("out", [S, D], F32, kind="ExternalOutput").ap()
    dbg = debug is not None
    mix_d = nc.dram_tensor("mix_d", [S, D], BF16, kind="ExternalOutput" if dbg else "Internal").ap()
    xres_d = nc.dram_tensor("xres_d", [S, D], F32, kind="ExternalOutput" if dbg else "Internal").ap()

    with ExitStack() as st:
        P = Prog(nc, st)
        op = P.op

        sfx = [""]

        def T(name, shape, dt, stack=None):
            return (stack or st).enter_context(nc.sbuf_tensor(name + sfx[0], shape, dt))

        B = [st.enter_context(nc.psum_tensor("B%d" % i, [128, 512], F32)) for i in range(7)]
        BT = st.enter_context(nc.psum_tensor("BT", [128, 8, 128], BF16))

        ident_b = T("ident_b", [128, 128], BF16)
        ident_f = T("ident_f", [128, 128], F32)
        tri_f = T("tri_f", [128, 128], F32)
        tri_b = T("tri_b", [128, 128], BF16)
        ntri_b = T("ntri_b", [128, 128], BF16)
        ones_f = T("ones_f", [128, 128], F32)
        ind8 = T("ind8", [8, 512], F32)
        A_big = T("A_big", [16, 512], BF16)
        Bm4 = T("Bm4", [16, 4, 128], BF16)
        VM = T("VM", [128, 128], F32)
        AMk = T("AMk", [128, 128], F32)
        tmpc = T("tmpc", [128, 512], F32)
        tmpc2 = T("tmpc2", [128, 512], F32)

        def G(fn, reads=(), writes=()):
            op("gpsimd", fn, reads=reads, writes=writes)

        def asel(t, pattern, cmp_, fill, base, cm, key):
            G(lambda e: e.affine_select(out=t, in_=t, pattern=pattern, compare_op=cmp_, fill=fill,
                                        base=base, channel_multiplier=cm), reads=[key], writes=[key])

        G(lambda e: e.memset(ones_f[:], 1.0), writes=["ones_f"])
        G(lambda e: e.memset(ident_f[:], 1.0), writes=["ident_f"])
        asel(ident_f[:], [[-1, 128]], ALU.is_equal, 0.0, 0, 1, "ident_f")
        G(lambda e: e.tensor_copy(ident_b[:], ident_f[:]), reads=["ident_f"], writes=["ident_b"])
        G(lambda e: e.memset(tri_f[:], 1.0), writes=["tri_f"])
        asel(tri_f[:], [[1, 128]], ALU.is_ge, 0.0, 0, -1, "tri_f")
        G(lambda e: e.tensor_copy(tri_b[:], tri_f[:]), reads=["tri_f"], writes=["tri_b"])
        G(lambda e: e.memset(tmpc[:, 0:128], 1.0), writes=["tmpc"])
        asel(tmpc[:, 0:128], [[-1, 128]], ALU.is_gt, 0.0, 0, 1, "tmpc")
        G(lambda e: e.tensor_copy(ntri_b[:], tmpc[:, 0:128]), reads=["tmpc"], writes=["ntri_b"])
        G(lambda e: e.memset(ind8[:], 1.0), writes=["ind8"])
        asel(ind8[:].rearrange("p (h d) -> p h d", h=8), [[1, 8], [0, 64]], ALU.is_equal, 0.0, 0, -1, "ind8")
        G(lambda e: e.memset(tmpc[0:16, :], 1.0), writes=["tmpc"])
        asel(tmpc[0:16, :], [[1, 512]], ALU.is_equal, 0.0, -255, -1, "tmpc")
        G(lambda e: e.memset(tmpc2[0:16, :], 1.0), writes=["tmpc2"])
        asel(tmpc2[0:16, :], [[1, 512]], ALU.is_ge, 0.0, -263, 0, "tmpc2")
        asel(tmpc2[0:16, :], [[0, 512]], ALU.is_equal, 0.0, -8, 1, "tmpc2")
        asel(tmpc[0:16, :], [[0, 512]], ALU.is_ge, 0.0, 7, -1, "tmpc")
        G(lambda e: e.tensor_tensor(out=A_big[:], in0=tmpc[0:16, :], in1=tmpc2[0:16, :], op=ALU.add),
          reads=["tmpc", "tmpc2"], writes=["A_big"])
        G(lambda e: e.memset(tmpc[0:16, :], NEG), writes=["tmpc"])
        asel(tmpc[0:16, :].rearrange("p (g t) -> p g t", g=4), [[0, 4], [-1, 128]], ALU.is_gt, 0.0, 15, 16, "tmpc")
        asel(tmpc[0:16, :], [[0, 512]], ALU.is_ge, 0.0, 7, -1, "tmpc")
        G(lambda e: e.memset(tmpc2[0:16, :], NEG), writes=["tmpc2"])
        asel(tmpc2[0:16, :], [[0, 512]], ALU.is_equal, 0.0, -8, 1, "tmpc2")
        G(lambda e: e.tensor_tensor(out=Bm4[:].rearrange("p g t -> p (g t)"), in0=tmpc[0:16, :], in1=tmpc2[0:16, :], op=ALU.add),
          reads=["tmpc", "tmpc2"], writes=["Bm4"])
        G(lambda e: e.memset(VM[:], 1.0), writes=["VM"])
        asel(VM[:], [[-64, 128]], ALU.is_ge, 0.0, 64 * 64 - 128, 1, "VM")
        G(lambda e: e.memset(AMk[:], 10000.0), writes=["AMk"])
        asel(AMk[:], [[-64, 128]], ALU.is_ge, 0.0, 64 * 64, 1, "AMk")
        asel(AMk[:], [[64, 128]], ALU.is_ge, 0.0, -64 * 64 + 63, -1, "AMk")
        G(lambda e: e.memset(tmpc[:, 0:128], 10001.0), writes=["tmpc"])
        asel(tmpc[:, 0:128], [[-64, 128]], ALU.is_ge, 0.0, 64 * 64 - 64, 1, "tmpc")
        asel(tmpc[:, 0:128], [[64, 128]], ALU.is_ge, 0.0, -64 * 64 + 64 + 63, -1, "tmpc")
        G(lambda e: e.tensor_tensor(out=AMk[:], in0=AMk[:], in1=tmpc[:, 0:128], op=ALU.add), reads=["AMk", "tmpc"], writes=["AMk"])
        G(lambda e: e.memset(tmpc2[:, 0:128], -10000.0), writes=["tmpc2"])
        asel(tmpc2[:, 0:128], [[64, 128]], ALU.is_ge, 0.0, -64 * 64 - 1, -1, "tmpc2")
        G(lambda e: e.tensor_tensor(out=AMk[:], in0=AMk[:], in1=tmpc2[:, 0:128], op=ALU.add), reads=["AMk", "tmpc2"], writes=["AMk"])


        def rms_stats(src_ap, width, key_src, junk, ssum, rstd, tag):
            sc = float(width) ** -0.5
            op("scalar", lambda e: e.activation(out=junk, in_=src_ap, func=AF.Square, scale=sc, accum_out=ssum[:, 0:1]),
               reads=[key_src], writes=["junk" + tag, "ss" + tag])
            op("vector", lambda e: e.tensor_scalar(rstd[:, 0:1], ssum[:, 0:1], 1e-6, None, ALU.add), reads=["ss" + tag], writes=["rstd" + tag])
            op("scalar", lambda e: e.activation(out=rstd[:, 0:1], in_=rstd[:, 0:1], func=AF.Sqrt), reads=["rstd" + tag], writes=["rstd" + tag])
            op("vector", lambda e: e.reciprocal(rstd[:, 0:1], rstd[:, 0:1]), reads=["rstd" + tag], writes=["rstd" + tag])

        def layer(l):
            sfx[0] = "_L%d" % l
            xsrc = x_in if l == 0 else xres_d
            last = (l == nlayers - 1)
            P.barrier()
            with ExitStack() as s1:
                nmw = T("nmw", [128, D], F32, s1)
                gnw = T("gnw", [128, 512], F32, s1)
                onaw = T("onaw", [128, 512], F32, s1)
                onbw = T("onbw", [128, 512], F32, s1)
                gbt = T("gbt", [128, 24], F32, s1)
                bs8 = T("bs8", [8, 128], F32, s1)
                gates = T("gates", [128, NT, 24], F32, s1)
                P.dma("sync", nmw[:], norm_mix_w[l].partition_broadcast(128), writes=["nmw"])
                P.dma("sync", gnw[:], gmlp_norm_w[l].partition_broadcast(128), writes=["gnw"])
                P.dma("sync", onaw[:], out_norm_a_w[l].partition_broadcast(128), writes=["onaw"])
                P.dma("sync", onbw[:], out_norm_b_w[l].partition_broadcast(128), writes=["onbw"])
                P.dma("sync", gbt[:], gate_b[l].partition_broadcast(128), writes=["gbt"])
                P.dma("sync", bs8[:], gmlp_bs[l], writes=["bs8"])

                wtm = T("wtm", [128, 8, 1304], BF16, s1)
                wfm = T("wfm", [128, 8, 1024], BF16, s1)
                wsT = T("wsT", [128, 8, 128], BF16, s1)
                kTE = [T("kTE%d" % k, [128, S], BF16, s1) for k in range(2)]
                kTw = T("kTw", [128, S], BF16, s1)
                kcT = T("kcT", [128, S], BF16, s1)
                vcT = T("vcT", [128, S], BF16, s1)
                vs_aug = T("vs_aug", [128, NT, 2, 65], BF16, s1)
                vw_aug = T("vw_aug", [128, NT, 2, 65], BF16, s1)
                qT_all = T("qT_all", [128, 4, S], BF16, s1)
                w2k_pad = T("w2k_pad", [128, 2, 2, 128], BF16, s1)
                w2v = T("w2v", [128, 2, 64], BF16, s1)
                hidT = T("hidT", [128, 2, 256], BF16, s1)
                kcmp = T("kcmp", [128, 256], BF16, s1)
                R_cmp = T("R_cmp", [128, 2, 2, 129], BF16, s1)

                with ExitStack() as s0:
                    stage = [T("stage%d" % i, [128, 2328], F32, s0) for i in range(2)]
                    for c in range(8):
                        sg = stage[c % 2]
                        sk = "stage%d" % (c % 2)
                        P.dma("sync", sg[:], w_in[l, c * 128:(c + 1) * 128, :], writes=[sk])
                        cp = [
                            (wtm[:, c, 0:1024], sg[:, 0:1024]),
                            (wtm[:, c, 1024:1152], sg[:, 1920:2048]),
                            (wtm[:, c, 1152:1280], sg[:, 2176:2304]),
                            (wtm[:, c, 1280:1304], sg[:, 2304:2328]),
                            (wfm[:, c, 0:512].rearrange("p (g k d) -> p g k d", g=4, k=2),
                             sg[:, 1024:1536].rearrange("p (k g d) -> p g k d", k=2, g=4)),
                            (wfm[:, c, 512:640], sg[:, 1536:1664]),
                            (wfm[:, c, 640:768], sg[:, 1664:1792]),
                            (wfm[:, c, 768:896], sg[:, 1792:1920]),
                            (wfm[:, c, 896:1024], sg[:, 2048:2176]),
                        ]
                        for ci, (o_, i_) in enumerate(cp):
                            if ci % 2 == 0:
                                op("scalar", lambda e, o_=o_, i_=i_: e.copy(o_, i_), reads=[sk], writes=["wtm" if ci < 4 else "wfm"])
                            else:
                                op("vector", lambda e, o_=o_, i_=i_: e.tensor_copy(o_, i_), reads=[sk], writes=["wtm" if ci < 4 else "wfm"])
                    wsf = T("wsf", [128, 8, 128], F32, s0)
                    P.dma("sync", wsf[:], gmlp_ws[l].rearrange("h t s -> t h s"), writes=["wsf"])
                    for h in range(8):
                        bk = B[h % 2]
                        op("tensor", lambda e, h=h, bk=bk: e.transpose(bk[:, 0:128], wsf[:, h, :], ident_f[:]),
                           reads=["wsf", "ident_f"], writes=["B%d" % (h % 2)])
                        op("vector", lambda e, h=h, bk=bk: e.tensor_tensor(out=wsT[:, h, :], in0=bk[:, 0:128], in1=tri_f[:], op=ALU.mult),
                           reads=["B%d" % (h % 2), "tri_f"], writes=["wsT"])
                    for nt_ in range(2):
                        G(lambda e: e.memset(tmpc[:, 0:64], 1.0), writes=["tmpc"])
                        asel(tmpc[:, 0:64], [[-64, 64]], ALU.is_ge, 0.0, 2048 * nt_ + 31, 16, "tmpc")
                        asel(tmpc[:, 0:64], [[64, 64]], ALU.is_ge, 0.0, 63 - 2048 * nt_, -16, "tmpc")
                        for k in range(2):
                            G(lambda e, nt_=nt_, k=k: e.tensor_copy(R_cmp[:, nt_, k, 64:128], tmpc[:, 0:64]), reads=["tmpc"], writes=["R_cmp"])
                    G(lambda e: e.memset(R_cmp[:, :, :, 128:129], 1.0), writes=["R_cmp"])
                    G(lambda e: e.memset(vs_aug[:, :, :, 64:65], 1.0), writes=["vs_aug"])
                    G(lambda e: e.memset(vw_aug[:, :, :, 64:65], 1.0), writes=["vw_aug"])
                    G(lambda e: e.memset(kTE[0][:], 1.0), writes=["kTE0"])
                    asel(kTE[0][:].rearrange("p (j r) -> p j r", j=64), [[1, 64], [0, 64]], ALU.is_equal, 0.0, 64, -1, "kTE0")
                    G(lambda e: e.memset(kTE[1][:], 1.0), writes=["kTE1"])
                    asel(kTE[1][:].rearrange("p (j r) -> p j r", j=64), [[1, 64], [0, 64]], ALU.is_equal, 0.0, 0, -1, "kTE1")
                    P.barrier()

                with ExitStack() as s2:
                    xt = [T("xt%d" % i, [128, D], F32, s2) for i in range(2)]
                    junk = T("junk", [128, D], BF16, s2)
                    ssx = T("ssx", [128, 1], F32, s2)
                    rsx = T("rsx", [128, 1], F32, s2)
                    hb = T("hb", [128, D], BF16, s2)
                    hT = [T("hT%d" % i, [128, 8, 512], BF16, s2) for i in range(2)]
                    gu = T("gu", [128, 512], F32, s2)
                    gv = T("gv", [128, 512], F32, s2)
                    ssv = T("ssv", [128, 1], F32, s2)
                    rsv = T("rsv", [128, 1], F32, s2)
                    vn = T("vn", [128, 512], BF16, s2)
                    a_t = T("a_t", [128, 512], F32, s2)
                    ssa = T("ssa", [128, 1], F32, s2)
                    rsa = T("rsa", [128, 1], F32, s2)
                    mixA = [T("mixA%d" % i, [128, 512], BF16, s2) for i in range(2)]
                    gpre = T("gpre", [128, 24], F32, s2)
                    for TT in range(8):
                        hTt = hT[TT % 2]
                        hk = "hT%d" % (TT % 2)
                        for tt in range(4):
                            i = TT * 4 + tt
                            xti = xt[i % 2]
                            xk = "xt%d" % (i % 2)
                            P.dma("sync", xti[:], xsrc[i * 128:(i + 1) * 128, :], writes=[xk])
                            rms_stats(xti[:], D, xk, junk[:], ssx, rsx, "x")
                            op("vector", lambda e, xti=xti: e.scalar_tensor_tensor(out=hb[:], in0=xti[:], scalar=rsx[:, 0:1], in1=nmw[:], op0=ALU.mult, op1=ALU.mult),
                               reads=[xk, "rstdx", "nmw"], writes=["hb"])
                            for c in range(8):
                                op("tensor", lambda e, c=c: e.transpose(BT[:, c, :], hb[:, c * 128:(c + 1) * 128], ident_b[:]),
                                   reads=["hb", "ident_b"], writes=["BT"], signal=(c == 7))
                            op("vector", lambda e, hTt=hTt, tt=tt: e.tensor_copy(hTt[:, :, tt * 128:(tt + 1) * 128], BT[:]), reads=["BT"], writes=[hk])
                            for cb, (c0, c1) in enumerate(((0, 512), (512, 1024), (1024, 1304))):
                                for c in range(8):
                                    op("tensor", lambda e, cb=cb, c=c, c0=c0, c1=c1, hTt=hTt, tt=tt: e.matmul(B[cb][:, 0:c1 - c0], lhsT=hTt[:, c, tt * 128:(tt + 1) * 128], rhs=wtm[:, c, c0:c1],
                                                                                                      start=(c == 0), stop=(c == 7)),
                                       reads=[hk, "wtm"], writes=["B%d" % cb], signal=(c == 7))
                            op("scalar", lambda e: e.activation(out=gu[:], in_=B[0][:], func=AF.Gelu_apprx_tanh), reads=["B0"], writes=["gu"])
                            op("scalar", lambda e: e.activation(out=gv[:], in_=B[1][:], func=AF.Gelu_apprx_tanh), reads=["B1"], writes=["gv"])
                            rms_stats(gv[:], 512, "gv", junk[:, 0:512], ssv, rsv, "v")
                            op("vector", lambda e: e.scalar_tensor_tensor(out=vn[:], in0=gv[:], scalar=rsv[:, 0:1], in1=gnw[:], op0=ALU.mult, op1=ALU.mult),
                               reads=["gv", "rstdv", "gnw"], writes=["vn"])
                            op("vector", lambda e, i=i: e.tensor_copy(vs_aug[:, i, :, 0:64], B[2][:, 0:128].rearrange("p (k d) -> p k d", k=2)), reads=["B2"], writes=["vs_aug"])
                            op("vector", lambda e, i=i: e.tensor_copy(vw_aug[:, i, :, 0:64], B[2][:, 128:256].rearrange("p (k d) -> p k d", k=2)), reads=["B2"], writes=["vw_aug"])
                            op("vector", lambda e: e.tensor_tensor(out=gpre[:], in0=B[2][:, 256:280], in1=gbt[:], op=ALU.add), reads=["B2", "gbt"], writes=["gpre"])
                            op("scalar", lambda e, i=i: e.activation(out=gates[:, i, :], in_=gpre[:], func=AF.Sigmoid), reads=["gpre"], writes=["gates"])
                            op("tensor", lambda e: e.matmul(B[3][:], lhsT=bs8[:], rhs=ind8[:], start=True, stop=False), reads=["bs8", "ind8"], writes=["B3"], signal=False)
                            for h in range(8):
                                op("tensor", lambda e, h=h: e.matmul(B[3][:, h * 64:(h + 1) * 64], lhsT=wsT[:, h, :], rhs=vn[:, h * 64:(h + 1) * 64], start=False, stop=(h == 7)),
                                   reads=["wsT", "vn"], writes=["B3"], signal=(h == 7))
                            op("vector", lambda e: e.tensor_tensor(out=a_t[:], in0=B[3][:], in1=gu[:], op=ALU.mult), reads=["B3", "gu"], writes=["a_t"])
                            rms_stats(a_t[:], 512, "a_t", junk[:, 512:1024], ssa, rsa, "a")
                            mA = mixA[i % 2]
                            mk = "mixA%d" % (i % 2)
                            op("vector", lambda e, mA=mA: e.scalar_tensor_tensor(out=mA[:], in0=a_t[:], scalar=rsa[:, 0:1], in1=onaw[:], op0=ALU.mult, op1=ALU.mult),
                               reads=["a_t", "rstda", "onaw"], writes=[mk])
                            P.dma("gpsimd", mix_d[i * 128:(i + 1) * 128, 0:512], mA[:], reads=[mk], writes=["mix_d"])
                        tok = slice(TT * 512, (TT + 1) * 512)
                        for ch in range(8):
                            bk = B[4 + ch % 2]
                            bkk = "B%d" % (4 + ch % 2)
                            for c in range(8):
                                op("tensor", lambda e, bk=bk, ch=ch, c=c, hTt=hTt: e.matmul(bk[:], lhsT=wfm[:, c, ch * 128:(ch + 1) * 128], rhs=hTt[:, c, :], start=(c == 0), stop=(c == 7)),
                                   reads=[hk, "wfm"], writes=[bkk], signal=(c == 7))
                            if ch < 4:
                                op("scalar", lambda e, bk=bk, ch=ch, tok=tok: e.activation(out=qT_all[:, ch, tok], in_=bk[:], func=AF.Copy, scale=0.125), reads=[bkk], writes=["qT_all"])
                            elif ch == 4:
                                op("vector", lambda e, bk=bk, tok=tok: e.tensor_copy(kcT[:, tok], bk[:]), reads=[bkk], writes=["kcT"])
                            elif ch == 5:
                                op("vector", lambda e, bk=bk, tok=tok: e.tensor_copy(vcT[:, tok], bk[:]), reads=[bkk], writes=["vcT"])
                            elif ch == 6:
                                op("vector", lambda e, bk=bk, tok=tok: e.tensor_copy(kTE[0][0:64, tok], bk[0:64, :]), reads=[bkk], writes=["kTE0"])
                                op("vector", lambda e, bk=bk, tok=tok: e.tensor_copy(kTE[1][64:128, tok], bk[64:128, :]), reads=[bkk], writes=["kTE1"])
                            else:
                                op("vector", lambda e, bk=bk, tok=tok: e.tensor_copy(kTw[:, tok], bk[:]), reads=[bkk], writes=["kTw"])
                    P.barrier()

                with ExitStack() as s25:
                    w1A = {kv: T("w1A" + kv, [128, 32, 256], BF16, s25) for kv in "kv"}
                    posT = {kv: T("posT" + kv, [128, 32], BF16, s25) for kv in "kv"}
                    bcol = {kv: T("bcol" + kv, [128, 2], F32, s25) for kv in "kv"}
                    w1f = T("w1f", [128, 8, 256], F32, s25)
                    posf = T("posf", [32, 64], F32, s25)
                    w2f = T("w2f", [128, 2, 64], F32, s25)
                    G(lambda e: e.memset(w2k_pad[:], 0.0), writes=["w2k_pad"])
                    for kv in "kv":
                        src = cmp_w1[kv][l].rearrange("(l d) n -> d l n", d=64)
                        for q4 in range(4):
                            P.dma("sync", w1f[0:64], src[:, q4 * 8:(q4 + 1) * 8, :], writes=["w1f"])
                            P.dma("sync", w1f[64:128], src[:, q4 * 8:(q4 + 1) * 8, :], writes=["w1f"])
                            if q4 % 2 == 0:
                                op("scalar", lambda e, kv=kv, q4=q4: e.copy(w1A[kv][:, q4 * 8:(q4 + 1) * 8, :], w1f[:]), reads=["w1f"], writes=["w1A" + kv])
                            else:
                                op("vector", lambda e, kv=kv, q4=q4: e.tensor_copy(w1A[kv][:, q4 * 8:(q4 + 1) * 8, :], w1f[:]), reads=["w1f"], writes=["w1A" + kv])
                        P.dma("sync", posf[:], cmp_pos[kv][l], writes=["posf"])
                        op("tensor", lambda e: e.transpose(B[2][0:64, 0:32], posf[:, :], ident_f[0:32, 0:32]), reads=["posf", "ident_f"], writes=["B2"])
                        op("vector", lambda e, kv=kv: e.tensor_copy(posT[kv][0:64, :], B[2][0:64, 0:32]), reads=["B2"], writes=["posT" + kv])
                        for hc in range(2):
                            for ll in range(32):
                                op("tensor", lambda e, kv=kv, hc=hc, ll=ll: e.matmul(B[3][:, hc:hc + 1], lhsT=w1A[kv][0:64, ll, hc * 128:(hc + 1) * 128],
                                                                                 rhs=posT[kv][0:64, ll:ll + 1], start=(ll == 0), stop=(ll == 31)),
                                   reads=["w1A" + kv, "posT" + kv], writes=["B3"], signal=(ll == 31))
                        op("vector", lambda e, kv=kv: e.tensor_copy(bcol[kv][:], B[3][:, 0:2]), reads=["B3"], writes=["bcol" + kv])
                        P.dma("sync", w2f[:], cmp_w2[kv][l].rearrange("(c p) d -> p c d", p=128), writes=["w2f"])
                        if kv == "k":
                            for k in range(2):
                                G(lambda e, k=k: e.tensor_copy(w2k_pad[:, :, k, 64 * k:64 * k + 64], w2f[:]), reads=["w2f"], writes=["w2k_pad"])
                        else:
                            G(lambda e: e.tensor_copy(w2v[:], w2f[:]), reads=["w2f"], writes=["w2v"])
                    for kv, srcT in (("k", kcT), ("v", vcT)):
                        for k in range(2):
                            pr = slice(64 * k, 64 * k + 64)
                            for hc in range(2):
                                bk = B[hc]
                                for ll in range(32):
                                    op("tensor", lambda e, kv=kv, hc=hc, ll=ll, bk=bk, pr=pr, srcT=srcT: e.matmul(bk[:, 0:255], lhsT=w1A[kv][pr, ll, hc * 128:(hc + 1) * 128],
                                                                                                       rhs=srcT[pr, ll:ll + 16 * 254 + 1:16], start=(ll == 0), stop=(ll == 31)),
                                       reads=["w1A" + kv, "kcT", "vcT"], writes=["B%d" % hc], signal=(ll == 31))
                                op("scalar", lambda e, kv=kv, hc=hc, bk=bk: e.activation(out=hidT[:, hc, 0:255], in_=bk[:, 0:255], func=AF.Gelu_apprx_tanh, bias=bcol[kv][:, hc:hc + 1]),
                                   reads=["B%d" % hc, "bcol" + kv], writes=["hidT"])
                            if kv == "k":
                                for hc in range(2):
                                    op("tensor", lambda e, hc=hc, k=k: e.matmul(B[2][:, 0:255], lhsT=w2k_pad[:, hc, k, :], rhs=hidT[:, hc, 0:255], start=(hc == 0), stop=(hc == 1)),
                                       reads=["w2k_pad", "hidT"], writes=["B2"], signal=(hc == 1))
                                op("vector", lambda e, pr=pr: e.tensor_copy(kcmp[pr, 0:255], B[2][pr, 0:255]), reads=["B2"], writes=["kcmp"])
                            else:
                                for nt_, M in ((0, 128), (1, 127)):
                                    for hc in range(2):
                                        op("tensor", lambda e, hc=hc, nt_=nt_, M=M: e.matmul(B[3][0:M, 0:64], lhsT=hidT[:, hc, nt_ * 128:nt_ * 128 + M], rhs=w2v[:, hc, :], start=(hc == 0), stop=(hc == 1)),
                                           reads=["w2v", "hidT"], writes=["B3"], signal=(hc == 1))
                                    op("vector", lambda e, nt_=nt_, M=M, k=k: e.tensor_copy(R_cmp[0:M, nt_, k, 0:64], B[3][0:M, 0:64]), reads=["B3"], writes=["R_cmp"])
                    P.barrier()

                with ExitStack() as s3:
                    qB = [T("qB%d" % k, [128, 4, 128], BF16, s3) for k in range(2)]
                    PTc = T("PTc", [128, 2, 512], BF16, s3)
                    PT = [T("PT%d" % i, [128, 512], BF16, s3) for i in range(4)]
                    oc2 = [T("oc%d" % j, [128, 4, 129], F32, s3) for j in range(2)]
                    rc2 = [T("rc%d" % j, [128, 4], F32, s3) for j in range(2)]
                    imp = T("imp", [128, 64], F32, s3)
                    score = T("score", [128, 64], F32, s3)
                    sc2 = T("sc2", [128, 64], F32, s3)
                    m8 = T("m8", [128, 16], F32, s3)
                    biasP = [T("biasP%d" % k, [128, 128], BF16, s3) for k in range(2)]
                    oT_sb = T("oT_sb", [128, 2, 512], F32, s3)
                    rr = T("rr", [128, 2, 4], F32, s3)
                    rg = T("rg", [128, 3, 4], F32, s3)
                    bfull2 = [T("bfull%d" % j, [128, 512], F32, s3) for j in range(2)]
                    btmp2 = [T("btmp%d" % j, [128, 4, 64], F32, s3) for j in range(2)]
                    ssb = T("ssb", [128, 1], F32, s3)
                    rsb = T("rsb", [128, 1], F32, s3)
                    junkb = T("junkb", [128, 512], F32, s3)
                    mixB = [T("mixB%d" % i, [128, 512], BF16, s3) for i in range(2)]
                    for k in range(2):
                        G(lambda e, k=k: e.memset(qB[k][:], 0.0), writes=["qB%d" % k])
                        G(lambda e, k=k: e.memset(biasP[k][:], 0.0), writes=["biasP%d" % k])
                    psC = [B[6][:, 260:389], B[5][:, 0:129], B[5][:, 129:258], B[5][:, 258:387]]
                    psCk = ["B6", "B5", "B5", "B5"]
                    psB = BT[:, 0, :]
                    psT = B[6][:, 0:260].rearrange("p (g d) -> p g d", g=4)
                    sidx = [0]
                    pidx = [0]

                    def stageA(i, k, n):
                        pr = slice(64 * k, 64 * k + 64)
                        br_ = slice(64, 128) if k == 0 else slice(0, 64)
                        qk = "qB%d" % k
                        ocn, rcn = oc2[n % 2], rc2[n % 2]
                        ock, rck = "oc%d" % (n % 2), "rc%d" % (n % 2)
                        G(lambda e: e.tensor_copy(qB[k][pr, :, :], qT_all[pr, :, i * 128:(i + 1) * 128]), reads=["qT_all"], writes=[qk])
                        n_tiles = 1 if i <= 15 else 2
                        for nt_ in range(n_tiles):
                            M = 128 if nt_ == 0 else 127
                            bs_ = B[5]
                            bsk = "B5"
                            a0 = 256 - 8 * i + nt_ * 128
                            op("tensor", lambda e, bs_=bs_, M=M, nt_=nt_: e.matmul(bs_[0:M, :], lhsT=kcmp[pr, nt_ * 128:nt_ * 128 + M], rhs=qT_all[pr, :, i * 128:(i + 1) * 128], start=True, stop=False),
                               reads=["kcmp", "qT_all"], writes=[bsk], signal=False)
                            op("tensor", lambda e, bs_=bs_, M=M, a0=a0: e.matmul(bs_[0:M, :], lhsT=A_big[0:9, a0:a0 + M], rhs=Bm4[0:9, :, :], start=False, stop=True),
                               reads=["A_big", "Bm4"], writes=[bsk])
                            yield
                            yield
                            op("scalar", lambda e, bs_=bs_, M=M, nt_=nt_: e.activation(out=PTc[0:M, nt_, :], in_=bs_[0:M, :], func=AF.Exp), reads=[bsk], writes=["PTc"])
                            yield
                            yield
                        for g in range(4):
                            for nt_ in range(n_tiles):
                                M = 128 if nt_ == 0 else 127
                                op("tensor", lambda e, g=g, nt_=nt_, M=M: e.matmul(psC[g], lhsT=PTc[0:M, nt_, g * 128:(g + 1) * 128], rhs=R_cmp[0:M, nt_, k, :],
                                                                                start=(nt_ == 0), stop=(nt_ == n_tiles - 1)),
                                   reads=["PTc", "R_cmp"], writes=[psCk[g]], signal=(nt_ == n_tiles - 1))
                        yield
                        yield
                        for g in range(4):
                            op("vector", lambda e, g=g: e.tensor_copy(ocn[:, g, :], psC[g]), reads=[psCk[g]], writes=[ock])
                        op("vector", lambda e: e.tensor_scalar(rcn[:], ocn[:, :, 128], 1e-30, None, ALU.max), reads=[ock], writes=[rck])
                        op("vector", lambda e: e.reciprocal(rcn[:], rcn[:]), reads=[rck], writes=[rck])
                        yield
                        if i >= 8:
                            op("vector", lambda e: e.tensor_scalar(imp[:], ocn[:, 0, 64:128], rcn[:, 0:1], None, ALU.mult), reads=[ock, rck], writes=["imp"])
                            for g in range(1, 4):
                                op("vector", lambda e, g=g: e.scalar_tensor_tensor(out=imp[:], in0=ocn[:, g, 64:128], scalar=rcn[:, g:g + 1], in1=imp[:], op0=ALU.mult, op1=ALU.add),
                                   reads=[ock, rck, "imp"], writes=["imp"])
                            c0 = 64 - 2 * i
                            op("vector", lambda e: e.tensor_tensor(out=score[:], in0=imp[:], in1=VM[:, c0:c0 + 64], op=ALU.mult), reads=["imp", "VM"], writes=["score"])
                            op("vector", lambda e: e.tensor_tensor(out=score[:], in0=score[:], in1=AMk[:, c0:c0 + 64], op=ALU.add), reads=["score", "AMk"], writes=["score"])
                            op("vector", lambda e: e.memset(score[:, 0:1], 10002.0), reads=["score"], writes=["score"])
                            yield
                            op("vector", lambda e: e.max(out=m8[:, 0:8], in_=score[:]), reads=["score"], writes=["m8"])
                            op("vector", lambda e: e.match_replace(out=sc2[:], in_to_replace=m8[:, 0:8], in_values=score[:], imm_value=NEG), reads=["score", "m8"], writes=["sc2"])
                            op("vector", lambda e: e.max(out=m8[:, 8:16], in_=sc2[:]), reads=["sc2"], writes=["m8"])
                            bo = 64 if k == 0 else 0
                            op("vector", lambda e: e.tensor_scalar(biasP[k][:, bo:bo + 64], score[:], m8[:, 15:16], NEG, ALU.is_lt, ALU.mult),
                               reads=["score", "m8"], writes=["biasP%d" % k])
                            yield
                            yield
                            yield
                            yield
                            op("tensor", lambda e: e.transpose(psB, biasP[k][:], ident_b[:]), reads=["biasP%d" % k, "ident_b"], writes=["BT"])
                            yield
                            yield
                            op("vector", lambda e: e.tensor_copy(qB[k][br_, :, :], BT[br_, 0, :].unsqueeze(1).to_broadcast([64, 4, 128])),
                               reads=["BT"], writes=[qk])
                            yield

                    def adv(gen, cnt):
                        for _ in range(cnt):
                            try:
                                next(gen)
                            except StopIteration:
                                return

                    def makeB(i, k, n):
                        pr = slice(64 * k, 64 * k + 64)
                        qk = "qB%d" % k
                        ocn, rcn = oc2[n % 2], rc2[n % 2]
                        ock, rck = "oc%d" % (n % 2), "rc%d" % (n % 2)
                        bfl = bfull2[i % 2]
                        bfk = "bfull%d" % (i % 2)
                        cfg = ((kTE[k], vs_aug, list(range(0, i + 1))), (kTw, vw_aug, list(range(max(0, i - 4), i + 1))))
                        stp = [(bi, ji, jt, len(cfg[bi][2])) for bi in range(2) for ji, jt in enumerate(cfg[bi][2])]
                        banks = {}

                        def emitS(idx):
                            bi, ji, jt, nj = stp[idx]
                            cache = cfg[bi][0]
                            ck = ("kTE%d" % k) if bi == 0 else "kTw"
                            bnum = (0, 1, 4)[sidx[0] % 3]
                            bs_ = B[bnum]
                            bsk = "B%d" % bnum
                            sidx[0] += 1
                            banks[idx] = (bs_, bsk)
                            if bi == 0:
                                op("tensor", lambda e: e.matmul(bs_[:], lhsT=cache[:, jt * 128:(jt + 1) * 128], rhs=qB[k][:, :, :], start=True, stop=True),
                                   reads=[ck, qk], writes=[bsk])
                            else:
                                op("tensor", lambda e: e.matmul(bs_[:], lhsT=cache[pr, jt * 128:(jt + 1) * 128], rhs=qT_all[pr, :, i * 128:(i + 1) * 128], start=True, stop=True),
                                   reads=[ck, "qT_all"], writes=[bsk])

                        def emitPV(idx):
                            bi, ji, jt, nj = stp[idx]
                            vaug = cfg[bi][1]
                            pso = B[2 + bi]
                            psok = "B%d" % (2 + bi)
                            bs_, bsk = banks[idx]
                            pt = PT[pidx[0] % 4]
                            ptk = "PT%d" % (pidx[0] % 4)
                            pidx[0] += 1
                            op("scalar", lambda e: e.activation(out=pt[:], in_=bs_[:], func=AF.Exp), reads=[bsk], writes=[ptk])
                            if jt == i:
                                G(lambda e: e.tensor_tensor(out=pt[:].rearrange("p (g t) -> p g t", g=4), in0=pt[:].rearrange("p (g t) -> p g t", g=4),
                                                            in1=tri_b[:, :].unsqueeze(1).to_broadcast([128, 4, 128]), op=ALU.mult), reads=[ptk, "tri_b"], writes=[ptk])
                            if bi == 1 and jt == i - 4:
                                G(lambda e: e.tensor_tensor(out=pt[:].rearrange("p (g t) -> p g t", g=4), in0=pt[:].rearrange("p (g t) -> p g t", g=4),
                                                            in1=ntri_b[:, :].unsqueeze(1).to_broadcast([128, 4, 128]), op=ALU.mult), reads=[ptk, "ntri_b"], writes=[ptk])
                            op("tensor", lambda e: e.matmul(pso[0:65, :], lhsT=vaug[:, jt, k, :], rhs=pt[:], start=(ji == 0), stop=(ji == nj - 1)),
                               reads=[ptk, "vs_aug", "vw_aug"], writes=[psok], signal=(ji == nj - 1))
                            if ji == nj - 1:
                                op("vector", lambda e: e.tensor_copy(oT_sb[0:65, bi, :], pso[0:65, :]), reads=[psok], writes=["oT_sb%d" % bi])

                        def begin():
                            emitS(0)
                            if len(stp) > 1:
                                emitS(1)

                        def loop(gnext, fprev, f0):
                            nst = len(stp)
                            fidx = min(2, nst - 1)
                            f0idx = min(i + 2, nst - 1)
                            for idx in range(nst):
                                if idx == fidx:
                                    fprev()
                                if idx + 2 < nst:
                                    emitS(idx + 2)
                                emitPV(idx)
                                if idx == f0idx:
                                    f0()
                                adv(gnext, 1)
                            adv(gnext, 1000)

                        return begin, loop

                    def makeFin(i, k, n):
                        ocn, rcn = oc2[n % 2], rc2[n % 2]
                        ock, rck = "oc%d" % (n % 2), "rc%d" % (n % 2)
                        bfl = bfull2[i % 2]
                        bfk = "bfull%d" % (i % 2)
                        gsl = lambda br: gates[:, i, br * 8 + k * 4: br * 8 + k * 4 + 4]

                        def branch(bi):
                            for g in range(4):
                                op("tensor", lambda e, g=g: e.transpose(psT[:, g, :], oT_sb[0:65, bi, g * 128:(g + 1) * 128], ident_f[0:65, 0:65]),
                                   reads=["oT_sb%d" % bi, "ident_f"], writes=["B6"], signal=(g == 3))
                            op("vector", lambda e: e.tensor_scalar(rr[:, bi, :], psT[:, :, 64], 1e-30, None, ALU.max), reads=["B6"], writes=["rr"])
                            op("vector", lambda e: e.reciprocal(rr[:, bi, :], rr[:, bi, :]), reads=["rr"], writes=["rr"])
                            op("vector", lambda e: e.tensor_tensor(out=rg[:, 1 + bi, :], in0=rr[:, bi, :], in1=gsl(1 + bi), op=ALU.mult), reads=["rr", "gates"], writes=["rg"])
                            for g in range(4):
                                cs = slice(k * 256 + g * 64, k * 256 + g * 64 + 64)
                                op("vector", lambda e, g=g, cs=cs: e.scalar_tensor_tensor(out=bfl[:, cs], in0=psT[:, g, 0:64], scalar=rg[:, 1 + bi, g:g + 1], in1=bfl[:, cs], op0=ALU.mult, op1=ALU.add),
                                   reads=["B6", "rg", bfk], writes=[bfk])

                        def f0():
                            op("vector", lambda e: e.tensor_tensor(out=rg[:, 0, :], in0=rcn[:], in1=gsl(0), op=ALU.mult), reads=[rck, "gates"], writes=["rg"])
                            op("vector", lambda e: e.tensor_tensor(out=bfl[:, k * 256:(k + 1) * 256].rearrange("p (g d) -> p g d", g=4), in0=ocn[:, :, 0:64],
                                                                   in1=rg[:, 0, :].unsqueeze(2).to_broadcast([128, 4, 64]), op=ALU.mult), reads=[ock, "rg"], writes=[bfk])
                            branch(0)

                        def f1():
                            branch(1)
                            if k == 1:
                                rms_stats(bfl[:], 512, bfk, junkb[:], ssb, rsb, "b")
                                mB = mixB[i % 2]
                                mk = "mixB%d" % (i % 2)
                                op("vector", lambda e: e.scalar_tensor_tensor(out=mB[:], in0=bfl[:], scalar=rsb[:, 0:1], in1=onbw[:], op0=ALU.mult, op1=ALU.mult),
                                   reads=[bfk, "rstdb", "onbw"], writes=[mk])
                                P.dma("gpsimd", mix_d[i * 128:(i + 1) * 128, 512:1024], mB[:], reads=[mk], writes=["mix_d"])

                        return f0, f1

                    steps = [(i, k) for i in range(NT) for k in range(2)]
                    adv(stageA(steps[0][0], steps[0][1], 0), 1000)
                    Bcur = makeB(steps[0][0], steps[0][1], 0)
                    Bcur[0]()
                    fprev = lambda: None
                    for n, (i, k) in enumerate(steps):
                        gnext = stageA(steps[n + 1][0], steps[n + 1][1], n + 1) if n + 1 < len(steps) else iter(())
                        f0, f1 = makeFin(i, k, n)
                        Bcur[1](gnext, fprev, f0)
                        if n + 1 < len(steps):
                            Bcur = makeB(steps[n + 1][0], steps[n + 1][1], n + 1)
                            Bcur[0]()
                        fprev = f1
                    fprev()
                    P.barrier()
            if debug == "p3":
                return
            with ExitStack() as s4:
                wo_b = T("wo_b", [128, 8, D], BF16, s4)
                wg_b = T("wg_b", [128, 8, DFF], BF16, s4)
                wu_b = T("wu_b", [128, 8, DFF], BF16, s4)
                wd_b = T("wd_b", [128, NFC, D], BF16, s4)
                nfw = T("nfw", [128, D], F32, s4)
                fnw = T("fnw", [128, D], F32, s4)
                P.dma("sync", nfw[:], norm_ffn_w[l].partition_broadcast(128), writes=["nfw"])
                P.dma("sync", fnw[:], final_norm_w.partition_broadcast(128), writes=["fnw"])
                s4a = ExitStack()
                stg = [T("stg%d" % i, [128, DFF], F32, s4a) for i in range(3)]
                si = 0
                for (wsrc, wdst, nck, wid, key) in ((w_o, wo_b, 8, D, "wo_b"), (w_gate, wg_b, 8, DFF, "wg_b"), (w_up, wu_b, 8, DFF, "wu_b"), (w_down, wd_b, NFC, D, "wd_b")):
                    for c in range(nck):
                        sg = stg[si % 3]
                        sk = "stg%d" % (si % 3)
                        P.dma("sync" if si % 2 == 0 else "gpsimd", sg[:, 0:wid], wsrc[l, c * 128:(c + 1) * 128, :], writes=[sk])
                        if si % 2 == 0:
                            op("scalar", lambda e, wdst=wdst, c=c, sg=sg, wid=wid: e.copy(wdst[:, c, :], sg[:, 0:wid]), reads=[sk], writes=[key])
                        else:
                            op("vector", lambda e, wdst=wdst, c=c, sg=sg, wid=wid: e.tensor_copy(wdst[:, c, :], sg[:, 0:wid]), reads=[sk], writes=[key])
                        si += 1
                P.barrier()
                s4a.close()
                mixt = [T("mixt%d" % i, [128, D], BF16, s4) for i in range(2)]
                mixT = T("mixT", [128, 8, 128], BF16, s4)
                x1 = T("x1", [128, 2, D], F32, s4)
                junk4 = T("junk4", [128, D], BF16, s4)
                ss4 = T("ss4", [128, 1], F32, s4)
                rs4 = T("rs4", [128, 1], F32, s4)
                h2 = T("h2", [128, D], BF16, s4)
                h2T = T("h2T", [128, 8, 256], BF16, s4)
                sgt = [T("sgt%d" % i, [128, 256], F32, s4) for i in range(2)]
                actT = T("actT", [128, NFC, 256], BF16, s4)
                x2 = [T("x2_%d" % i, [128, D], F32, s4) for i in range(1)]
                ss5 = T("ss5", [128, 1], F32, s4)
                rs5 = T("rs5", [128, 1], F32, s4)
                for TT in range(16):
                    for tt in range(2):
                        i = TT * 2 + tt
                        mt = mixt[i % 2]
                        mtk = "mixt%d" % (i % 2)
                        P.dma("sync", mt[:], mix_d[i * 128:(i + 1) * 128, :], reads=["mix_d"], writes=[mtk])
                        P.dma("scalar", x1[:, tt, :], xsrc[i * 128:(i + 1) * 128, :], reads=["xres_d"], writes=["x1"])
                        for c in range(8):
                            op("tensor", lambda e, c=c, mt=mt: e.transpose(BT[:, c, :], mt[:, c * 128:(c + 1) * 128], ident_b[:]), reads=[mtk, "ident_b"], writes=["BT"], signal=(c == 7))
                        op("vector", lambda e: e.tensor_copy(mixT[:], BT[:]), reads=["BT"], writes=["mixT"])
                        for half in range(2):
                            for c in range(8):
                                op("tensor", lambda e, half=half, c=c: e.matmul(B[half][:], lhsT=mixT[:, c, :], rhs=wo_b[:, c, half * 512:(half + 1) * 512], start=(c == 0), stop=(c == 7)),
                                   reads=["mixT", "wo_b"], writes=["B%d" % half], signal=(c == 7))
                            op("vector", lambda e, half=half, tt=tt: e.tensor_tensor(out=x1[:, tt, half * 512:(half + 1) * 512], in0=B[half][:], in1=x1[:, tt, half * 512:(half + 1) * 512], op=ALU.add),
                               reads=["B%d" % half, "x1"], writes=["x1"])
                        rms_stats(x1[:, tt, :], D, "x1", junk4[:], ss4, rs4, "4")
                        op("vector", lambda e, tt=tt: e.scalar_tensor_tensor(out=h2[:], in0=x1[:, tt, :], scalar=rs4[:, 0:1], in1=nfw[:], op0=ALU.mult, op1=ALU.mult),
                           reads=["x1", "rstd4", "nfw"], writes=["h2"])
                        for c in range(8):
                            op("tensor", lambda e, c=c: e.transpose(BT[:, c, :], h2[:, c * 128:(c + 1) * 128], ident_b[:]), reads=["h2", "ident_b"], writes=["BT"], signal=(c == 7))
                        op("vector", lambda e, tt=tt: e.tensor_copy(h2T[:, :, tt * 128:(tt + 1) * 128], BT[:]), reads=["BT"], writes=["h2T"])
                    for fc in range(NFC):
                        pg = B[2 + 2 * (fc % 2)]
                        pu = B[3 + 2 * (fc % 2)]
                        pgk = "B%d" % (2 + 2 * (fc % 2))
                        puk = "B%d" % (3 + 2 * (fc % 2))
                        for c in range(8):
                            op("tensor", lambda e, pg=pg, c=c, fc=fc: e.matmul(pg[:, 0:256], lhsT=wg_b[:, c, fc * 128:(fc + 1) * 128], rhs=h2T[:, c, :], start=(c == 0), stop=(c == 7)),
                               reads=["wg_b", "h2T"], writes=[pgk], signal=(c == 7))
                        for c in range(8):
                            op("tensor", lambda e, pu=pu, c=c, fc=fc: e.matmul(pu[:, 0:256], lhsT=wu_b[:, c, fc * 128:(fc + 1) * 128], rhs=h2T[:, c, :], start=(c == 0), stop=(c == 7)),
                               reads=["wu_b", "h2T"], writes=[puk], signal=(c == 7))
                        sg_ = sgt[fc % 2]
                        sgk = "sgt%d" % (fc % 2)
                        op("scalar", lambda e, sg_=sg_, pg=pg: e.activation(out=sg_[:], in_=pg[:, 0:256], func=AF.Silu), reads=[pgk], writes=[sgk])
                        op("vector", lambda e, sg_=sg_, pu=pu, fc=fc: e.tensor_tensor(out=actT[:, fc, :], in0=pu[:, 0:256], in1=sg_[:], op=ALU.mult), reads=[puk, sgk], writes=["actT"])
                    for tt in range(2):
                        i = TT * 2 + tt
                        x2i = x2[0]
                        x2k = "x2_0"
                        for half in range(2):
                            pd = B[(0, 6)[half]]
                            pdk = "B%d" % ((0, 6)[half])
                            for fc in range(NFC):
                                op("tensor", lambda e, pd=pd, fc=fc, tt=tt, half=half: e.matmul(pd[:], lhsT=actT[:, fc, tt * 128:(tt + 1) * 128], rhs=wd_b[:, fc, half * 512:(half + 1) * 512],
                                                                                      start=(fc == 0), stop=(fc == NFC - 1)),
                                   reads=["actT", "wd_b"], writes=[pdk], signal=(fc == NFC - 1))
                            op("vector", lambda e, pd=pd, half=half, tt=tt, x2i=x2i: e.tensor_tensor(out=x2i[:, half * 512:(half + 1) * 512], in0=pd[:], in1=x1[:, tt, half * 512:(half + 1) * 512], op=ALU.add),
                               reads=[pdk, "x1"], writes=[x2k])
                        if not last:
                            P.dma("gpsimd", xres_d[i * 128:(i + 1) * 128, :], x2i[:], reads=[x2k], writes=["xres_d"])
                        else:
                            rms_stats(x2i[:], D, x2k, junk4[:], ss5, rs5, "5")
                            op("vector", lambda e, x2i=x2i: e.scalar_tensor_tensor(out=x2i[:], in0=x2i[:], scalar=rs5[:, 0:1], in1=fnw[:], op0=ALU.mult, op1=ALU.mult),
                               reads=[x2k, "rstd5", "fnw"], writes=[x2k])
                            P.dma("gpsimd", out[i * 128:(i + 1) * 128, :], x2i[:], reads=[x2k], writes=["out"])
                P.barrier()
        for l_ in range(nlayers):
            layer(l_)
        P.finish("gpsimd", ["out", "mix_d", "xres_d"])
        P.barrier()
        with nc.Block() as block:
            P.emit(block)
    print("instructions:", P.n_inst)
    return nc


_NAMES = ["norm_mix_w", "w_in", "gmlp_norm_w", "gmlp_ws", "gmlp_bs", "cmp_pos_k", "cmp_pos_v", "cmp_k_w1", "cmp_k_w2",
          "cmp_v_w1", "cmp_v_w2", "gate_b", "out_norm_a_w", "out_norm_b_w", "w_o", "norm_ffn_w", "w_gate", "w_up",
          "w_down", "final_norm_w"]


def kernel(**inputs):
    x = np.ascontiguousarray(np.asarray(inputs["x"], dtype=np.float32))
    shared = {n: np.ascontiguousarray(np.asarray(inputs[n], dtype=np.float32)) for n in _NAMES}
    nc = build()
    in_maps = [dict(shared, x=x[b]) for b in range(8)]
    res = run_bass_kernel_spmd(nc, in_maps, core_ids=list(range(8)))
    return np.stack([np.asarray(r["out"], dtype=np.float32) for r in res.results], axis=0)
```

```python
import numpy as np
import concourse.bass as bass
import concourse.mybir as mybir
from concourse.bass_utils import run_bass_kernel_spmd

F32 = mybir.dt.float32
BF16 = mybir.dt.bfloat16
AF = mybir.ActivationFunctionType
ALU = mybir.AluOpType
AX = mybir.AxisListType


class Prog:
    ENGS = ("sync", "scalar", "vector", "gpsimd", "tensor")
    NDMA = 8
    R = 8

    def __init__(self, nc, stack):
        self.nc = nc
        self.q = {e: [] for e in self.ENGS}
        self.sem = {e: [stack.enter_context(nc.semaphore("s_%s%d" % (e, i))) for i in range(self.R)]
                    for e in self.ENGS}
        self.cnt = {e: 0 for e in self.ENGS}
        self.dsem = {e: [stack.enter_context(nc.semaphore("d_%s%d" % (e, i))) for i in range(self.NDMA)]
                     for e in ("sync", "scalar", "gpsimd")}
        self.dcnt = {e: 0 for e in self.dsem}
        self.semobj = {}
        for e in self.ENGS:
            for i in range(self.R):
                self.semobj[("c", e, i)] = self.sem[e][i]
        for e in self.dsem:
            for i in range(self.NDMA):
                self.semobj[("d", e, i)] = self.dsem[e][i]
        self.waited = {e: {} for e in self.ENGS}
        self.lastw = {}
        self.readers = {}
        self.n_inst = 0

    def _waits(self, eng, deps):
        need = {}
        for (sid, val) in deps:
            if sid[0] == "c" and sid[1] == eng and (val - 1) * self.R + sid[2] + 1 > self.cnt[eng]:
                continue
            if self.waited[eng].get(sid, 0) >= val:
                continue
            if need.get(sid, 0) < val:
                need[sid] = val
        out = []
        for sid, val in need.items():
            self.waited[eng][sid] = val
            out.append((self.semobj[sid], val))
        return out

    def _deps(self, reads, writes):
        deps = []
        for k in reads:
            if k in self.lastw:
                deps.append(self.lastw[k])
        for k in writes:
            if k in self.lastw:
                deps.append(self.lastw[k])
            deps.extend(self.readers.get(k, ()))
        return deps

    def _commit(self, tok, reads, writes):
        for k in reads:
            self.readers.setdefault(k, []).append(tok)
        for k in writes:
            self.lastw[k] = tok
            self.readers[k] = []

    def op(self, eng, fn, reads=(), writes=(), signal=True):
        waits = self._waits(eng, self._deps(reads, writes))
        n = self.cnt[eng]
        tok = (("c", eng, n % self.R), n // self.R + 1)
        if signal:
            self.cnt[eng] += 1
        sem = self.sem[eng][n % self.R]

        def run(e, fn=fn, waits=waits, signal=signal, sem=sem):
            for (s, v) in waits:
                e.wait_ge(s, v)
            ins = fn(e)
            if signal:
                ins.then_inc(sem, 1)

        self.q[eng].append(run)
        self._commit(tok, reads, writes)
        self.n_inst += 1

    def dma(self, eng, out, in_, reads=(), writes=(), **kw):
        n = self.dcnt[eng]
        self.dcnt[eng] += 1
        slot = n % self.NDMA
        val = 16 * (n // self.NDMA + 1)
        sid = ("d", eng, slot)
        deps = self._deps(reads, writes)
        if val > 16:
            deps.append((sid, val - 16))
        waits = self._waits(eng, deps)
        sem = self.dsem[eng][slot]

        def run(e, waits=waits, sem=sem, out=out, in_=in_, kw=kw):
            for (s, v) in waits:
                e.wait_ge(s, v)
            e.dma_start(out=out, in_=in_, **kw).then_inc(sem, 16)

        self.q[eng].append(run)
        self._commit((sid, val), reads, writes)
        self.n_inst += 1

    def barrier(self):
        deps = []
        for e in self.ENGS:
            n = self.cnt[e]
            for i in range(self.R):
                if n >= i + 1:
                    deps.append((("c", e, i), (n - 1 - i) // self.R + 1))
        for e in self.dsem:
            n = self.dcnt[e]
            for i in range(self.NDMA):
                if n >= i + 1:
                    deps.append((("d", e, i), 16 * ((n - 1 - i) // self.NDMA + 1)))
        for e in self.ENGS:
            waits = self._waits(e, deps)

            def run(en, waits=waits):
                for (s, v) in waits:
                    en.wait_ge(s, v)

            self.q[e].append(run)

    def finish(self, eng, keys):
        waits = self._waits(eng, self._deps(keys, ()))

        def run(e, waits=waits):
            for (s, v) in waits:
                e.wait_ge(s, v)

        self.q[eng].append(run)

    def emit(self, block):
        q = self.q

        @block.sync
        def _(e):
            for f in q["sync"]:
                f(e)

        @block.scalar
        def _(e):
            for f in q["scalar"]:
                f(e)

        @block.vector
        def _(e):
            for f in q["vector"]:
                f(e)

        @block.gpsimd
        def _(e):
            for f in q["gpsimd"]:
                f(e)

        @block.tensor
        def _(e):
            for f in q["tensor"]:
                f(e)


S = 4096
D = 1024
NT = 32
DFF = 2816
NFC = 22
NEG = -30000.0
L = 2


def build(debug=None, nlayers=L):
    from contextlib import ExitStack
    nc = bass.Bass("TRN2", target_bir_lowering=False)
    dt_in = lambda name, shape: nc.dram_tensor(name, shape, F32, kind="ExternalInput").ap()
    x_in = dt_in("x", [S, D])
    norm_mix_w = dt_in("norm_mix_w", [L, D])
    w_in = dt_in("w_in", [L, D, 2328])
    gmlp_norm_w = dt_in("gmlp_norm_w", [L, 512])
    gmlp_ws = dt_in("gmlp_ws", [L, 8, 128, 128])
    gmlp_bs = dt_in("gmlp_bs", [L, 8, 128])
    cmp_pos = {"k": dt_in("cmp_pos_k", [L, 32, 64]), "v": dt_in("cmp_pos_v", [L, 32, 64])}
    cmp_w1 = {"k": dt_in("cmp_k_w1", [L, 2048, 256]), "v": dt_in("cmp_v_w1", [L, 2048, 256])}
    cmp_w2 = {"k": dt_in("cmp_k_w2", [L, 256, 64]), "v": dt_in("cmp_v_w2", [L, 256, 64])}
    gate_b = dt_in("gate_b", [L, 24])
    out_norm_a_w = dt_in("out_norm_a_w", [L, 512])
    out_norm_b_w = dt_in("out_norm_b_w", [L, 512])
    w_o = dt_in("w_o", [L, D, D])
    norm_ffn_w = dt_in("norm_ffn_w", [L, D])
    w_gate = dt_in("w_gate", [L, D, DFF])
    w_up = dt_in("w_up", [L, D, DFF])
    w_down = dt_in("w_down", [L, DFF, D])
    final_norm_w = dt_in("final_norm_w", [D])
    out = nc.dram_tensor("out", [S, D], F32, kind="ExternalOutput").ap()
    dbg = debug is not None
    mix_d = nc.dram_tensor("mix_d", [S, D], BF16, kind="ExternalOutput" if dbg else "Internal").ap()
    xres_d = nc.dram_tensor("xres_d", [S, D], F32, kind="ExternalOutput" if dbg else "Internal").ap()

    with ExitStack() as st:
        P = Prog(nc, st)
        op = P.op

        sfx = [""]

        def T(name, shape, dt, stack=None):
            return (stack or st).enter_context(nc.sbuf_tensor(name + sfx[0], shape, dt))

        B = [st.enter_context(nc.psum_tensor("B%d" % i, [128, 512], F32)) for i in range(7)]
        BT = st.enter_context(nc.psum_tensor("BT", [128, 8, 128], BF16))

        ident_b = T("ident_b", [128, 128], BF16)
        ident_f = T("ident_f", [128, 128], F32)
        tri_f = T("tri_f", [128, 128], F32)
        tri_b = T("tri_b", [128, 128], BF16)
        ntri_b = T("ntri_b", [128, 128], BF16)
        ones_f = T("ones_f", [128, 128], F32)
        ind8 = T("ind8", [8, 512], F32)
        A_big = T("A_big", [16, 512], BF16)
        Bm4 = T("Bm4", [16, 4, 128], BF16)
        VM = T("VM", [128, 128], F32)
        AMk = T("AMk", [128, 128], F32)
        tmpc = T("tmpc", [128, 512], F32)
        tmpc2 = T("tmpc2", [128, 512], F32)

        def G(fn, reads=(), writes=()):
            op("gpsimd", fn, reads=reads, writes=writes)

        def asel(t, pattern, cmp_, fill, base, cm, key):
            G(lambda e: e.affine_select(out=t, in_=t, pattern=pattern, compare_op=cmp_, fill=fill,
                                        base=base, channel_multiplier=cm), reads=[key], writes=[key])

        G(lambda e: e.memset(ones_f[:], 1.0), writes=["ones_f"])
        G(lambda e: e.memset(ident_f[:], 1.0), writes=["ident_f"])
        asel(ident_f[:], [[-1, 128]], ALU.is_equal, 0.0, 0, 1, "ident_f")
        G(lambda e: e.tensor_copy(ident_b[:], ident_f[:]), reads=["ident_f"], writes=["ident_b"])
        G(lambda e: e.memset(tri_f[:], 1.0), writes=["tri_f"])
        asel(tri_f[:], [[1, 128]], ALU.is_ge, 0.0, 0, -1, "tri_f")
        G(lambda e: e.tensor_copy(tri_b[:], tri_f[:]), reads=["tri_f"], writes=["tri_b"])
        G(lambda e: e.memset(tmpc[:, 0:128], 1.0), writes=["tmpc"])
        asel(tmpc[:, 0:128], [[-1, 128]], ALU.is_gt, 0.0, 0, 1, "tmpc")
        G(lambda e: e.tensor_copy(ntri_b[:], tmpc[:, 0:128]), reads=["tmpc"], writes=["ntri_b"])
        G(lambda e: e.memset(ind8[:], 1.0), writes=["ind8"])
        asel(ind8[:].rearrange("p (h d) -> p h d", h=8), [[1, 8], [0, 64]], ALU.is_equal, 0.0, 0, -1, "ind8")
        G(lambda e: e.memset(tmpc[0:16, :], 1.0), writes=["tmpc"])
        asel(tmpc[0:16, :], [[1, 512]], ALU.is_equal, 0.0, -255, -1, "tmpc")
        G(lambda e: e.memset(tmpc2[0:16, :], 1.0), writes=["tmpc2"])
        asel(tmpc2[0:16, :], [[1, 512]], ALU.is_ge, 0.0, -263, 0, "tmpc2")
        asel(tmpc2[0:16, :], [[0, 512]], ALU.is_equal, 0.0, -8, 1, "tmpc2")
        asel(tmpc[0:16, :], [[0, 512]], ALU.is_ge, 0.0, 7, -1, "tmpc")
        G(lambda e: e.tensor_tensor(out=A_big[:], in0=tmpc[0:16, :], in1=tmpc2[0:16, :], op=ALU.add),
          reads=["tmpc", "tmpc2"], writes=["A_big"])
        G(lambda e: e.memset(tmpc[0:16, :], NEG), writes=["tmpc"])
        asel(tmpc[0:16, :].rearrange("p (g t) -> p g t", g=4), [[0, 4], [-1, 128]], ALU.is_gt, 0.0, 15, 16, "tmpc")
        asel(tmpc[0:16, :], [[0, 512]], ALU.is_ge, 0.0, 7, -1, "tmpc")
        G(lambda e: e.memset(tmpc2[0:16, :], NEG), writes=["tmpc2"])
        asel(tmpc2[0:16, :], [[0, 512]], ALU.is_equal, 0.0, -8, 1, "tmpc2")
        G(lambda e: e.tensor_tensor(out=Bm4[:].rearrange("p g t -> p (g t)"), in0=tmpc[0:16, :], in1=tmpc2[0:16, :], op=ALU.add),
          reads=["tmpc", "tmpc2"], writes=["Bm4"])
        G(lambda e: e.memset(VM[:], 1.0), writes=["VM"])
        asel(VM[:], [[-64, 128]], ALU.is_ge, 0.0, 64 * 64 - 128, 1, "VM")
        G(lambda e: e.memset(AMk[:], 10000.0), writes=["AMk"])
        asel(AMk[:], [[-64, 128]], ALU.is_ge, 0.0, 64 * 64, 1, "AMk")
        asel(AMk[:], [[64, 128]], ALU.is_ge, 0.0, -64 * 64 + 63, -1, "AMk")
        G(lambda e: e.memset(tmpc[:, 0:128], 10001.0), writes=["tmpc"])
        asel(tmpc[:, 0:128], [[-64, 128]], ALU.is_ge, 0.0, 64 * 64 - 64, 1, "tmpc")
        asel(tmpc[:, 0:128], [[64, 128]], ALU.is_ge, 0.0, -64 * 64 + 64 + 63, -1, "tmpc")
        G(lambda e: e.tensor_tensor(out=AMk[:], in0=AMk[:], in1=tmpc[:, 0:128], op=ALU.add), reads=["AMk", "tmpc"], writes=["AMk"])
        G(lambda e: e.memset(tmpc2[:, 0:128], -10000.0), writes=["tmpc2"])
        asel(tmpc2[:, 0:128], [[64, 128]], ALU.is_ge, 0.0, -64 * 64 - 1, -1, "tmpc2")
        G(lambda e: e.tensor_tensor(out=AMk[:], in0=AMk[:], in1=tmpc2[:, 0:128], op=ALU.add), reads=["AMk", "tmpc2"], writes=["AMk"])


        eps_t = T("eps_t", [128, 1], F32)
        G(lambda e: e.memset(eps_t[:], 1e-6), writes=["eps_t"])

        def rms_stats_exp(src_ap, width, key_src, junk, ssum, rstd, tag):
            sc = float(width) ** -0.5
            op("scalar", lambda e: e.activation(out=junk, in_=src_ap, func=AF.Square, scale=sc, accum_out=ssum[:, 0:1]),
               reads=[key_src], writes=["junk" + tag, "ss" + tag])
            op("scalar", lambda e: e.activation(out=rstd[:, 0:1], in_=ssum[:, 0:1], func=AF.Ln, bias=eps_t[:, 0:1]), reads=["ss" + tag, "eps_t"], writes=["rstd" + tag])
            op("scalar", lambda e: e.activation(out=rstd[:, 0:1], in_=rstd[:, 0:1], func=AF.Exp, scale=-0.5), reads=["rstd" + tag], writes=["rstd" + tag])

        def rms_stats(src_ap, width, key_src, junk, ssum, rstd, tag):
            sc = float(width) ** -0.5
            op("scalar", lambda e: e.activation(out=junk, in_=src_ap, func=AF.Square, scale=sc, accum_out=ssum[:, 0:1]),
               reads=[key_src], writes=["junk" + tag, "ss" + tag])
            op("vector", lambda e: e.tensor_scalar(rstd[:, 0:1], ssum[:, 0:1], 1e-6, None, ALU.add), reads=["ss" + tag], writes=["rstd" + tag])
            op("scalar", lambda e: e.activation(out=rstd[:, 0:1], in_=rstd[:, 0:1], func=AF.Sqrt), reads=["rstd" + tag], writes=["rstd" + tag])
            op("vector", lambda e: e.reciprocal(rstd[:, 0:1], rstd[:, 0:1]), reads=["rstd" + tag], writes=["rstd" + tag])

        def layer(l):
            sfx[0] = "_L%d" % l
            xsrc = x_in if l == 0 else xres_d
            last = (l == nlayers - 1)
            P.barrier()
            with ExitStack() as s1:
                nmw = T("nmw", [128, D], F32, s1)
                gnw = T("gnw", [128, 512], F32, s1)
                onaw = T("onaw", [128, 512], F32, s1)
                onbw = T("onbw", [128, 512], F32, s1)
                gbt = T("gbt", [128, 24], F32, s1)
                bs8 = T("bs8", [8, 128], F32, s1)
                gates = T("gates", [128, NT, 24], F32, s1)
                P.dma("sync", nmw[:], norm_mix_w[l].partition_broadcast(128), writes=["nmw"])
                P.dma("sync", gnw[:], gmlp_norm_w[l].partition_broadcast(128), writes=["gnw"])
                P.dma("sync", onaw[:], out_norm_a_w[l].partition_broadcast(128), writes=["onaw"])
                P.dma("sync", onbw[:], out_norm_b_w[l].partition_broadcast(128), writes=["onbw"])
                P.dma("sync", gbt[:], gate_b[l].partition_broadcast(128), writes=["gbt"])
                P.dma("sync", bs8[:], gmlp_bs[l], writes=["bs8"])

                wtm = T("wtm", [128, 8, 1304], BF16, s1)
                wfm = T("wfm", [128, 8, 1024], BF16, s1)
                wsT = T("wsT", [128, 8, 128], BF16, s1)
                kTE = [T("kTE%d" % k, [128, S], BF16, s1) for k in range(2)]
                kTw = T("kTw", [128, S], BF16, s1)
                kcT = T("kcT", [128, S], BF16, s1)
                vcT = T("vcT", [128, S], BF16, s1)
                vs_aug = T("vs_aug", [128, NT, 2, 65], BF16, s1)
                vw_aug = T("vw_aug", [128, NT, 2, 65], BF16, s1)
                qT_all = T("qT_all", [128, 4, S], BF16, s1)
                w2k_pad = T("w2k_pad", [128, 2, 2, 128], BF16, s1)
                w2v = T("w2v", [128, 2, 64], BF16, s1)
                hidT = T("hidT", [128, 2, 256], BF16, s1)
                kcmp = T("kcmp", [128, 256], BF16, s1)
                R_cmp = T("R_cmp", [128, 2, 2, 129], BF16, s1)

                with ExitStack() as s0:
                    stage = [T("stage%d" % i, [128, 2328], F32, s0) for i in range(2)]
                    for c in range(8):
                        sg = stage[c % 2]
                        sk = "stage%d" % (c % 2)
                        P.dma("sync", sg[:], w_in[l, c * 128:(c + 1) * 128, :], writes=[sk])
                        cp = [
                            (wtm[:, c, 0:1024], sg[:, 0:1024]),
                            (wtm[:, c, 1024:1152], sg[:, 1920:2048]),
                            (wtm[:, c, 1152:1280], sg[:, 2176:2304]),
                            (wtm[:, c, 1280:1304], sg[:, 2304:2328]),
                            (wfm[:, c, 0:512].rearrange("p (g k d) -> p g k d", g=4, k=2),
                             sg[:, 1024:1536].rearrange("p (k g d) -> p g k d", k=2, g=4)),
                            (wfm[:, c, 512:640], sg[:, 1536:1664]),
                            (wfm[:, c, 640:768], sg[:, 1664:1792]),
                            (wfm[:, c, 768:896], sg[:, 1792:1920]),
                            (wfm[:, c, 896:1024], sg[:, 2048:2176]),
                        ]
                        for ci, (o_, i_) in enumerate(cp):
                            if ci % 2 == 0:
                                op("scalar", lambda e, o_=o_, i_=i_: e.copy(o_, i_), reads=[sk], writes=["wtm" if ci < 4 else "wfm"])
                            else:
                                op("vector", lambda e, o_=o_, i_=i_: e.tensor_copy(o_, i_), reads=[sk], writes=["wtm" if ci < 4 else "wfm"])
                    wsf = T("wsf", [128, 8, 128], F32, s0)
                    P.dma("sync", wsf[:], gmlp_ws[l].rearrange("h t s -> t h s"), writes=["wsf"])
                    for h in range(8):
                        bk = B[h % 2]
                        op("tensor", lambda e, h=h, bk=bk: e.transpose(bk[:, 0:128], wsf[:, h, :], ident_f[:]),
                           reads=["wsf", "ident_f"], writes=["B%d" % (h % 2)])
                        op("vector", lambda e, h=h, bk=bk: e.tensor_tensor(out=wsT[:, h, :], in0=bk[:, 0:128], in1=tri_f[:], op=ALU.mult),
                           reads=["B%d" % (h % 2), "tri_f"], writes=["wsT"])
                    for nt_ in range(2):
                        G(lambda e: e.memset(tmpc[:, 0:64], 1.0), writes=["tmpc"])
                        asel(tmpc[:, 0:64], [[-64, 64]], ALU.is_ge, 0.0, 2048 * nt_ + 31, 16, "tmpc")
                        asel(tmpc[:, 0:64], [[64, 64]], ALU.is_ge, 0.0, 63 - 2048 * nt_, -16, "tmpc")
                        for k in range(2):
                            G(lambda e, nt_=nt_, k=k: e.tensor_copy(R_cmp[:, nt_, k, 64:128], tmpc[:, 0:64]), reads=["tmpc"], writes=["R_cmp"])
                    G(lambda e: e.memset(R_cmp[:, :, :, 128:129], 1.0), writes=["R_cmp"])
                    G(lambda e: e.memset(vs_aug[:, :, :, 64:65], 1.0), writes=["vs_aug"])
                    G(lambda e: e.memset(vw_aug[:, :, :, 64:65], 1.0), writes=["vw_aug"])
                    G(lambda e: e.memset(kTE[0][:], 1.0), writes=["kTE0"])
                    asel(kTE[0][:].rearrange("p (j r) -> p j r", j=64), [[1, 64], [0, 64]], ALU.is_equal, 0.0, 64, -1, "kTE0")
                    G(lambda e: e.memset(kTE[1][:], 1.0), writes=["kTE1"])
                    asel(kTE[1][:].rearrange("p (j r) -> p j r", j=64), [[1, 64], [0, 64]], ALU.is_equal, 0.0, 0, -1, "kTE1")
                    P.barrier()

                with ExitStack() as s2:
                    xt = [T("xt%d" % i, [128, D], F32, s2) for i in range(2)]
                    junk = T("junk", [128, D], BF16, s2)
                    ssx = T("ssx", [128, 1], F32, s2)
                    rsx = T("rsx", [128, 1], F32, s2)
                    hb = T("hb", [128, D], BF16, s2)
                    hT = [T("hT%d" % i, [128, 8, 512], BF16, s2) for i in range(2)]
                    gu = T("gu", [128, 512], F32, s2)
                    gv = T("gv", [128, 512], F32, s2)
                    ssv = T("ssv", [128, 1], F32, s2)
                    rsv = T("rsv", [128, 1], F32, s2)
                    vn = T("vn", [128, 512], BF16, s2)
                    a_t = T("a_t", [128, 512], F32, s2)
                    ssa = T("ssa", [128, 1], F32, s2)
                    rsa = T("rsa", [128, 1], F32, s2)
                    mixA = [T("mixA%d" % i, [128, 512], BF16, s2) for i in range(2)]
                    gpre = T("gpre", [128, 24], F32, s2)
                    for TT in range(8):
                        hTt = hT[TT % 2]
                        hk = "hT%d" % (TT % 2)
                        for tt in range(4):
                            i = TT * 4 + tt
                            xti = xt[i % 2]
                            xk = "xt%d" % (i % 2)
                            P.dma("sync", xti[:], xsrc[i * 128:(i + 1) * 128, :], writes=[xk])
                            rms_stats_exp(xti[:], D, xk, junk[:], ssx, rsx, "x")
                            op("vector", lambda e, xti=xti: e.scalar_tensor_tensor(out=hb[:], in0=xti[:], scalar=rsx[:, 0:1], in1=nmw[:], op0=ALU.mult, op1=ALU.mult),
                               reads=[xk, "rstdx", "nmw"], writes=["hb"])
                            for c in range(8):
                                op("tensor", lambda e, c=c: e.transpose(BT[:, c, :], hb[:, c * 128:(c + 1) * 128], ident_b[:]),
                                   reads=["hb", "ident_b"], writes=["BT"], signal=(c == 7))
                            op("vector", lambda e, hTt=hTt, tt=tt: e.tensor_copy(hTt[:, :, tt * 128:(tt + 1) * 128], BT[:]), reads=["BT"], writes=[hk])
                            for cb, (c0, c1) in enumerate(((0, 512), (512, 1024), (1024, 1304))):
                                for c in range(8):
                                    op("tensor", lambda e, cb=cb, c=c, c0=c0, c1=c1, hTt=hTt, tt=tt: e.matmul(B[cb][:, 0:c1 - c0], lhsT=hTt[:, c, tt * 128:(tt + 1) * 128], rhs=wtm[:, c, c0:c1],
                                                                                                      start=(c == 0), stop=(c == 7)),
                                       reads=[hk, "wtm"], writes=["B%d" % cb], signal=(c == 7))
                            op("scalar", lambda e: e.activation(out=gu[:], in_=B[0][:], func=AF.Gelu_apprx_tanh), reads=["B0"], writes=["gu"])
                            op("scalar", lambda e: e.activation(out=gv[:], in_=B[1][:], func=AF.Gelu_apprx_tanh), reads=["B1"], writes=["gv"])
                            rms_stats_exp(gv[:], 512, "gv", junk[:, 0:512], ssv, rsv, "v")
                            op("vector", lambda e: e.scalar_tensor_tensor(out=vn[:], in0=gv[:], scalar=rsv[:, 0:1], in1=gnw[:], op0=ALU.mult, op1=ALU.mult),
                               reads=["gv", "rstdv", "gnw"], writes=["vn"])
                            op("vector", lambda e, i=i: e.tensor_copy(vs_aug[:, i, :, 0:64], B[2][:, 0:128].rearrange("p (k d) -> p k d", k=2)), reads=["B2"], writes=["vs_aug"])
                            op("vector", lambda e, i=i: e.tensor_copy(vw_aug[:, i, :, 0:64], B[2][:, 128:256].rearrange("p (k d) -> p k d", k=2)), reads=["B2"], writes=["vw_aug"])
                            op("vector", lambda e, i=i: e.tensor_tensor(out=gates[:, i, :], in0=B[2][:, 256:280], in1=gbt[:], op=ALU.add), reads=["B2", "gbt"], writes=["gates"])
                            op("tensor", lambda e: e.matmul(B[3][:], lhsT=bs8[:], rhs=ind8[:], start=True, stop=False), reads=["bs8", "ind8"], writes=["B3"], signal=False)
                            for h in range(8):
                                op("tensor", lambda e, h=h: e.matmul(B[3][:, h * 64:(h + 1) * 64], lhsT=wsT[:, h, :], rhs=vn[:, h * 64:(h + 1) * 64], start=False, stop=(h == 7)),
                                   reads=["wsT", "vn"], writes=["B3"], signal=(h == 7))
                            op("vector", lambda e: e.tensor_tensor(out=a_t[:], in0=B[3][:], in1=gu[:], op=ALU.mult), reads=["B3", "gu"], writes=["a_t"])
                            rms_stats_exp(a_t[:], 512, "a_t", junk[:, 512:1024], ssa, rsa, "a")
                            mA = mixA[i % 2]
                            mk = "mixA%d" % (i % 2)
                            op("vector", lambda e, mA=mA: e.scalar_tensor_tensor(out=mA[:], in0=a_t[:], scalar=rsa[:, 0:1], in1=onaw[:], op0=ALU.mult, op1=ALU.mult),
                               reads=["a_t", "rstda", "onaw"], writes=[mk])
                            P.dma("gpsimd", mix_d[i * 128:(i + 1) * 128, 0:512], mA[:], reads=[mk], writes=["mix_d"])
                        tok = slice(TT * 512, (TT + 1) * 512)
                        for ch in range(8):
                            bk = B[4 + ch % 2]
                            bkk = "B%d" % (4 + ch % 2)
                            for c in range(8):
                                op("tensor", lambda e, bk=bk, ch=ch, c=c, hTt=hTt: e.matmul(bk[:], lhsT=wfm[:, c, ch * 128:(ch + 1) * 128], rhs=hTt[:, c, :], start=(c == 0), stop=(c == 7)),
                                   reads=[hk, "wfm"], writes=[bkk], signal=(c == 7))
                            if ch < 4:
                                op("scalar", lambda e, bk=bk, ch=ch, tok=tok: e.activation(out=qT_all[:, ch, tok], in_=bk[:], func=AF.Copy, scale=0.125), reads=[bkk], writes=["qT_all"])
                            elif ch == 4:
                                op("vector", lambda e, bk=bk, tok=tok: e.tensor_copy(kcT[:, tok], bk[:]), reads=[bkk], writes=["kcT"])
                            elif ch == 5:
                                op("vector", lambda e, bk=bk, tok=tok: e.tensor_copy(vcT[:, tok], bk[:]), reads=[bkk], writes=["vcT"])
                            elif ch == 6:
                                op("vector", lambda e, bk=bk, tok=tok: e.tensor_copy(kTE[0][0:64, tok], bk[0:64, :]), reads=[bkk], writes=["kTE0"])
                                op("vector", lambda e, bk=bk, tok=tok: e.tensor_copy(kTE[1][64:128, tok], bk[64:128, :]), reads=[bkk], writes=["kTE1"])
                            else:
                                op("vector", lambda e, bk=bk, tok=tok: e.tensor_copy(kTw[:, tok], bk[:]), reads=[bkk], writes=["kTw"])
                    op("scalar", lambda e: e.activation(out=gates[:], in_=gates[:], func=AF.Sigmoid), reads=["gates"], writes=["gates"])
                    P.barrier()

                with ExitStack() as s25:
                    w1A = {kv: T("w1A" + kv, [128, 32, 256], BF16, s25) for kv in "kv"}
                    posT = {kv: T("posT" + kv, [128, 32], BF16, s25) for kv in "kv"}
                    bcol = {kv: T("bcol" + kv, [128, 2], F32, s25) for kv in "kv"}
                    w1f = T("w1f", [128, 8, 256], F32, s25)
                    posf = T("posf", [32, 64], F32, s25)
                    w2f = T("w2f", [128, 2, 64], F32, s25)
                    G(lambda e: e.memset(w2k_pad[:], 0.0), writes=["w2k_pad"])
                    for kv in "kv":
                        src = cmp_w1[kv][l].rearrange("(l d) n -> d l n", d=64)
                        for q4 in range(4):
                            P.dma("sync", w1f[0:64], src[:, q4 * 8:(q4 + 1) * 8, :], writes=["w1f"])
                            P.dma("sync", w1f[64:128], src[:, q4 * 8:(q4 + 1) * 8, :], writes=["w1f"])
                            if q4 % 2 == 0:
                                op("scalar", lambda e, kv=kv, q4=q4: e.copy(w1A[kv][:, q4 * 8:(q4 + 1) * 8, :], w1f[:]), reads=["w1f"], writes=["w1A" + kv])
                            else:
                                op("vector", lambda e, kv=kv, q4=q4: e.tensor_copy(w1A[kv][:, q4 * 8:(q4 + 1) * 8, :], w1f[:]), reads=["w1f"], writes=["w1A" + kv])
                        P.dma("sync", posf[:], cmp_pos[kv][l], writes=["posf"])
                        op("tensor", lambda e: e.transpose(B[2][0:64, 0:32], posf[:, :], ident_f[0:32, 0:32]), reads=["posf", "ident_f"], writes=["B2"])
                        op("vector", lambda e, kv=kv: e.tensor_copy(posT[kv][0:64, :], B[2][0:64, 0:32]), reads=["B2"], writes=["posT" + kv])
                        for hc in range(2):
                            for ll in range(32):
                                op("tensor", lambda e, kv=kv, hc=hc, ll=ll: e.matmul(B[3][:, hc:hc + 1], lhsT=w1A[kv][0:64, ll, hc * 128:(hc + 1) * 128],
                                                                                 rhs=posT[kv][0:64, ll:ll + 1], start=(ll == 0), stop=(ll == 31)),
                                   reads=["w1A" + kv, "posT" + kv], writes=["B3"], signal=(ll == 31))
                        op("vector", lambda e, kv=kv: e.tensor_copy(bcol[kv][:], B[3][:, 0:2]), reads=["B3"], writes=["bcol" + kv])
                        P.dma("sync", w2f[:], cmp_w2[kv][l].rearrange("(c p) d -> p c d", p=128), writes=["w2f"])
                        if kv == "k":
                            for k in range(2):
                                G(lambda e, k=k: e.tensor_copy(w2k_pad[:, :, k, 64 * k:64 * k + 64], w2f[:]), reads=["w2f"], writes=["w2k_pad"])
                        else:
                            G(lambda e: e.tensor_copy(w2v[:], w2f[:]), reads=["w2f"], writes=["w2v"])
                    for kv, srcT in (("k", kcT), ("v", vcT)):
                        for k in range(2):
                            pr = slice(64 * k, 64 * k + 64)
                            for hc in range(2):
                                bk = B[hc]
                                for ll in range(32):
                                    op("tensor", lambda e, kv=kv, hc=hc, ll=ll, bk=bk, pr=pr, srcT=srcT: e.matmul(bk[:, 0:255], lhsT=w1A[kv][pr, ll, hc * 128:(hc + 1) * 128],
                                                                                                       rhs=srcT[pr, ll:ll + 16 * 254 + 1:16], start=(ll == 0), stop=(ll == 31)),
                                       reads=["w1A" + kv, "kcT", "vcT"], writes=["B%d" % hc], signal=(ll == 31))
                                op("scalar", lambda e, kv=kv, hc=hc, bk=bk: e.activation(out=hidT[:, hc, 0:255], in_=bk[:, 0:255], func=AF.Gelu_apprx_tanh, bias=bcol[kv][:, hc:hc + 1]),
                                   reads=["B%d" % hc, "bcol" + kv], writes=["hidT"])
                            if kv == "k":
                                for hc in range(2):
                                    op("tensor", lambda e, hc=hc, k=k: e.matmul(B[2][:, 0:255], lhsT=w2k_pad[:, hc, k, :], rhs=hidT[:, hc, 0:255], start=(hc == 0), stop=(hc == 1)),
                                       reads=["w2k_pad", "hidT"], writes=["B2"], signal=(hc == 1))
                                op("vector", lambda e, pr=pr: e.tensor_copy(kcmp[pr, 0:255], B[2][pr, 0:255]), reads=["B2"], writes=["kcmp"])
                            else:
                                for nt_, M in ((0, 128), (1, 127)):
                                    for hc in range(2):
                                        op("tensor", lambda e, hc=hc, nt_=nt_, M=M: e.matmul(B[3][0:M, 0:64], lhsT=hidT[:, hc, nt_ * 128:nt_ * 128 + M], rhs=w2v[:, hc, :], start=(hc == 0), stop=(hc == 1)),
                                           reads=["w2v", "hidT"], writes=["B3"], signal=(hc == 1))
                                    op("vector", lambda e, nt_=nt_, M=M, k=k: e.tensor_copy(R_cmp[0:M, nt_, k, 0:64], B[3][0:M, 0:64]), reads=["B3"], writes=["R_cmp"])
                    P.barrier()

                with ExitStack() as s3:
                    qB = [T("qB%d" % k, [128, 4, 128], BF16, s3) for k in range(2)]
                    PTc = T("PTc", [128, 2, 512], BF16, s3)
                    PT = [T("PT%d" % i, [128, 512], BF16, s3) for i in range(4)]
                    oc2 = [T("oc%d" % j, [128, 4, 129], F32, s3) for j in range(2)]
                    rc2 = [T("rc%d" % j, [128, 4], F32, s3) for j in range(2)]
                    imp = T("imp", [128, 64], F32, s3)
                    score = T("score", [128, 64], F32, s3)
                    sc2 = T("sc2", [128, 64], F32, s3)
                    m8 = T("m8", [128, 16], F32, s3)
                    biasP = [T("biasP%d" % k, [128, 128], BF16, s3) for k in range(2)]
                    oT_sb = T("oT_sb", [128, 2, 512], F32, s3)
                    rr = T("rr", [128, 2, 4], F32, s3)
                    rg = T("rg", [128, 3, 4], F32, s3)
                    bfull2 = [T("bfull%d" % j, [128, 512], F32, s3) for j in range(2)]
                    btmp2 = [T("btmp%d" % j, [128, 4, 64], F32, s3) for j in range(2)]
                    ssb = T("ssb", [128, 1], F32, s3)
                    rsb = T("rsb", [128, 1], F32, s3)
                    junkb = T("junkb", [128, 512], F32, s3)
                    mixB = [T("mixB%d" % i, [128, 512], BF16, s3) for i in range(2)]
                    for k in range(2):
                        G(lambda e, k=k: e.memset(qB[k][:], 0.0), writes=["qB%d" % k])
                        G(lambda e, k=k: e.memset(biasP[k][:], 0.0), writes=["biasP%d" % k])
                    psC = [B[6][:, 260:389], B[5][:, 0:129], B[5][:, 129:258], B[5][:, 258:387]]
                    psCk = ["B6", "B5", "B5", "B5"]
                    psB = BT[:, 0, :]
                    psT = B[6][:, 0:260].rearrange("p (g d) -> p g d", g=4)
                    sidx = [0]
                    pidx = [0]

                    def stageA(i, k, n):
                        pr = slice(64 * k, 64 * k + 64)
                        br_ = slice(64, 128) if k == 0 else slice(0, 64)
                        qk = "qB%d" % k
                        ocn, rcn = oc2[n % 2], rc2[n % 2]
                        ock, rck = "oc%d" % (n % 2), "rc%d" % (n % 2)
                        G(lambda e: e.tensor_copy(qB[k][pr, :, :], qT_all[pr, :, i * 128:(i + 1) * 128]), reads=["qT_all"], writes=[qk])
                        n_tiles = 1 if i <= 15 else 2
                        for nt_ in range(n_tiles):
                            M = 128 if nt_ == 0 else 127
                            bs_ = B[5]
                            bsk = "B5"
                            a0 = 256 - 8 * i + nt_ * 128
                            op("tensor", lambda e, bs_=bs_, M=M, nt_=nt_: e.matmul(bs_[0:M, :], lhsT=kcmp[pr, nt_ * 128:nt_ * 128 + M], rhs=qT_all[pr, :, i * 128:(i + 1) * 128], start=True, stop=False),
                               reads=["kcmp", "qT_all"], writes=[bsk], signal=False)
                            op("tensor", lambda e, bs_=bs_, M=M, a0=a0: e.matmul(bs_[0:M, :], lhsT=A_big[0:9, a0:a0 + M], rhs=Bm4[0:9, :, :], start=False, stop=True),
                               reads=["A_big", "Bm4"], writes=[bsk])
                            yield
                            yield
                            op("scalar", lambda e, bs_=bs_, M=M, nt_=nt_: e.activation(out=PTc[0:M, nt_, :], in_=bs_[0:M, :], func=AF.Exp), reads=[bsk], writes=["PTc"])
                            yield
                            yield
                        for g in range(4):
                            for nt_ in range(n_tiles):
                                M = 128 if nt_ == 0 else 127
                                op("tensor", lambda e, g=g, nt_=nt_, M=M: e.matmul(psC[g], lhsT=PTc[0:M, nt_, g * 128:(g + 1) * 128], rhs=R_cmp[0:M, nt_, k, :],
                                                                                start=(nt_ == 0), stop=(nt_ == n_tiles - 1)),
                                   reads=["PTc", "R_cmp"], writes=[psCk[g]], signal=(nt_ == n_tiles - 1))
                        yield
                        yield
                        for g in range(4):
                            op("vector", lambda e, g=g: e.tensor_copy(ocn[:, g, :], psC[g]), reads=[psCk[g]], writes=[ock])
                        op("vector", lambda e: e.tensor_scalar(rcn[:], ocn[:, :, 128], 1e-30, None, ALU.max), reads=[ock], writes=[rck])
                        op("vector", lambda e: e.reciprocal(rcn[:], rcn[:]), reads=[rck], writes=[rck])
                        yield
                        if i >= 8:
                            op("vector", lambda e: e.tensor_scalar(imp[:], ocn[:, 0, 64:128], rcn[:, 0:1], None, ALU.mult), reads=[ock, rck], writes=["imp"])
                            for g in range(1, 4):
                                op("vector", lambda e, g=g: e.scalar_tensor_tensor(out=imp[:], in0=ocn[:, g, 64:128], scalar=rcn[:, g:g + 1], in1=imp[:], op0=ALU.mult, op1=ALU.add),
                                   reads=[ock, rck, "imp"], writes=["imp"])
                            c0 = 64 - 2 * i
                            op("vector", lambda e: e.tensor_tensor(out=score[:], in0=imp[:], in1=VM[:, c0:c0 + 64], op=ALU.mult), reads=["imp", "VM"], writes=["score"])
                            op("vector", lambda e: e.tensor_tensor(out=score[:], in0=score[:], in1=AMk[:, c0:c0 + 64], op=ALU.add), reads=["score", "AMk"], writes=["score"])
                            op("vector", lambda e: e.memset(score[:, 0:1], 10002.0), reads=["score"], writes=["score"])
                            yield
                            op("vector", lambda e: e.max(out=m8[:, 0:8], in_=score[:]), reads=["score"], writes=["m8"])
                            op("vector", lambda e: e.match_replace(out=sc2[:], in_to_replace=m8[:, 0:8], in_values=score[:], imm_value=NEG), reads=["score", "m8"], writes=["sc2"])
                            op("vector", lambda e: e.max(out=m8[:, 8:16], in_=sc2[:]), reads=["sc2"], writes=["m8"])
                            bo = 64 if k == 0 else 0
                            op("vector", lambda e: e.tensor_scalar(biasP[k][:, bo:bo + 64], score[:], m8[:, 15:16], NEG, ALU.is_lt, ALU.mult),
                               reads=["score", "m8"], writes=["biasP%d" % k])
                            yield
                            yield
                            yield
                            yield
                            op("tensor", lambda e: e.transpose(psB, biasP[k][:], ident_b[:]), reads=["biasP%d" % k, "ident_b"], writes=["BT"])
                            yield
                            yield
                            op("vector", lambda e: e.tensor_copy(qB[k][br_, :, :], BT[br_, 0, :].unsqueeze(1).to_broadcast([64, 4, 128])),
                               reads=["BT"], writes=[qk])
                            yield

                    def adv(gen, cnt):
                        for _ in range(cnt):
                            try:
                                next(gen)
                            except StopIteration:
                                return

                    def makeB(i, k, n):
                        pr = slice(64 * k, 64 * k + 64)
                        qk = "qB%d" % k
                        ocn, rcn = oc2[n % 2], rc2[n % 2]
                        ock, rck = "oc%d" % (n % 2), "rc%d" % (n % 2)
                        bfl = bfull2[i % 2]
                        bfk = "bfull%d" % (i % 2)
                        cfg = ((kTE[k], vs_aug, list(range(0, i + 1))), (kTw, vw_aug, list(range(max(0, i - 4), i + 1))))
                        stp = [(bi, ji, jt, len(cfg[bi][2])) for bi in range(2) for ji, jt in enumerate(cfg[bi][2])]
                        banks = {}

                        def emitS(idx):
                            bi, ji, jt, nj = stp[idx]
                            cache = cfg[bi][0]
                            ck = ("kTE%d" % k) if bi == 0 else "kTw"
                            bnum = (0, 1, 4)[sidx[0] % 3]
                            bs_ = B[bnum]
                            bsk = "B%d" % bnum
                            sidx[0] += 1
                            banks[idx] = (bs_, bsk)
                            if bi == 0:
                                op("tensor", lambda e: e.matmul(bs_[:], lhsT=cache[:, jt * 128:(jt + 1) * 128], rhs=qB[k][:, :, :], start=True, stop=True),
                                   reads=[ck, qk], writes=[bsk])
                            else:
                                op("tensor", lambda e: e.matmul(bs_[:], lhsT=cache[pr, jt * 128:(jt + 1) * 128], rhs=qT_all[pr, :, i * 128:(i + 1) * 128], start=True, stop=True),
                                   reads=[ck, "qT_all"], writes=[bsk])

                        def emitPV(idx):
                            bi, ji, jt, nj = stp[idx]
                            vaug = cfg[bi][1]
                            pso = B[2 + bi]
                            psok = "B%d" % (2 + bi)
                            bs_, bsk = banks[idx]
                            pt = PT[pidx[0] % 4]
                            ptk = "PT%d" % (pidx[0] % 4)
                            pidx[0] += 1
                            op("scalar", lambda e: e.activation(out=pt[:], in_=bs_[:], func=AF.Exp), reads=[bsk], writes=[ptk])
                            if jt == i:
                                G(lambda e: e.tensor_tensor(out=pt[:].rearrange("p (g t) -> p g t", g=4), in0=pt[:].rearrange("p (g t) -> p g t", g=4),
                                                            in1=tri_b[:, :].unsqueeze(1).to_broadcast([128, 4, 128]), op=ALU.mult), reads=[ptk, "tri_b"], writes=[ptk])
                            if bi == 1 and jt == i - 4:
                                G(lambda e: e.tensor_tensor(out=pt[:].rearrange("p (g t) -> p g t", g=4), in0=pt[:].rearrange("p (g t) -> p g t", g=4),
                                                            in1=ntri_b[:, :].unsqueeze(1).to_broadcast([128, 4, 128]), op=ALU.mult), reads=[ptk, "ntri_b"], writes=[ptk])
                            op("tensor", lambda e: e.matmul(pso[0:65, :], lhsT=vaug[:, jt, k, :], rhs=pt[:], start=(ji == 0), stop=(ji == nj - 1)),
                               reads=[ptk, "vs_aug", "vw_aug"], writes=[psok], signal=(ji == nj - 1))
                            if ji == nj - 1:
                                op("vector", lambda e: e.tensor_copy(oT_sb[0:65, bi, :], pso[0:65, :]), reads=[psok], writes=["oT_sb%d" % bi])

                        def begin():
                            emitS(0)
                            if len(stp) > 1:
                                emitS(1)

                        def loop(gnext, fprev, f0):
                            nst = len(stp)
                            fidx = min(2, nst - 1)
                            f0idx = min(i + 2, nst - 1)
                            for idx in range(nst):
                                if idx == fidx:
                                    fprev()
                                if idx + 2 < nst:
                                    emitS(idx + 2)
                                emitPV(idx)
                                if idx == f0idx:
                                    f0()
                                adv(gnext, 1)
                            adv(gnext, 1000)

                        return begin, loop

                    def makeFin(i, k, n):
                        ocn, rcn = oc2[n % 2], rc2[n % 2]
                        ock, rck = "oc%d" % (n % 2), "rc%d" % (n % 2)
                        bfl = bfull2[i % 2]
                        bfk = "bfull%d" % (i % 2)
                        gsl = lambda br: gates[:, i, br * 8 + k * 4: br * 8 + k * 4 + 4]

                        def branch(bi):
                            for g in range(4):
                                op("tensor", lambda e, g=g: e.transpose(psT[:, g, :], oT_sb[0:65, bi, g * 128:(g + 1) * 128], ident_f[0:65, 0:65]),
                                   reads=["oT_sb%d" % bi, "ident_f"], writes=["B6"], signal=(g == 3))
                            op("vector", lambda e: e.tensor_scalar(rr[:, bi, :], psT[:, :, 64], 1e-30, None, ALU.max), reads=["B6"], writes=["rr"])
                            op("vector", lambda e: e.reciprocal(rr[:, bi, :], rr[:, bi, :]), reads=["rr"], writes=["rr"])
                            op("vector", lambda e: e.tensor_tensor(out=rg[:, 1 + bi, :], in0=rr[:, bi, :], in1=gsl(1 + bi), op=ALU.mult), reads=["rr", "gates"], writes=["rg"])
                            for g in range(4):
                                cs = slice(k * 256 + g * 64, k * 256 + g * 64 + 64)
                                op("vector", lambda e, g=g, cs=cs: e.scalar_tensor_tensor(out=bfl[:, cs], in0=psT[:, g, 0:64], scalar=rg[:, 1 + bi, g:g + 1], in1=bfl[:, cs], op0=ALU.mult, op1=ALU.add),
                                   reads=["B6", "rg", bfk], writes=[bfk])

                        def f0():
                            op("vector", lambda e: e.tensor_tensor(out=rg[:, 0, :], in0=rcn[:], in1=gsl(0), op=ALU.mult), reads=[rck, "gates"], writes=["rg"])
                            op("vector", lambda e: e.tensor_tensor(out=bfl[:, k * 256:(k + 1) * 256].rearrange("p (g d) -> p g d", g=4), in0=ocn[:, :, 0:64],
                                                                   in1=rg[:, 0, :].unsqueeze(2).to_broadcast([128, 4, 64]), op=ALU.mult), reads=[ock, "rg"], writes=[bfk])
                            branch(0)

                        def f1():
                            branch(1)
                            if k == 1:
                                rms_stats_exp(bfl[:], 512, bfk, junkb[:], ssb, rsb, "b")
                                mB = mixB[i % 2]
                                mk = "mixB%d" % (i % 2)
                                op("vector", lambda e: e.scalar_tensor_tensor(out=mB[:], in0=bfl[:], scalar=rsb[:, 0:1], in1=onbw[:], op0=ALU.mult, op1=ALU.mult),
                                   reads=[bfk, "rstdb", "onbw"], writes=[mk])
                                P.dma("gpsimd", mix_d[i * 128:(i + 1) * 128, 512:1024], mB[:], reads=[mk], writes=["mix_d"])

                        return f0, f1

                    steps = [(i, k) for i in range(NT) for k in range(2)]
                    adv(stageA(steps[0][0], steps[0][1], 0), 1000)
                    Bcur = makeB(steps[0][0], steps[0][1], 0)
                    Bcur[0]()
                    fprev = lambda: None
                    for n, (i, k) in enumerate(steps):
                        gnext = stageA(steps[n + 1][0], steps[n + 1][1], n + 1) if n + 1 < len(steps) else iter(())
                        f0, f1 = makeFin(i, k, n)
                        Bcur[1](gnext, fprev, f0)
                        if n + 1 < len(steps):
                            Bcur = makeB(steps[n + 1][0], steps[n + 1][1], n + 1)
                            Bcur[0]()
                        fprev = f1
                    fprev()
                    P.barrier()
            if debug == "p3":
                return
            with ExitStack() as s4:
                wo_b = T("wo_b", [128, 8, D], BF16, s4)
                wg_b = T("wg_b", [128, 8, DFF], BF16, s4)
                wu_b = T("wu_b", [128, 8, DFF], BF16, s4)
                wd_b = T("wd_b", [128, NFC, D], BF16, s4)
                nfw = T("nfw", [128, D], F32, s4)
                fnw = T("fnw", [128, D], F32, s4)
                P.dma("sync", nfw[:], norm_ffn_w[l].partition_broadcast(128), writes=["nfw"])
                P.dma("sync", fnw[:], final_norm_w.partition_broadcast(128), writes=["fnw"])
                s4a = ExitStack()
                stg = [T("stg%d" % i, [128, DFF], F32, s4a) for i in range(3)]
                si = 0
                for (wsrc, wdst, nck, wid, key) in ((w_o, wo_b, 8, D, "wo_b"), (w_gate, wg_b, 8, DFF, "wg_b"), (w_up, wu_b, 8, DFF, "wu_b"), (w_down, wd_b, NFC, D, "wd_b")):
                    for c in range(nck):
                        sg = stg[si % 3]
                        sk = "stg%d" % (si % 3)
                        P.dma("sync" if si % 2 == 0 else "gpsimd", sg[:, 0:wid], wsrc[l, c * 128:(c + 1) * 128, :], writes=[sk])
                        if si % 2 == 0:
                            op("scalar", lambda e, wdst=wdst, c=c, sg=sg, wid=wid: e.copy(wdst[:, c, :], sg[:, 0:wid]), reads=[sk], writes=[key])
                        else:
                            op("vector", lambda e, wdst=wdst, c=c, sg=sg, wid=wid: e.tensor_copy(wdst[:, c, :], sg[:, 0:wid]), reads=[sk], writes=[key])
                        si += 1
                P.barrier()
                s4a.close()
                mixt = [T("mixt%d" % i, [128, D], BF16, s4) for i in range(2)]
                mixT = T("mixT", [128, 8, 128], BF16, s4)
                x1 = T("x1", [128, 2, D], F32, s4)
                junk4 = T("junk4", [128, D], BF16, s4)
                ss4 = T("ss4", [128, 1], F32, s4)
                rs4 = T("rs4", [128, 1], F32, s4)
                h2 = T("h2", [128, D], BF16, s4)
                h2T = T("h2T", [128, 8, 256], BF16, s4)
                sgt = [T("sgt%d" % i, [128, 256], F32, s4) for i in range(2)]
                actT = T("actT", [128, NFC, 256], BF16, s4)
                x2 = [T("x2_%d" % i, [128, D], F32, s4) for i in range(1)]
                ss5 = T("ss5", [128, 1], F32, s4)
                rs5 = T("rs5", [128, 1], F32, s4)
                for TT in range(16):
                    for tt in range(2):
                        i = TT * 2 + tt
                        mt = mixt[i % 2]
                        mtk = "mixt%d" % (i % 2)
                        P.dma("sync", mt[:], mix_d[i * 128:(i + 1) * 128, :], reads=["mix_d"], writes=[mtk])
                        P.dma("scalar", x1[:, tt, :], xsrc[i * 128:(i + 1) * 128, :], reads=["xres_d"], writes=["x1"])
                        for c in range(8):
                            op("tensor", lambda e, c=c, mt=mt: e.transpose(BT[:, c, :], mt[:, c * 128:(c + 1) * 128], ident_b[:]), reads=[mtk, "ident_b"], writes=["BT"], signal=(c == 7))
                        op("vector", lambda e: e.tensor_copy(mixT[:], BT[:]), reads=["BT"], writes=["mixT"])
                        for half in range(2):
                            for c in range(8):
                                op("tensor", lambda e, half=half, c=c: e.matmul(B[half][:], lhsT=mixT[:, c, :], rhs=wo_b[:, c, half * 512:(half + 1) * 512], start=(c == 0), stop=(c == 7)),
                                   reads=["mixT", "wo_b"], writes=["B%d" % half], signal=(c == 7))
                            op("vector", lambda e, half=half, tt=tt: e.tensor_tensor(out=x1[:, tt, half * 512:(half + 1) * 512], in0=B[half][:], in1=x1[:, tt, half * 512:(half + 1) * 512], op=ALU.add),
                               reads=["B%d" % half, "x1"], writes=["x1"])
                        rms_stats_exp(x1[:, tt, :], D, "x1", junk4[:], ss4, rs4, "4")
                        op("vector", lambda e, tt=tt: e.scalar_tensor_tensor(out=h2[:], in0=x1[:, tt, :], scalar=rs4[:, 0:1], in1=nfw[:], op0=ALU.mult, op1=ALU.mult),
                           reads=["x1", "rstd4", "nfw"], writes=["h2"])
                        for c in range(8):
                            op("tensor", lambda e, c=c: e.transpose(BT[:, c, :], h2[:, c * 128:(c + 1) * 128], ident_b[:]), reads=["h2", "ident_b"], writes=["BT"], signal=(c == 7))
                        op("vector", lambda e, tt=tt: e.tensor_copy(h2T[:, :, tt * 128:(tt + 1) * 128], BT[:]), reads=["BT"], writes=["h2T"])
                    for fc in range(NFC):
                        pg = B[2 + 2 * (fc % 2)]
                        pu = B[3 + 2 * (fc % 2)]
                        pgk = "B%d" % (2 + 2 * (fc % 2))
                        puk = "B%d" % (3 + 2 * (fc % 2))
                        for c in range(8):
                            op("tensor", lambda e, pg=pg, c=c, fc=fc: e.matmul(pg[:, 0:256], lhsT=wg_b[:, c, fc * 128:(fc + 1) * 128], rhs=h2T[:, c, :], start=(c == 0), stop=(c == 7)),
                               reads=["wg_b", "h2T"], writes=[pgk], signal=(c == 7))
                        for c in range(8):
                            op("tensor", lambda e, pu=pu, c=c, fc=fc: e.matmul(pu[:, 0:256], lhsT=wu_b[:, c, fc * 128:(fc + 1) * 128], rhs=h2T[:, c, :], start=(c == 0), stop=(c == 7)),
                               reads=["wu_b", "h2T"], writes=[puk], signal=(c == 7))
                        sg_ = sgt[fc % 2]
                        sgk = "sgt%d" % (fc % 2)
                        op("scalar", lambda e, sg_=sg_, pg=pg: e.activation(out=sg_[:], in_=pg[:, 0:256], func=AF.Silu), reads=[pgk], writes=[sgk])
                        op("vector", lambda e, sg_=sg_, pu=pu, fc=fc: e.tensor_tensor(out=actT[:, fc, :], in0=pu[:, 0:256], in1=sg_[:], op=ALU.mult), reads=[puk, sgk], writes=["actT"])
                    for tt in range(2):
                        i = TT * 2 + tt
                        x2i = x2[0]
                        x2k = "x2_0"
                        for half in range(2):
                            pd = B[(0, 6)[half]]
                            pdk = "B%d" % ((0, 6)[half])
                            for fc in range(NFC):
                                op("tensor", lambda e, pd=pd, fc=fc, tt=tt, half=half: e.matmul(pd[:], lhsT=actT[:, fc, tt * 128:(tt + 1) * 128], rhs=wd_b[:, fc, half * 512:(half + 1) * 512],
                                                                                      start=(fc == 0), stop=(fc == NFC - 1)),
                                   reads=["actT", "wd_b"], writes=[pdk], signal=(fc == NFC - 1))
                            op("vector", lambda e, pd=pd, half=half, tt=tt, x2i=x2i: e.tensor_tensor(out=x2i[:, half * 512:(half + 1) * 512], in0=pd[:], in1=x1[:, tt, half * 512:(half + 1) * 512], op=ALU.add),
                               reads=[pdk, "x1"], writes=[x2k])
                        if not last:
                            P.dma("gpsimd", xres_d[i * 128:(i + 1) * 128, :], x2i[:], reads=[x2k], writes=["xres_d"])
                        else:
                            rms_stats_exp(x2i[:], D, x2k, junk4[:], ss5, rs5, "5")
                            op("vector", lambda e, x2i=x2i: e.scalar_tensor_tensor(out=x2i[:], in0=x2i[:], scalar=rs5[:, 0:1], in1=fnw[:], op0=ALU.mult, op1=ALU.mult),
                               reads=[x2k, "rstd5", "fnw"], writes=[x2k])
                            P.dma("gpsimd", out[i * 128:(i + 1) * 128, :], x2i[:], reads=[x2k], writes=["out"])
                P.barrier()
        for l_ in range(nlayers):
            layer(l_)
        P.finish("gpsimd", ["out", "mix_d", "xres_d"])
        P.barrier()
        with nc.Block() as block:
            P.emit(block)
    print("instructions:", P.n_inst)
    return nc


_NAMES = ["norm_mix_w", "w_in", "gmlp_norm_w", "gmlp_ws", "gmlp_bs", "cmp_pos_k", "cmp_pos_v", "cmp_k_w1", "cmp_k_w2",
          "cmp_v_w1", "cmp_v_w2", "gate_b", "out_norm_a_w", "out_norm_b_w", "w_o", "norm_ffn_w", "w_gate", "w_up",
          "w_down", "final_norm_w"]


def kernel(**inputs):
    x = np.ascontiguousarray(np.asarray(inputs["x"], dtype=np.float32))
    shared = {n: np.ascontiguousarray(np.asarray(inputs[n], dtype=np.float32)) for n in _NAMES}
    nc = build()
    in_maps = [dict(shared, x=x[b]) for b in range(8)]
    res = run_bass_kernel_spmd(nc, in_maps, core_ids=list(range(8)))
    return np.stack([np.asarray(r["out"], dtype=np.float32) for r in res.results], axis=0)
```
